# Optimizing a Trainium2 kernel written in Bass

```python
import jax, jax.numpy as jnp
from jax import lax
import numpy as np

D_MODEL = 2048
BATCH = 2
SEQ = 16384
DEPTH = 1

FOX_HEADS = 8
FOX_HEAD_DIM = 128
FOX_WIDTH = FOX_HEADS * FOX_HEAD_DIM
ATTN_BLOCK = 128
MLSTM_HEADS = 4
MLSTM_QK_DIM = 128
MLSTM_V_DIM = 256
MLSTM_QK_WIDTH = MLSTM_HEADS * MLSTM_QK_DIM
MLSTM_V_WIDTH = MLSTM_HEADS * MLSTM_V_DIM
MLSTM_CONV = 4
MLSTM_CHUNK = 128
MIX_WIDTH = FOX_WIDTH + MLSTM_V_WIDTH
IN_SPLITS = (FOX_WIDTH, FOX_WIDTH, FOX_WIDTH, FOX_HEADS, MLSTM_QK_WIDTH, MLSTM_QK_WIDTH, MLSTM_V_WIDTH, MLSTM_HEADS, MLSTM_HEADS, MLSTM_V_WIDTH)
IN_WIDTH = 3 * FOX_WIDTH + FOX_HEADS + 2 * MLSTM_QK_WIDTH + 2 * MLSTM_V_WIDTH + 2 * MLSTM_HEADS
PEER_HEADS = 8
PEER_KEY_DIM = 256
PEER_N_KEYS = 128
PEER_TOPK = 16
PEER_N_EXPERTS = PEER_N_KEYS * PEER_N_KEYS
PEER_TOKEN_BLOCK = 128
NORM_EPS = 1e-6

kernel_name = "fox_mlstm_peer_hybrid_block"


def rmsnorm(x, w):
    xf = x.astype(jnp.float32)
    y = xf * lax.rsqrt(jnp.mean(xf * xf, axis=-1, keepdims=True) + NORM_EPS)
    return (y * w).astype(x.dtype)


def head_rmsnorm(h, w):
    B, S, H, dh = h.shape
    hf = h.astype(jnp.float32)
    hf = hf * lax.rsqrt(jnp.mean(hf * hf, axis=-1, keepdims=True) + NORM_EPS)
    return (hf.reshape(B, S, H * dh) * w).astype(h.dtype)


def causal_depthwise_conv(x, w, b):
    K = w.shape[0]
    S = x.shape[1]
    xp = jnp.pad(x, ((0, 0), (K - 1, 0), (0, 0)))
    y = b
    for j in range(K):
        y = y + w[j] * xp[:, j:j + S]
    return y


def forgetting_attention(q, k, v, f_logit):
    B, S, H, Dh = q.shape
    nb = S // ATTN_BLOCK
    F = jnp.cumsum(jax.nn.log_sigmoid(f_logit.astype(jnp.float32)), axis=1).transpose(0, 2, 1)
    qh = q.transpose(0, 2, 1, 3) * (Dh ** -0.5)
    kh = k.transpose(0, 2, 1, 3)
    vh = v.transpose(0, 2, 1, 3)
    q_blocks = qh.reshape(B, H, nb, ATTN_BLOCK, Dh).transpose(2, 0, 1, 3, 4)
    F_blocks = F.reshape(B, H, nb, ATTN_BLOCK).transpose(2, 0, 1, 3)
    q_pos = jnp.arange(S).reshape(nb, ATTN_BLOCK)
    k_pos = jnp.arange(S)

    def block(args):
        qb, Fb, pos = args
        s = jnp.einsum('bhqd,bhkd->bhqk', qb, kh, preferred_element_type=jnp.float32)
        s = s + Fb[..., :, None] - F[..., None, :]
        s = jnp.where(pos[:, None] >= k_pos[None, :], s, -jnp.inf)
        p = jax.nn.softmax(s, axis=-1)
        return jnp.einsum('bhqk,bhkd->bhqd', p.astype(vh.dtype), vh)

    out = lax.map(block, (q_blocks, F_blocks, q_pos))
    return out.transpose(1, 0, 3, 2, 4).reshape(B, S, H, Dh)


def mlstm_chunkwise(q, k, v, i_pre, f_pre):
    B, S, H, dk = q.shape
    dv = v.shape[-1]
    L = MLSTM_CHUNK
    nc = S // L
    f32 = jnp.float32

    def to_chunks(a):
        a = jnp.moveaxis(a, 2, 1)
        return a.reshape(B, H, nc, L, *a.shape[3:])

    qc = to_chunks(q).astype(f32)
    kc = to_chunks(k).astype(f32) * (dk ** -0.5)
    vc = to_chunks(v).astype(f32)
    ic = to_chunks(i_pre).astype(f32)
    b = jnp.cumsum(jax.nn.log_sigmoid(to_chunks(f_pre).astype(f32)), axis=-1)
    b_last = b[..., -1]

    w_end = b_last[..., None] - b + ic
    m_loc = jnp.max(w_end, axis=-1)
    a_end = jnp.exp(w_end - m_loc[..., None])
    C_loc = jnp.einsum('bhcl,bhcld,bhcle->bhcde', a_end, kc, vc)
    n_loc = jnp.einsum('bhcl,bhcld->bhcd', a_end, kc)

    def step(carry, inp):
        C, n, m = carry
        Cl, nl, ml, bl = inp
        m_new = jnp.maximum(bl + m, ml)
        a_prev = jnp.exp(bl + m - m_new)
        a_loc = jnp.exp(ml - m_new)
        C_new = a_prev[..., None, None] * C + a_loc[..., None, None] * Cl
        n_new = a_prev[..., None] * n + a_loc[..., None] * nl
        return (C_new, n_new, m_new), (C, n, m)

    init = (jnp.zeros((B, H, dk, dv), f32), jnp.zeros((B, H, dk), f32), jnp.zeros((B, H), f32))
    xs = (jnp.moveaxis(C_loc, 2, 0), jnp.moveaxis(n_loc, 2, 0), jnp.moveaxis(m_loc, 2, 0), jnp.moveaxis(b_last, 2, 0))
    _, (C_prev, n_prev, m_prev) = lax.scan(step, init, xs)
    C_prev = jnp.moveaxis(C_prev, 0, 2)
    n_prev = jnp.moveaxis(n_prev, 0, 2)
    m_prev = jnp.moveaxis(m_prev, 0, 2)

    g = b + m_prev[..., None]
    D = b[..., :, None] - b[..., None, :] + ic[..., None, :]
    causal = jnp.tril(jnp.ones((L, L), dtype=bool))
    D = jnp.where(causal, D, -jnp.inf)
    m_t = jnp.maximum(g, jnp.max(D, axis=-1))
    Sm = jnp.einsum('bhctd,bhcsd->bhcts', qc, kc) * jnp.exp(D - m_t[..., None])
    inter = jnp.exp(g - m_t)
    num = jnp.einsum('bhcts,bhcse->bhcte', Sm, vc) + inter[..., None] * jnp.einsum('bhctd,bhcde->bhcte', qc, C_prev)
    den = jnp.sum(Sm, axis=-1) + inter * jnp.einsum('bhctd,bhcd->bhct', qc, n_prev)
    h = num / jnp.maximum(jnp.abs(den), jnp.exp(-m_t))[..., None]
    return h.reshape(B, H, S, dv).transpose(0, 2, 1, 3).astype(v.dtype)


def peer_ffn(x, w_q, keys, u, v):
    B, S, D = x.shape
    T = B * S
    xt = x.reshape(T, D)
    q = (xt @ w_q).reshape(T, PEER_HEADS, 2, PEER_KEY_DIM // 2)
    scores = jnp.einsum('thpd,pnd->thpn', q, keys, preferred_element_type=jnp.float32)
    s_top, i_top = lax.top_k(scores, PEER_TOPK)
    cand = (s_top[:, :, 0, :, None] + s_top[:, :, 1, None, :]).reshape(T, PEER_HEADS, PEER_TOPK * PEER_TOPK)
    cand_idx = (i_top[:, :, 0, :, None] * PEER_N_KEYS + i_top[:, :, 1, None, :]).reshape(T, PEER_HEADS, PEER_TOPK * PEER_TOPK)
    s_fin, pos = lax.top_k(cand, PEER_TOPK)
    idx = jnp.take_along_axis(cand_idx, pos, axis=-1)
    gate = jax.nn.softmax(s_fin, axis=-1)
    nblk = T // PEER_TOKEN_BLOCK

    def blk(args):
        xb, ib, gb = args
        ub = jnp.take(u, ib, axis=0)
        act = jax.nn.gelu(jnp.einsum('thkd,td->thk', ub, xb, preferred_element_type=jnp.float32), approximate=False) * gb
        vb = jnp.take(v, ib, axis=0)
        return jnp.einsum('thk,thkd->td', act.astype(vb.dtype), vb)

    y = lax.map(blk, (xt.reshape(nblk, PEER_TOKEN_BLOCK, D),
                      idx.reshape(nblk, PEER_TOKEN_BLOCK, PEER_HEADS, PEER_TOPK),
                      gate.reshape(nblk, PEER_TOKEN_BLOCK, PEER_HEADS, PEER_TOPK)))
    return y.reshape(B, S, D)


def setup_inputs(seed: int = 0) -> dict:
    key = jax.random.key(seed)
    ks = jax.random.split(key, 18)
    f32 = jnp.float32

    def nrm(k, shape, scale):
        return jax.random.normal(k, shape, f32) * scale

    return {
        "x": nrm(ks[0], (BATCH, SEQ, D_MODEL), 1.0),
        "norm1_w": 1.0 + nrm(ks[1], (DEPTH, D_MODEL), 0.02),
        "w_in": nrm(ks[2], (DEPTH, D_MODEL, IN_WIDTH), D_MODEL ** -0.5),
        "fox_f_bias": jnp.linspace(2.0, 6.0, FOX_HEADS, dtype=f32)[None] + nrm(ks[3], (DEPTH, FOX_HEADS), 0.1),
        "mlstm_conv_w": nrm(ks[4], (DEPTH, MLSTM_CONV, 2 * MLSTM_QK_WIDTH), MLSTM_CONV ** -0.5),
        "mlstm_conv_b": nrm(ks[5], (DEPTH, 2 * MLSTM_QK_WIDTH), 0.01),
        "mlstm_i_bias": nrm(ks[6], (DEPTH, MLSTM_HEADS), 0.1),
        "mlstm_f_bias": jnp.linspace(3.0, 6.0, MLSTM_HEADS, dtype=f32)[None] + nrm(ks[7], (DEPTH, MLSTM_HEADS), 0.1),
        "fox_out_norm_w": 1.0 + nrm(ks[8], (DEPTH, FOX_WIDTH), 0.02),
        "mlstm_out_norm_w": 1.0 + nrm(ks[9], (DEPTH, MLSTM_V_WIDTH), 0.02),
        "w_out": nrm(ks[10], (DEPTH, MIX_WIDTH, D_MODEL), MIX_WIDTH ** -0.5),
        "norm2_w": 1.0 + nrm(ks[11], (DEPTH, D_MODEL), 0.02),
        "peer_w_q": nrm(ks[12], (DEPTH, D_MODEL, PEER_HEADS * PEER_KEY_DIM), D_MODEL ** -0.5),
        "peer_keys": nrm(ks[13], (DEPTH, 2, PEER_N_KEYS, PEER_KEY_DIM // 2), (PEER_KEY_DIM // 2) ** -0.5),
        "peer_u": nrm(ks[14], (DEPTH, PEER_N_EXPERTS, D_MODEL), D_MODEL ** -0.5),
        "peer_v": nrm(ks[15], (DEPTH, PEER_N_EXPERTS, D_MODEL), PEER_HEADS ** -0.5),
        "final_norm_w": 1.0 + nrm(ks[16], (D_MODEL,), 0.02),
    }


def reference(x, norm1_w, w_in, fox_f_bias, mlstm_conv_w, mlstm_conv_b, mlstm_i_bias, mlstm_f_bias,
              fox_out_norm_w, mlstm_out_norm_w, w_out, norm2_w, peer_w_q, peer_keys, peer_u, peer_v,
              final_norm_w):
    B, S, _ = x.shape
    split_points = [int(p) for p in np.cumsum(IN_SPLITS)[:-1]]
    for l in range(DEPTH):
        h = rmsnorm(x, norm1_w[l])
        z = h @ w_in[l]
        fq, fk, fv, ff, mq, mk, mv, mi, mf, mo = jnp.split(z, split_points, axis=-1)

        att = forgetting_attention(fq.reshape(B, S, FOX_HEADS, FOX_HEAD_DIM),
                                   fk.reshape(B, S, FOX_HEADS, FOX_HEAD_DIM),
                                   fv.reshape(B, S, FOX_HEADS, FOX_HEAD_DIM),
                                   ff + fox_f_bias[l])

        mqk = jax.nn.silu(causal_depthwise_conv(jnp.concatenate([mq, mk], axis=-1), mlstm_conv_w[l], mlstm_conv_b[l]))
        mq_c, mk_c = jnp.split(mqk, 2, axis=-1)
        cell = mlstm_chunkwise(mq_c.reshape(B, S, MLSTM_HEADS, MLSTM_QK_DIM),
                               mk_c.reshape(B, S, MLSTM_HEADS, MLSTM_QK_DIM),
                               mv.reshape(B, S, MLSTM_HEADS, MLSTM_V_DIM),
                               mi + mlstm_i_bias[l], mf + mlstm_f_bias[l])
        cell = jax.nn.sigmoid(mo.astype(jnp.float32)).astype(cell.dtype).reshape(B, S, MLSTM_HEADS, MLSTM_V_DIM) * cell

        mixed = jnp.concatenate([head_rmsnorm(att, fox_out_norm_w[l]),
                                 head_rmsnorm(cell, mlstm_out_norm_w[l])], axis=-1)
        x = x + mixed @ w_out[l]

        x = x + peer_ffn(rmsnorm(x, norm2_w[l]), peer_w_q[l], peer_keys[l], peer_u[l], peer_v[l])
    return rmsnorm(x, final_norm_w)
```

```python
import numpy as np
import ml_dtypes
from contextlib import ExitStack
import concourse.bass as bass
import concourse.mybir as mybir
from concourse.bass_utils import run_bass_kernel_spmd

F32 = mybir.dt.float32
BF16 = mybir.dt.bfloat16
I32 = mybir.dt.int32
U32 = mybir.dt.uint32
AF = mybir.ActivationFunctionType
ALU = mybir.AluOpType
AX = mybir.AxisListType

D = 2048
KC = 16
EPS = 1e-6


class Trk:
    def __init__(self, nc):
        self.nc = nc
        self.engs = {"pe": nc.tensor, "act": nc.scalar, "dve": nc.vector,
                     "pool": nc.gpsimd, "sp": nc.sync}
        self.sem = {}
        self.cnt = {}
        for e in ("pe", "act", "dve", "pool"):
            self.sem[e] = nc.alloc_semaphore(name="s_" + e)
            self.cnt[e] = 0
        self.dsem = {}
        self.waited = {}
        self.lastw = {}
        self.readers = {}
        self.ninst = 0
        self.lazy_keys = set()

    def _wait(self, eng, ev):
        sem, val, src, key = ev
        k = (eng, key)
        if self.waited.get(k, 0) >= val:
            return
        self.engs[eng].wait_ge(sem, val)
        self.waited[k] = val

    def _deps(self, eng, reads, writes):
        for b in reads:
            ev = self.lastw.get(b)
            if ev is not None and not (ev[2] == eng and eng == "pe"):
                self._wait(eng, ev)
        for b in writes:
            ev = self.lastw.get(b)
            if ev is not None and ev[2] != eng:
                self._wait(eng, ev)
            for ev in self.readers.get(b, ()):
                if ev[2] != eng:
                    self._wait(eng, ev)

    def _record(self, ev, reads, writes):
        for b in reads:
            self.readers.setdefault(b, []).append(ev)
        for b in writes:
            self.lastw[b] = ev
            self.readers[b] = []

    def op(self, eng, fn, reads=(), writes=()):
        self._deps(eng, reads, writes)
        inst = fn(self.engs[eng])
        self.cnt[eng] += 1
        inst.then_inc(self.sem[eng], 1)
        ev = (self.sem[eng], self.cnt[eng], eng, eng)
        self._record(ev, reads, writes)
        self.ninst += 1
        return inst

    def dma(self, q, out, in_, reads=(), writes=(), key=None, **kw):
        if key is None:
            key = writes[0] if writes else reads[0]
        if key not in self.dsem:
            self.dsem[key] = [self.nc.alloc_semaphore(name="d%d" % len(self.dsem)), 0]
        self._deps(q, reads, writes)
        ds = self.dsem[key]
        inst = self.engs[q].dma_start(out=out, in_=in_, **kw)
        ds[1] += 16
        inst.then_inc(ds[0], 16)
        ev = (ds[0], ds[1], "dma", ("d", key))
        self._record(ev, reads, writes)
        self.ninst += 1
        return inst

    def coll(self, fn, reads=(), writes=()):
        if "cc" not in self.sem:
            self.sem["cc"] = self.nc.alloc_semaphore(name="s_cc")
            self.cnt["cc"] = 0
        self._deps("pool", reads, writes)
        inst = fn(self.engs["pool"])
        self.cnt["cc"] += 1
        inst.then_inc(self.sem["cc"])
        ev = (self.sem["cc"], self.cnt["cc"], "cc", "cc")
        self._record(ev, reads, writes)
        self.ninst += 1
        return inst

    def barrier(self):
        evs = [(self.sem[e], self.cnt[e], e, e) for e in self.sem if self.cnt[e] > 0]
        evs += [(v[0], v[1], "dma", ("d", k)) for k, v in self.dsem.items()
                if v[1] > 0 and k not in self.lazy_keys]
        for e in ("pe", "act", "dve", "pool", "sp"):
            for ev in evs:
                if ev[2] != e:
                    self._wait(e, ev)
        keep = {b: ev for b, ev in self.lastw.items() if ev[2] == "dma" and ev[3][1] in self.lazy_keys}
        self.lastw = keep
        self.readers = {}

    def final_wait(self, q="sp"):
        for k, v in self.dsem.items():
            if v[1] > 0:
                self._wait(q, (v[0], v[1], "dma", ("d", k)))


import os
PH = os.environ.get('PH', 'PAM')
SK = os.environ.get('SK', '')

NCOL = 1540
C_FQ0, C_FQ1, C_FK0, C_FK1, C_FV, C_MQ, C_MK, C_MVO, C_G = 0, 128, 256, 384, 512, 768, 896, 1024, 1536


def host_consts():
    c = {}
    c["ident_bf"] = np.eye(128, dtype=np.float32).astype(ml_dtypes.bfloat16)
    c["ident_f"] = np.eye(128, dtype=np.float32)
    r = np.arange(128)
    c["triu"] = (r[:, None] <= r[None, :]).astype(np.float32)
    c["ones_f"] = np.ones((128, 128), np.float32)
    es = np.zeros((128, 128), np.float32); es[0, :] = 1.0
    c["esel"] = es
    t = np.arange(512)
    m = np.stack([(r[:, None] + 128 * rr <= t[None, :]) for rr in range(4)], axis=1)
    c["amask"] = m.astype(np.float32).astype(ml_dtypes.bfloat16)
    c["mmask"] = ((r[:, None] <= r[None, :]).astype(np.float32) * (128 ** -0.5)).astype(np.float32)
    return c


def build_phase_a(nc, S, tk=None, mix_dst=None):
    NT = S // 128
    NB = S // 512
    own_tk = tk is None
    if own_tk:
        tk = Trk(nc)
    dt = nc.dram_tensor
    x = dt("x", [int(os.environ.get("XROWS", S)), D], F32, kind="ExternalInput").ap()
    wc = dt("wc", [D, NCOL], F32, kind="ExternalInput").ap()
    n1w = dt("n1w", [128, KC], F32, kind="ExternalInput").ap()
    gbias = dt("gbias", [128, 4], F32, kind="ExternalInput").ap()
    convp = dt("convp", [128, 10], F32, kind="ExternalInput").ap()
    hnw = dt("hnw", [128, 512], F32, kind="ExternalInput").ap()
    ident_bf_d = dt("ident_bf", [128, 128], BF16, kind="ExternalInput").ap()
    ident_f_d = dt("ident_f", [128, 128], F32, kind="ExternalInput").ap()
    triu_d = dt("triu", [128, 128], F32, kind="ExternalInput").ap()
    ones_d = dt("ones_f", [128, 128], F32, kind="ExternalInput").ap()
    esel_d = dt("esel", [128, 128], F32, kind="ExternalInput").ap()
    amask_d = dt("amask", [128, 4, 512], BF16, kind="ExternalInput").ap()
    mmask_d = dt("mmask", [128, 128], F32, kind="ExternalInput").ap()
    if mix_dst is None:
        mixed = dt("mixed", [int(os.environ.get("MROWS", S)), 512], BF16, kind="ExternalOutput").ap()
        mix_dst = lambda t0, c0, c1: mixed[t0:t0 + 128, c0:c1]
    qt_d = [dt("qt%d" % h, [128, S], BF16, kind="Internal").ap() for h in range(2)]
    kt_d = [dt("kt%d" % h, [128, S], BF16, kind="Internal").ap() for h in range(2)]
    va_d = [dt("va%d" % h, [128, S // 128, 130], BF16, kind="Internal").ap() for h in range(2)]
    mq_d = dt("mqT", [128, S], F32, kind="Internal").ap()
    mk_d = dt("mkT", [128, S], F32, kind="Internal").ap()
    mvo_d = dt("mvo", [S, 512], F32, kind="Internal").ap()
    if os.environ.get("DUMMY"):
        dummy_d = dt("dummyx", [int(os.environ["DUMMY"]), 512], F32, kind="Internal").ap()

    with ExitStack() as es0:
        def sb(name, shape, dtype, es=es0):
            return es.enter_context(nc.sbuf_tensor(name, shape, dtype))
        ident_bf = sb("ident_bf_s", [128, 128], BF16)
        ident_f = sb("ident_f_s", [128, 128], F32)
        triu = sb("triu_s", [128, 128], F32)
        ones_f = sb("ones_s", [128, 128], F32)
        esel = sb("esel_s", [128, 128], F32)
        amask = sb("amask_s", [128, 4, 512], BF16)
        mmask = sb("mmask_s", [128, 128], F32)
        gb = sb("gb_s", [128, 4], F32)
        ngb = sb("ngb_s", [128, 4], F32)
        cvp = sb("cvp_s", [128, 10], F32)
        hnw_s = sb("hnw_s", [128, 512], F32)
        n1w_s = sb("n1w_s", [128, KC], F32)
        gates = sb("gates_s", [128, NT, 4], F32)
        epsc = sb("epsc", [128, 1], F32)
        onec = sb("onec", [128, 1], F32)
        psum = [es0.enter_context(nc.psum_tensor("ps%d" % i, [128, 512], F32)) for i in range(8)]
        for i, (s_, d_) in enumerate([(ident_bf, ident_bf_d), (ident_f, ident_f_d), (triu, triu_d),
                                      (ones_f, ones_d), (esel, esel_d), (amask, amask_d), (mmask, mmask_d),
                                      (gb, gbias), (cvp, convp), (hnw_s, hnw), (n1w_s, n1w)]):
            tk.dma("sp", s_[:], d_, writes=["c%d" % i], key="const")
        tk.barrier()
        tk.op("dve", lambda e: e.memset(epsc[:], EPS), writes=["epsc"])
        tk.op("dve", lambda e: e.memset(onec[:], 1.0), writes=["onec"])
        tk.op("dve", lambda e: e.tensor_scalar(out=ngb[:], in0=gb[:], scalar1=-1.0, scalar2=None, op0=ALU.mult),
              reads=["c7"], writes=["ngb"])

        with ExitStack() as es1:
            W = sb("W", [128, KC, NCOL], BF16, es1)
            wst = [sb("wst%d" % i, [128, NCOL], F32, es1) for i in range(2)]
            xt = [sb("xt%d" % i, [128, D], F32, es1) for i in range(2)]
            xn = [sb("xn%d" % i, [128, D], BF16, es1) for i in range(2)]
            junk = sb("junk", [128, D], BF16, es1)
            hT = [sb("hT%d" % i, [128, KC, 512], BF16, es1) for i in range(2)]
            ss = sb("ss", [128, 4], F32, es1)
            fst = [sb("fst%d" % i, [128, 512], BF16, es1) for i in range(4)]
            mst = [sb("mst%d" % i, [128, 512], F32, es1) for i in range(4)]
            vst = [sb("vst%d" % i, [128, 2, 130], BF16, es1) for i in range(2)]
            for i in range(2):
                tk.op("dve", lambda e, i=i: e.memset(vst[i][:, :, 128:129], 1.0), writes=["vst%d" % i])
                tk.op("dve", lambda e, i=i: e.memset(vst[i][:, :, 129:130], 0.0), writes=["vst%d" % i])
            for kc in range(KC):
                s = kc % 2
                tk.dma("sp", wst[s][:], wc[kc * 128:(kc + 1) * 128, :], writes=["wst%d" % s])
                tk.op("dve", lambda e, kc=kc, s=s: e.tensor_scalar(
                    out=W[:, kc, :], in0=wst[s][:], scalar1=n1w_s[:, kc:kc + 1], scalar2=None, op0=ALU.mult),
                    reads=["wst%d" % s, "c10"], writes=["W"])
            pb = [2]

            def nbank():
                b = pb[0]
                pb[0] = 2 + (pb[0] - 2 + 1) % 6
                return b
            fcnt = [0]
            mcnt = [0]
            for jb in range(min(NB, int(os.environ.get('NBLIM', '999')))):
                hs = jb % 2
                for tt in range(4):
                    ti = jb * 4 + tt
                    s = ti % 2
                    tk.dma("sp", xt[s][:], x[ti * 128:(ti + 1) * 128, :], writes=["xt%d" % s])
                    tk.op("act", lambda e, s=s: e.activation(out=junk[:], in_=xt[s][:], func=AF.Square,
                                                             accum_out=ss[:, 0:1]),
                          reads=["xt%d" % s], writes=["junk", "ss0"])
                    tk.op("act", lambda e: e.activation(out=ss[:, 1:2], in_=ss[:, 0:1], func=AF.Ln,
                                                        scale=1.0 / D, bias=epsc[:]),
                          reads=["ss0", "epsc"], writes=["ss1"])
                    tk.op("act", lambda e: e.activation(out=ss[:, 2:3], in_=ss[:, 1:2], func=AF.Exp, scale=-0.5),
                          reads=["ss1"], writes=["ss2"])
                    tk.op("dve", lambda e, s=s: e.tensor_scalar(out=xn[s][:], in0=xt[s][:], scalar1=ss[:, 2:3],
                                                                scalar2=None, op0=ALU.mult),
                          reads=["xt%d" % s, "ss2"], writes=["xn%d" % s])
                    for half in range(2):
                        pst = psum[half][:, :].bitcast(BF16)
                        for k8 in range(8):
                            kc = half * 8 + k8
                            tk.op("pe", lambda e, kc=kc, k8=k8, pst=pst, s=s: e.transpose(
                                out=pst[:, k8 * 128:(k8 + 1) * 128], in_=xn[s][:, kc * 128:(kc + 1) * 128],
                                identity=ident_bf[:]),
                                reads=["xn%d" % s, "c0"], writes=["psb%d" % half])
                        eng = "act" if half == 0 else "dve"
                        src = pst.rearrange("p (k t) -> p k t", k=8)
                        dst = hT[hs][:, half * 8:(half + 1) * 8, tt * 128:(tt + 1) * 128]
                        if eng == "act":
                            tk.op("act", lambda e, src=src, dst=dst: e.copy(out=dst, in_=src),
                                  reads=["psb%d" % half], writes=["hT%d" % hs])
                        else:
                            tk.op("dve", lambda e, src=src, dst=dst: e.tensor_copy(out=dst, in_=src),
                                  reads=["psb%d" % half], writes=["hT%d" % hs])
                for (c0, kind, dst_d) in [(C_FQ0, "f", qt_d[0]), (C_FQ1, "f", qt_d[1]), (C_FK0, "f", kt_d[0]),
                                          (C_FK1, "f", kt_d[1]), (C_MQ, "m", mq_d), (C_MK, "m", mk_d)]:
                    b = nbank()
                    for kc in range(KC):
                        tk.op("pe", lambda e, kc=kc, b=b, c0=c0: e.matmul(
                            psum[b][:, :], lhsT=W[:, kc, c0:c0 + 128], rhs=hT[hs][:, kc, :],
                            start=(kc == 0), stop=(kc == KC - 1)),
                            reads=["W", "hT%d" % hs], writes=["psb%d" % b])
                    if kind == "f":
                        f = fcnt[0] % 4
                        fcnt[0] += 1
                        tk.op("act", lambda e, b=b, f=f: e.copy(out=fst[f][:], in_=psum[b][:, :]),
                              reads=["psb%d" % b], writes=["fst%d" % f])
                        if 'f' not in SK:
                            tk.dma("pool", dst_d[:, jb * 512:(jb + 1) * 512], fst[f][:], reads=["fst%d" % f],
                                   key="fst%d" % f)
                    else:
                        f = mcnt[0] % 4
                        mcnt[0] += 1
                        tk.op("dve", lambda e, b=b, f=f: e.tensor_copy(out=mst[f][:], in_=psum[b][:, :]),
                              reads=["psb%d" % b], writes=["mst%d" % f])
                        if 'm' not in SK:
                            tk.dma("pool", dst_d[:, jb * 512:(jb + 1) * 512], mst[f][:], reads=["mst%d" % f],
                                   key="mst%d" % f)
                for tt in range(4):
                    ti = jb * 4 + tt
                    tsl = slice(tt * 128, (tt + 1) * 128)
                    b = nbank()
                    for kc in range(KC):
                        tk.op("pe", lambda e, kc=kc, b=b, tsl=tsl: e.matmul(
                            psum[b][:, 0:256], lhsT=hT[hs][:, kc, tsl], rhs=W[:, kc, C_FV:C_FV + 256],
                            start=(kc == 0), stop=(kc == KC - 1)),
                            reads=["W", "hT%d" % hs], writes=["psb%d" % b])
                    vs = ti % 2
                    tk.op("act", lambda e, b=b, vs=vs: e.copy(
                        out=vst[vs][:, :, 0:128], in_=psum[b][:, 0:256].rearrange("p (h d) -> p h d", h=2)),
                        reads=["psb%d" % b], writes=["vst%d" % vs])
                    for h in (range(2) if 'v' not in SK else []):
                        tk.dma("pool", va_d[h][:, ti, :], vst[vs][:, h, :],
                               reads=["vst%d" % vs], key="vst%d_%d" % (vs, h))
                    b = nbank()
                    for kc in range(KC):
                        tk.op("pe", lambda e, kc=kc, b=b, tsl=tsl: e.matmul(
                            psum[b][:, :], lhsT=hT[hs][:, kc, tsl], rhs=W[:, kc, C_MVO:C_MVO + 512],
                            start=(kc == 0), stop=(kc == KC - 1)),
                            reads=["W", "hT%d" % hs], writes=["psb%d" % b])
                    f = mcnt[0] % 4
                    mcnt[0] += 1
                    tk.op("dve", lambda e, b=b, f=f: e.tensor_copy(out=mst[f][:], in_=psum[b][:, :]),
                          reads=["psb%d" % b], writes=["mst%d" % f])
                    if 'o' not in SK:
                        tk.dma("pool", mvo_d[ti * 128:(ti + 1) * 128, :], mst[f][:], reads=["mst%d" % f],
                               key="mst%d" % f)
                    b = nbank()
                    for kc in range(KC):
                        tk.op("pe", lambda e, kc=kc, b=b, tsl=tsl: e.matmul(
                            psum[b][:, 0:4], lhsT=hT[hs][:, kc, tsl], rhs=W[:, kc, C_G:C_G + 4],
                            start=(kc == 0), stop=(kc == KC - 1)),
                            reads=["W", "hT%d" % hs], writes=["psb%d" % b])
                    tk.op("act", lambda e, b=b, ti=ti: e.copy(out=gates[:, ti, :], in_=psum[b][:, 0:4]),
                          reads=["psb%d" % b], writes=["gates"])
            tk.barrier()

        NQ = NB
        SC = 128 ** -0.5
        with ExitStack() as es2:
            KT = sb("KT", [128, S], BF16, es2)
            VA = sb("VA", [128, NT, 130], BF16, es2)
            qt = [sb("qtb%d" % i, [128, 512], BF16, es2) for i in range(2)]
            PT = [sb("PT%d" % i, [128, 512], BF16, es2) for i in range(3)]
            lfn = sb("lfn", [128, NT], F32, es2)
            tmpg = sb("tmpg", [128, NT], F32, es2)
            tot = sb("tot", [128, NT], F32, es2)
            offi = sb("offi", [128, NT], F32, es2)
            onesn = sb("onesn", [128, NT], F32, es2)
            G = sb("G", [128, NT], F32, es2)
            cb = sb("cb", [128, NQ], F32, es2)
            biasj = [sb("biasj%d" % i, [128, NT], F32, es2) for i in range(2)]
            sm = sb("sm", [128, 8], F32, es2)
            ot = [sb("ot%d" % i, [128, 128], F32, es2) for i in range(2)]
            oj = sb("oj", [128, 128], F32, es2)
            ob = [sb("ob%d" % i, [128, 128], BF16, es2) for i in range(2)]
            tk.op("dve", lambda e: e.memset(onesn[:], 1.0), writes=["onesn"])
            ptc = [0]
            oc = [0]
            for hh in (range(2) if 'A' in PH else []):
                for c0 in range(0, S, 2048):
                    tk.dma("sp", KT[:, c0:min(S, c0 + 2048)], kt_d[hh][:, c0:min(S, c0 + 2048)], writes=["KT"])
                for c0 in range(0, NT, 16):
                    tk.dma("sp", VA[:, c0:min(NT, c0 + 16), :], va_d[hh][:, c0:min(NT, c0 + 16), :], writes=["VA"])
                tk.op("act", lambda e, hh=hh: e.activation(out=tmpg[:], in_=gates[:, :, hh], func=AF.Exp,
                                                           scale=-1.0, bias=ngb[:, hh:hh + 1]),
                      reads=["gates", "ngb"], writes=["tmpg"])
                tk.op("act", lambda e: e.activation(out=lfn[:], in_=tmpg[:], func=AF.Ln, scale=1.0, bias=onec[:]),
                      reads=["tmpg", "onec"], writes=["lfn"])
                for c0 in range(0, NT, 32):
                    c1 = min(NT, c0 + 32)
                    tk.op("pe", lambda e, c0=c0, c1=c1: e.matmul(psum[0][:, c0:c1], lhsT=triu[:], rhs=lfn[:, c0:c1],
                                                                 start=True, stop=True),
                          reads=["lfn", "c2"], writes=["psb0"])
                    tk.op("pe", lambda e, c0=c0, c1=c1: e.matmul(psum[1][:, c0:c1], lhsT=ones_f[:], rhs=lfn[:, c0:c1],
                                                                 start=True, stop=True),
                          reads=["lfn", "c3"], writes=["psb1"])
                tk.op("act", lambda e: e.copy(out=tot[:], in_=psum[1][:, 0:NT]), reads=["psb1"], writes=["tot"])
                tk.op("act", lambda e: e.copy(out=G[:], in_=psum[0][:, 0:NT]), reads=["psb0"], writes=["G"])
                tk.op("dve", lambda e: e.tensor_tensor_scan(out=offi[:], data0=onesn[:], data1=tot[:], initial=0.0,
                                                            op0=ALU.mult, op1=ALU.add),
                      reads=["tot", "onesn"], writes=["offi"])
                tk.op("dve", lambda e: e.tensor_tensor(out=offi[:], in0=offi[:], in1=tot[:], op=ALU.subtract),
                      reads=["offi", "tot"], writes=["offi"])
                tk.op("dve", lambda e: e.tensor_tensor(out=G[:], in0=G[:], in1=offi[:], op=ALU.add),
                      reads=["G", "offi"], writes=["G"])
                Gsel = G[:].rearrange("p (j r) -> p j r", r=4)[:, :, 2]
                tk.op("dve", lambda e, Gsel=Gsel: e.tensor_copy(out=tmpg[:, 0:NQ], in_=Gsel), reads=["G"],
                      writes=["tmpg"])
                tk.op("pe", lambda e: e.matmul(psum[1][:, 0:NQ], lhsT=esel[:], rhs=tmpg[:, 0:NQ], start=True, stop=True),
                      reads=["tmpg", "c4"], writes=["psb1"])
                tk.op("act", lambda e: e.copy(out=cb[:], in_=psum[1][:, 0:NQ]), reads=["psb1"], writes=["cb"])
                for j in range(NQ):
                    qs = j % 2
                    tk.dma("sp", qt[qs][:], qt_d[hh][:, j * 512:(j + 1) * 512], writes=["qt%d" % qs])
                    bj = j % 2
                    nk = 4 * j + 4
                    tk.op("dve", lambda e, bj=bj, j=j, nk=nk: e.tensor_scalar(
                        out=biasj[bj][:, 0:nk], in0=G[:, 0:nk], scalar1=cb[:, j:j + 1], scalar2=None,
                        op0=ALU.subtract),
                        reads=["G", "cb"], writes=["bj%d" % bj])
                    for i in range(nk):
                        r = i - 4 * j
                        sb_ = 2 + (i % 2)
                        tk.op("pe", lambda e, i=i, qs=qs, sb_=sb_: e.matmul(
                            psum[sb_][:, :], lhsT=KT[:, i * 128:(i + 1) * 128], rhs=qt[qs][:], start=True, stop=True),
                            reads=["KT", "qt%d" % qs], writes=["psb%d" % sb_])
                        p = ptc[0] % 3
                        ptc[0] += 1
                        tk.op("act", lambda e, p=p, sb_=sb_, bj=bj, i=i: e.activation(
                            out=PT[p][:], in_=psum[sb_][:, :], func=AF.Exp, scale=SC, bias=biasj[bj][:, i:i + 1]),
                            reads=["psb%d" % sb_, "bj%d" % bj], writes=["PT%d" % p])
                        if r >= 0:
                            tk.op("dve", lambda e, p=p, r=r: e.tensor_tensor(
                                out=PT[p][:], in0=PT[p][:], in1=amask[:, r, :], op=ALU.mult),
                                reads=["PT%d" % p, "c5"], writes=["PT%d" % p])
                        for u in range(4):
                            if r > u:
                                continue
                            last = 4 * j + u
                            tk.op("pe", lambda e, p=p, u=u, i=i, last=last: e.matmul(
                                psum[4 + u][:, 0:130], lhsT=PT[p][:, u * 128:(u + 1) * 128], rhs=VA[:, i, :],
                                start=(i == 0), stop=(i == last)),
                                reads=["PT%d" % p, "VA"], writes=["psb%d" % (4 + u)])
                    for u in range(4):
                        o = oc[0] % 2
                        oc[0] += 1
                        pu = psum[4 + u]
                        tk.op("dve", lambda e, pu=pu: e.reciprocal(out=sm[:, 0:1], in_=pu[:, 128:129]),
                              reads=["psb%d" % (4 + u)], writes=["sm0"])
                        tk.op("dve", lambda e, pu=pu, o=o: e.tensor_scalar(
                            out=ot[o][:], in0=pu[:, 0:128], scalar1=sm[:, 0:1], scalar2=None, op0=ALU.mult),
                            reads=["psb%d" % (4 + u), "sm0"], writes=["ot%d" % o])
                        tk.op("act", lambda e, o=o: e.activation(out=oj[:], in_=ot[o][:], func=AF.Square,
                                                                 accum_out=sm[:, 1:2]),
                              reads=["ot%d" % o], writes=["oj", "sm1"])
                        tk.op("act", lambda e: e.activation(out=sm[:, 2:3], in_=sm[:, 1:2], func=AF.Ln,
                                                            scale=1.0 / 128, bias=epsc[:]),
                              reads=["sm1", "epsc"], writes=["sm2"])
                        tk.op("act", lambda e: e.activation(out=sm[:, 3:4], in_=sm[:, 2:3], func=AF.Exp, scale=-0.5),
                              reads=["sm2"], writes=["sm3"])
                        tk.op("dve", lambda e, o=o, hh=hh: e.scalar_tensor_tensor(
                            out=ob[o][:], in0=ot[o][:], scalar=sm[:, 3:4], in1=hnw_s[:, hh * 128:(hh + 1) * 128],
                            op0=ALU.mult, op1=ALU.mult),
                            reads=["ot%d" % o, "sm3", "c9"], writes=["ob%d" % o])
                        t0 = j * 512 + u * 128
                        tk.dma("pool", mix_dst(t0, hh * 128, (hh + 1) * 128), ob[o][:], reads=["ob%d" % o],
                               key="ob%d" % o)
                tk.barrier()

        NCH = NT
        with ExitStack() as es3:
            zq = [sb("zq%d" % i, [128, 515], F32, es3) for i in range(2)]
            zk = [sb("zk%d" % i, [128, 515], F32, es3) for i in range(2)]
            acc = sb("acc", [128, 512], F32, es3)
            ex = sb("ex", [128, 512], F32, es3)
            qT = [sb("qT%d" % i, [128, 512], BF16, es3) for i in range(2)]
            kT = [sb("kT%d" % i, [128, 512], BF16, es3) for i in range(2)]
            mvo = [sb("mvo%d" % i, [128, 512], F32, es3) for i in range(2)]
            vt = [sb("vt%d" % i, [128, 258], BF16, es3) for i in range(2)]
            ktok = [sb("ktok%d" % i, [128, 128], BF16, es3) for i in range(2)]
            smt = [sb("smt%d" % i, [128, 128], BF16, es3) for i in range(2)]
            Cf = sb("Cf", [128, 258], F32, es3)
            Cb = [sb("Cb%d" % i, [128, 258], BF16, es3) for i in range(2)]
            lf = sb("mlf", [128, NCH], F32, es3)
            tmpm = sb("tmpm", [128, NCH], F32, es3)
            bcs = sb("bcs", [128, NCH], F32, es3)
            eb = sb("eb", [128, NCH], F32, es3)
            ek = sb("ek", [128, NCH], F32, es3)
            ebl = sb("ebl", [128, NCH], F32, es3)
            hsm = sb("hsm", [128, 8], F32, es3)
            hv = [sb("hv%d" % i, [128, 256], F32, es3) for i in range(2)]
            sg = sb("sg", [128, 256], F32, es3)
            hj = sb("hj", [128, 256], F32, es3)
            hb = [sb("hb%d" % i, [128, 256], BF16, es3) for i in range(2)]
            if 'M' in PH or os.environ.get('MLPRE'):
                _lim = int(os.environ.get('MLPRE', '99'))
                _real_op = tk.op
                _cnt = [0]
                def _lop(*a, **k):
                    _cnt[0] += 1
                    if _cnt[0] <= _lim:
                        return _real_op(*a, **k)
                tk.op = _lop
                tk.op("act", lambda e: e.activation(out=tmpm[:], in_=gates[:, :, 3], func=AF.Exp, scale=-1.0,
                                                    bias=ngb[:, 3:4]), reads=["gates", "ngb"], writes=["tmpm"])
                tk.op("act", lambda e: e.activation(out=lf[:], in_=tmpm[:], func=AF.Ln, scale=1.0, bias=onec[:]),
                      reads=["tmpm", "onec"], writes=["mlf"])
                tk.op("dve", lambda e: e.tensor_scalar(out=lf[:], in0=lf[:], scalar1=-1.0, scalar2=None, op0=ALU.mult),
                      reads=["mlf"], writes=["mlf"])
                for c0 in range(0, NCH, 32):
                    c1 = min(NCH, c0 + 32)
                    tk.op("pe", lambda e, c0=c0, c1=c1: e.matmul(psum[0][:, c0:c1], lhsT=triu[:], rhs=lf[:, c0:c1],
                                                                 start=True, stop=True),
                          reads=["mlf", "c2"], writes=["psb0"])
                    tk.op("pe", lambda e, c0=c0, c1=c1: e.matmul(psum[1][:, c0:c1], lhsT=ones_f[:], rhs=lf[:, c0:c1],
                                                                 start=True, stop=True),
                          reads=["mlf", "c3"], writes=["psb1"])
                tk.op("act", lambda e: e.activation(out=eb[:], in_=psum[0][:, 0:NCH], func=AF.Exp),
                      reads=["psb0"], writes=["eb"])
                tk.op("act", lambda e: e.activation(out=ebl[:], in_=psum[1][:, 0:NCH], func=AF.Exp),
                      reads=["psb1"], writes=["ebl"])
                tk.op("act", lambda e: e.copy(out=tmpm[:], in_=psum[0][:, 0:NCH]), reads=["psb0"], writes=["tmpm"])
                tk.op("act", lambda e: e.copy(out=bcs[:], in_=gates[:, :, 2]), reads=["gates"], writes=["bcs"])
                tk.op("dve", lambda e: e.tensor_tensor(out=bcs[:], in0=bcs[:], in1=tmpm[:], op=ALU.subtract),
                      reads=["bcs", "tmpm"], writes=["bcs"])
                tk.op("act", lambda e: e.activation(out=ek[:], in_=bcs[:], func=AF.Exp, bias=gb[:, 2:3], scale=1.0),
                      reads=["bcs", "c7"], writes=["ek"])
            if 'M' in PH or os.environ.get('MLPRE'):
                tk.op = _real_op
            tk.op("dve", lambda e: e.memset(Cf[:], 0.0), writes=["Cf"])
            tk.op("dve", lambda e: e.memset(Cb[0][:], 0.0), writes=["Cb0"])
            for i in range(2):
                tk.op("dve", lambda e, i=i: e.memset(zq[i][:, 0:3], 0.0), writes=["zq%d" % i])
                tk.op("dve", lambda e, i=i: e.memset(zk[i][:, 0:3], 0.0), writes=["zk%d" % i])
                tk.op("dve", lambda e, i=i: e.memset(vt[i][:], 0.0), writes=["vt%d" % i])
            for jb in (range(NB) if 'M' in PH else []):
                s = jb % 2
                for (z, zn, zd, wo, bo, dstT, dn) in [(zq, "zq", mq_d, 0, 8, qT, "qT"), (zk, "zk", mk_d, 4, 9, kT, "kT")]:
                    tk.dma("sp", z[s][:, 3:515], zd[:, jb * 512:(jb + 1) * 512], writes=["%s%d" % (zn, s)])
                    if jb > 0:
                        tk.op("act", lambda e, z=z, s=s: e.copy(out=z[s][:, 0:3], in_=z[1 - s][:, 512:515]),
                              reads=["%s%d" % (zn, 1 - s)], writes=["%s%d" % (zn, s)])
                    tk.op("dve", lambda e, z=z, s=s, wo=wo, bo=bo: e.tensor_scalar(
                        out=acc[:], in0=z[s][:, 0:512], scalar1=cvp[:, wo:wo + 1], scalar2=cvp[:, bo:bo + 1],
                        op0=ALU.mult, op1=ALU.add), reads=["%s%d" % (zn, s), "c8"], writes=["acc"])
                    for jj in range(1, 4):
                        tk.op("dve", lambda e, z=z, s=s, wo=wo, jj=jj: e.scalar_tensor_tensor(
                            out=acc[:], in0=z[s][:, jj:jj + 512], scalar=cvp[:, wo + jj:wo + jj + 1], in1=acc[:],
                            op0=ALU.mult, op1=ALU.add), reads=["%s%d" % (zn, s), "c8", "acc"], writes=["acc"])
                    tk.op("act", lambda e: e.activation(out=ex[:], in_=acc[:], func=AF.Exp, scale=-1.0),
                          reads=["acc"], writes=["ex"])
                    tk.op("dve", lambda e: e.tensor_scalar(out=ex[:], in0=ex[:], scalar1=1.0, scalar2=None, op0=ALU.add),
                          reads=["ex"], writes=["ex"])
                    tk.op("dve", lambda e: e.reciprocal(out=ex[:], in_=ex[:]), reads=["ex"], writes=["ex"])
                    tk.op("dve", lambda e, dstT=dstT, s=s: e.tensor_tensor(out=dstT[s][:], in0=acc[:], in1=ex[:],
                                                                           op=ALU.mult),
                          reads=["acc", "ex"], writes=["%s%d" % (dn, s)])
                for cc in range(4):
                    c = jb * 4 + cc
                    cs = c % 2
                    csl = slice(cc * 128, (cc + 1) * 128)
                    tk.dma("sp", mvo[cs][:], mvo_d[c * 128:(c + 1) * 128, :], writes=["mvo%d" % cs])
                    pkt = psum[2][:, :].bitcast(BF16)
                    tk.op("pe", lambda e, s=s, csl=csl, pkt=pkt: e.transpose(out=pkt[:, 0:128], in_=kT[s][:, csl],
                                                                             identity=ident_bf[:]),
                          reads=["kT%d" % s, "c0"], writes=["psb2"])
                    tk.op("act", lambda e, cs=cs, pkt=pkt: e.activation(out=ktok[cs][:], in_=pkt[:, 0:128],
                                                                        func=AF.Copy, scale=SC),
                          reads=["psb2"], writes=["ktok%d" % cs])
                    tk.op("dve", lambda e, cs=cs, c=c: e.tensor_scalar(
                        out=vt[cs][:, 0:256], in0=mvo[cs][:, 0:256], scalar1=ek[:, c:c + 1], scalar2=None, op0=ALU.mult),
                        reads=["mvo%d" % cs, "ek"], writes=["vt%d" % cs])
                    tk.op("dve", lambda e, cs=cs, c=c: e.tensor_copy(out=vt[cs][:, 256:257], in_=ek[:, c:c + 1]),
                          reads=["ek"], writes=["vt%d" % cs])
                    tk.op("pe", lambda e, s=s, csl=csl: e.matmul(psum[3][:, 0:128], lhsT=kT[s][:, csl], rhs=qT[s][:, csl],
                                                                 start=True, stop=True),
                          reads=["kT%d" % s, "qT%d" % s], writes=["psb3"])
                    tk.op("dve", lambda e, cs=cs: e.tensor_tensor(out=smt[cs][:], in0=psum[3][:, 0:128], in1=mmask[:],
                                                                  op=ALU.mult),
                          reads=["psb3", "c6"], writes=["smt%d" % cs])
                    tk.op("pe", lambda e, cs=cs: e.matmul(psum[4][:, 0:258], lhsT=smt[cs][:], rhs=vt[cs][:],
                                                          start=True, stop=False),
                          reads=["smt%d" % cs, "vt%d" % cs], writes=["psb4"])
                    tk.op("pe", lambda e, s=s, csl=csl, cs=cs: e.matmul(psum[4][:, 0:258], lhsT=qT[s][:, csl],
                                                                        rhs=Cb[cs][:], start=False, stop=True),
                          reads=["qT%d" % s, "Cb%d" % cs], writes=["psb4"])
                    tk.op("pe", lambda e, cs=cs: e.matmul(psum[5][:, 0:258], lhsT=ktok[cs][:], rhs=vt[cs][:],
                                                          start=True, stop=True),
                          reads=["ktok%d" % cs, "vt%d" % cs], writes=["psb5"])
                    tk.op("dve", lambda e: e.tensor_tensor(out=Cf[:], in0=psum[5][:, 0:258], in1=Cf[:], op=ALU.add),
                          reads=["psb5", "Cf"], writes=["Cf"])
                    tk.op("dve", lambda e, c=c: e.tensor_scalar(out=Cf[:], in0=Cf[:], scalar1=ebl[:, c:c + 1],
                                                                scalar2=None, op0=ALU.mult),
                          reads=["Cf", "ebl"], writes=["Cf"])
                    tk.op("act", lambda e, cs=cs: e.copy(out=Cb[1 - cs][:], in_=Cf[:]), reads=["Cf"],
                          writes=["Cb%d" % (1 - cs)])
                    tk.op("act", lambda e, c=c: e.activation(out=hsm[:, 6:7], in_=psum[4][:, 256:257], func=AF.Abs,
                                                             scale=eb[:, c:c + 1]),
                          reads=["psb4", "eb"], writes=["hsm6"])
                    tk.op("dve", lambda e: e.tensor_scalar(out=hsm[:, 0:1], in0=hsm[:, 6:7], scalar1=1.0, scalar2=None,
                                                           op0=ALU.max),
                          reads=["hsm6"], writes=["hsm0"])
                    tk.op("dve", lambda e: e.reciprocal(out=hsm[:, 1:2], in_=hsm[:, 0:1]), reads=["hsm0"], writes=["hsm1"])
                    tk.op("dve", lambda e, c=c: e.tensor_tensor(out=hsm[:, 2:3], in0=hsm[:, 1:2], in1=eb[:, c:c + 1],
                                                                op=ALU.mult), reads=["hsm1", "eb"], writes=["hsm2"])
                    tk.op("act", lambda e, cs=cs: e.activation(out=sg[:], in_=mvo[cs][:, 256:512], func=AF.Exp, scale=-1.0),
                          reads=["mvo%d" % cs], writes=["sg"])
                    tk.op("dve", lambda e: e.tensor_scalar(out=sg[:], in0=sg[:], scalar1=1.0, scalar2=None, op0=ALU.add),
                          reads=["sg"], writes=["sg"])
                    tk.op("dve", lambda e: e.reciprocal(out=sg[:], in_=sg[:]), reads=["sg"], writes=["sg"])
                    tk.op("dve", lambda e, cs=cs: e.scalar_tensor_tensor(
                        out=hv[cs][:], in0=psum[4][:, 0:256], scalar=hsm[:, 2:3], in1=sg[:], op0=ALU.mult, op1=ALU.mult),
                        reads=["psb4", "hsm2", "sg"], writes=["hv%d" % cs])
                    tk.op("act", lambda e, cs=cs: e.activation(out=hj[:], in_=hv[cs][:], func=AF.Square,
                                                               accum_out=hsm[:, 3:4]),
                          reads=["hv%d" % cs], writes=["hj", "hsm3"])
                    tk.op("act", lambda e: e.activation(out=hsm[:, 4:5], in_=hsm[:, 3:4], func=AF.Ln, scale=1.0 / 256,
                                                        bias=epsc[:]), reads=["hsm3", "epsc"], writes=["hsm4"])
                    tk.op("act", lambda e: e.activation(out=hsm[:, 5:6], in_=hsm[:, 4:5], func=AF.Exp, scale=-0.5),
                          reads=["hsm4"], writes=["hsm5"])
                    tk.op("dve", lambda e, cs=cs: e.scalar_tensor_tensor(
                        out=hb[cs][:], in0=hv[cs][:], scalar=hsm[:, 5:6], in1=hnw_s[:, 256:512], op0=ALU.mult,
                        op1=ALU.mult), reads=["hv%d" % cs, "hsm5", "c9"], writes=["hb%d" % cs])
                    tk.dma("pool", mix_dst(c * 128, 256, 512), hb[cs][:], reads=["hb%d" % cs],
                           key="hb%d" % cs)
            tk.barrier()
        if own_tk:
            tk.final_wait("sp")
    print("phase A instructions:", tk.ninst)
    return nc


def host_inputs_a(inp, S):
    c = host_consts()
    w_in = np.asarray(inp["w_in"][0])
    maps = []
    for core in range(8):
        b, g = core // 4, core % 4
        h0, h1 = 2 * g, 2 * g + 1
        cols = []
        for base in (0, 1024, 2048):
            cols += list(range(base + h0 * 128, base + h0 * 128 + 128)) + list(range(base + h1 * 128, base + h1 * 128 + 128))
        cols += list(range(3080 + g * 128, 3080 + g * 128 + 128))
        cols += list(range(3592 + g * 128, 3592 + g * 128 + 128))
        cols += list(range(4104 + g * 256, 4104 + g * 256 + 256))
        cols += list(range(5136 + g * 256, 5136 + g * 256 + 256))
        cols += [3072 + h0, 3072 + h1, 5128 + g, 5132 + g]
        wcs = np.ascontiguousarray(w_in[:, cols])
        gbv = np.array([inp["fox_f_bias"][0][h0], inp["fox_f_bias"][0][h1], inp["mlstm_i_bias"][0][g],
                        inp["mlstm_f_bias"][0][g]], np.float32)
        cw = np.asarray(inp["mlstm_conv_w"][0])
        cbv = np.asarray(inp["mlstm_conv_b"][0])
        convp = np.concatenate([cw[:, g * 128:(g + 1) * 128].T, cw[:, 512 + g * 128:512 + (g + 1) * 128].T,
                                cbv[g * 128:(g + 1) * 128][:, None], cbv[512 + g * 128:512 + (g + 1) * 128][:, None]],
                               axis=1).astype(np.float32)
        hn = np.concatenate([np.asarray(inp["fox_out_norm_w"][0])[h0 * 128:(h1 + 1) * 128],
                             np.asarray(inp["mlstm_out_norm_w"][0])[g * 256:(g + 1) * 256]])
        m = {"x": np.ascontiguousarray(np.asarray(inp["x"])[b, :int(os.environ.get("XROWS", S))]),
             "wc": wcs,
             "n1w": np.ascontiguousarray(np.asarray(inp["norm1_w"][0]).reshape(KC, 128).T),
             "gbias": np.ascontiguousarray(np.broadcast_to(gbv[None, :], (128, 4))),
             "convp": np.ascontiguousarray(convp),
             "hnw": np.ascontiguousarray(np.broadcast_to(hn[None, :], (128, 512))).astype(np.float32)}
        m.update(c)
        maps.append(m)
    return maps


import os


def host_consts_b():
    c = {}
    c["ident_bf"] = np.eye(128, dtype=np.float32).astype(ml_dtypes.bfloat16)
    c["ident_f"] = np.eye(128, dtype=np.float32)
    c["iota_row"] = np.ascontiguousarray(np.broadcast_to(np.arange(128, dtype=np.float32)[None, :], (128, 128)))
    c["thr16"] = np.ascontiguousarray(np.broadcast_to((16.0 * np.arange(16, dtype=np.float32))[None, :], (128, 16)))
    c["iota16"] = np.ascontiguousarray(np.broadcast_to(np.arange(16, dtype=np.float32)[None, :], (128, 16)))
    return c


def build_phase_b(nc, TC, NJ=128, tk=None, gath=None, pfx="", pre=None):
    NTT = TC // 128
    PB = min(256, TC)
    NBLK = TC // PB
    TPB = PB // 128
    own_tk = tk is None
    if own_tk:
        tk = Trk(nc)
    _dt = nc.dram_tensor

    def dt(name, *a, **k):
        return _dt(pfx + name, *a, **k)
    x = dt("x", [TC, D], F32, kind="ExternalInput").ap()
    if gath is None:
        mixed = dt("mixed", [TC, D], BF16, kind="ExternalInput").ap()
    else:
        wsel_d = dt("wsel", [128, 8], F32, kind="ExternalInput").ap()
    wout = dt("wout", [D, D], F32, kind="ExternalInput").ap()
    wq = dt("wq", [D, D], F32, kind="ExternalInput").ap()
    n2w = dt("n2w", [128, D], F32, kind="ExternalInput").ap()
    fnw = dt("fnw", [128, D], F32, kind="ExternalInput").ap()
    keysT = dt("keysT", [128, 2, 128], F32, kind="ExternalInput").ap()
    if pre is None:
        uh = dt("uh", [128, 128, D], F32, kind="ExternalInput").ap()
        vh = dt("vh", [128, 128, D], F32, kind="ExternalInput").ap()
    ident_bf_d = dt("ident_bf", [128, 128], BF16, kind="ExternalInput").ap()
    ident_f_d = dt("ident_f", [128, 128], F32, kind="ExternalInput").ap()
    iota_row_d = dt("iota_row", [128, 128], F32, kind="ExternalInput").ap()
    thr16_d = dt("thr16", [128, 16], F32, kind="ExternalInput").ap()
    iota16_d = dt("iota16", [128, 16], F32, kind="ExternalInput").ap()
    out = dt("out", [TC, D], F32, kind="ExternalOutput").ap()
    if pre is None:
        u16 = dt("u16", [128, 128, D], BF16, kind="Internal").ap()
        v16 = dt("v16", [128, 128, D], BF16, kind="Internal").ap()
    else:
        u16, v16 = pre
    x1_d = dt("x1_d", [TC, D], F32, kind="Internal").ap()
    h2T_d = dt("h2T_d", [128, KC, TC], BF16, kind="Internal").ap()
    slot_d = dt("slot_d", [128, 3, TC], F32, kind="Internal").ap()

    with ExitStack() as es0:
        def sb(name, shape, dtype, es=es0):
            return es.enter_context(nc.sbuf_tensor(pfx + name, shape, dtype))
        ident_bf = sb("ident_bf_s", [128, 128], BF16)
        ident_f = sb("ident_f_s", [128, 128], F32)
        iota_row = sb("iota_row_s", [128, 128], F32)
        thr16 = sb("thr16_s", [128, 16], F32)
        iota16 = sb("iota16_s", [128, 16], F32)
        epsc = sb("epsc", [128, 1], F32)
        psum = [es0.enter_context(nc.psum_tensor(pfx + "ps%d" % i, [128, 512], F32)) for i in range(8)]
        for i, (s_, d_) in enumerate([(ident_bf, ident_bf_d), (ident_f, ident_f_d), (iota_row, iota_row_d),
                                      (thr16, thr16_d), (iota16, iota16_d)]):
            tk.dma("sp", s_[:], d_, writes=["c%d" % i], key="const")
        for j in (range(NJ) if pre is None else []):
            tk.dma("pool", u16[j], uh[j], writes=["u16"], key="cvt")
            tk.dma("pool", v16[j], vh[j], writes=["v16"], key="cvt")
        tk.op("dve", lambda e: e.memset(epsc[:], EPS), writes=["epsc"])

        def rms_rstd(src_ap, srckey, ss, junk):
            tk.op("act", lambda e: e.activation(out=junk[:], in_=src_ap, func=AF.Square, accum_out=ss[:, 0:1]),
                  reads=[srckey], writes=["junk", "ss0"])
            tk.op("act", lambda e: e.activation(out=ss[:, 1:2], in_=ss[:, 0:1], func=AF.Ln, scale=1.0 / D, bias=epsc[:]),
                  reads=["ss0", "epsc"], writes=["ss1"])
            tk.op("act", lambda e: e.activation(out=ss[:, 2:3], in_=ss[:, 1:2], func=AF.Exp, scale=-0.5),
                  reads=["ss1"], writes=["ss2"])

        def transpose16(src, srckey, dst, dstkey, banks):
            for half in range(2):
                bk = banks[half]
                pst = psum[bk][:, :].bitcast(BF16)
                for k8 in range(8):
                    kc = half * 8 + k8
                    tk.op("pe", lambda e, kc=kc, k8=k8, pst=pst: e.transpose(
                        out=pst[:, k8 * 128:(k8 + 1) * 128], in_=src[:, kc * 128:(kc + 1) * 128], identity=ident_bf[:]),
                        reads=[srckey, "c0"], writes=["psb%d" % bk])
                srcv = pst.rearrange("p (k t) -> p k t", k=8)
                dstv = dst[:, half * 8:(half + 1) * 8, :]
                if half == 0:
                    tk.op("act", lambda e, srcv=srcv, dstv=dstv: e.copy(out=dstv, in_=srcv),
                          reads=["psb%d" % bk], writes=[dstkey])
                else:
                    tk.op("dve", lambda e, srcv=srcv, dstv=dstv: e.tensor_copy(out=dstv, in_=srcv),
                          reads=["psb%d" % bk], writes=[dstkey])

        with ExitStack() as es1:
            Wo = sb("Wo", [128, KC, D], BF16, es1)
            n2b = sb("n2b", [128, D], F32, es1)
            mx = [sb("mx%d" % i, [128, D], BF16, es1) for i in range(2)]
            xt = [sb("xt%d" % i, [128, D], F32, es1) for i in range(2)]
            x1 = [sb("x1_%d" % i, [128, D], F32, es1) for i in range(2)]
            mT = sb("mT", [128, KC, 128], BF16, es1)
            h2 = sb("h2", [128, D], BF16, es1)
            h2t = [sb("h2t%d" % i, [128, KC, 128], BF16, es1) for i in range(2)]
            junk = sb("junk", [128, D], BF16, es1)
            ss = sb("ss", [128, 4], F32, es1)
            for kc in range(KC):
                tk.dma("pool", Wo[:, kc, :], wout[kc * 128:(kc + 1) * 128, :], writes=["Wo"], key="wload")
            tk.dma("sp", n2b[:], n2w, writes=["n2b"])
            ccnt = [0]
            if gath is not None:
                cand = [sb("cand%d" % i, [128, D], BF16, es1) for i in range(3)]
                wsel = sb("wsel_s", [128, 8], F32, es1)
                tk.dma("sp", wsel[:], wsel_d, writes=["wsel"])
            for ti in range(NTT):
                s = ti % 2
                rows = slice(ti * 128, (ti + 1) * 128)
                if gath is None:
                    tk.dma("sp", mx[s][:], mixed[rows, :], writes=["mx%d" % s])
                else:
                    bb_, lt = ti // (NTT // 2), ti % (NTT // 2)
                    for dp in range(8):
                        cs_ = ccnt[0] % 3
                        ccnt[0] += 1
                        gt_ = dp * (NTT // 2) + lt
                        kq = gt_ // 4
                        src = gath[kq].ap().rearrange("(r t) c -> r t c", r=8)[bb_ * 4:(bb_ + 1) * 4,
                                                                              (gt_ % 4) * 128:(gt_ % 4 + 1) * 128, :]
                        tk.dma("sp", cand[cs_][:].rearrange("p (g c) -> p g c", g=4), src.rearrange("g t c -> t g c"),
                               reads=["gath%d" % kq], writes=["cand%d" % cs_])
                        if dp == 0:
                            tk.op("dve", lambda e, cs_=cs_, s=s: e.tensor_scalar(
                                out=mx[s][:], in0=cand[cs_][:], scalar1=wsel[:, 0:1], scalar2=None, op0=ALU.mult),
                                reads=["cand%d" % cs_, "wsel"], writes=["mx%d" % s])
                        else:
                            tk.op("dve", lambda e, cs_=cs_, s=s, dp=dp: e.scalar_tensor_tensor(
                                out=mx[s][:], in0=cand[cs_][:], scalar=wsel[:, dp:dp + 1], in1=mx[s][:], op0=ALU.mult,
                                op1=ALU.add), reads=["cand%d" % cs_, "wsel", "mx%d" % s], writes=["mx%d" % s])
                tk.dma("sp", xt[s][:], x[rows, :], writes=["xt%d" % s])
                transpose16(mx[s], "mx%d" % s, mT, "mT", (0, 1))
                for cg in range(4):
                    b = 2 + cg
                    for kc in range(KC):
                        tk.op("pe", lambda e, kc=kc, b=b, cg=cg: e.matmul(
                            psum[b][:, :], lhsT=mT[:, kc, :], rhs=Wo[:, kc, cg * 512:(cg + 1) * 512],
                            start=(kc == 0), stop=(kc == KC - 1)), reads=["mT", "Wo"], writes=["psb%d" % b])
                    tk.op("dve", lambda e, b=b, cg=cg, s=s: e.tensor_tensor(
                        out=x1[s][:, cg * 512:(cg + 1) * 512], in0=psum[b][:, :], in1=xt[s][:, cg * 512:(cg + 1) * 512],
                        op=ALU.add), reads=["psb%d" % b, "xt%d" % s], writes=["x1_%d" % s])
                tk.dma("pool", x1_d[rows, :], x1[s][:], reads=["x1_%d" % s], key="x1st%d" % s)
                rms_rstd(x1[s][:], "x1_%d" % s, ss, junk)
                tk.op("dve", lambda e, s=s: e.scalar_tensor_tensor(out=h2[:], in0=x1[s][:], scalar=ss[:, 2:3], in1=n2b[:],
                                                                   op0=ALU.mult, op1=ALU.mult),
                      reads=["x1_%d" % s, "ss2", "n2b"], writes=["h2"])
                transpose16(h2, "h2", h2t[s], "h2t%d" % s, (6, 7))
                tk.dma("pool", h2T_d[:, :, rows], h2t[s][:], reads=["h2t%d" % s], key="h2st%d" % s)
            tk.barrier()

        if os.environ.get('BSTOP') == '1':
            tk.final_wait('sp')
            return nc
        with ExitStack() as es2:
            Wq = sb("Wq", [128, KC, D], BF16, es2)
            kT = sb("kTs", [128, 2, 128], BF16, es2)
            h2t = [sb("h2tb%d" % i, [128, KC, 128], BF16, es2) for i in range(2)]
            qpT = [sb("qpT%d" % i, [128, 128], BF16, es2) for i in range(2)]
            sc = sb("sc", [128, 16, 128], F32, es2)
            tmp1 = sb("tmp1", [128, 128], F32, es2)
            st = sb("st", [128, 16, 16], F32, es2)
            iu = sb("iu", [128, 16, 16], U32, es2)
            itf = sb("itf", [128, 16, 16], F32, es2)
            dd = sb("dd", [128, 16, 16], F32, es2)
            cand = sb("cand", [128, 8, 256], F32, es2)
            tmp2 = sb("tmp2", [128, 256], F32, es2)
            cf = sb("cf", [128, 8, 16], F32, es2)
            pu = sb("pu", [128, 8, 16], U32, es2)
            posf = sb("posf", [128, 8, 16], F32, es2)
            cs = sb("cs", [128, 8, 16], F32, es2)
            zs = sb("zs", [128, 8], F32, es2)
            ge = sb("ge", [128, 8, 16, 16], F32, es2)
            prod = sb("prod", [128, 8, 16, 16], F32, es2)
            k1s = sb("k1s", [128, 8, 16], F32, es2)
            k2f = sb("k2f", [128, 8, 16], F32, es2)
            res = sb("res", [128, 3, 128], F32, es2)
            rst = [sb("rst%d" % i, [128, 3, 128], F32, es2) for i in range(2)]
            for kc in range(KC):
                tk.dma("pool", Wq[:, kc, :], wq[kc * 128:(kc + 1) * 128, :], writes=["Wq"], key="wload")
            tk.dma("pool", kT[:], keysT, writes=["kT"], key="wload")
            st4 = st[:].rearrange("p (h q) k -> p h q k", q=2)
            itf4 = itf[:].rearrange("p (h q) k -> p h q k", q=2)
            dd4 = dd[:].rearrange("p (h q) k -> p h q k", q=2)
            for ti in range(NTT):
                s = ti % 2
                cols = slice(ti * 128, (ti + 1) * 128)
                tk.dma("sp", h2t[s][:], h2T_d[:, :, cols], writes=["h2tb%d" % s])
                for blk in range(16):
                    b = blk % 2
                    p = blk % 2
                    for kc in range(KC):
                        tk.op("pe", lambda e, kc=kc, b=b, blk=blk: e.matmul(
                            psum[b][:, 0:128], lhsT=Wq[:, kc, blk * 128:(blk + 1) * 128], rhs=h2t[s][:, kc, :],
                            start=(kc == 0), stop=(kc == KC - 1)), reads=["Wq", "h2tb%d" % s], writes=["psb%d" % b])
                    tk.op("act", lambda e, b=b: e.copy(out=qpT[b][:], in_=psum[b][:, 0:128]),
                          reads=["psb%d" % b], writes=["qpT%d" % b])
                    sbk = 2 + blk // 4
                    tk.op("pe", lambda e, b=b, p=p, sbk=sbk, blk=blk: e.matmul(
                        psum[sbk][:, (blk % 4) * 128:(blk % 4 + 1) * 128], lhsT=qpT[b][:], rhs=kT[:, p, :],
                        start=True, stop=True), reads=["qpT%d" % b, "kT"], writes=["psb%d" % sbk])
                    if blk % 4 == 3:
                        tk.op("dve", lambda e, sbk=sbk, blk=blk: e.tensor_copy(
                            out=sc[:, blk - 3:blk + 1, :], in_=psum[sbk][:, :].rearrange("p (a n) -> p a n", a=4)),
                            reads=["psb%d" % sbk], writes=["sc"])
                for blk in range(16):
                    tk.op("dve", lambda e, blk=blk: e.max(out=st[:, blk, 0:8], in_=sc[:, blk, :]),
                          reads=["sc"], writes=["st"])
                    tk.op("dve", lambda e, blk=blk: e.max_index(out=iu[:, blk, 0:8], in_max=st[:, blk, 0:8],
                                                                in_values=sc[:, blk, :]),
                          reads=["sc", "st"], writes=["iu"])
                    tk.op("dve", lambda e, blk=blk: e.match_replace(out=tmp1[:], in_to_replace=st[:, blk, 0:8],
                                                                    in_values=sc[:, blk, :], imm_value=-1e30),
                          reads=["sc", "st"], writes=["tmp1"])
                    tk.op("dve", lambda e, blk=blk: e.max(out=st[:, blk, 8:16], in_=tmp1[:]),
                          reads=["tmp1"], writes=["st"])
                    tk.op("dve", lambda e, blk=blk: e.max_index(out=iu[:, blk, 8:16], in_max=st[:, blk, 8:16],
                                                                in_values=tmp1[:]),
                          reads=["tmp1", "st"], writes=["iu"])
                tk.op("dve", lambda e: e.tensor_copy(out=itf[:], in_=iu[:]), reads=["iu"], writes=["itf"])
                tk.op("dve", lambda e: e.tensor_copy(out=dd[:, :, 0:1], in_=itf[:, :, 0:1]), reads=["itf"], writes=["dd"])
                tk.op("dve", lambda e: e.tensor_tensor(out=dd[:, :, 1:16], in0=itf[:, :, 1:16], in1=itf[:, :, 0:15],
                                                       op=ALU.subtract), reads=["itf"], writes=["dd"])
                a0 = st4[:, :, 0, :].unsqueeze(3).broadcast_to([128, 8, 16, 16])
                a1 = st4[:, :, 1, :].unsqueeze(2).broadcast_to([128, 8, 16, 16])
                cand4 = cand[:].rearrange("p h (a b) -> p h a b", a=16)
                tk.op("dve", lambda e: e.tensor_tensor(out=cand4, in0=a0, in1=a1, op=ALU.add), reads=["st"], writes=["cand"])
                for h in range(8):
                    tk.op("dve", lambda e, h=h: e.max(out=cf[:, h, 0:8], in_=cand[:, h, :]), reads=["cand"], writes=["cf"])
                    tk.op("dve", lambda e, h=h: e.max_index(out=pu[:, h, 0:8], in_max=cf[:, h, 0:8], in_values=cand[:, h, :]),
                          reads=["cand", "cf"], writes=["pu"])
                    tk.op("dve", lambda e, h=h: e.match_replace(out=tmp2[:], in_to_replace=cf[:, h, 0:8],
                                                                in_values=cand[:, h, :], imm_value=-1e30),
                          reads=["cand", "cf"], writes=["tmp2"])
                    tk.op("dve", lambda e, h=h: e.max(out=cf[:, h, 8:16], in_=tmp2[:]), reads=["tmp2"], writes=["cf"])
                    tk.op("dve", lambda e, h=h: e.max_index(out=pu[:, h, 8:16], in_max=cf[:, h, 8:16], in_values=tmp2[:]),
                          reads=["tmp2", "cf"], writes=["pu"])
                tk.op("dve", lambda e: e.tensor_copy(out=posf[:], in_=pu[:]), reads=["pu"], writes=["posf"])
                tk.op("dve", lambda e: e.tensor_tensor(out=cs[:], in0=cf[:], in1=cf[:, :, 0:1].broadcast_to([128, 8, 16]),
                                                       op=ALU.subtract), reads=["cf"], writes=["cs"])
                tk.op("act", lambda e: e.activation(out=cs[:], in_=cs[:], func=AF.Exp), reads=["cs"], writes=["cs"])
                tk.op("dve", lambda e: e.tensor_reduce(out=zs[:], in_=cs[:], axis=AX.X, op=ALU.add), reads=["cs"], writes=["zs"])
                tk.op("dve", lambda e: e.reciprocal(out=zs[:], in_=zs[:]), reads=["zs"], writes=["zs"])
                res_g = res[:, 2, :].rearrange("p (h k) -> p h k", h=8)
                tk.op("dve", lambda e: e.tensor_tensor(out=res_g, in0=cs[:], in1=zs[:].unsqueeze(2).broadcast_to([128, 8, 16]),
                                                       op=ALU.mult), reads=["cs", "zs"], writes=["res"])
                pos_b = posf[:].unsqueeze(3).broadcast_to([128, 8, 16, 16])
                thr_b = thr16[:].unsqueeze(1).unsqueeze(1).broadcast_to([128, 8, 16, 16])
                tk.op("dve", lambda e: e.tensor_tensor(out=ge[:], in0=pos_b, in1=thr_b, op=ALU.is_ge),
                      reads=["posf", "c3"], writes=["ge"])
                d1_b = dd4[:, :, 0, :].unsqueeze(2).broadcast_to([128, 8, 16, 16])
                tk.op("dve", lambda e: e.tensor_tensor(out=prod[:], in0=ge[:], in1=d1_b, op=ALU.mult),
                      reads=["ge", "dd"], writes=["prod"])
                res_i = res[:, 0, :].rearrange("p (h k) -> p h k", h=8)
                tk.op("dve", lambda e: e.tensor_reduce(out=res_i, in_=prod[:], axis=AX.X, op=ALU.add),
                      reads=["prod"], writes=["res"])
                tk.op("dve", lambda e: e.tensor_reduce(out=k1s[:], in_=ge[:], axis=AX.X, op=ALU.add), reads=["ge"], writes=["k1s"])
                tk.op("dve", lambda e: e.tensor_scalar(out=k1s[:], in0=k1s[:], scalar1=-16.0, scalar2=16.0, op0=ALU.mult,
                                                       op1=ALU.add), reads=["k1s"], writes=["k1s"])
                tk.op("dve", lambda e: e.tensor_tensor(out=k2f[:], in0=posf[:], in1=k1s[:], op=ALU.add),
                      reads=["posf", "k1s"], writes=["k2f"])
                k2_b = k2f[:].unsqueeze(3).broadcast_to([128, 8, 16, 16])
                io_b = iota16[:].unsqueeze(1).unsqueeze(1).broadcast_to([128, 8, 16, 16])
                tk.op("dve", lambda e: e.tensor_tensor(out=ge[:], in0=k2_b, in1=io_b, op=ALU.is_ge),
                      reads=["k2f", "c4"], writes=["ge"])
                d2_b = dd4[:, :, 1, :].unsqueeze(2).broadcast_to([128, 8, 16, 16])
                tk.op("dve", lambda e: e.tensor_tensor(out=prod[:], in0=ge[:], in1=d2_b, op=ALU.mult),
                      reads=["ge", "dd"], writes=["prod"])
                res_j = res[:, 1, :].rearrange("p (h k) -> p h k", h=8)
                tk.op("dve", lambda e: e.tensor_reduce(out=res_j, in_=prod[:], axis=AX.X, op=ALU.add),
                      reads=["prod"], writes=["res"])
                for q in range(3):
                    tk.op("pe", lambda e, q=q: e.transpose(out=psum[6][:, q * 128:(q + 1) * 128], in_=res[:, q, :],
                                                           identity=ident_f[:]), reads=["res", "c1"], writes=["psb6"])
                tk.op("act", lambda e, s=s: e.copy(out=rst[s][:], in_=psum[6][:, 0:384].rearrange("p (q t) -> p q t", q=3)),
                      reads=["psb6"], writes=["rst%d" % s])
                tk.dma("pool", slot_d[:, :, cols], rst[s][:], reads=["rst%d" % s], key="rst%d" % s)
            tk.barrier()

        if os.environ.get('BSTOP') == '2':
            tk.final_wait('sp')
            return nc
        with ExitStack() as es3:
            G = sb("G", [128, 128, PB], BF16, es3)
            ut = [sb("ut%d" % i, [128, KC, 128], BF16, es3) for i in range(3)]
            vt = [sb("vt%d" % i, [128, D], BF16, es3) for i in range(3)]
            h2b = [sb("h2b%d" % i, [128, KC, PB], BF16, es3) for i in range(2)]
            slots = [sb("slots%d" % i, [128, 3, PB], F32, es3) for i in range(2)]
            gl = [sb("gl%d" % i, [128, PB], BF16, es3) for i in range(2)]
            ohi = [sb("ohi%d" % i, [128, 128], BF16, es3) for i in range(4)]
            ohj = [sb("ohj%d" % i, [128, 128], BF16, es3) for i in range(4)]
            x1t = sb("x1t", [128, D], F32, es3)
            xo = [sb("xo%d" % i, [128, D], F32, es3) for i in range(2)]
            junk = sb("junk3", [128, D], BF16, es3)
            fnb = sb("fnb", [128, D], F32, es3)
            ss = sb("ss3", [128, 4], F32, es3)
            tk.dma("sp", fnb[:], fnw, writes=["fnb"])
            ucnt = [0]
            vcnt = [0]
            ocnt = [0]
            for blk in range(NBLK):
                bs = blk % 2
                cols = slice(blk * PB, (blk + 1) * PB)
                tk.dma("sp", slots[bs][:], slot_d[:, :, cols], writes=["slots%d" % bs])
                tk.dma("sp", h2b[bs][:], h2T_d[:, :, cols], writes=["h2b%d" % bs])
                for t in range(PB):
                    o = t % 4
                    tk.op("dve", lambda e, o=o, t=t, bs=bs: e.tensor_scalar(
                        out=ohi[o][:], in0=iota_row[:], scalar1=slots[bs][:, 0, t:t + 1], scalar2=slots[bs][:, 2, t:t + 1],
                        op0=ALU.is_equal, op1=ALU.mult), reads=["slots%d" % bs, "c2"], writes=["ohi%d" % o])
                    tk.op("dve", lambda e, o=o, t=t, bs=bs: e.tensor_scalar(
                        out=ohj[o][:], in0=iota_row[:], scalar1=slots[bs][:, 1, t:t + 1], scalar2=None,
                        op0=ALU.is_equal), reads=["slots%d" % bs, "c2"], writes=["ohj%d" % o])
                    gb_ = (t // 4) % 2
                    tk.op("pe", lambda e, o=o, gb_=gb_: e.matmul(psum[gb_][:, o * 128:(o + 1) * 128], lhsT=ohi[o][:],
                                                                 rhs=ohj[o][:], start=True, stop=True),
                          reads=["ohi%d" % o, "ohj%d" % o], writes=["psb%d" % gb_])
                    if o == 3:
                        t0 = t - 3
                        tk.op("act", lambda e, gb_=gb_, t0=t0: e.copy(
                            out=G[:, :, t0:t0 + 4], in_=psum[gb_][:, :].rearrange("p (t j) -> p j t", t=4)),
                            reads=["psb%d" % gb_], writes=["G"])
                for j in range(NJ):
                    us = ucnt[0] % 3
                    ucnt[0] += 1
                    tk.dma("sp", ut[us][:], u16[j].rearrange("p (k i) -> p k i", k=KC), writes=["ut%d" % us])
                    b = 2 + j % 2
                    for kc in range(KC):
                        tk.op("pe", lambda e, kc=kc, b=b, us=us, bs=bs: e.matmul(
                            psum[b][:, 0:PB], lhsT=ut[us][:, kc, :], rhs=h2b[bs][:, kc, :], start=(kc == 0),
                            stop=(kc == KC - 1)), reads=["ut%d" % us, "h2b%d" % bs], writes=["psb%d" % b])
                    g = j % 2
                    tk.op("act", lambda e, b=b, g=g: e.activation(out=gl[g][:], in_=psum[b][:, 0:PB], func=AF.Gelu),
                          reads=["psb%d" % b], writes=["gl%d" % g])
                    tk.op("dve", lambda e, g=g, j=j: e.tensor_tensor(out=G[:, j, :], in0=gl[g][:], in1=G[:, j, :], op=ALU.mult),
                          reads=["gl%d" % g, "G"], writes=["G"])
                for j in range(NJ):
                    vs = vcnt[0] % 3
                    vcnt[0] += 1
                    tk.dma("sp", vt[vs][:], v16[j], writes=["vt%d" % vs])
                    for tt in range(TPB):
                        for cg in range(4):
                            b = tt * 4 + cg
                            tk.op("pe", lambda e, j=j, tt=tt, cg=cg, b=b, vs=vs: e.matmul(
                                psum[b][:, :], lhsT=G[:, j, tt * 128:(tt + 1) * 128], rhs=vt[vs][:, cg * 512:(cg + 1) * 512],
                                start=(j == 0), stop=(j == NJ - 1)), reads=["G", "vt%d" % vs], writes=["psb%d" % b])
                for tt in range(TPB):
                    rows = slice(blk * PB + tt * 128, blk * PB + (tt + 1) * 128)
                    o = ocnt[0] % 2
                    ocnt[0] += 1
                    tk.dma("sp", x1t[:], x1_d[rows, :], writes=["x1t"])
                    for cg in range(4):
                        b = tt * 4 + cg
                        tk.op("dve", lambda e, b=b, cg=cg, o=o: e.tensor_tensor(
                            out=xo[o][:, cg * 512:(cg + 1) * 512], in0=psum[b][:, :], in1=x1t[:, cg * 512:(cg + 1) * 512],
                            op=ALU.add), reads=["psb%d" % b, "x1t"], writes=["xo%d" % o])
                    rms_rstd(xo[o][:], "xo%d" % o, ss, junk)
                    tk.op("dve", lambda e, o=o: e.scalar_tensor_tensor(out=xo[o][:], in0=xo[o][:], scalar=ss[:, 2:3],
                                                                       in1=fnb[:], op0=ALU.mult, op1=ALU.mult),
                          reads=["xo%d" % o, "ss2", "fnb"], writes=["xo%d" % o])
                    tk.dma("pool", out[rows, :], xo[o][:], reads=["xo%d" % o], key="ost%d" % o)
            tk.barrier()
        tk.final_wait("sp")
    print("phase B instructions (cumulative):", tk.ninst)
    return nc


def host_inputs_b(inp, xflat, mixed_full, TCs):
    c = host_consts_b()
    U = np.asarray(inp["peer_u"][0])
    V = np.asarray(inp["peer_v"][0])
    uh = np.ascontiguousarray(U.reshape(128, 128, KC, 128).transpose(1, 3, 2, 0)).reshape(128, 128, D)
    vh = np.ascontiguousarray(V.reshape(128, 128, D).transpose(1, 0, 2))
    keys = np.asarray(inp["peer_keys"][0])
    keysT = np.ascontiguousarray(keys.transpose(2, 0, 1))
    wout = np.ascontiguousarray(np.asarray(inp["w_out"][0]))
    wq = np.ascontiguousarray(np.asarray(inp["peer_w_q"][0]))
    n2w = np.ascontiguousarray(np.broadcast_to(np.asarray(inp["norm2_w"][0])[None, :], (128, D)))
    fnw = np.ascontiguousarray(np.broadcast_to(np.asarray(inp["final_norm_w"])[None, :], (128, D)))
    maps = []
    for core in range(8):
        r0 = core * TCs
        m = {"x": np.ascontiguousarray(xflat[r0:r0 + TCs]),
             "mixed": np.ascontiguousarray(mixed_full[r0:r0 + TCs]),
             "wout": wout, "wq": wq, "n2w": n2w, "fnw": fnw, "keysT": keysT, "uh": uh, "vh": vh}
        m.update(c)
        maps.append(m)
    return maps


def build_fused(nc, S):
    TC = 2 * S // 8
    NK = S // 512
    tk = Trk(nc)
    mixloc = [nc.dram_tensor("mixloc%d" % k, [512, 512], BF16) for k in range(NK)]
    gath = [nc.dram_tensor("gath%d" % k, [8 * 512, 512], BF16) for k in range(NK)]
    uh = nc.dram_tensor("b_uh", [128, 128, D], F32, kind="ExternalInput").ap()
    vh = nc.dram_tensor("b_vh", [128, 128, D], F32, kind="ExternalInput").ap()
    u16 = nc.dram_tensor("b_u16", [128, 128, D], BF16, kind="Internal").ap()
    v16 = nc.dram_tensor("b_v16", [128, 128, D], BF16, kind="Internal").ap()
    tk.lazy_keys.add("cvt")
    for j in range(128):
        tk.dma("pool", u16[j], uh[j], writes=["u16"], key="cvt")
        tk.dma("pool", v16[j], vh[j], writes=["v16"], key="cvt")
    build_phase_a(nc, S, tk=tk, mix_dst=lambda t0, c0, c1: mixloc[t0 // 512][t0 % 512:t0 % 512 + 128, c0:c1])
    for k in range(NK):
        tk.coll(lambda g, k=k: g.collective_compute("AllGather", ALU.bypass, replica_groups=[list(range(8))],
                                                    ins=[mixloc[k].ap().opt()], outs=[gath[k].ap().opt()]),
                writes=["gath%d" % k])
    tk.lazy_keys.discard("cvt")
    build_phase_b(nc, TC, tk=tk, gath=gath, pfx="b_", pre=(u16, v16))
    return nc


def wout_perm():
    perm = []
    for g in range(4):
        perm += list(range(g * 256, (g + 1) * 256)) + list(range(1024 + g * 256, 1024 + (g + 1) * 256))
    return np.array(perm)


def host_inputs_fused(inp, S):
    maps = host_inputs_a(inp, S)
    c = host_consts_b()
    TC = 2 * S // 8
    H = S // 8
    U = np.asarray(inp["peer_u"][0])
    V = np.asarray(inp["peer_v"][0])
    uh = np.ascontiguousarray(U.reshape(128, 128, KC, 128).transpose(1, 3, 2, 0)).reshape(128, 128, D)
    vh = np.ascontiguousarray(V.reshape(128, 128, D).transpose(1, 0, 2))
    keysT = np.ascontiguousarray(np.asarray(inp["peer_keys"][0]).transpose(2, 0, 1))
    wout = np.ascontiguousarray(np.asarray(inp["w_out"][0])[wout_perm(), :])
    wq = np.ascontiguousarray(np.asarray(inp["peer_w_q"][0]))
    n2w = np.ascontiguousarray(np.broadcast_to(np.asarray(inp["norm2_w"][0])[None, :], (128, D)))
    fnw = np.ascontiguousarray(np.broadcast_to(np.asarray(inp["final_norm_w"])[None, :], (128, D)))
    x = np.asarray(inp["x"])
    for d in range(8):
        m = maps[d]
        m["b_x"] = np.ascontiguousarray(np.concatenate([x[0, d * H:(d + 1) * H], x[1, d * H:(d + 1) * H]], axis=0))
        ws = np.zeros((128, 8), np.float32)
        ws[:, d] = 1.0
        m["b_wsel"] = ws
        m.update({"b_wout": wout, "b_wq": wq, "b_n2w": n2w, "b_fnw": fnw, "b_keysT": keysT, "b_uh": uh, "b_vh": vh})
        for k, v in c.items():
            m["b_" + k] = v
    return maps


def gather_out(res, S):
    H = S // 8
    out = np.zeros((2, S, D), np.float32)
    for d in range(8):
        o = res.results[d]["b_out"]
        out[0, d * H:(d + 1) * H] = o[0:H]
        out[1, d * H:(d + 1) * H] = o[H:2 * H]
    return out


S_FULL = 16384


def kernel(**inputs):
    inp = {k: np.asarray(v) for k, v in inputs.items()}
    S = S_FULL
    nc = bass.Bass("TRN2", target_bir_lowering=False)
    build_fused(nc, S)
    maps = host_inputs_fused(inp, S)
    res = run_bass_kernel_spmd(nc, maps, core_ids=list(range(8)))
    return gather_out(res, S).astype(np.float32)
```

```python
import numpy as np
import ml_dtypes
from contextlib import ExitStack
import concourse.bass as bass
import concourse.mybir as mybir
from concourse.bass_utils import run_bass_kernel_spmd

F32 = mybir.dt.float32
BF16 = mybir.dt.bfloat16
I32 = mybir.dt.int32
U32 = mybir.dt.uint32
AF = mybir.ActivationFunctionType
ALU = mybir.AluOpType
AX = mybir.AxisListType

D = 2048
KC = 16
EPS = 1e-6


class Trk:
    def __init__(self, nc):
        self.nc = nc
        self.engs = {"pe": nc.tensor, "act": nc.scalar, "dve": nc.vector,
                     "pool": nc.gpsimd, "sp": nc.sync}
        self.sem = {}
        self.cnt = {}
        for e in ("pe", "act", "dve", "pool"):
            self.sem[e] = nc.alloc_semaphore(name="s_" + e)
            self.cnt[e] = 0
        self.dsem = {}
        self.waited = {}
        self.lastw = {}
        self.readers = {}
        self.ninst = 0
        self.lazy_keys = set()

    def _wait(self, eng, ev):
        sem, val, src, key = ev
        k = (eng, key)
        if self.waited.get(k, 0) >= val:
            return
        self.engs[eng].wait_ge(sem, val)
        self.waited[k] = val

    def _deps(self, eng, reads, writes):
        for b in reads:
            ev = self.lastw.get(b)
            if ev is not None and not (ev[2] == eng and eng == "pe"):
                self._wait(eng, ev)
        for b in writes:
            ev = self.lastw.get(b)
            if ev is not None and ev[2] != eng:
                self._wait(eng, ev)
            for ev in self.readers.get(b, ()):
                if ev[2] != eng:
                    self._wait(eng, ev)

    def _record(self, ev, reads, writes):
        for b in reads:
            self.readers.setdefault(b, []).append(ev)
        for b in writes:
            self.lastw[b] = ev
            self.readers[b] = []

    def op(self, eng, fn, reads=(), writes=()):
        self._deps(eng, reads, writes)
        inst = fn(self.engs[eng])
        self.cnt[eng] += 1
        inst.then_inc(self.sem[eng], 1)
        ev = (self.sem[eng], self.cnt[eng], eng, eng)
        self._record(ev, reads, writes)
        self.ninst += 1
        return inst

    def dma(self, q, out, in_, reads=(), writes=(), key=None, **kw):
        if key is None:
            key = writes[0] if writes else reads[0]
        if key not in self.dsem:
            self.dsem[key] = [self.nc.alloc_semaphore(name="d%d" % len(self.dsem)), 0]
        self._deps(q, reads, writes)
        ds = self.dsem[key]
        inst = self.engs[q].dma_start(out=out, in_=in_, **kw)
        ds[1] += 16
        inst.then_inc(ds[0], 16)
        ev = (ds[0], ds[1], "dma", ("d", key))
        self._record(ev, reads, writes)
        self.ninst += 1
        return inst

    def coll(self, fn, reads=(), writes=()):
        if "cc" not in self.sem:
            self.sem["cc"] = self.nc.alloc_semaphore(name="s_cc")
            self.cnt["cc"] = 0
        self._deps("pool", reads, writes)
        inst = fn(self.engs["pool"])
        self.cnt["cc"] += 1
        inst.then_inc(self.sem["cc"])
        ev = (self.sem["cc"], self.cnt["cc"], "cc", "cc")
        self._record(ev, reads, writes)
        self.ninst += 1
        return inst

    def barrier(self):
        evs = [(self.sem[e], self.cnt[e], e, e) for e in self.sem if self.cnt[e] > 0]
        evs += [(v[0], v[1], "dma", ("d", k)) for k, v in self.dsem.items()
                if v[1] > 0 and k not in self.lazy_keys]
        for e in ("pe", "act", "dve", "pool", "sp"):
            for ev in evs:
                if ev[2] != e:
                    self._wait(e, ev)
        keep = {b: ev for b, ev in self.lastw.items() if ev[2] == "dma" and ev[3][1] in self.lazy_keys}
        self.lastw = keep
        self.readers = {}

    def final_wait(self, q="sp"):
        for k, v in self.dsem.items():
            if v[1] > 0:
                self._wait(q, (v[0], v[1], "dma", ("d", k)))


import os
PH = os.environ.get('PH', 'PAM')
SK = os.environ.get('SK', '')

NCOL = 1540
C_FQ0, C_FQ1, C_FK0, C_FK1, C_FV, C_MQ, C_MK, C_MVO, C_G = 0, 128, 256, 384, 512, 768, 896, 1024, 1536


def host_consts():
    c = {}
    c["ident_bf"] = np.eye(128, dtype=np.float32).astype(ml_dtypes.bfloat16)
    c["ident_f"] = np.eye(128, dtype=np.float32)
    r = np.arange(128)
    c["triu"] = (r[:, None] <= r[None, :]).astype(np.float32)
    c["ones_f"] = np.ones((128, 128), np.float32)
    es = np.zeros((128, 128), np.float32); es[0, :] = 1.0
    c["esel"] = es
    t = np.arange(512)
    m = np.stack([(r[:, None] + 128 * rr <= t[None, :]) for rr in range(4)], axis=1)
    c["amask"] = m.astype(np.float32).astype(ml_dtypes.bfloat16)
    c["mmask"] = ((r[:, None] <= r[None, :]).astype(np.float32) * (128 ** -0.5)).astype(np.float32)
    return c


def build_phase_a(nc, S, tk=None, mix_dst=None):
    NT = S // 128
    NB = S // 512
    own_tk = tk is None
    if own_tk:
        tk = Trk(nc)
    dt = nc.dram_tensor
    x = dt("x", [int(os.environ.get("XROWS", S)), D], F32, kind="ExternalInput").ap()
    wc = dt("wc", [D, NCOL], F32, kind="ExternalInput").ap()
    n1w = dt("n1w", [128, KC], F32, kind="ExternalInput").ap()
    gbias = dt("gbias", [128, 4], F32, kind="ExternalInput").ap()
    convp = dt("convp", [128, 10], F32, kind="ExternalInput").ap()
    hnw = dt("hnw", [128, 512], F32, kind="ExternalInput").ap()
    ident_bf_d = dt("ident_bf", [128, 128], BF16, kind="ExternalInput").ap()
    ident_f_d = dt("ident_f", [128, 128], F32, kind="ExternalInput").ap()
    triu_d = dt("triu", [128, 128], F32, kind="ExternalInput").ap()
    ones_d = dt("ones_f", [128, 128], F32, kind="ExternalInput").ap()
    esel_d = dt("esel", [128, 128], F32, kind="ExternalInput").ap()
    amask_d = dt("amask", [128, 4, 512], BF16, kind="ExternalInput").ap()
    mmask_d = dt("mmask", [128, 128], F32, kind="ExternalInput").ap()
    if mix_dst is None:
        mixed = dt("mixed", [int(os.environ.get("MROWS", S)), 512], BF16, kind="ExternalOutput").ap()
        mix_dst = lambda t0, c0, c1: mixed[t0:t0 + 128, c0:c1]
    qt_d = [dt("qt%d" % h, [128, S], BF16, kind="Internal").ap() for h in range(2)]
    kt_d = [dt("kt%d" % h, [128, S], BF16, kind="Internal").ap() for h in range(2)]
    va_d = [dt("va%d" % h, [128, S // 128, 130], BF16, kind="Internal").ap() for h in range(2)]
    mq_d = dt("mqT", [128, S], F32, kind="Internal").ap()
    mk_d = dt("mkT", [128, S], F32, kind="Internal").ap()
    mvo_d = dt("mvo", [S, 512], F32, kind="Internal").ap()
    if os.environ.get("DUMMY"):
        dummy_d = dt("dummyx", [int(os.environ["DUMMY"]), 512], F32, kind="Internal").ap()

    with ExitStack() as es0:
        def sb(name, shape, dtype, es=es0):
            return es.enter_context(nc.sbuf_tensor(name, shape, dtype))
        ident_bf = sb("ident_bf_s", [128, 128], BF16)
        ident_f = sb("ident_f_s", [128, 128], F32)
        triu = sb("triu_s", [128, 128], F32)
        ones_f = sb("ones_s", [128, 128], F32)
        esel = sb("esel_s", [128, 128], F32)
        amask = sb("amask_s", [128, 4, 512], BF16)
        mmask = sb("mmask_s", [128, 128], F32)
        gb = sb("gb_s", [128, 4], F32)
        ngb = sb("ngb_s", [128, 4], F32)
        cvp = sb("cvp_s", [128, 10], F32)
        hnw_s = sb("hnw_s", [128, 512], F32)
        n1w_s = sb("n1w_s", [128, KC], F32)
        gates = sb("gates_s", [128, NT, 4], F32)
        epsc = sb("epsc", [128, 1], F32)
        onec = sb("onec", [128, 1], F32)
        psum = [es0.enter_context(nc.psum_tensor("ps%d" % i, [128, 512], F32)) for i in range(8)]
        for i, (s_, d_) in enumerate([(ident_bf, ident_bf_d), (ident_f, ident_f_d), (triu, triu_d),
                                      (ones_f, ones_d), (esel, esel_d), (amask, amask_d), (mmask, mmask_d),
                                      (gb, gbias), (cvp, convp), (hnw_s, hnw), (n1w_s, n1w)]):
            tk.dma("sp", s_[:], d_, writes=["c%d" % i], key="const")
        tk.barrier()
        tk.op("dve", lambda e: e.memset(epsc[:], EPS), writes=["epsc"])
        tk.op("dve", lambda e: e.memset(onec[:], 1.0), writes=["onec"])
        tk.op("dve", lambda e: e.tensor_scalar(out=ngb[:], in0=gb[:], scalar1=-1.0, scalar2=None, op0=ALU.mult),
              reads=["c7"], writes=["ngb"])

        with ExitStack() as es1:
            W = sb("W", [128, KC, NCOL], BF16, es1)
            wst = [sb("wst%d" % i, [128, NCOL], F32, es1) for i in range(2)]
            xt = [sb("xt%d" % i, [128, D], F32, es1) for i in range(2)]
            xn = [sb("xn%d" % i, [128, D], BF16, es1) for i in range(2)]
            junk = sb("junk", [128, D], BF16, es1)
            hT = [sb("hT%d" % i, [128, KC, 512], BF16, es1) for i in range(2)]
            ss = sb("ss", [128, 4], F32, es1)
            fst = [sb("fst%d" % i, [128, 512], BF16, es1) for i in range(4)]
            mst = [sb("mst%d" % i, [128, 512], F32, es1) for i in range(4)]
            vst = [sb("vst%d" % i, [128, 2, 130], BF16, es1) for i in range(2)]
            for i in range(2):
                tk.op("dve", lambda e, i=i: e.memset(vst[i][:, :, 128:129], 1.0), writes=["vst%d" % i])
                tk.op("dve", lambda e, i=i: e.memset(vst[i][:, :, 129:130], 0.0), writes=["vst%d" % i])
            for kc in range(KC):
                s = kc % 2
                tk.dma("sp", wst[s][:], wc[kc * 128:(kc + 1) * 128, :], writes=["wst%d" % s])
                tk.op("dve", lambda e, kc=kc, s=s: e.tensor_scalar(
                    out=W[:, kc, :], in0=wst[s][:], scalar1=n1w_s[:, kc:kc + 1], scalar2=None, op0=ALU.mult),
                    reads=["wst%d" % s, "c10"], writes=["W"])
            pb = [2]

            def nbank():
                b = pb[0]
                pb[0] = 2 + (pb[0] - 2 + 1) % 6
                return b
            fcnt = [0]
            mcnt = [0]
            for jb in range(min(NB, int(os.environ.get('NBLIM', '999')))):
                hs = jb % 2
                for tt in range(4):
                    ti = jb * 4 + tt
                    s = ti % 2
                    tk.dma("sp", xt[s][:], x[ti * 128:(ti + 1) * 128, :], writes=["xt%d" % s])
                    tk.op("act", lambda e, s=s: e.activation(out=junk[:], in_=xt[s][:], func=AF.Square,
                                                             accum_out=ss[:, 0:1]),
                          reads=["xt%d" % s], writes=["junk", "ss0"])
                    tk.op("act", lambda e: e.activation(out=ss[:, 1:2], in_=ss[:, 0:1], func=AF.Ln,
                                                        scale=1.0 / D, bias=epsc[:]),
                          reads=["ss0", "epsc"], writes=["ss1"])
                    tk.op("act", lambda e: e.activation(out=ss[:, 2:3], in_=ss[:, 1:2], func=AF.Exp, scale=-0.5),
                          reads=["ss1"], writes=["ss2"])
                    tk.op("dve", lambda e, s=s: e.tensor_scalar(out=xn[s][:], in0=xt[s][:], scalar1=ss[:, 2:3],
                                                                scalar2=None, op0=ALU.mult),
                          reads=["xt%d" % s, "ss2"], writes=["xn%d" % s])
                    for half in range(2):
                        pst = psum[half][:, :].bitcast(BF16)
                        for k8 in range(8):
                            kc = half * 8 + k8
                            tk.op("pe", lambda e, kc=kc, k8=k8, pst=pst, s=s: e.transpose(
                                out=pst[:, k8 * 128:(k8 + 1) * 128], in_=xn[s][:, kc * 128:(kc + 1) * 128],
                                identity=ident_bf[:]),
                                reads=["xn%d" % s, "c0"], writes=["psb%d" % half])
                        eng = "act" if half == 0 else "dve"
                        src = pst.rearrange("p (k t) -> p k t", k=8)
                        dst = hT[hs][:, half * 8:(half + 1) * 8, tt * 128:(tt + 1) * 128]
                        if eng == "act":
                            tk.op("act", lambda e, src=src, dst=dst: e.copy(out=dst, in_=src),
                                  reads=["psb%d" % half], writes=["hT%d" % hs])
                        else:
                            tk.op("dve", lambda e, src=src, dst=dst: e.tensor_copy(out=dst, in_=src),
                                  reads=["psb%d" % half], writes=["hT%d" % hs])
                for (c0, kind, dst_d) in [(C_FQ0, "f", qt_d[0]), (C_FQ1, "f", qt_d[1]), (C_FK0, "f", kt_d[0]),
                                          (C_FK1, "f", kt_d[1]), (C_MQ, "m", mq_d), (C_MK, "m", mk_d)]:
                    b = nbank()
                    for kc in range(KC):
                        tk.op("pe", lambda e, kc=kc, b=b, c0=c0: e.matmul(
                            psum[b][:, :], lhsT=W[:, kc, c0:c0 + 128], rhs=hT[hs][:, kc, :],
                            start=(kc == 0), stop=(kc == KC - 1)),
                            reads=["W", "hT%d" % hs], writes=["psb%d" % b])
                    if kind == "f":
                        f = fcnt[0] % 4
                        fcnt[0] += 1
                        tk.op("act", lambda e, b=b, f=f: e.copy(out=fst[f][:], in_=psum[b][:, :]),
                              reads=["psb%d" % b], writes=["fst%d" % f])
                        if 'f' not in SK:
                            tk.dma("pool", dst_d[:, jb * 512:(jb + 1) * 512], fst[f][:], reads=["fst%d" % f],
                                   key="fst%d" % f)
                    else:
                        f = mcnt[0] % 4
                        mcnt[0] += 1
                        tk.op("dve", lambda e, b=b, f=f: e.tensor_copy(out=mst[f][:], in_=psum[b][:, :]),
                              reads=["psb%d" % b], writes=["mst%d" % f])
                        if 'm' not in SK:
                            tk.dma("pool", dst_d[:, jb * 512:(jb + 1) * 512], mst[f][:], reads=["mst%d" % f],
                                   key="mst%d" % f)
                for tt in range(4):
                    ti = jb * 4 + tt
                    tsl = slice(tt * 128, (tt + 1) * 128)
                    b = nbank()
                    for kc in range(KC):
                        tk.op("pe", lambda e, kc=kc, b=b, tsl=tsl: e.matmul(
                            psum[b][:, 0:256], lhsT=hT[hs][:, kc, tsl], rhs=W[:, kc, C_FV:C_FV + 256],
                            start=(kc == 0), stop=(kc == KC - 1)),
                            reads=["W", "hT%d" % hs], writes=["psb%d" % b])
                    vs = ti % 2
                    tk.op("act", lambda e, b=b, vs=vs: e.copy(
                        out=vst[vs][:, :, 0:128], in_=psum[b][:, 0:256].rearrange("p (h d) -> p h d", h=2)),
                        reads=["psb%d" % b], writes=["vst%d" % vs])
                    for h in (range(2) if 'v' not in SK else []):
                        tk.dma("pool", va_d[h][:, ti, :], vst[vs][:, h, :],
                               reads=["vst%d" % vs], key="vst%d_%d" % (vs, h))
                    b = nbank()
                    for kc in range(KC):
                        tk.op("pe", lambda e, kc=kc, b=b, tsl=tsl: e.matmul(
                            psum[b][:, :], lhsT=hT[hs][:, kc, tsl], rhs=W[:, kc, C_MVO:C_MVO + 512],
                            start=(kc == 0), stop=(kc == KC - 1)),
                            reads=["W", "hT%d" % hs], writes=["psb%d" % b])
                    f = mcnt[0] % 4
                    mcnt[0] += 1
                    tk.op("dve", lambda e, b=b, f=f: e.tensor_copy(out=mst[f][:], in_=psum[b][:, :]),
                          reads=["psb%d" % b], writes=["mst%d" % f])
                    if 'o' not in SK:
                        tk.dma("pool", mvo_d[ti * 128:(ti + 1) * 128, :], mst[f][:], reads=["mst%d" % f],
                               key="mst%d" % f)
                    b = nbank()
                    for kc in range(KC):
                        tk.op("pe", lambda e, kc=kc, b=b, tsl=tsl: e.matmul(
                            psum[b][:, 0:4], lhsT=hT[hs][:, kc, tsl], rhs=W[:, kc, C_G:C_G + 4],
                            start=(kc == 0), stop=(kc == KC - 1)),
                            reads=["W", "hT%d" % hs], writes=["psb%d" % b])
                    tk.op("act", lambda e, b=b, ti=ti: e.copy(out=gates[:, ti, :], in_=psum[b][:, 0:4]),
                          reads=["psb%d" % b], writes=["gates"])
            tk.barrier()

        NQ = NB
        SC = 128 ** -0.5
        with ExitStack() as es2:
            KT = sb("KT", [128, S], BF16, es2)
            VA = sb("VA", [128, NT, 130], BF16, es2)
            qt = [sb("qtb%d" % i, [128, 512], BF16, es2) for i in range(2)]
            PT = [sb("PT%d" % i, [128, 512], BF16, es2) for i in range(4)]
            lfn = sb("lfn", [128, NT], F32, es2)
            tmpg = sb("tmpg", [128, NT], F32, es2)
            tot = sb("tot", [128, NT], F32, es2)
            offi = sb("offi", [128, NT], F32, es2)
            onesn = sb("onesn", [128, NT], F32, es2)
            G = sb("G", [128, NT], F32, es2)
            cb = sb("cb", [128, NQ], F32, es2)
            biasj = [sb("biasj%d" % i, [128, NT], F32, es2) for i in range(2)]
            sm = sb("sm", [128, 8], F32, es2)
            ot = [sb("ot%d" % i, [128, 128], F32, es2) for i in range(2)]
            oj = sb("oj", [128, 128], F32, es2)
            ob = [sb("ob%d" % i, [128, 128], BF16, es2) for i in range(2)]
            tk.op("dve", lambda e: e.memset(onesn[:], 1.0), writes=["onesn"])
            ptc = [0]
            oc = [0]
            for hh in (range(2) if 'A' in PH else []):
                for c0 in range(0, S, 2048):
                    tk.dma("sp", KT[:, c0:min(S, c0 + 2048)], kt_d[hh][:, c0:min(S, c0 + 2048)], writes=["KT"])
                for c0 in range(0, NT, 16):
                    tk.dma("sp", VA[:, c0:min(NT, c0 + 16), :], va_d[hh][:, c0:min(NT, c0 + 16), :], writes=["VA"])
                tk.op("act", lambda e, hh=hh: e.activation(out=tmpg[:], in_=gates[:, :, hh], func=AF.Exp,
                                                           scale=-1.0, bias=ngb[:, hh:hh + 1]),
                      reads=["gates", "ngb"], writes=["tmpg"])
                tk.op("act", lambda e: e.activation(out=lfn[:], in_=tmpg[:], func=AF.Ln, scale=1.0, bias=onec[:]),
                      reads=["tmpg", "onec"], writes=["lfn"])
                for c0 in range(0, NT, 32):
                    c1 = min(NT, c0 + 32)
                    tk.op("pe", lambda e, c0=c0, c1=c1: e.matmul(psum[0][:, c0:c1], lhsT=triu[:], rhs=lfn[:, c0:c1],
                                                                 start=True, stop=True),
                          reads=["lfn", "c2"], writes=["psb0"])
                    tk.op("pe", lambda e, c0=c0, c1=c1: e.matmul(psum[1][:, c0:c1], lhsT=ones_f[:], rhs=lfn[:, c0:c1],
                                                                 start=True, stop=True),
                          reads=["lfn", "c3"], writes=["psb1"])
                tk.op("act", lambda e: e.copy(out=tot[:], in_=psum[1][:, 0:NT]), reads=["psb1"], writes=["tot"])
                tk.op("act", lambda e: e.copy(out=G[:], in_=psum[0][:, 0:NT]), reads=["psb0"], writes=["G"])
                tk.op("dve", lambda e: e.tensor_tensor_scan(out=offi[:], data0=onesn[:], data1=tot[:], initial=0.0,
                                                            op0=ALU.mult, op1=ALU.add),
                      reads=["tot", "onesn"], writes=["offi"])
                tk.op("dve", lambda e: e.tensor_tensor(out=offi[:], in0=offi[:], in1=tot[:], op=ALU.subtract),
                      reads=["offi", "tot"], writes=["offi"])
                tk.op("dve", lambda e: e.tensor_tensor(out=G[:], in0=G[:], in1=offi[:], op=ALU.add),
                      reads=["G", "offi"], writes=["G"])
                Gsel = G[:].rearrange("p (j r) -> p j r", r=4)[:, :, 2]
                tk.op("dve", lambda e, Gsel=Gsel: e.tensor_copy(out=tmpg[:, 0:NQ], in_=Gsel), reads=["G"],
                      writes=["tmpg"])
                tk.op("pe", lambda e: e.matmul(psum[1][:, 0:NQ], lhsT=esel[:], rhs=tmpg[:, 0:NQ], start=True, stop=True),
                      reads=["tmpg", "c4"], writes=["psb1"])
                tk.op("act", lambda e: e.copy(out=cb[:], in_=psum[1][:, 0:NQ]), reads=["psb1"], writes=["cb"])
                blocks = [(j, i) for j in range(NQ) for i in range(4 * j + 4)]
                NBK = len(blocks)
                LOOK = 2

                def load_q(j):
                    tk.dma("sp", qt[j % 2][:], qt_d[hh][:, j * 512:(j + 1) * 512], writes=["qt%d" % (j % 2)])

                def make_bias(j):
                    nk = 4 * j + 4
                    tk.op("dve", lambda e, j=j, nk=nk: e.tensor_scalar(
                        out=biasj[j % 2][:, 0:nk], in0=G[:, 0:nk], scalar1=cb[:, j:j + 1], scalar2=None,
                        op0=ALU.subtract), reads=["G", "cb"], writes=["bj%d" % (j % 2)])

                def st_qk(n):
                    j, i = blocks[n]
                    if i == 0 and j + 1 < NQ:
                        load_q(j + 1)
                    sb_ = n % 4
                    qs = j % 2
                    tk.op("pe", lambda e, i=i, qs=qs, sb_=sb_: e.matmul(
                        psum[sb_][:, :], lhsT=KT[:, i * 128:(i + 1) * 128], rhs=qt[qs][:], start=True, stop=True),
                        reads=["KT", "qt%d" % qs], writes=["psb%d" % sb_])

                def st_ex(n):
                    j, i = blocks[n]
                    if i == 0 and j + 1 < NQ:
                        make_bias(j + 1)
                    r = i - 4 * j
                    sb_ = n % 4
                    p = n % 4
                    bj = j % 2
                    tk.op("act", lambda e, p=p, sb_=sb_, bj=bj, i=i: e.activation(
                        out=PT[p][:], in_=psum[sb_][:, :], func=AF.Exp, scale=SC, bias=biasj[bj][:, i:i + 1]),
                        reads=["psb%d" % sb_, "bj%d" % bj], writes=["PT%d" % p])
                    if r >= 0:
                        tk.op("dve", lambda e, p=p, r=r: e.tensor_tensor(
                            out=PT[p][:], in0=PT[p][:], in1=amask[:, r, :], op=ALU.mult),
                            reads=["PT%d" % p, "c5"], writes=["PT%d" % p])

                def st_pv(n):
                    j, i = blocks[n]
                    r = i - 4 * j
                    p = n % 4
                    for u in range(4):
                        if r > u:
                            continue
                        last = 4 * j + u
                        tk.op("pe", lambda e, p=p, u=u, i=i, last=last: e.matmul(
                            psum[4 + u][:, 0:130], lhsT=PT[p][:, u * 128:(u + 1) * 128], rhs=VA[:, i, :],
                            start=(i == 0), stop=(i == last)),
                            reads=["PT%d" % p, "VA"], writes=["psb%d" % (4 + u)])
                    if i == 4 * j + 3:
                        epilogue(j)

                def epilogue(j):
                    for u in range(4):
                        o = oc[0] % 2
                        oc[0] += 1
                        pu = psum[4 + u]
                        tk.op("dve", lambda e, pu=pu: e.reciprocal(out=sm[:, 0:1], in_=pu[:, 128:129]),
                              reads=["psb%d" % (4 + u)], writes=["sm0"])
                        tk.op("dve", lambda e, pu=pu, o=o: e.tensor_scalar(
                            out=ot[o][:], in0=pu[:, 0:128], scalar1=sm[:, 0:1], scalar2=None, op0=ALU.mult),
                            reads=["psb%d" % (4 + u), "sm0"], writes=["ot%d" % o])
                        tk.op("act", lambda e, o=o: e.activation(out=oj[:], in_=ot[o][:], func=AF.Square,
                                                                 accum_out=sm[:, 1:2]),
                              reads=["ot%d" % o], writes=["oj", "sm1"])
                        tk.op("act", lambda e: e.activation(out=sm[:, 2:3], in_=sm[:, 1:2], func=AF.Ln,
                                                            scale=1.0 / 128, bias=epsc[:]),
                              reads=["sm1", "epsc"], writes=["sm2"])
                        tk.op("act", lambda e: e.activation(out=sm[:, 3:4], in_=sm[:, 2:3], func=AF.Exp, scale=-0.5),
                              reads=["sm2"], writes=["sm3"])
                        tk.op("dve", lambda e, o=o, hh=hh: e.scalar_tensor_tensor(
                            out=ob[o][:], in0=ot[o][:], scalar=sm[:, 3:4], in1=hnw_s[:, hh * 128:(hh + 1) * 128],
                            op0=ALU.mult, op1=ALU.mult),
                            reads=["ot%d" % o, "sm3", "c9"], writes=["ob%d" % o])
                        t0 = j * 512 + u * 128
                        tk.dma("pool", mix_dst(t0, hh * 128, (hh + 1) * 128), ob[o][:], reads=["ob%d" % o],
                               key="ob%d" % o)

                load_q(0)
                make_bias(0)
                for n in range(NBK + LOOK):
                    if n < NBK:
                        st_qk(n)
                    if n - LOOK >= 0:
                        st_ex(n - LOOK)
                        st_pv(n - LOOK)
                tk.barrier()

        NCH = NT
        with ExitStack() as es3:
            zq = [sb("zq%d" % i, [128, 515], F32, es3) for i in range(2)]
            zk = [sb("zk%d" % i, [128, 515], F32, es3) for i in range(2)]
            acc = sb("acc", [128, 512], F32, es3)
            ex = sb("ex", [128, 512], F32, es3)
            qT = [sb("qT%d" % i, [128, 512], BF16, es3) for i in range(2)]
            kT = [sb("kT%d" % i, [128, 512], BF16, es3) for i in range(2)]
            mvo = [sb("mvo%d" % i, [128, 512], F32, es3) for i in range(2)]
            vt = [sb("vt%d" % i, [128, 258], BF16, es3) for i in range(2)]
            ktok = [sb("ktok%d" % i, [128, 128], BF16, es3) for i in range(2)]
            smt = [sb("smt%d" % i, [128, 128], BF16, es3) for i in range(2)]
            Cf = sb("Cf", [128, 258], F32, es3)
            Cb = [sb("Cb%d" % i, [128, 258], BF16, es3) for i in range(2)]
            lf = sb("mlf", [128, NCH], F32, es3)
            tmpm = sb("tmpm", [128, NCH], F32, es3)
            bcs = sb("bcs", [128, NCH], F32, es3)
            eb = sb("eb", [128, NCH], F32, es3)
            ek = sb("ek", [128, NCH], F32, es3)
            ebl = sb("ebl", [128, NCH], F32, es3)
            hsm = sb("hsm", [128, 8], F32, es3)
            hv = [sb("hv%d" % i, [128, 256], F32, es3) for i in range(2)]
            sg = sb("sg", [128, 256], F32, es3)
            hj = sb("hj", [128, 256], F32, es3)
            hb = [sb("hb%d" % i, [128, 256], BF16, es3) for i in range(2)]
            if 'M' in PH or os.environ.get('MLPRE'):
                _lim = int(os.environ.get('MLPRE', '99'))
                _real_op = tk.op
                _cnt = [0]
                def _lop(*a, **k):
                    _cnt[0] += 1
                    if _cnt[0] <= _lim:
                        return _real_op(*a, **k)
                tk.op = _lop
                tk.op("act", lambda e: e.activation(out=tmpm[:], in_=gates[:, :, 3], func=AF.Exp, scale=-1.0,
                                                    bias=ngb[:, 3:4]), reads=["gates", "ngb"], writes=["tmpm"])
                tk.op("act", lambda e: e.activation(out=lf[:], in_=tmpm[:], func=AF.Ln, scale=1.0, bias=onec[:]),
                      reads=["tmpm", "onec"], writes=["mlf"])
                tk.op("dve", lambda e: e.tensor_scalar(out=lf[:], in0=lf[:], scalar1=-1.0, scalar2=None, op0=ALU.mult),
                      reads=["mlf"], writes=["mlf"])
                for c0 in range(0, NCH, 32):
                    c1 = min(NCH, c0 + 32)
                    tk.op("pe", lambda e, c0=c0, c1=c1: e.matmul(psum[0][:, c0:c1], lhsT=triu[:], rhs=lf[:, c0:c1],
                                                                 start=True, stop=True),
                          reads=["mlf", "c2"], writes=["psb0"])
                    tk.op("pe", lambda e, c0=c0, c1=c1: e.matmul(psum[1][:, c0:c1], lhsT=ones_f[:], rhs=lf[:, c0:c1],
                                                                 start=True, stop=True),
                          reads=["mlf", "c3"], writes=["psb1"])
                tk.op("act", lambda e: e.activation(out=eb[:], in_=psum[0][:, 0:NCH], func=AF.Exp),
                      reads=["psb0"], writes=["eb"])
                tk.op("act", lambda e: e.activation(out=ebl[:], in_=psum[1][:, 0:NCH], func=AF.Exp),
                      reads=["psb1"], writes=["ebl"])
                tk.op("act", lambda e: e.copy(out=tmpm[:], in_=psum[0][:, 0:NCH]), reads=["psb0"], writes=["tmpm"])
                tk.op("act", lambda e: e.copy(out=bcs[:], in_=gates[:, :, 2]), reads=["gates"], writes=["bcs"])
                tk.op("dve", lambda e: e.tensor_tensor(out=bcs[:], in0=bcs[:], in1=tmpm[:], op=ALU.subtract),
                      reads=["bcs", "tmpm"], writes=["bcs"])
                tk.op("act", lambda e: e.activation(out=ek[:], in_=bcs[:], func=AF.Exp, bias=gb[:, 2:3], scale=1.0),
                      reads=["bcs", "c7"], writes=["ek"])
            if 'M' in PH or os.environ.get('MLPRE'):
                tk.op = _real_op
            tk.op("dve", lambda e: e.memset(Cf[:], 0.0), writes=["Cf"])
            tk.op("dve", lambda e: e.memset(Cb[0][:], 0.0), writes=["Cb0"])
            for i in range(2):
                tk.op("dve", lambda e, i=i: e.memset(zq[i][:, 0:3], 0.0), writes=["zq%d" % i])
                tk.op("dve", lambda e, i=i: e.memset(zk[i][:, 0:3], 0.0), writes=["zk%d" % i])
                tk.op("dve", lambda e, i=i: e.memset(vt[i][:], 0.0), writes=["vt%d" % i])
            for jb in (range(NB) if 'M' in PH else []):
                s = jb % 2
                for (z, zn, zd, wo, bo, dstT, dn) in [(zq, "zq", mq_d, 0, 8, qT, "qT"), (zk, "zk", mk_d, 4, 9, kT, "kT")]:
                    tk.dma("sp", z[s][:, 3:515], zd[:, jb * 512:(jb + 1) * 512], writes=["%s%d" % (zn, s)])
                    if jb > 0:
                        tk.op("act", lambda e, z=z, s=s: e.copy(out=z[s][:, 0:3], in_=z[1 - s][:, 512:515]),
                              reads=["%s%d" % (zn, 1 - s)], writes=["%s%d" % (zn, s)])
                    tk.op("dve", lambda e, z=z, s=s, wo=wo, bo=bo: e.tensor_scalar(
                        out=acc[:], in0=z[s][:, 0:512], scalar1=cvp[:, wo:wo + 1], scalar2=cvp[:, bo:bo + 1],
                        op0=ALU.mult, op1=ALU.add), reads=["%s%d" % (zn, s), "c8"], writes=["acc"])
                    for jj in range(1, 4):
                        tk.op("dve", lambda e, z=z, s=s, wo=wo, jj=jj: e.scalar_tensor_tensor(
                            out=acc[:], in0=z[s][:, jj:jj + 512], scalar=cvp[:, wo + jj:wo + jj + 1], in1=acc[:],
                            op0=ALU.mult, op1=ALU.add), reads=["%s%d" % (zn, s), "c8", "acc"], writes=["acc"])
                    tk.op("act", lambda e: e.activation(out=ex[:], in_=acc[:], func=AF.Exp, scale=-1.0),
                          reads=["acc"], writes=["ex"])
                    tk.op("dve", lambda e: e.tensor_scalar(out=ex[:], in0=ex[:], scalar1=1.0, scalar2=None, op0=ALU.add),
                          reads=["ex"], writes=["ex"])
                    tk.op("dve", lambda e: e.reciprocal(out=ex[:], in_=ex[:]), reads=["ex"], writes=["ex"])
                    tk.op("dve", lambda e, dstT=dstT, s=s: e.tensor_tensor(out=dstT[s][:], in0=acc[:], in1=ex[:],
                                                                           op=ALU.mult),
                          reads=["acc", "ex"], writes=["%s%d" % (dn, s)])
                for cc in range(4):
                    c = jb * 4 + cc
                    cs = c % 2
                    csl = slice(cc * 128, (cc + 1) * 128)
                    tk.dma("sp", mvo[cs][:], mvo_d[c * 128:(c + 1) * 128, :], writes=["mvo%d" % cs])
                    pkt = psum[2][:, :].bitcast(BF16)
                    tk.op("pe", lambda e, s=s, csl=csl, pkt=pkt: e.transpose(out=pkt[:, 0:128], in_=kT[s][:, csl],
                                                                             identity=ident_bf[:]),
                          reads=["kT%d" % s, "c0"], writes=["psb2"])
                    tk.op("act", lambda e, cs=cs, pkt=pkt: e.activation(out=ktok[cs][:], in_=pkt[:, 0:128],
                                                                        func=AF.Copy, scale=SC),
                          reads=["psb2"], writes=["ktok%d" % cs])
                    tk.op("dve", lambda e, cs=cs, c=c: e.tensor_scalar(
                        out=vt[cs][:, 0:256], in0=mvo[cs][:, 0:256], scalar1=ek[:, c:c + 1], scalar2=None, op0=ALU.mult),
                        reads=["mvo%d" % cs, "ek"], writes=["vt%d" % cs])
                    tk.op("dve", lambda e, cs=cs, c=c: e.tensor_copy(out=vt[cs][:, 256:257], in_=ek[:, c:c + 1]),
                          reads=["ek"], writes=["vt%d" % cs])
                    tk.op("pe", lambda e, s=s, csl=csl: e.matmul(psum[3][:, 0:128], lhsT=kT[s][:, csl], rhs=qT[s][:, csl],
                                                                 start=True, stop=True),
                          reads=["kT%d" % s, "qT%d" % s], writes=["psb3"])
                    tk.op("dve", lambda e, cs=cs: e.tensor_tensor(out=smt[cs][:], in0=psum[3][:, 0:128], in1=mmask[:],
                                                                  op=ALU.mult),
                          reads=["psb3", "c6"], writes=["smt%d" % cs])
                    tk.op("pe", lambda e, cs=cs: e.matmul(psum[4][:, 0:258], lhsT=smt[cs][:], rhs=vt[cs][:],
                                                          start=True, stop=False),
                          reads=["smt%d" % cs, "vt%d" % cs], writes=["psb4"])
                    tk.op("pe", lambda e, s=s, csl=csl, cs=cs: e.matmul(psum[4][:, 0:258], lhsT=qT[s][:, csl],
                                                                        rhs=Cb[cs][:], start=False, stop=True),
                          reads=["qT%d" % s, "Cb%d" % cs], writes=["psb4"])
                    tk.op("pe", lambda e, cs=cs: e.matmul(psum[5][:, 0:258], lhsT=ktok[cs][:], rhs=vt[cs][:],
                                                          start=True, stop=True),
                          reads=["ktok%d" % cs, "vt%d" % cs], writes=["psb5"])
                    tk.op("dve", lambda e: e.tensor_tensor(out=Cf[:], in0=psum[5][:, 0:258], in1=Cf[:], op=ALU.add),
                          reads=["psb5", "Cf"], writes=["Cf"])
                    tk.op("dve", lambda e, c=c: e.tensor_scalar(out=Cf[:], in0=Cf[:], scalar1=ebl[:, c:c + 1],
                                                                scalar2=None, op0=ALU.mult),
                          reads=["Cf", "ebl"], writes=["Cf"])
                    tk.op("act", lambda e, cs=cs: e.copy(out=Cb[1 - cs][:], in_=Cf[:]), reads=["Cf"],
                          writes=["Cb%d" % (1 - cs)])
                    tk.op("act", lambda e, c=c: e.activation(out=hsm[:, 6:7], in_=psum[4][:, 256:257], func=AF.Abs,
                                                             scale=eb[:, c:c + 1]),
                          reads=["psb4", "eb"], writes=["hsm6"])
                    tk.op("dve", lambda e: e.tensor_scalar(out=hsm[:, 0:1], in0=hsm[:, 6:7], scalar1=1.0, scalar2=None,
                                                           op0=ALU.max),
                          reads=["hsm6"], writes=["hsm0"])
                    tk.op("dve", lambda e: e.reciprocal(out=hsm[:, 1:2], in_=hsm[:, 0:1]), reads=["hsm0"], writes=["hsm1"])
                    tk.op("dve", lambda e, c=c: e.tensor_tensor(out=hsm[:, 2:3], in0=hsm[:, 1:2], in1=eb[:, c:c + 1],
                                                                op=ALU.mult), reads=["hsm1", "eb"], writes=["hsm2"])
                    tk.op("act", lambda e, cs=cs: e.activation(out=sg[:], in_=mvo[cs][:, 256:512], func=AF.Exp, scale=-1.0),
                          reads=["mvo%d" % cs], writes=["sg"])
                    tk.op("dve", lambda e: e.tensor_scalar(out=sg[:], in0=sg[:], scalar1=1.0, scalar2=None, op0=ALU.add),
                          reads=["sg"], writes=["sg"])
                    tk.op("dve", lambda e: e.reciprocal(out=sg[:], in_=sg[:]), reads=["sg"], writes=["sg"])
                    tk.op("dve", lambda e, cs=cs: e.scalar_tensor_tensor(
                        out=hv[cs][:], in0=psum[4][:, 0:256], scalar=hsm[:, 2:3], in1=sg[:], op0=ALU.mult, op1=ALU.mult),
                        reads=["psb4", "hsm2", "sg"], writes=["hv%d" % cs])
                    tk.op("act", lambda e, cs=cs: e.activation(out=hj[:], in_=hv[cs][:], func=AF.Square,
                                                               accum_out=hsm[:, 3:4]),
                          reads=["hv%d" % cs], writes=["hj", "hsm3"])
                    tk.op("act", lambda e: e.activation(out=hsm[:, 4:5], in_=hsm[:, 3:4], func=AF.Ln, scale=1.0 / 256,
                                                        bias=epsc[:]), reads=["hsm3", "epsc"], writes=["hsm4"])
                    tk.op("act", lambda e: e.activation(out=hsm[:, 5:6], in_=hsm[:, 4:5], func=AF.Exp, scale=-0.5),
                          reads=["hsm4"], writes=["hsm5"])
                    tk.op("dve", lambda e, cs=cs: e.scalar_tensor_tensor(
                        out=hb[cs][:], in0=hv[cs][:], scalar=hsm[:, 5:6], in1=hnw_s[:, 256:512], op0=ALU.mult,
                        op1=ALU.mult), reads=["hv%d" % cs, "hsm5", "c9"], writes=["hb%d" % cs])
                    tk.dma("pool", mix_dst(c * 128, 256, 512), hb[cs][:], reads=["hb%d" % cs],
                           key="hb%d" % cs)
            tk.barrier()
        if own_tk:
            tk.final_wait("sp")
    print("phase A instructions:", tk.ninst)
    return nc


def host_inputs_a(inp, S):
    c = host_consts()
    w_in = np.asarray(inp["w_in"][0])
    maps = []
    for core in range(8):
        b, g = core // 4, core % 4
        h0, h1 = 2 * g, 2 * g + 1
        cols = []
        for base in (0, 1024, 2048):
            cols += list(range(base + h0 * 128, base + h0 * 128 + 128)) + list(range(base + h1 * 128, base + h1 * 128 + 128))
        cols += list(range(3080 + g * 128, 3080 + g * 128 + 128))
        cols += list(range(3592 + g * 128, 3592 + g * 128 + 128))
        cols += list(range(4104 + g * 256, 4104 + g * 256 + 256))
        cols += list(range(5136 + g * 256, 5136 + g * 256 + 256))
        cols += [3072 + h0, 3072 + h1, 5128 + g, 5132 + g]
        wcs = np.ascontiguousarray(w_in[:, cols])
        gbv = np.array([inp["fox_f_bias"][0][h0], inp["fox_f_bias"][0][h1], inp["mlstm_i_bias"][0][g],
                        inp["mlstm_f_bias"][0][g]], np.float32)
        cw = np.asarray(inp["mlstm_conv_w"][0])
        cbv = np.asarray(inp["mlstm_conv_b"][0])
        convp = np.concatenate([cw[:, g * 128:(g + 1) * 128].T, cw[:, 512 + g * 128:512 + (g + 1) * 128].T,
                                cbv[g * 128:(g + 1) * 128][:, None], cbv[512 + g * 128:512 + (g + 1) * 128][:, None]],
                               axis=1).astype(np.float32)
        hn = np.concatenate([np.asarray(inp["fox_out_norm_w"][0])[h0 * 128:(h1 + 1) * 128],
                             np.asarray(inp["mlstm_out_norm_w"][0])[g * 256:(g + 1) * 256]])
        m = {"x": np.ascontiguousarray(np.asarray(inp["x"])[b, :int(os.environ.get("XROWS", S))]),
             "wc": wcs,
             "n1w": np.ascontiguousarray(np.asarray(inp["norm1_w"][0]).reshape(KC, 128).T),
             "gbias": np.ascontiguousarray(np.broadcast_to(gbv[None, :], (128, 4))),
             "convp": np.ascontiguousarray(convp),
             "hnw": np.ascontiguousarray(np.broadcast_to(hn[None, :], (128, 512))).astype(np.float32)}
        m.update(c)
        maps.append(m)
    return maps


import os


def host_consts_b():
    c = {}
    c["ident_bf"] = np.eye(128, dtype=np.float32).astype(ml_dtypes.bfloat16)
    c["ident_f"] = np.eye(128, dtype=np.float32)
    c["iota_row"] = np.ascontiguousarray(np.broadcast_to(np.arange(128, dtype=np.float32)[None, :], (128, 128)))
    c["thr16"] = np.ascontiguousarray(np.broadcast_to((16.0 * np.arange(16, dtype=np.float32))[None, :], (128, 16)))
    c["iota16"] = np.ascontiguousarray(np.broadcast_to(np.arange(16, dtype=np.float32)[None, :], (128, 16)))
    return c


def build_phase_b(nc, TC, NJ=128, tk=None, gath=None, pfx="", pre=None):
    NTT = TC // 128
    PB = min(256, TC)
    NBLK = TC // PB
    TPB = PB // 128
    own_tk = tk is None
    if own_tk:
        tk = Trk(nc)
    _dt = nc.dram_tensor

    def dt(name, *a, **k):
        return _dt(pfx + name, *a, **k)
    x = dt("x", [TC, D], F32, kind="ExternalInput").ap()
    if gath is None:
        mixed = dt("mixed", [TC, D], BF16, kind="ExternalInput").ap()
    else:
        wsel_d = dt("wsel", [128, 8], F32, kind="ExternalInput").ap()
    wout = dt("wout", [D, D], F32, kind="ExternalInput").ap()
    wq = dt("wq", [D, D], F32, kind="ExternalInput").ap()
    n2w = dt("n2w", [128, D], F32, kind="ExternalInput").ap()
    fnw = dt("fnw", [128, D], F32, kind="ExternalInput").ap()
    keysT = dt("keysT", [128, 2, 128], F32, kind="ExternalInput").ap()
    if pre is None:
        uh = dt("uh", [128, 128, D], F32, kind="ExternalInput").ap()
        vh = dt("vh", [128, 128, D], F32, kind="ExternalInput").ap()
    ident_bf_d = dt("ident_bf", [128, 128], BF16, kind="ExternalInput").ap()
    ident_f_d = dt("ident_f", [128, 128], F32, kind="ExternalInput").ap()
    iota_row_d = dt("iota_row", [128, 128], F32, kind="ExternalInput").ap()
    thr16_d = dt("thr16", [128, 16], F32, kind="ExternalInput").ap()
    iota16_d = dt("iota16", [128, 16], F32, kind="ExternalInput").ap()
    out = dt("out", [TC, D], F32, kind="ExternalOutput").ap()
    if pre is None:
        u16 = dt("u16", [128, 128, D], BF16, kind="Internal").ap()
        v16 = dt("v16", [128, 128, D], BF16, kind="Internal").ap()
    else:
        u16, v16 = pre
    x1_d = dt("x1_d", [TC, D], F32, kind="Internal").ap()
    h2T_d = dt("h2T_d", [128, KC, TC], BF16, kind="Internal").ap()
    slot_d = dt("slot_d", [128, 3, TC], F32, kind="Internal").ap()

    with ExitStack() as es0:
        def sb(name, shape, dtype, es=es0):
            return es.enter_context(nc.sbuf_tensor(pfx + name, shape, dtype))
        ident_bf = sb("ident_bf_s", [128, 128], BF16)
        ident_f = sb("ident_f_s", [128, 128], F32)
        iota_row = sb("iota_row_s", [128, 128], F32)
        thr16 = sb("thr16_s", [128, 16], F32)
        iota16 = sb("iota16_s", [128, 16], F32)
        epsc = sb("epsc", [128, 1], F32)
        psum = [es0.enter_context(nc.psum_tensor(pfx + "ps%d" % i, [128, 512], F32)) for i in range(8)]
        for i, (s_, d_) in enumerate([(ident_bf, ident_bf_d), (ident_f, ident_f_d), (iota_row, iota_row_d),
                                      (thr16, thr16_d), (iota16, iota16_d)]):
            tk.dma("sp", s_[:], d_, writes=["c%d" % i], key="const")
        for j in (range(NJ) if pre is None else []):
            tk.dma("pool", u16[j], uh[j], writes=["u16"], key="cvt")
            tk.dma("pool", v16[j], vh[j], writes=["v16"], key="cvt")
        tk.op("dve", lambda e: e.memset(epsc[:], EPS), writes=["epsc"])

        def rms_rstd(src_ap, srckey, ss, junk):
            tk.op("act", lambda e: e.activation(out=junk[:], in_=src_ap, func=AF.Square, accum_out=ss[:, 0:1]),
                  reads=[srckey], writes=["junk", "ss0"])
            tk.op("act", lambda e: e.activation(out=ss[:, 1:2], in_=ss[:, 0:1], func=AF.Ln, scale=1.0 / D, bias=epsc[:]),
                  reads=["ss0", "epsc"], writes=["ss1"])
            tk.op("act", lambda e: e.activation(out=ss[:, 2:3], in_=ss[:, 1:2], func=AF.Exp, scale=-0.5),
                  reads=["ss1"], writes=["ss2"])

        def transpose16(src, srckey, dst, dstkey, banks):
            for half in range(2):
                bk = banks[half]
                pst = psum[bk][:, :].bitcast(BF16)
                for k8 in range(8):
                    kc = half * 8 + k8
                    tk.op("pe", lambda e, kc=kc, k8=k8, pst=pst: e.transpose(
                        out=pst[:, k8 * 128:(k8 + 1) * 128], in_=src[:, kc * 128:(kc + 1) * 128], identity=ident_bf[:]),
                        reads=[srckey, "c0"], writes=["psb%d" % bk])
                srcv = pst.rearrange("p (k t) -> p k t", k=8)
                dstv = dst[:, half * 8:(half + 1) * 8, :]
                if half == 0:
                    tk.op("act", lambda e, srcv=srcv, dstv=dstv: e.copy(out=dstv, in_=srcv),
                          reads=["psb%d" % bk], writes=[dstkey])
                else:
                    tk.op("dve", lambda e, srcv=srcv, dstv=dstv: e.tensor_copy(out=dstv, in_=srcv),
                          reads=["psb%d" % bk], writes=[dstkey])

        with ExitStack() as es1:
            Wo = sb("Wo", [128, KC, D], BF16, es1)
            n2b = sb("n2b", [128, D], F32, es1)
            mx = [sb("mx%d" % i, [128, D], BF16, es1) for i in range(2)]
            xt = [sb("xt%d" % i, [128, D], F32, es1) for i in range(2)]
            x1 = [sb("x1_%d" % i, [128, D], F32, es1) for i in range(2)]
            mT = sb("mT", [128, KC, 128], BF16, es1)
            h2 = sb("h2", [128, D], BF16, es1)
            h2t = [sb("h2t%d" % i, [128, KC, 128], BF16, es1) for i in range(2)]
            junk = sb("junk", [128, D], BF16, es1)
            ss = sb("ss", [128, 4], F32, es1)
            for kc in range(KC):
                tk.dma("pool", Wo[:, kc, :], wout[kc * 128:(kc + 1) * 128, :], writes=["Wo"], key="wload")
            tk.dma("sp", n2b[:], n2w, writes=["n2b"])
            ccnt = [0]
            if gath is not None:
                cand = [sb("cand%d" % i, [128, D], BF16, es1) for i in range(3)]
                wsel = sb("wsel_s", [128, 8], F32, es1)
                tk.dma("sp", wsel[:], wsel_d, writes=["wsel"])
            for ti in range(NTT):
                s = ti % 2
                rows = slice(ti * 128, (ti + 1) * 128)
                if gath is None:
                    tk.dma("sp", mx[s][:], mixed[rows, :], writes=["mx%d" % s])
                else:
                    bb_, lt = ti // (NTT // 2), ti % (NTT // 2)
                    for dp in range(8):
                        cs_ = ccnt[0] % 3
                        ccnt[0] += 1
                        gt_ = dp * (NTT // 2) + lt
                        kq = gt_ // 4
                        src = gath[kq].ap().rearrange("(r t) c -> r t c", r=8)[bb_ * 4:(bb_ + 1) * 4,
                                                                              (gt_ % 4) * 128:(gt_ % 4 + 1) * 128, :]
                        tk.dma("sp", cand[cs_][:].rearrange("p (g c) -> p g c", g=4), src.rearrange("g t c -> t g c"),
                               reads=["gath%d" % kq], writes=["cand%d" % cs_])
                        if dp == 0:
                            tk.op("dve", lambda e, cs_=cs_, s=s: e.tensor_scalar(
                                out=mx[s][:], in0=cand[cs_][:], scalar1=wsel[:, 0:1], scalar2=None, op0=ALU.mult),
                                reads=["cand%d" % cs_, "wsel"], writes=["mx%d" % s])
                        else:
                            tk.op("dve", lambda e, cs_=cs_, s=s, dp=dp: e.scalar_tensor_tensor(
                                out=mx[s][:], in0=cand[cs_][:], scalar=wsel[:, dp:dp + 1], in1=mx[s][:], op0=ALU.mult,
                                op1=ALU.add), reads=["cand%d" % cs_, "wsel", "mx%d" % s], writes=["mx%d" % s])
                tk.dma("sp", xt[s][:], x[rows, :], writes=["xt%d" % s])
                transpose16(mx[s], "mx%d" % s, mT, "mT", (0, 1))
                for cg in range(4):
                    b = 2 + cg
                    for kc in range(KC):
                        tk.op("pe", lambda e, kc=kc, b=b, cg=cg: e.matmul(
                            psum[b][:, :], lhsT=mT[:, kc, :], rhs=Wo[:, kc, cg * 512:(cg + 1) * 512],
                            start=(kc == 0), stop=(kc == KC - 1)), reads=["mT", "Wo"], writes=["psb%d" % b])
                    tk.op("dve", lambda e, b=b, cg=cg, s=s: e.tensor_tensor(
                        out=x1[s][:, cg * 512:(cg + 1) * 512], in0=psum[b][:, :], in1=xt[s][:, cg * 512:(cg + 1) * 512],
                        op=ALU.add), reads=["psb%d" % b, "xt%d" % s], writes=["x1_%d" % s])
                tk.dma("pool", x1_d[rows, :], x1[s][:], reads=["x1_%d" % s], key="x1st%d" % s)
                rms_rstd(x1[s][:], "x1_%d" % s, ss, junk)
                tk.op("dve", lambda e, s=s: e.scalar_tensor_tensor(out=h2[:], in0=x1[s][:], scalar=ss[:, 2:3], in1=n2b[:],
                                                                   op0=ALU.mult, op1=ALU.mult),
                      reads=["x1_%d" % s, "ss2", "n2b"], writes=["h2"])
                transpose16(h2, "h2", h2t[s], "h2t%d" % s, (6, 7))
                tk.dma("pool", h2T_d[:, :, rows], h2t[s][:], reads=["h2t%d" % s], key="h2st%d" % s)
            tk.barrier()

        if os.environ.get('BSTOP') == '1':
            tk.final_wait('sp')
            return nc
        with ExitStack() as es2:
            Wq = sb("Wq", [128, KC, D], BF16, es2)
            kT = sb("kTs", [128, 2, 128], BF16, es2)
            h2t = [sb("h2tb%d" % i, [128, KC, 128], BF16, es2) for i in range(2)]
            qpT = [sb("qpT%d" % i, [128, 128], BF16, es2) for i in range(2)]
            sc = sb("sc", [128, 16, 128], F32, es2)
            tmp1 = sb("tmp1", [128, 128], F32, es2)
            st = sb("st", [128, 16, 16], F32, es2)
            iu = sb("iu", [128, 16, 16], U32, es2)
            itf = sb("itf", [128, 16, 16], F32, es2)
            dd = sb("dd", [128, 16, 16], F32, es2)
            cand = sb("cand", [128, 8, 256], F32, es2)
            tmp2 = sb("tmp2", [128, 256], F32, es2)
            cf = sb("cf", [128, 8, 16], F32, es2)
            pu = sb("pu", [128, 8, 16], U32, es2)
            posf = sb("posf", [128, 8, 16], F32, es2)
            cs = sb("cs", [128, 8, 16], F32, es2)
            zs = sb("zs", [128, 8], F32, es2)
            ge = sb("ge", [128, 8, 16, 16], F32, es2)
            prod = sb("prod", [128, 8, 16, 16], F32, es2)
            k1s = sb("k1s", [128, 8, 16], F32, es2)
            k2f = sb("k2f", [128, 8, 16], F32, es2)
            res = sb("res", [128, 3, 128], F32, es2)
            rst = [sb("rst%d" % i, [128, 3, 128], F32, es2) for i in range(2)]
            for kc in range(KC):
                tk.dma("pool", Wq[:, kc, :], wq[kc * 128:(kc + 1) * 128, :], writes=["Wq"], key="wload")
            tk.dma("pool", kT[:], keysT, writes=["kT"], key="wload")
            st4 = st[:].rearrange("p (h q) k -> p h q k", q=2)
            itf4 = itf[:].rearrange("p (h q) k -> p h q k", q=2)
            dd4 = dd[:].rearrange("p (h q) k -> p h q k", q=2)
            for ti in range(NTT):
                s = ti % 2
                cols = slice(ti * 128, (ti + 1) * 128)
                tk.dma("sp", h2t[s][:], h2T_d[:, :, cols], writes=["h2tb%d" % s])
                for blk in range(16):
                    b = blk % 2
                    p = blk % 2
                    for kc in range(KC):
                        tk.op("pe", lambda e, kc=kc, b=b, blk=blk: e.matmul(
                            psum[b][:, 0:128], lhsT=Wq[:, kc, blk * 128:(blk + 1) * 128], rhs=h2t[s][:, kc, :],
                            start=(kc == 0), stop=(kc == KC - 1)), reads=["Wq", "h2tb%d" % s], writes=["psb%d" % b])
                    tk.op("act", lambda e, b=b: e.copy(out=qpT[b][:], in_=psum[b][:, 0:128]),
                          reads=["psb%d" % b], writes=["qpT%d" % b])
                    sbk = 2 + blk // 4
                    tk.op("pe", lambda e, b=b, p=p, sbk=sbk, blk=blk: e.matmul(
                        psum[sbk][:, (blk % 4) * 128:(blk % 4 + 1) * 128], lhsT=qpT[b][:], rhs=kT[:, p, :],
                        start=True, stop=True), reads=["qpT%d" % b, "kT"], writes=["psb%d" % sbk])
                    if blk % 4 == 3:
                        tk.op("dve", lambda e, sbk=sbk, blk=blk: e.tensor_copy(
                            out=sc[:, blk - 3:blk + 1, :], in_=psum[sbk][:, :].rearrange("p (a n) -> p a n", a=4)),
                            reads=["psb%d" % sbk], writes=["sc"])
                for blk in range(16):
                    tk.op("dve", lambda e, blk=blk: e.max(out=st[:, blk, 0:8], in_=sc[:, blk, :]),
                          reads=["sc"], writes=["st"])
                    tk.op("dve", lambda e, blk=blk: e.max_index(out=iu[:, blk, 0:8], in_max=st[:, blk, 0:8],
                                                                in_values=sc[:, blk, :]),
                          reads=["sc", "st"], writes=["iu"])
                    tk.op("dve", lambda e, blk=blk: e.match_replace(out=tmp1[:], in_to_replace=st[:, blk, 0:8],
                                                                    in_values=sc[:, blk, :], imm_value=-1e30),
                          reads=["sc", "st"], writes=["tmp1"])
                    tk.op("dve", lambda e, blk=blk: e.max(out=st[:, blk, 8:16], in_=tmp1[:]),
                          reads=["tmp1"], writes=["st"])
                    tk.op("dve", lambda e, blk=blk: e.max_index(out=iu[:, blk, 8:16], in_max=st[:, blk, 8:16],
                                                                in_values=tmp1[:]),
                          reads=["tmp1", "st"], writes=["iu"])
                tk.op("dve", lambda e: e.tensor_copy(out=itf[:], in_=iu[:]), reads=["iu"], writes=["itf"])
                tk.op("dve", lambda e: e.tensor_copy(out=dd[:, :, 0:1], in_=itf[:, :, 0:1]), reads=["itf"], writes=["dd"])
                tk.op("dve", lambda e: e.tensor_tensor(out=dd[:, :, 1:16], in0=itf[:, :, 1:16], in1=itf[:, :, 0:15],
                                                       op=ALU.subtract), reads=["itf"], writes=["dd"])
                a0 = st4[:, :, 0, :].unsqueeze(3).broadcast_to([128, 8, 16, 16])
                a1 = st4[:, :, 1, :].unsqueeze(2).broadcast_to([128, 8, 16, 16])
                cand4 = cand[:].rearrange("p h (a b) -> p h a b", a=16)
                tk.op("dve", lambda e: e.tensor_tensor(out=cand4, in0=a0, in1=a1, op=ALU.add), reads=["st"], writes=["cand"])
                for h in range(8):
                    tk.op("dve", lambda e, h=h: e.max(out=cf[:, h, 0:8], in_=cand[:, h, :]), reads=["cand"], writes=["cf"])
                    tk.op("dve", lambda e, h=h: e.max_index(out=pu[:, h, 0:8], in_max=cf[:, h, 0:8], in_values=cand[:, h, :]),
                          reads=["cand", "cf"], writes=["pu"])
                    tk.op("dve", lambda e, h=h: e.match_replace(out=tmp2[:], in_to_replace=cf[:, h, 0:8],
                                                                in_values=cand[:, h, :], imm_value=-1e30),
                          reads=["cand", "cf"], writes=["tmp2"])
                    tk.op("dve", lambda e, h=h: e.max(out=cf[:, h, 8:16], in_=tmp2[:]), reads=["tmp2"], writes=["cf"])
                    tk.op("dve", lambda e, h=h: e.max_index(out=pu[:, h, 8:16], in_max=cf[:, h, 8:16], in_values=tmp2[:]),
                          reads=["tmp2", "cf"], writes=["pu"])
                tk.op("dve", lambda e: e.tensor_copy(out=posf[:], in_=pu[:]), reads=["pu"], writes=["posf"])
                tk.op("dve", lambda e: e.tensor_tensor(out=cs[:], in0=cf[:], in1=cf[:, :, 0:1].broadcast_to([128, 8, 16]),
                                                       op=ALU.subtract), reads=["cf"], writes=["cs"])
                tk.op("act", lambda e: e.activation(out=cs[:], in_=cs[:], func=AF.Exp), reads=["cs"], writes=["cs"])
                tk.op("dve", lambda e: e.tensor_reduce(out=zs[:], in_=cs[:], axis=AX.X, op=ALU.add), reads=["cs"], writes=["zs"])
                tk.op("dve", lambda e: e.reciprocal(out=zs[:], in_=zs[:]), reads=["zs"], writes=["zs"])
                res_g = res[:, 2, :].rearrange("p (h k) -> p h k", h=8)
                tk.op("dve", lambda e: e.tensor_tensor(out=res_g, in0=cs[:], in1=zs[:].unsqueeze(2).broadcast_to([128, 8, 16]),
                                                       op=ALU.mult), reads=["cs", "zs"], writes=["res"])
                pos_b = posf[:].unsqueeze(3).broadcast_to([128, 8, 16, 16])
                thr_b = thr16[:].unsqueeze(1).unsqueeze(1).broadcast_to([128, 8, 16, 16])
                tk.op("dve", lambda e: e.tensor_tensor(out=ge[:], in0=pos_b, in1=thr_b, op=ALU.is_ge),
                      reads=["posf", "c3"], writes=["ge"])
                d1_b = dd4[:, :, 0, :].unsqueeze(2).broadcast_to([128, 8, 16, 16])
                tk.op("dve", lambda e: e.tensor_tensor(out=prod[:], in0=ge[:], in1=d1_b, op=ALU.mult),
                      reads=["ge", "dd"], writes=["prod"])
                res_i = res[:, 0, :].rearrange("p (h k) -> p h k", h=8)
                tk.op("dve", lambda e: e.tensor_reduce(out=res_i, in_=prod[:], axis=AX.X, op=ALU.add),
                      reads=["prod"], writes=["res"])
                tk.op("dve", lambda e: e.tensor_reduce(out=k1s[:], in_=ge[:], axis=AX.X, op=ALU.add), reads=["ge"], writes=["k1s"])
                tk.op("dve", lambda e: e.tensor_scalar(out=k1s[:], in0=k1s[:], scalar1=-16.0, scalar2=16.0, op0=ALU.mult,
                                                       op1=ALU.add), reads=["k1s"], writes=["k1s"])
                tk.op("dve", lambda e: e.tensor_tensor(out=k2f[:], in0=posf[:], in1=k1s[:], op=ALU.add),
                      reads=["posf", "k1s"], writes=["k2f"])
                k2_b = k2f[:].unsqueeze(3).broadcast_to([128, 8, 16, 16])
                io_b = iota16[:].unsqueeze(1).unsqueeze(1).broadcast_to([128, 8, 16, 16])
                tk.op("dve", lambda e: e.tensor_tensor(out=ge[:], in0=k2_b, in1=io_b, op=ALU.is_ge),
                      reads=["k2f", "c4"], writes=["ge"])
                d2_b = dd4[:, :, 1, :].unsqueeze(2).broadcast_to([128, 8, 16, 16])
                tk.op("dve", lambda e: e.tensor_tensor(out=prod[:], in0=ge[:], in1=d2_b, op=ALU.mult),
                      reads=["ge", "dd"], writes=["prod"])
                res_j = res[:, 1, :].rearrange("p (h k) -> p h k", h=8)
                tk.op("dve", lambda e: e.tensor_reduce(out=res_j, in_=prod[:], axis=AX.X, op=ALU.add),
                      reads=["prod"], writes=["res"])
                for q in range(3):
                    tk.op("pe", lambda e, q=q: e.transpose(out=psum[6][:, q * 128:(q + 1) * 128], in_=res[:, q, :],
                                                           identity=ident_f[:]), reads=["res", "c1"], writes=["psb6"])
                tk.op("act", lambda e, s=s: e.copy(out=rst[s][:], in_=psum[6][:, 0:384].rearrange("p (q t) -> p q t", q=3)),
                      reads=["psb6"], writes=["rst%d" % s])
                tk.dma("pool", slot_d[:, :, cols], rst[s][:], reads=["rst%d" % s], key="rst%d" % s)
            tk.barrier()

        if os.environ.get('BSTOP') == '2':
            tk.final_wait('sp')
            return nc
        with ExitStack() as es3:
            G = sb("G", [128, 128, PB], BF16, es3)
            ut = [sb("ut%d" % i, [128, KC, 128], BF16, es3) for i in range(3)]
            vt = [sb("vt%d" % i, [128, D], BF16, es3) for i in range(3)]
            h2b = [sb("h2b%d" % i, [128, KC, PB], BF16, es3) for i in range(2)]
            slots = [sb("slots%d" % i, [128, 3, PB], F32, es3) for i in range(2)]
            gl = [sb("gl%d" % i, [128, PB], BF16, es3) for i in range(2)]
            ohi = [sb("ohi%d" % i, [128, 128], BF16, es3) for i in range(4)]
            ohj = [sb("ohj%d" % i, [128, 128], BF16, es3) for i in range(4)]
            x1t = sb("x1t", [128, D], F32, es3)
            xo = [sb("xo%d" % i, [128, D], F32, es3) for i in range(2)]
            junk = sb("junk3", [128, D], BF16, es3)
            fnb = sb("fnb", [128, D], F32, es3)
            ss = sb("ss3", [128, 4], F32, es3)
            tk.dma("sp", fnb[:], fnw, writes=["fnb"])
            ucnt = [0]
            vcnt = [0]
            ocnt = [0]
            for blk in range(NBLK):
                bs = blk % 2
                cols = slice(blk * PB, (blk + 1) * PB)
                tk.dma("sp", slots[bs][:], slot_d[:, :, cols], writes=["slots%d" % bs])
                tk.dma("sp", h2b[bs][:], h2T_d[:, :, cols], writes=["h2b%d" % bs])
                for t in range(PB):
                    o = t % 4
                    tk.op("dve", lambda e, o=o, t=t, bs=bs: e.tensor_scalar(
                        out=ohi[o][:], in0=iota_row[:], scalar1=slots[bs][:, 0, t:t + 1], scalar2=slots[bs][:, 2, t:t + 1],
                        op0=ALU.is_equal, op1=ALU.mult), reads=["slots%d" % bs, "c2"], writes=["ohi%d" % o])
                    tk.op("dve", lambda e, o=o, t=t, bs=bs: e.tensor_scalar(
                        out=ohj[o][:], in0=iota_row[:], scalar1=slots[bs][:, 1, t:t + 1], scalar2=None,
                        op0=ALU.is_equal), reads=["slots%d" % bs, "c2"], writes=["ohj%d" % o])
                    gb_ = (t // 4) % 2
                    tk.op("pe", lambda e, o=o, gb_=gb_: e.matmul(psum[gb_][:, o * 128:(o + 1) * 128], lhsT=ohi[o][:],
                                                                 rhs=ohj[o][:], start=True, stop=True),
                          reads=["ohi%d" % o, "ohj%d" % o], writes=["psb%d" % gb_])
                    if o == 3:
                        t0 = t - 3
                        tk.op("act", lambda e, gb_=gb_, t0=t0: e.copy(
                            out=G[:, :, t0:t0 + 4], in_=psum[gb_][:, :].rearrange("p (t j) -> p j t", t=4)),
                            reads=["psb%d" % gb_], writes=["G"])
                for j in range(NJ):
                    us = ucnt[0] % 3
                    ucnt[0] += 1
                    tk.dma("sp", ut[us][:], u16[j].rearrange("p (k i) -> p k i", k=KC), writes=["ut%d" % us])
                    b = 2 + j % 2
                    for kc in range(KC):
                        tk.op("pe", lambda e, kc=kc, b=b, us=us, bs=bs: e.matmul(
                            psum[b][:, 0:PB], lhsT=ut[us][:, kc, :], rhs=h2b[bs][:, kc, :], start=(kc == 0),
                            stop=(kc == KC - 1)), reads=["ut%d" % us, "h2b%d" % bs], writes=["psb%d" % b])
                    g = j % 2
                    tk.op("act", lambda e, b=b, g=g: e.activation(out=gl[g][:], in_=psum[b][:, 0:PB], func=AF.Gelu),
                          reads=["psb%d" % b], writes=["gl%d" % g])
                    tk.op("dve", lambda e, g=g, j=j: e.tensor_tensor(out=G[:, j, :], in0=gl[g][:], in1=G[:, j, :], op=ALU.mult),
                          reads=["gl%d" % g, "G"], writes=["G"])
                for j in range(NJ):
                    vs = vcnt[0] % 3
                    vcnt[0] += 1
                    tk.dma("sp", vt[vs][:], v16[j], writes=["vt%d" % vs])
                    for tt in range(TPB):
                        for cg in range(4):
                            b = tt * 4 + cg
                            tk.op("pe", lambda e, j=j, tt=tt, cg=cg, b=b, vs=vs: e.matmul(
                                psum[b][:, :], lhsT=G[:, j, tt * 128:(tt + 1) * 128], rhs=vt[vs][:, cg * 512:(cg + 1) * 512],
                                start=(j == 0), stop=(j == NJ - 1)), reads=["G", "vt%d" % vs], writes=["psb%d" % b])
                for tt in range(TPB):
                    rows = slice(blk * PB + tt * 128, blk * PB + (tt + 1) * 128)
                    o = ocnt[0] % 2
                    ocnt[0] += 1
                    tk.dma("sp", x1t[:], x1_d[rows, :], writes=["x1t"])
                    for cg in range(4):
                        b = tt * 4 + cg
                        tk.op("dve", lambda e, b=b, cg=cg, o=o: e.tensor_tensor(
                            out=xo[o][:, cg * 512:(cg + 1) * 512], in0=psum[b][:, :], in1=x1t[:, cg * 512:(cg + 1) * 512],
                            op=ALU.add), reads=["psb%d" % b, "x1t"], writes=["xo%d" % o])
                    rms_rstd(xo[o][:], "xo%d" % o, ss, junk)
                    tk.op("dve", lambda e, o=o: e.scalar_tensor_tensor(out=xo[o][:], in0=xo[o][:], scalar=ss[:, 2:3],
                                                                       in1=fnb[:], op0=ALU.mult, op1=ALU.mult),
                          reads=["xo%d" % o, "ss2", "fnb"], writes=["xo%d" % o])
                    tk.dma("pool", out[rows, :], xo[o][:], reads=["xo%d" % o], key="ost%d" % o)
            tk.barrier()
        tk.final_wait("sp")
    print("phase B instructions (cumulative):", tk.ninst)
    return nc


def host_inputs_b(inp, xflat, mixed_full, TCs):
    c = host_consts_b()
    U = np.asarray(inp["peer_u"][0])
    V = np.asarray(inp["peer_v"][0])
    uh = np.ascontiguousarray(U.reshape(128, 128, KC, 128).transpose(1, 3, 2, 0)).reshape(128, 128, D)
    vh = np.ascontiguousarray(V.reshape(128, 128, D).transpose(1, 0, 2))
    keys = np.asarray(inp["peer_keys"][0])
    keysT = np.ascontiguousarray(keys.transpose(2, 0, 1))
    wout = np.ascontiguousarray(np.asarray(inp["w_out"][0]))
    wq = np.ascontiguousarray(np.asarray(inp["peer_w_q"][0]))
    n2w = np.ascontiguousarray(np.broadcast_to(np.asarray(inp["norm2_w"][0])[None, :], (128, D)))
    fnw = np.ascontiguousarray(np.broadcast_to(np.asarray(inp["final_norm_w"])[None, :], (128, D)))
    maps = []
    for core in range(8):
        r0 = core * TCs
        m = {"x": np.ascontiguousarray(xflat[r0:r0 + TCs]),
             "mixed": np.ascontiguousarray(mixed_full[r0:r0 + TCs]),
             "wout": wout, "wq": wq, "n2w": n2w, "fnw": fnw, "keysT": keysT, "uh": uh, "vh": vh}
        m.update(c)
        maps.append(m)
    return maps


def build_fused(nc, S):
    TC = 2 * S // 8
    NK = S // 512
    tk = Trk(nc)
    mixloc = [nc.dram_tensor("mixloc%d" % k, [512, 512], BF16) for k in range(NK)]
    gath = [nc.dram_tensor("gath%d" % k, [8 * 512, 512], BF16) for k in range(NK)]
    uh = nc.dram_tensor("b_uh", [128, 128, D], F32, kind="ExternalInput").ap()
    vh = nc.dram_tensor("b_vh", [128, 128, D], F32, kind="ExternalInput").ap()
    u16 = nc.dram_tensor("b_u16", [128, 128, D], BF16, kind="Internal").ap()
    v16 = nc.dram_tensor("b_v16", [128, 128, D], BF16, kind="Internal").ap()
    tk.lazy_keys.add("cvt")
    for j in range(128):
        tk.dma("pool", u16[j], uh[j], writes=["u16"], key="cvt")
        tk.dma("pool", v16[j], vh[j], writes=["v16"], key="cvt")
    build_phase_a(nc, S, tk=tk, mix_dst=lambda t0, c0, c1: mixloc[t0 // 512][t0 % 512:t0 % 512 + 128, c0:c1])
    for k in range(NK):
        tk.coll(lambda g, k=k: g.collective_compute("AllGather", ALU.bypass, replica_groups=[list(range(8))],
                                                    ins=[mixloc[k].ap().opt()], outs=[gath[k].ap().opt()]),
                writes=["gath%d" % k])
    tk.lazy_keys.discard("cvt")
    build_phase_b(nc, TC, tk=tk, gath=gath, pfx="b_", pre=(u16, v16))
    return nc


def wout_perm():
    perm = []
    for g in range(4):
        perm += list(range(g * 256, (g + 1) * 256)) + list(range(1024 + g * 256, 1024 + (g + 1) * 256))
    return np.array(perm)


def host_inputs_fused(inp, S):
    maps = host_inputs_a(inp, S)
    c = host_consts_b()
    TC = 2 * S // 8
    H = S // 8
    U = np.asarray(inp["peer_u"][0])
    V = np.asarray(inp["peer_v"][0])
    uh = np.ascontiguousarray(U.reshape(128, 128, KC, 128).transpose(1, 3, 2, 0)).reshape(128, 128, D)
    vh = np.ascontiguousarray(V.reshape(128, 128, D).transpose(1, 0, 2))
    keysT = np.ascontiguousarray(np.asarray(inp["peer_keys"][0]).transpose(2, 0, 1))
    wout = np.ascontiguousarray(np.asarray(inp["w_out"][0])[wout_perm(), :])
    wq = np.ascontiguousarray(np.asarray(inp["peer_w_q"][0]))
    n2w = np.ascontiguousarray(np.broadcast_to(np.asarray(inp["norm2_w"][0])[None, :], (128, D)))
    fnw = np.ascontiguousarray(np.broadcast_to(np.asarray(inp["final_norm_w"])[None, :], (128, D)))
    x = np.asarray(inp["x"])
    for d in range(8):
        m = maps[d]
        m["b_x"] = np.ascontiguousarray(np.concatenate([x[0, d * H:(d + 1) * H], x[1, d * H:(d + 1) * H]], axis=0))
        ws = np.zeros((128, 8), np.float32)
        ws[:, d] = 1.0
        m["b_wsel"] = ws
        m.update({"b_wout": wout, "b_wq": wq, "b_n2w": n2w, "b_fnw": fnw, "b_keysT": keysT, "b_uh": uh, "b_vh": vh})
        for k, v in c.items():
            m["b_" + k] = v
    return maps


def gather_out(res, S):
    H = S // 8
    out = np.zeros((2, S, D), np.float32)
    for d in range(8):
        o = res.results[d]["b_out"]
        out[0, d * H:(d + 1) * H] = o[0:H]
        out[1, d * H:(d + 1) * H] = o[H:2 * H]
    return out


S_FULL = 16384


def kernel(**inputs):
    inp = {k: np.asarray(v) for k, v in inputs.items()}
    S = S_FULL
    nc = bass.Bass("TRN2", target_bir_lowering=False)
    build_fused(nc, S)
    maps = host_inputs_fused(inp, S)
    res = run_bass_kernel_spmd(nc, maps, core_ids=list(range(8)))
    return gather_out(res, S).astype(np.float32)
```

```python
import numpy as np
import ml_dtypes
from contextlib import ExitStack
import concourse.bass as bass
import concourse.mybir as mybir
from concourse.bass_utils import run_bass_kernel_spmd

F32 = mybir.dt.float32
BF16 = mybir.dt.bfloat16
I32 = mybir.dt.int32
U32 = mybir.dt.uint32
AF = mybir.ActivationFunctionType
ALU = mybir.AluOpType
AX = mybir.AxisListType

D = 2048
KC = 16
EPS = 1e-6


class Trk:
    def __init__(self, nc):
        self.nc = nc
        self.engs = {"pe": nc.tensor, "act": nc.scalar, "dve": nc.vector,
                     "pool": nc.gpsimd, "sp": nc.sync}
        self.sem = {}
        self.cnt = {}
        for e in ("pe", "act", "dve", "pool"):
            self.sem[e] = nc.alloc_semaphore(name="s_" + e)
            self.cnt[e] = 0
        self.dsem = {}
        self.waited = {}
        self.lastw = {}
        self.readers = {}
        self.ninst = 0
        self.lazy_keys = set()

    def _wait(self, eng, ev):
        sem, val, src, key = ev
        k = (eng, key)
        if self.waited.get(k, 0) >= val:
            return
        self.engs[eng].wait_ge(sem, val)
        self.waited[k] = val

    def _deps(self, eng, reads, writes):
        for b in reads:
            ev = self.lastw.get(b)
            if ev is not None and not (ev[2] == eng and eng == "pe"):
                self._wait(eng, ev)
        for b in writes:
            ev = self.lastw.get(b)
            if ev is not None and ev[2] != eng:
                self._wait(eng, ev)
            for ev in self.readers.get(b, ()):
                if ev[2] != eng:
                    self._wait(eng, ev)

    def _record(self, ev, reads, writes):
        for b in reads:
            self.readers.setdefault(b, []).append(ev)
        for b in writes:
            self.lastw[b] = ev
            self.readers[b] = []

    def op(self, eng, fn, reads=(), writes=(), inc=True):
        self._deps(eng, reads, writes)
        inst = fn(self.engs[eng])
        if inc:
            self.cnt[eng] += 1
            inst.then_inc(self.sem[eng], 1)
            ev = (self.sem[eng], self.cnt[eng], eng, eng)
        else:
            assert eng == "pe"
            ev = (self.sem[eng], self.cnt[eng] + 1, eng, eng)
        self._record(ev, reads, writes)
        self.ninst += 1
        return inst

    def dma(self, q, out, in_, reads=(), writes=(), key=None, **kw):
        if key is None:
            key = writes[0] if writes else reads[0]
        if key not in self.dsem:
            self.dsem[key] = [self.nc.alloc_semaphore(name="d%d" % len(self.dsem)), 0]
        self._deps(q, reads, writes)
        ds = self.dsem[key]
        inst = self.engs[q].dma_start(out=out, in_=in_, **kw)
        ds[1] += 16
        inst.then_inc(ds[0], 16)
        ev = (ds[0], ds[1], "dma", ("d", key))
        self._record(ev, reads, writes)
        self.ninst += 1
        return inst

    def coll(self, fn, reads=(), writes=()):
        if "cc" not in self.sem:
            self.sem["cc"] = self.nc.alloc_semaphore(name="s_cc")
            self.cnt["cc"] = 0
        self._deps("pool", reads, writes)
        inst = fn(self.engs["pool"])
        self.cnt["cc"] += 1
        inst.then_inc(self.sem["cc"])
        ev = (self.sem["cc"], self.cnt["cc"], "cc", "cc")
        self._record(ev, reads, writes)
        self.ninst += 1
        return inst

    def barrier(self):
        evs = [(self.sem[e], self.cnt[e], e, e) for e in self.sem if self.cnt[e] > 0]
        evs += [(v[0], v[1], "dma", ("d", k)) for k, v in self.dsem.items()
                if v[1] > 0 and k not in self.lazy_keys]
        for e in ("pe", "act", "dve", "pool", "sp"):
            for ev in evs:
                if ev[2] != e:
                    self._wait(e, ev)
        keep = {b: ev for b, ev in self.lastw.items() if ev[2] == "dma" and ev[3][1] in self.lazy_keys}
        self.lastw = keep
        self.readers = {}

    def final_wait(self, q="sp"):
        for k, v in self.dsem.items():
            if v[1] > 0:
                self._wait(q, (v[0], v[1], "dma", ("d", k)))


class OpQueue:
    def __init__(self):
        self.q = []
        self.suffix = ""
        self.priv = set()

    def _fix(self, k):
        for nm in ("reads", "writes"):
            if nm in k:
                k[nm] = [x + self.suffix if x in self.priv else x for x in k[nm]]
        return k

    def op(self, *a, **k):
        self.q.append(("op", a, self._fix(k)))

    def dma(self, *a, **k):
        self.q.append(("dma", a, self._fix(k)))


def emit_interleaved(tk, queues):
    n = max(len(q.q) for q in queues)
    for i in range(n):
        for q in queues:
            if i < len(q.q):
                kind, a, k = q.q[i]
                getattr(tk, kind)(*a, **k)


import os
PH = os.environ.get('PH', 'PAM')
SK = os.environ.get('SK', '')

NCOL = 1540
C_FQ0, C_FQ1, C_FK0, C_FK1, C_FV, C_MQ, C_MK, C_MVO, C_G = 0, 128, 256, 384, 512, 768, 896, 1024, 1536


def host_consts():
    c = {}
    c["ident_bf"] = np.eye(128, dtype=np.float32).astype(ml_dtypes.bfloat16)
    c["ident_f"] = np.eye(128, dtype=np.float32)
    r = np.arange(128)
    c["triu"] = (r[:, None] <= r[None, :]).astype(np.float32)
    c["ones_f"] = np.ones((128, 128), np.float32)
    es = np.zeros((128, 128), np.float32); es[0, :] = 1.0
    c["esel"] = es
    t = np.arange(512)
    m = np.stack([(r[:, None] + 128 * rr <= t[None, :]) for rr in range(4)], axis=1)
    c["amask"] = m.astype(np.float32).astype(ml_dtypes.bfloat16)
    c["mmask"] = ((r[:, None] <= r[None, :]).astype(np.float32) * (128 ** -0.5)).astype(np.float32)
    return c


def build_phase_a(nc, S, tk=None, mix_dst=None):
    NT = S // 128
    NB = S // 512
    own_tk = tk is None
    if own_tk:
        tk = Trk(nc)
    dt = nc.dram_tensor
    x = dt("x", [int(os.environ.get("XROWS", S)), D], F32, kind="ExternalInput").ap()
    wc = dt("wc", [D, NCOL], F32, kind="ExternalInput").ap()
    n1w = dt("n1w", [128, KC], F32, kind="ExternalInput").ap()
    gbias = dt("gbias", [128, 4], F32, kind="ExternalInput").ap()
    convp = dt("convp", [128, 10], F32, kind="ExternalInput").ap()
    hnw = dt("hnw", [128, 512], F32, kind="ExternalInput").ap()
    ident_bf_d = dt("ident_bf", [128, 128], BF16, kind="ExternalInput").ap()
    ident_f_d = dt("ident_f", [128, 128], F32, kind="ExternalInput").ap()
    triu_d = dt("triu", [128, 128], F32, kind="ExternalInput").ap()
    ones_d = dt("ones_f", [128, 128], F32, kind="ExternalInput").ap()
    esel_d = dt("esel", [128, 128], F32, kind="ExternalInput").ap()
    amask_d = dt("amask", [128, 4, 512], BF16, kind="ExternalInput").ap()
    mmask_d = dt("mmask", [128, 128], F32, kind="ExternalInput").ap()
    if mix_dst is None:
        mixed = dt("mixed", [int(os.environ.get("MROWS", S)), 512], BF16, kind="ExternalOutput").ap()
        mix_dst = lambda t0, c0, c1: mixed[t0:t0 + 128, c0:c1]
    qt_d = [dt("qt%d" % h, [128, S], BF16, kind="Internal").ap() for h in range(2)]
    kt_d = [dt("kt%d" % h, [128, S], BF16, kind="Internal").ap() for h in range(2)]
    va_d = [dt("va%d" % h, [128, S // 128, 130], BF16, kind="Internal").ap() for h in range(2)]
    mq_d = dt("mqT", [128, S], F32, kind="Internal").ap()
    mk_d = dt("mkT", [128, S], F32, kind="Internal").ap()
    mvo_d = dt("mvo", [S, 512], F32, kind="Internal").ap()
    if os.environ.get("DUMMY"):
        dummy_d = dt("dummyx", [int(os.environ["DUMMY"]), 512], F32, kind="Internal").ap()

    with ExitStack() as es0:
        def sb(name, shape, dtype, es=es0):
            return es.enter_context(nc.sbuf_tensor(name, shape, dtype))
        ident_bf = sb("ident_bf_s", [128, 128], BF16)
        ident_f = sb("ident_f_s", [128, 128], F32)
        triu = sb("triu_s", [128, 128], F32)
        ones_f = sb("ones_s", [128, 128], F32)
        esel = sb("esel_s", [128, 128], F32)
        amask = sb("amask_s", [128, 4, 512], BF16)
        mmask = sb("mmask_s", [128, 128], F32)
        gb = sb("gb_s", [128, 4], F32)
        ngb = sb("ngb_s", [128, 4], F32)
        cvp = sb("cvp_s", [128, 10], F32)
        hnw_s = sb("hnw_s", [128, 512], F32)
        n1w_s = sb("n1w_s", [128, KC], F32)
        gates = sb("gates_s", [128, NT, 4], F32)
        epsc = sb("epsc", [128, 1], F32)
        onec = sb("onec", [128, 1], F32)
        psum = [es0.enter_context(nc.psum_tensor("ps%d" % i, [128, 512], F32)) for i in range(8)]
        for i, (s_, d_) in enumerate([(ident_bf, ident_bf_d), (ident_f, ident_f_d), (triu, triu_d),
                                      (ones_f, ones_d), (esel, esel_d), (amask, amask_d), (mmask, mmask_d),
                                      (gb, gbias), (cvp, convp), (hnw_s, hnw), (n1w_s, n1w)]):
            tk.dma("sp", s_[:], d_, writes=["c%d" % i], key="const")
        tk.barrier()
        tk.op("dve", lambda e: e.memset(epsc[:], EPS), writes=["epsc"])
        tk.op("dve", lambda e: e.memset(onec[:], 1.0), writes=["onec"])
        tk.op("dve", lambda e: e.tensor_scalar(out=ngb[:], in0=gb[:], scalar1=-1.0, scalar2=None, op0=ALU.mult),
              reads=["c7"], writes=["ngb"])

        with ExitStack() as es1:
            W = sb("W", [128, KC, NCOL], BF16, es1)
            wst = [sb("wst%d" % i, [128, NCOL], F32, es1) for i in range(2)]
            xt = [sb("xt%d" % i, [128, D], F32, es1) for i in range(2)]
            xn = [sb("xn%d" % i, [128, D], BF16, es1) for i in range(2)]
            junk = sb("junk", [128, D], BF16, es1)
            hT = [sb("hT%d" % i, [128, KC, 512], BF16, es1) for i in range(2)]
            ss = sb("ss", [128, 4], F32, es1)
            fst = [sb("fst%d" % i, [128, 512], BF16, es1) for i in range(4)]
            mst = [sb("mst%d" % i, [128, 512], F32, es1) for i in range(4)]
            vst = [sb("vst%d" % i, [128, 2, 130], BF16, es1) for i in range(2)]
            for i in range(2):
                tk.op("dve", lambda e, i=i: e.memset(vst[i][:, :, 128:129], 1.0), writes=["vst%d" % i])
                tk.op("dve", lambda e, i=i: e.memset(vst[i][:, :, 129:130], 0.0), writes=["vst%d" % i])
            for kc in range(KC):
                s = kc % 2
                tk.dma("sp", wst[s][:], wc[kc * 128:(kc + 1) * 128, :], writes=["wst%d" % s])
                tk.op("dve", lambda e, kc=kc, s=s: e.tensor_scalar(
                    out=W[:, kc, :], in0=wst[s][:], scalar1=n1w_s[:, kc:kc + 1], scalar2=None, op0=ALU.mult),
                    reads=["wst%d" % s, "c10"], writes=["W"])
            pb = [2]

            def nbank():
                b = pb[0]
                pb[0] = 2 + (pb[0] - 2 + 1) % 6
                return b
            fcnt = [0]
            mcnt = [0]
            def p_stage1(jb):
                hs = jb % 2
                for tt in range(4):
                    ti = jb * 4 + tt
                    s = ti % 2
                    tk.dma("sp", xt[s][:], x[ti * 128:(ti + 1) * 128, :], writes=["xt%d" % s])
                    tk.op("act", lambda e, s=s: e.activation(out=junk[:], in_=xt[s][:], func=AF.Square,
                                                             accum_out=ss[:, 0:1]),
                          reads=["xt%d" % s], writes=["junk", "ss0"])
                    tk.op("act", lambda e: e.activation(out=ss[:, 1:2], in_=ss[:, 0:1], func=AF.Ln,
                                                        scale=1.0 / D, bias=epsc[:]),
                          reads=["ss0", "epsc"], writes=["ss1"])
                    tk.op("act", lambda e: e.activation(out=ss[:, 2:3], in_=ss[:, 1:2], func=AF.Exp, scale=-0.5),
                          reads=["ss1"], writes=["ss2"])
                    tk.op("dve", lambda e, s=s: e.tensor_scalar(out=xn[s][:], in0=xt[s][:], scalar1=ss[:, 2:3],
                                                                scalar2=None, op0=ALU.mult),
                          reads=["xt%d" % s, "ss2"], writes=["xn%d" % s])
                    for half in range(2):
                        pst = psum[half][:, :].bitcast(BF16)
                        for k8 in range(8):
                            kc = half * 8 + k8
                            tk.op("pe", lambda e, kc=kc, k8=k8, pst=pst, s=s: e.transpose(
                                out=pst[:, k8 * 128:(k8 + 1) * 128], in_=xn[s][:, kc * 128:(kc + 1) * 128],
                                identity=ident_bf[:]),
                                reads=["xn%d" % s, "c0"], writes=["psb%d" % half], inc=(k8 == 7))
                        eng = "act" if half == 0 else "dve"
                        src = pst.rearrange("p (k t) -> p k t", k=8)
                        dst = hT[hs][:, half * 8:(half + 1) * 8, tt * 128:(tt + 1) * 128]
                        if eng == "act":
                            tk.op("act", lambda e, src=src, dst=dst: e.copy(out=dst, in_=src),
                                  reads=["psb%d" % half], writes=["hT%d" % hs])
                        else:
                            tk.op("dve", lambda e, src=src, dst=dst: e.tensor_copy(out=dst, in_=src),
                                  reads=["psb%d" % half], writes=["hT%d" % hs])

            def p_stage2(jb):
                hs = jb % 2
                for (c0, kind, dst_d) in [(C_FQ0, "f", qt_d[0]), (C_FQ1, "f", qt_d[1]), (C_FK0, "f", kt_d[0]),
                                          (C_FK1, "f", kt_d[1]), (C_MQ, "m", mq_d), (C_MK, "m", mk_d)]:
                    b = nbank()
                    for kc in range(KC):
                        tk.op("pe", lambda e, kc=kc, b=b, c0=c0: e.matmul(
                            psum[b][:, :], lhsT=W[:, kc, c0:c0 + 128], rhs=hT[hs][:, kc, :],
                            start=(kc == 0), stop=(kc == KC - 1)),
                            reads=["W", "hT%d" % hs], writes=["psb%d" % b], inc=(kc == KC - 1))
                    if kind == "f":
                        f = fcnt[0] % 4
                        fcnt[0] += 1
                        tk.op("act", lambda e, b=b, f=f: e.copy(out=fst[f][:], in_=psum[b][:, :]),
                              reads=["psb%d" % b], writes=["fst%d" % f])
                        if 'f' not in SK:
                            tk.dma("pool", dst_d[:, jb * 512:(jb + 1) * 512], fst[f][:], reads=["fst%d" % f],
                                   key="fst%d" % f)
                    else:
                        f = mcnt[0] % 4
                        mcnt[0] += 1
                        tk.op("dve", lambda e, b=b, f=f: e.tensor_copy(out=mst[f][:], in_=psum[b][:, :]),
                              reads=["psb%d" % b], writes=["mst%d" % f])
                        if 'm' not in SK:
                            tk.dma("pool", dst_d[:, jb * 512:(jb + 1) * 512], mst[f][:], reads=["mst%d" % f],
                                   key="mst%d" % f)
                for tt in range(4):
                    ti = jb * 4 + tt
                    tsl = slice(tt * 128, (tt + 1) * 128)
                    b = nbank()
                    for kc in range(KC):
                        tk.op("pe", lambda e, kc=kc, b=b, tsl=tsl: e.matmul(
                            psum[b][:, 0:256], lhsT=hT[hs][:, kc, tsl], rhs=W[:, kc, C_FV:C_FV + 256],
                            start=(kc == 0), stop=(kc == KC - 1)),
                            reads=["W", "hT%d" % hs], writes=["psb%d" % b], inc=(kc == KC - 1))
                    vs = ti % 2
                    tk.op("act", lambda e, b=b, vs=vs: e.copy(
                        out=vst[vs][:, :, 0:128], in_=psum[b][:, 0:256].rearrange("p (h d) -> p h d", h=2)),
                        reads=["psb%d" % b], writes=["vst%d" % vs])
                    for h in (range(2) if 'v' not in SK else []):
                        tk.dma("pool", va_d[h][:, ti, :], vst[vs][:, h, :],
                               reads=["vst%d" % vs], key="vst%d_%d" % (vs, h))
                    b = nbank()
                    for kc in range(KC):
                        tk.op("pe", lambda e, kc=kc, b=b, tsl=tsl: e.matmul(
                            psum[b][:, :], lhsT=hT[hs][:, kc, tsl], rhs=W[:, kc, C_MVO:C_MVO + 512],
                            start=(kc == 0), stop=(kc == KC - 1)),
                            reads=["W", "hT%d" % hs], writes=["psb%d" % b], inc=(kc == KC - 1))
                    f = mcnt[0] % 4
                    mcnt[0] += 1
                    tk.op("dve", lambda e, b=b, f=f: e.tensor_copy(out=mst[f][:], in_=psum[b][:, :]),
                          reads=["psb%d" % b], writes=["mst%d" % f])
                    if 'o' not in SK:
                        tk.dma("pool", mvo_d[ti * 128:(ti + 1) * 128, :], mst[f][:], reads=["mst%d" % f],
                               key="mst%d" % f)
                    b = nbank()
                    for kc in range(KC):
                        tk.op("pe", lambda e, kc=kc, b=b, tsl=tsl: e.matmul(
                            psum[b][:, 0:4], lhsT=hT[hs][:, kc, tsl], rhs=W[:, kc, C_G:C_G + 4],
                            start=(kc == 0), stop=(kc == KC - 1)),
                            reads=["W", "hT%d" % hs], writes=["psb%d" % b], inc=(kc == KC - 1))
                    tk.op("act", lambda e, b=b, ti=ti: e.copy(out=gates[:, ti, :], in_=psum[b][:, 0:4]),
                          reads=["psb%d" % b], writes=["gates"])

            NBP = min(NB, int(os.environ.get('NBLIM', '999')))
            p_stage1(0)
            for jb in range(NBP):
                if jb + 1 < NBP:
                    p_stage1(jb + 1)
                p_stage2(jb)
            tk.barrier()

        NQ = NB
        SC = 128 ** -0.5
        with ExitStack() as es2:
            KT = sb("KT", [128, S], BF16, es2)
            VA = sb("VA", [128, NT, 130], BF16, es2)
            qt = [sb("qtb%d" % i, [128, 512], BF16, es2) for i in range(2)]
            PT = [sb("PT%d" % i, [128, 512], BF16, es2) for i in range(4)]
            lfn = sb("lfn", [128, NT], F32, es2)
            tmpg = sb("tmpg", [128, NT], F32, es2)
            tot = sb("tot", [128, NT], F32, es2)
            offi = sb("offi", [128, NT], F32, es2)
            onesn = sb("onesn", [128, NT], F32, es2)
            G = sb("G", [128, NT], F32, es2)
            cb = sb("cb", [128, NQ], F32, es2)
            biasj = [sb("biasj%d" % i, [128, NT], F32, es2) for i in range(2)]
            sm = sb("sm", [128, 8], F32, es2)
            ot = [sb("ot%d" % i, [128, 128], F32, es2) for i in range(2)]
            oj = sb("oj", [128, 128], F32, es2)
            ob = [sb("ob%d" % i, [128, 128], BF16, es2) for i in range(2)]
            tk.op("dve", lambda e: e.memset(onesn[:], 1.0), writes=["onesn"])
            ptc = [0]
            oc = [0]
            for hh in (range(2) if 'A' in PH else []):
                for c0 in range(0, S, 2048):
                    tk.dma("sp", KT[:, c0:min(S, c0 + 2048)], kt_d[hh][:, c0:min(S, c0 + 2048)], writes=["KT"])
                for c0 in range(0, NT, 16):
                    tk.dma("sp", VA[:, c0:min(NT, c0 + 16), :], va_d[hh][:, c0:min(NT, c0 + 16), :], writes=["VA"])
                tk.op("act", lambda e, hh=hh: e.activation(out=tmpg[:], in_=gates[:, :, hh], func=AF.Exp,
                                                           scale=-1.0, bias=ngb[:, hh:hh + 1]),
                      reads=["gates", "ngb"], writes=["tmpg"])
                tk.op("act", lambda e: e.activation(out=lfn[:], in_=tmpg[:], func=AF.Ln, scale=1.0, bias=onec[:]),
                      reads=["tmpg", "onec"], writes=["lfn"])
                for c0 in range(0, NT, 32):
                    c1 = min(NT, c0 + 32)
                    tk.op("pe", lambda e, c0=c0, c1=c1: e.matmul(psum[0][:, c0:c1], lhsT=triu[:], rhs=lfn[:, c0:c1],
                                                                 start=True, stop=True),
                          reads=["lfn", "c2"], writes=["psb0"])
                    tk.op("pe", lambda e, c0=c0, c1=c1: e.matmul(psum[1][:, c0:c1], lhsT=ones_f[:], rhs=lfn[:, c0:c1],
                                                                 start=True, stop=True),
                          reads=["lfn", "c3"], writes=["psb1"])
                tk.op("act", lambda e: e.copy(out=tot[:], in_=psum[1][:, 0:NT]), reads=["psb1"], writes=["tot"])
                tk.op("act", lambda e: e.copy(out=G[:], in_=psum[0][:, 0:NT]), reads=["psb0"], writes=["G"])
                tk.op("dve", lambda e: e.tensor_tensor_scan(out=offi[:], data0=onesn[:], data1=tot[:], initial=0.0,
                                                            op0=ALU.mult, op1=ALU.add),
                      reads=["tot", "onesn"], writes=["offi"])
                tk.op("dve", lambda e: e.tensor_tensor(out=offi[:], in0=offi[:], in1=tot[:], op=ALU.subtract),
                      reads=["offi", "tot"], writes=["offi"])
                tk.op("dve", lambda e: e.tensor_tensor(out=G[:], in0=G[:], in1=offi[:], op=ALU.add),
                      reads=["G", "offi"], writes=["G"])
                Gsel = G[:].rearrange("p (j r) -> p j r", r=4)[:, :, 2]
                tk.op("dve", lambda e, Gsel=Gsel: e.tensor_copy(out=tmpg[:, 0:NQ], in_=Gsel), reads=["G"],
                      writes=["tmpg"])
                tk.op("pe", lambda e: e.matmul(psum[1][:, 0:NQ], lhsT=esel[:], rhs=tmpg[:, 0:NQ], start=True, stop=True),
                      reads=["tmpg", "c4"], writes=["psb1"])
                tk.op("act", lambda e: e.copy(out=cb[:], in_=psum[1][:, 0:NQ]), reads=["psb1"], writes=["cb"])
                blocks = [(j, i) for j in range(NQ) for i in range(4 * j + 4)]
                NBK = len(blocks)
                LOOK = 2

                def load_q(j):
                    tk.dma("sp", qt[j % 2][:], qt_d[hh][:, j * 512:(j + 1) * 512], writes=["qt%d" % (j % 2)])

                def make_bias(j):
                    nk = 4 * j + 4
                    tk.op("dve", lambda e, j=j, nk=nk: e.tensor_scalar(
                        out=biasj[j % 2][:, 0:nk], in0=G[:, 0:nk], scalar1=cb[:, j:j + 1], scalar2=None,
                        op0=ALU.subtract), reads=["G", "cb"], writes=["bj%d" % (j % 2)])

                def st_qk(n):
                    j, i = blocks[n]
                    if i == 0 and j + 1 < NQ:
                        load_q(j + 1)
                    sb_ = n % 4
                    qs = j % 2
                    tk.op("pe", lambda e, i=i, qs=qs, sb_=sb_: e.matmul(
                        psum[sb_][:, :], lhsT=KT[:, i * 128:(i + 1) * 128], rhs=qt[qs][:], start=True, stop=True),
                        reads=["KT", "qt%d" % qs], writes=["psb%d" % sb_])

                def st_ex(n):
                    j, i = blocks[n]
                    if i == 0 and j + 1 < NQ:
                        make_bias(j + 1)
                    r = i - 4 * j
                    sb_ = n % 4
                    p = n % 4
                    bj = j % 2
                    tk.op("act", lambda e, p=p, sb_=sb_, bj=bj, i=i: e.activation(
                        out=PT[p][:], in_=psum[sb_][:, :], func=AF.Exp, scale=SC, bias=biasj[bj][:, i:i + 1]),
                        reads=["psb%d" % sb_, "bj%d" % bj], writes=["PT%d" % p])
                    if r >= 0:
                        tk.op("dve", lambda e, p=p, r=r: e.tensor_tensor(
                            out=PT[p][:], in0=PT[p][:], in1=amask[:, r, :], op=ALU.mult),
                            reads=["PT%d" % p, "c5"], writes=["PT%d" % p])

                def st_pv(n):
                    j, i = blocks[n]
                    r = i - 4 * j
                    p = n % 4
                    for u in range(4):
                        if r > u:
                            continue
                        last = 4 * j + u
                        tk.op("pe", lambda e, p=p, u=u, i=i, last=last: e.matmul(
                            psum[4 + u][:, 0:130], lhsT=PT[p][:, u * 128:(u + 1) * 128], rhs=VA[:, i, :],
                            start=(i == 0), stop=(i == last)),
                            reads=["PT%d" % p, "VA"], writes=["psb%d" % (4 + u)], inc=(u == 3))
                    if i == 4 * j + 3:
                        epilogue(j)

                def epilogue(j):
                    for u in range(4):
                        o = oc[0] % 2
                        oc[0] += 1
                        pu = psum[4 + u]
                        tk.op("dve", lambda e, pu=pu: e.reciprocal(out=sm[:, 0:1], in_=pu[:, 128:129]),
                              reads=["psb%d" % (4 + u)], writes=["sm0"])
                        tk.op("dve", lambda e, pu=pu, o=o: e.tensor_scalar(
                            out=ot[o][:], in0=pu[:, 0:128], scalar1=sm[:, 0:1], scalar2=None, op0=ALU.mult),
                            reads=["psb%d" % (4 + u), "sm0"], writes=["ot%d" % o])
                        tk.op("act", lambda e, o=o: e.activation(out=oj[:], in_=ot[o][:], func=AF.Square,
                                                                 accum_out=sm[:, 1:2]),
                              reads=["ot%d" % o], writes=["oj", "sm1"])
                        tk.op("act", lambda e: e.activation(out=sm[:, 2:3], in_=sm[:, 1:2], func=AF.Ln,
                                                            scale=1.0 / 128, bias=epsc[:]),
                              reads=["sm1", "epsc"], writes=["sm2"])
                        tk.op("act", lambda e: e.activation(out=sm[:, 3:4], in_=sm[:, 2:3], func=AF.Exp, scale=-0.5),
                              reads=["sm2"], writes=["sm3"])
                        tk.op("dve", lambda e, o=o, hh=hh: e.scalar_tensor_tensor(
                            out=ob[o][:], in0=ot[o][:], scalar=sm[:, 3:4], in1=hnw_s[:, hh * 128:(hh + 1) * 128],
                            op0=ALU.mult, op1=ALU.mult),
                            reads=["ot%d" % o, "sm3", "c9"], writes=["ob%d" % o])
                        t0 = j * 512 + u * 128
                        tk.dma("pool", mix_dst(t0, hh * 128, (hh + 1) * 128), ob[o][:], reads=["ob%d" % o],
                               key="ob%d" % o)

                load_q(0)
                make_bias(0)
                for n in range(NBK + LOOK):
                    if n < NBK:
                        st_qk(n)
                    if n - LOOK >= 0:
                        st_ex(n - LOOK)
                        st_pv(n - LOOK)
                tk.barrier()

        NCH = NT
        with ExitStack() as es3:
            zq = [sb("zq%d" % i, [128, 515], F32, es3) for i in range(2)]
            zk = [sb("zk%d" % i, [128, 515], F32, es3) for i in range(2)]
            acc = sb("acc", [128, 512], F32, es3)
            ex = sb("ex", [128, 512], F32, es3)
            qT = [sb("qT%d" % i, [128, 512], BF16, es3) for i in range(2)]
            kT = [sb("kT%d" % i, [128, 512], BF16, es3) for i in range(2)]
            mvo = [sb("mvo%d" % i, [128, 512], F32, es3) for i in range(2)]
            vt = [sb("vt%d" % i, [128, 258], BF16, es3) for i in range(2)]
            ktok = [sb("ktok%d" % i, [128, 128], BF16, es3) for i in range(2)]
            smt = [sb("smt%d" % i, [128, 128], BF16, es3) for i in range(2)]
            Cf = sb("Cf", [128, 258], F32, es3)
            Cb = [sb("Cb%d" % i, [128, 258], BF16, es3) for i in range(2)]
            lf = sb("mlf", [128, NCH], F32, es3)
            tmpm = sb("tmpm", [128, NCH], F32, es3)
            bcs = sb("bcs", [128, NCH], F32, es3)
            eb = sb("eb", [128, NCH], F32, es3)
            ek = sb("ek", [128, NCH], F32, es3)
            ebl = sb("ebl", [128, NCH], F32, es3)
            hsm = sb("hsm", [128, 8], F32, es3)
            hv = [sb("hv%d" % i, [128, 256], F32, es3) for i in range(2)]
            sg = sb("sg", [128, 256], F32, es3)
            hj = sb("hj", [128, 256], F32, es3)
            hb = [sb("hb%d" % i, [128, 256], BF16, es3) for i in range(2)]
            if 'M' in PH or os.environ.get('MLPRE'):
                _lim = int(os.environ.get('MLPRE', '99'))
                _real_op = tk.op
                _cnt = [0]
                def _lop(*a, **k):
                    _cnt[0] += 1
                    if _cnt[0] <= _lim:
                        return _real_op(*a, **k)
                tk.op = _lop
                tk.op("act", lambda e: e.activation(out=tmpm[:], in_=gates[:, :, 3], func=AF.Exp, scale=-1.0,
                                                    bias=ngb[:, 3:4]), reads=["gates", "ngb"], writes=["tmpm"])
                tk.op("act", lambda e: e.activation(out=lf[:], in_=tmpm[:], func=AF.Ln, scale=1.0, bias=onec[:]),
                      reads=["tmpm", "onec"], writes=["mlf"])
                tk.op("dve", lambda e: e.tensor_scalar(out=lf[:], in0=lf[:], scalar1=-1.0, scalar2=None, op0=ALU.mult),
                      reads=["mlf"], writes=["mlf"])
                for c0 in range(0, NCH, 32):
                    c1 = min(NCH, c0 + 32)
                    tk.op("pe", lambda e, c0=c0, c1=c1: e.matmul(psum[0][:, c0:c1], lhsT=triu[:], rhs=lf[:, c0:c1],
                                                                 start=True, stop=True),
                          reads=["mlf", "c2"], writes=["psb0"])
                    tk.op("pe", lambda e, c0=c0, c1=c1: e.matmul(psum[1][:, c0:c1], lhsT=ones_f[:], rhs=lf[:, c0:c1],
                                                                 start=True, stop=True),
                          reads=["mlf", "c3"], writes=["psb1"])
                tk.op("act", lambda e: e.activation(out=eb[:], in_=psum[0][:, 0:NCH], func=AF.Exp),
                      reads=["psb0"], writes=["eb"])
                tk.op("act", lambda e: e.activation(out=ebl[:], in_=psum[1][:, 0:NCH], func=AF.Exp),
                      reads=["psb1"], writes=["ebl"])
                tk.op("act", lambda e: e.copy(out=tmpm[:], in_=psum[0][:, 0:NCH]), reads=["psb0"], writes=["tmpm"])
                tk.op("act", lambda e: e.copy(out=bcs[:], in_=gates[:, :, 2]), reads=["gates"], writes=["bcs"])
                tk.op("dve", lambda e: e.tensor_tensor(out=bcs[:], in0=bcs[:], in1=tmpm[:], op=ALU.subtract),
                      reads=["bcs", "tmpm"], writes=["bcs"])
                tk.op("act", lambda e: e.activation(out=ek[:], in_=bcs[:], func=AF.Exp, bias=gb[:, 2:3], scale=1.0),
                      reads=["bcs", "c7"], writes=["ek"])
            if 'M' in PH or os.environ.get('MLPRE'):
                tk.op = _real_op
            tk.op("dve", lambda e: e.memset(Cf[:], 0.0), writes=["Cf"])
            tk.op("dve", lambda e: e.memset(Cb[0][:], 0.0), writes=["Cb0"])
            for i in range(2):
                tk.op("dve", lambda e, i=i: e.memset(zq[i][:, 0:3], 0.0), writes=["zq%d" % i])
                tk.op("dve", lambda e, i=i: e.memset(zk[i][:, 0:3], 0.0), writes=["zk%d" % i])
                tk.op("dve", lambda e, i=i: e.memset(vt[i][:], 0.0), writes=["vt%d" % i])
            for jb in (range(NB) if 'M' in PH else []):
                s = jb % 2
                for (z, zn, zd, wo, bo, dstT, dn) in [(zq, "zq", mq_d, 0, 8, qT, "qT"), (zk, "zk", mk_d, 4, 9, kT, "kT")]:
                    tk.dma("sp", z[s][:, 3:515], zd[:, jb * 512:(jb + 1) * 512], writes=["%s%d" % (zn, s)])
                    if jb > 0:
                        tk.op("act", lambda e, z=z, s=s: e.copy(out=z[s][:, 0:3], in_=z[1 - s][:, 512:515]),
                              reads=["%s%d" % (zn, 1 - s)], writes=["%s%d" % (zn, s)])
                    tk.op("dve", lambda e, z=z, s=s, wo=wo, bo=bo: e.tensor_scalar(
                        out=acc[:], in0=z[s][:, 0:512], scalar1=cvp[:, wo:wo + 1], scalar2=cvp[:, bo:bo + 1],
                        op0=ALU.mult, op1=ALU.add), reads=["%s%d" % (zn, s), "c8"], writes=["acc"])
                    for jj in range(1, 4):
                        tk.op("dve", lambda e, z=z, s=s, wo=wo, jj=jj: e.scalar_tensor_tensor(
                            out=acc[:], in0=z[s][:, jj:jj + 512], scalar=cvp[:, wo + jj:wo + jj + 1], in1=acc[:],
                            op0=ALU.mult, op1=ALU.add), reads=["%s%d" % (zn, s), "c8", "acc"], writes=["acc"])
                    tk.op("act", lambda e: e.activation(out=ex[:], in_=acc[:], func=AF.Exp, scale=-1.0),
                          reads=["acc"], writes=["ex"])
                    tk.op("dve", lambda e: e.tensor_scalar(out=ex[:], in0=ex[:], scalar1=1.0, scalar2=None, op0=ALU.add),
                          reads=["ex"], writes=["ex"])
                    tk.op("dve", lambda e: e.reciprocal(out=ex[:], in_=ex[:]), reads=["ex"], writes=["ex"])
                    tk.op("dve", lambda e, dstT=dstT, s=s: e.tensor_tensor(out=dstT[s][:], in0=acc[:], in1=ex[:],
                                                                           op=ALU.mult),
                          reads=["acc", "ex"], writes=["%s%d" % (dn, s)])
                for cc in range(4):
                    c = jb * 4 + cc
                    cs = c % 2
                    csl = slice(cc * 128, (cc + 1) * 128)
                    tk.dma("sp", mvo[cs][:], mvo_d[c * 128:(c + 1) * 128, :], writes=["mvo%d" % cs])
                    pkt = psum[2][:, :].bitcast(BF16)
                    tk.op("pe", lambda e, s=s, csl=csl, pkt=pkt: e.transpose(out=pkt[:, 0:128], in_=kT[s][:, csl],
                                                                             identity=ident_bf[:]),
                          reads=["kT%d" % s, "c0"], writes=["psb2"])
                    tk.op("act", lambda e, cs=cs, pkt=pkt: e.activation(out=ktok[cs][:], in_=pkt[:, 0:128],
                                                                        func=AF.Copy, scale=SC),
                          reads=["psb2"], writes=["ktok%d" % cs])
                    tk.op("dve", lambda e, cs=cs, c=c: e.tensor_scalar(
                        out=vt[cs][:, 0:256], in0=mvo[cs][:, 0:256], scalar1=ek[:, c:c + 1], scalar2=None, op0=ALU.mult),
                        reads=["mvo%d" % cs, "ek"], writes=["vt%d" % cs])
                    tk.op("dve", lambda e, cs=cs, c=c: e.tensor_copy(out=vt[cs][:, 256:257], in_=ek[:, c:c + 1]),
                          reads=["ek"], writes=["vt%d" % cs])
                    tk.op("pe", lambda e, s=s, csl=csl: e.matmul(psum[3][:, 0:128], lhsT=kT[s][:, csl], rhs=qT[s][:, csl],
                                                                 start=True, stop=True),
                          reads=["kT%d" % s, "qT%d" % s], writes=["psb3"])
                    tk.op("dve", lambda e, cs=cs: e.tensor_tensor(out=smt[cs][:], in0=psum[3][:, 0:128], in1=mmask[:],
                                                                  op=ALU.mult),
                          reads=["psb3", "c6"], writes=["smt%d" % cs])
                    tk.op("pe", lambda e, cs=cs: e.matmul(psum[4][:, 0:258], lhsT=smt[cs][:], rhs=vt[cs][:],
                                                          start=True, stop=False),
                          reads=["smt%d" % cs, "vt%d" % cs], writes=["psb4"])
                    tk.op("pe", lambda e, s=s, csl=csl, cs=cs: e.matmul(psum[4][:, 0:258], lhsT=qT[s][:, csl],
                                                                        rhs=Cb[cs][:], start=False, stop=True),
                          reads=["qT%d" % s, "Cb%d" % cs], writes=["psb4"])
                    tk.op("pe", lambda e, cs=cs: e.matmul(psum[5][:, 0:258], lhsT=ktok[cs][:], rhs=vt[cs][:],
                                                          start=True, stop=True),
                          reads=["ktok%d" % cs, "vt%d" % cs], writes=["psb5"])
                    tk.op("dve", lambda e: e.tensor_tensor(out=Cf[:], in0=psum[5][:, 0:258], in1=Cf[:], op=ALU.add),
                          reads=["psb5", "Cf"], writes=["Cf"])
                    tk.op("dve", lambda e, c=c: e.tensor_scalar(out=Cf[:], in0=Cf[:], scalar1=ebl[:, c:c + 1],
                                                                scalar2=None, op0=ALU.mult),
                          reads=["Cf", "ebl"], writes=["Cf"])
                    tk.op("act", lambda e, cs=cs: e.copy(out=Cb[1 - cs][:], in_=Cf[:]), reads=["Cf"],
                          writes=["Cb%d" % (1 - cs)])
                    tk.op("act", lambda e, c=c: e.activation(out=hsm[:, 6:7], in_=psum[4][:, 256:257], func=AF.Abs,
                                                             scale=eb[:, c:c + 1]),
                          reads=["psb4", "eb"], writes=["hsm6"])
                    tk.op("dve", lambda e: e.tensor_scalar(out=hsm[:, 0:1], in0=hsm[:, 6:7], scalar1=1.0, scalar2=None,
                                                           op0=ALU.max),
                          reads=["hsm6"], writes=["hsm0"])
                    tk.op("dve", lambda e: e.reciprocal(out=hsm[:, 1:2], in_=hsm[:, 0:1]), reads=["hsm0"], writes=["hsm1"])
                    tk.op("dve", lambda e, c=c: e.tensor_tensor(out=hsm[:, 2:3], in0=hsm[:, 1:2], in1=eb[:, c:c + 1],
                                                                op=ALU.mult), reads=["hsm1", "eb"], writes=["hsm2"])
                    tk.op("act", lambda e, cs=cs: e.activation(out=sg[:], in_=mvo[cs][:, 256:512], func=AF.Exp, scale=-1.0),
                          reads=["mvo%d" % cs], writes=["sg"])
                    tk.op("dve", lambda e: e.tensor_scalar(out=sg[:], in0=sg[:], scalar1=1.0, scalar2=None, op0=ALU.add),
                          reads=["sg"], writes=["sg"])
                    tk.op("dve", lambda e: e.reciprocal(out=sg[:], in_=sg[:]), reads=["sg"], writes=["sg"])
                    tk.op("dve", lambda e, cs=cs: e.scalar_tensor_tensor(
                        out=hv[cs][:], in0=psum[4][:, 0:256], scalar=hsm[:, 2:3], in1=sg[:], op0=ALU.mult, op1=ALU.mult),
                        reads=["psb4", "hsm2", "sg"], writes=["hv%d" % cs])
                    tk.op("act", lambda e, cs=cs: e.activation(out=hj[:], in_=hv[cs][:], func=AF.Square,
                                                               accum_out=hsm[:, 3:4]),
                          reads=["hv%d" % cs], writes=["hj", "hsm3"])
                    tk.op("act", lambda e: e.activation(out=hsm[:, 4:5], in_=hsm[:, 3:4], func=AF.Ln, scale=1.0 / 256,
                                                        bias=epsc[:]), reads=["hsm3", "epsc"], writes=["hsm4"])
                    tk.op("act", lambda e: e.activation(out=hsm[:, 5:6], in_=hsm[:, 4:5], func=AF.Exp, scale=-0.5),
                          reads=["hsm4"], writes=["hsm5"])
                    tk.op("dve", lambda e, cs=cs: e.scalar_tensor_tensor(
                        out=hb[cs][:], in0=hv[cs][:], scalar=hsm[:, 5:6], in1=hnw_s[:, 256:512], op0=ALU.mult,
                        op1=ALU.mult), reads=["hv%d" % cs, "hsm5", "c9"], writes=["hb%d" % cs])
                    tk.dma("pool", mix_dst(c * 128, 256, 512), hb[cs][:], reads=["hb%d" % cs],
                           key="hb%d" % cs)
            tk.barrier()
        if own_tk:
            tk.final_wait("sp")
    print("phase A instructions:", tk.ninst)
    return nc


def host_inputs_a(inp, S):
    c = host_consts()
    w_in = np.asarray(inp["w_in"][0])
    maps = []
    for core in range(8):
        b, g = core // 4, core % 4
        h0, h1 = 2 * g, 2 * g + 1
        cols = []
        for base in (0, 1024, 2048):
            cols += list(range(base + h0 * 128, base + h0 * 128 + 128)) + list(range(base + h1 * 128, base + h1 * 128 + 128))
        cols += list(range(3080 + g * 128, 3080 + g * 128 + 128))
        cols += list(range(3592 + g * 128, 3592 + g * 128 + 128))
        cols += list(range(4104 + g * 256, 4104 + g * 256 + 256))
        cols += list(range(5136 + g * 256, 5136 + g * 256 + 256))
        cols += [3072 + h0, 3072 + h1, 5128 + g, 5132 + g]
        wcs = np.ascontiguousarray(w_in[:, cols])
        gbv = np.array([inp["fox_f_bias"][0][h0], inp["fox_f_bias"][0][h1], inp["mlstm_i_bias"][0][g],
                        inp["mlstm_f_bias"][0][g]], np.float32)
        cw = np.asarray(inp["mlstm_conv_w"][0])
        cbv = np.asarray(inp["mlstm_conv_b"][0])
        convp = np.concatenate([cw[:, g * 128:(g + 1) * 128].T, cw[:, 512 + g * 128:512 + (g + 1) * 128].T,
                                cbv[g * 128:(g + 1) * 128][:, None], cbv[512 + g * 128:512 + (g + 1) * 128][:, None]],
                               axis=1).astype(np.float32)
        hn = np.concatenate([np.asarray(inp["fox_out_norm_w"][0])[h0 * 128:(h1 + 1) * 128],
                             np.asarray(inp["mlstm_out_norm_w"][0])[g * 256:(g + 1) * 256]])
        m = {"x": np.ascontiguousarray(np.asarray(inp["x"])[b, :int(os.environ.get("XROWS", S))]),
             "wc": wcs,
             "n1w": np.ascontiguousarray(np.asarray(inp["norm1_w"][0]).reshape(KC, 128).T),
             "gbias": np.ascontiguousarray(np.broadcast_to(gbv[None, :], (128, 4))),
             "convp": np.ascontiguousarray(convp),
             "hnw": np.ascontiguousarray(np.broadcast_to(hn[None, :], (128, 512))).astype(np.float32)}
        m.update(c)
        maps.append(m)
    return maps


import os


def host_consts_b():
    c = {}
    c["ident_bf"] = np.eye(128, dtype=np.float32).astype(ml_dtypes.bfloat16)
    c["ident_f"] = np.eye(128, dtype=np.float32)
    c["iota_row"] = np.ascontiguousarray(np.broadcast_to(np.arange(128, dtype=np.float32)[None, :], (128, 128)))
    c["thr16"] = np.ascontiguousarray(np.broadcast_to((16.0 * np.arange(16, dtype=np.float32))[None, :], (128, 16)))
    c["iota16"] = np.ascontiguousarray(np.broadcast_to(np.arange(16, dtype=np.float32)[None, :], (128, 16)))
    return c


def build_phase_b(nc, TC, NJ=128, tk=None, gath=None, pfx="", pre=None):
    NTT = TC // 128
    PB = min(256, TC)
    NBLK = TC // PB
    TPB = PB // 128
    own_tk = tk is None
    if own_tk:
        tk = Trk(nc)
    _dt = nc.dram_tensor

    def dt(name, *a, **k):
        return _dt(pfx + name, *a, **k)
    x = dt("x", [TC, D], F32, kind="ExternalInput").ap()
    if gath is None:
        mixed = dt("mixed", [TC, D], BF16, kind="ExternalInput").ap()
    else:
        wsel_d = dt("wsel", [128, 8], F32, kind="ExternalInput").ap()
    wout = dt("wout", [D, D], F32, kind="ExternalInput").ap()
    wq = dt("wq", [D, D], F32, kind="ExternalInput").ap()
    n2w = dt("n2w", [128, D], F32, kind="ExternalInput").ap()
    fnw = dt("fnw", [128, D], F32, kind="ExternalInput").ap()
    keysT = dt("keysT", [128, 2, 128], F32, kind="ExternalInput").ap()
    if pre is None:
        uh = dt("uh", [128, 128, D], F32, kind="ExternalInput").ap()
        vh = dt("vh", [128, 128, D], F32, kind="ExternalInput").ap()
    ident_bf_d = dt("ident_bf", [128, 128], BF16, kind="ExternalInput").ap()
    ident_f_d = dt("ident_f", [128, 128], F32, kind="ExternalInput").ap()
    iota_row_d = dt("iota_row", [128, 128], F32, kind="ExternalInput").ap()
    thr16_d = dt("thr16", [128, 16], F32, kind="ExternalInput").ap()
    iota16_d = dt("iota16", [128, 16], F32, kind="ExternalInput").ap()
    out = dt("out", [TC, D], F32, kind="ExternalOutput").ap()
    if pre is None:
        u16 = dt("u16", [128, 128, D], BF16, kind="Internal").ap()
        v16 = dt("v16", [128, 128, D], BF16, kind="Internal").ap()
    else:
        u16, v16 = pre
    x1_d = dt("x1_d", [TC, D], F32, kind="Internal").ap()
    h2T_d = dt("h2T_d", [128, KC, TC], BF16, kind="Internal").ap()
    slot_d = dt("slot_d", [128, 3, TC], F32, kind="Internal").ap()

    with ExitStack() as es0:
        def sb(name, shape, dtype, es=es0):
            return es.enter_context(nc.sbuf_tensor(pfx + name, shape, dtype))
        ident_bf = sb("ident_bf_s", [128, 128], BF16)
        ident_f = sb("ident_f_s", [128, 128], F32)
        iota_row = sb("iota_row_s", [128, 128], F32)
        thr16 = sb("thr16_s", [128, 16], F32)
        iota16 = sb("iota16_s", [128, 16], F32)
        epsc = sb("epsc", [128, 1], F32)
        psum = [es0.enter_context(nc.psum_tensor(pfx + "ps%d" % i, [128, 512], F32)) for i in range(8)]
        for i, (s_, d_) in enumerate([(ident_bf, ident_bf_d), (ident_f, ident_f_d), (iota_row, iota_row_d),
                                      (thr16, thr16_d), (iota16, iota16_d)]):
            tk.dma("sp", s_[:], d_, writes=["c%d" % i], key="const")
        for j in (range(NJ) if pre is None else []):
            tk.dma("pool", u16[j], uh[j], writes=["u16"], key="cvt")
            tk.dma("pool", v16[j], vh[j], writes=["v16"], key="cvt")
        tk.op("dve", lambda e: e.memset(epsc[:], EPS), writes=["epsc"])

        def rms_rstd(src_ap, srckey, ss, junk):
            tk.op("act", lambda e: e.activation(out=junk[:], in_=src_ap, func=AF.Square, accum_out=ss[:, 0:1]),
                  reads=[srckey], writes=["junk", "ss0"])
            tk.op("act", lambda e: e.activation(out=ss[:, 1:2], in_=ss[:, 0:1], func=AF.Ln, scale=1.0 / D, bias=epsc[:]),
                  reads=["ss0", "epsc"], writes=["ss1"])
            tk.op("act", lambda e: e.activation(out=ss[:, 2:3], in_=ss[:, 1:2], func=AF.Exp, scale=-0.5),
                  reads=["ss1"], writes=["ss2"])

        def transpose16(src, srckey, dst, dstkey, banks):
            for half in range(2):
                bk = banks[half]
                pst = psum[bk][:, :].bitcast(BF16)
                for k8 in range(8):
                    kc = half * 8 + k8
                    tk.op("pe", lambda e, kc=kc, k8=k8, pst=pst: e.transpose(
                        out=pst[:, k8 * 128:(k8 + 1) * 128], in_=src[:, kc * 128:(kc + 1) * 128], identity=ident_bf[:]),
                        reads=[srckey, "c0"], writes=["psb%d" % bk], inc=(k8 == 7))
                srcv = pst.rearrange("p (k t) -> p k t", k=8)
                dstv = dst[:, half * 8:(half + 1) * 8, :]
                if half == 0:
                    tk.op("act", lambda e, srcv=srcv, dstv=dstv: e.copy(out=dstv, in_=srcv),
                          reads=["psb%d" % bk], writes=[dstkey])
                else:
                    tk.op("dve", lambda e, srcv=srcv, dstv=dstv: e.tensor_copy(out=dstv, in_=srcv),
                          reads=["psb%d" % bk], writes=[dstkey])

        with ExitStack() as es1:
            Wo = sb("Wo", [128, KC, D], BF16, es1)
            n2b = sb("n2b", [128, D], F32, es1)
            mx = [sb("mx%d" % i, [128, D], BF16, es1) for i in range(2)]
            xt = [sb("xt%d" % i, [128, D], F32, es1) for i in range(2)]
            x1 = [sb("x1_%d" % i, [128, D], F32, es1) for i in range(2)]
            mT = sb("mT", [128, KC, 128], BF16, es1)
            h2 = sb("h2", [128, D], BF16, es1)
            h2t = [sb("h2t%d" % i, [128, KC, 128], BF16, es1) for i in range(2)]
            junk = sb("junk", [128, D], BF16, es1)
            ss = sb("ss", [128, 4], F32, es1)
            for kc in range(KC):
                tk.dma("pool", Wo[:, kc, :], wout[kc * 128:(kc + 1) * 128, :], writes=["Wo"], key="wload")
            tk.dma("sp", n2b[:], n2w, writes=["n2b"])
            ccnt = [0]
            if gath is not None:
                cand = [sb("cand%d" % i, [128, D], BF16, es1) for i in range(3)]
                wsel = sb("wsel_s", [128, 8], F32, es1)
                tk.dma("sp", wsel[:], wsel_d, writes=["wsel"])
            for ti in range(NTT):
                s = ti % 2
                rows = slice(ti * 128, (ti + 1) * 128)
                if gath is None:
                    tk.dma("sp", mx[s][:], mixed[rows, :], writes=["mx%d" % s])
                else:
                    bb_, lt = ti // (NTT // 2), ti % (NTT // 2)
                    for dp in range(8):
                        cs_ = ccnt[0] % 3
                        ccnt[0] += 1
                        gt_ = dp * (NTT // 2) + lt
                        kq = gt_ // 4
                        src = gath[kq].ap().rearrange("(r t) c -> r t c", r=8)[bb_ * 4:(bb_ + 1) * 4,
                                                                              (gt_ % 4) * 128:(gt_ % 4 + 1) * 128, :]
                        tk.dma("sp", cand[cs_][:].rearrange("p (g c) -> p g c", g=4), src.rearrange("g t c -> t g c"),
                               reads=["gath%d" % kq], writes=["cand%d" % cs_])
                        if dp == 0:
                            tk.op("dve", lambda e, cs_=cs_, s=s: e.tensor_scalar(
                                out=mx[s][:], in0=cand[cs_][:], scalar1=wsel[:, 0:1], scalar2=None, op0=ALU.mult),
                                reads=["cand%d" % cs_, "wsel"], writes=["mx%d" % s])
                        else:
                            tk.op("dve", lambda e, cs_=cs_, s=s, dp=dp: e.scalar_tensor_tensor(
                                out=mx[s][:], in0=cand[cs_][:], scalar=wsel[:, dp:dp + 1], in1=mx[s][:], op0=ALU.mult,
                                op1=ALU.add), reads=["cand%d" % cs_, "wsel", "mx%d" % s], writes=["mx%d" % s])
                tk.dma("sp", xt[s][:], x[rows, :], writes=["xt%d" % s])
                transpose16(mx[s], "mx%d" % s, mT, "mT", (0, 1))
                for cg in range(4):
                    b = 2 + cg
                    for kc in range(KC):
                        tk.op("pe", lambda e, kc=kc, b=b, cg=cg: e.matmul(
                            psum[b][:, :], lhsT=mT[:, kc, :], rhs=Wo[:, kc, cg * 512:(cg + 1) * 512],
                            start=(kc == 0), stop=(kc == KC - 1)), reads=["mT", "Wo"], writes=["psb%d" % b], inc=(kc == KC - 1))
                    tk.op("dve", lambda e, b=b, cg=cg, s=s: e.tensor_tensor(
                        out=x1[s][:, cg * 512:(cg + 1) * 512], in0=psum[b][:, :], in1=xt[s][:, cg * 512:(cg + 1) * 512],
                        op=ALU.add), reads=["psb%d" % b, "xt%d" % s], writes=["x1_%d" % s])
                tk.dma("pool", x1_d[rows, :], x1[s][:], reads=["x1_%d" % s], key="x1st%d" % s)
                rms_rstd(x1[s][:], "x1_%d" % s, ss, junk)
                tk.op("dve", lambda e, s=s: e.scalar_tensor_tensor(out=h2[:], in0=x1[s][:], scalar=ss[:, 2:3], in1=n2b[:],
                                                                   op0=ALU.mult, op1=ALU.mult),
                      reads=["x1_%d" % s, "ss2", "n2b"], writes=["h2"])
                transpose16(h2, "h2", h2t[s], "h2t%d" % s, (6, 7))
                tk.dma("pool", h2T_d[:, :, rows], h2t[s][:], reads=["h2t%d" % s], key="h2st%d" % s)
            tk.barrier()

        if os.environ.get('BSTOP') == '1':
            tk.final_wait('sp')
            return nc
        with ExitStack() as es2:
            Wq = sb("Wq", [128, KC, D], BF16, es2)
            kT = sb("kTs", [128, 2, 128], BF16, es2)
            h2t = [sb("h2tb%d" % i, [128, KC, 128], BF16, es2) for i in range(2)]
            qpT = [sb("qpT%d" % i, [128, 128], BF16, es2) for i in range(2)]
            sc_l = [sb("sc_%d" % i_, [128, 16, 128], F32, es2) for i_ in range(2)]
            tmp1_l = [sb("tmp1_%d" % i_, [128, 128], F32, es2) for i_ in range(2)]
            st_l = [sb("st_%d" % i_, [128, 16, 16], F32, es2) for i_ in range(2)]
            iu_l = [sb("iu_%d" % i_, [128, 16, 16], U32, es2) for i_ in range(2)]
            itf_l = [sb("itf_%d" % i_, [128, 16, 16], F32, es2) for i_ in range(2)]
            dd_l = [sb("dd_%d" % i_, [128, 16, 16], F32, es2) for i_ in range(2)]
            cand_l = [sb("cand_%d" % i_, [128, 8, 256], F32, es2) for i_ in range(2)]
            tmp2_l = [sb("tmp2_%d" % i_, [128, 256], F32, es2) for i_ in range(2)]
            cf_l = [sb("cf_%d" % i_, [128, 8, 16], F32, es2) for i_ in range(2)]
            pu_l = [sb("pu_%d" % i_, [128, 8, 16], U32, es2) for i_ in range(2)]
            posf_l = [sb("posf_%d" % i_, [128, 8, 16], F32, es2) for i_ in range(2)]
            cs_l = [sb("cs_%d" % i_, [128, 8, 16], F32, es2) for i_ in range(2)]
            zs_l = [sb("zs_%d" % i_, [128, 8], F32, es2) for i_ in range(2)]
            ge_l = [sb("ge_%d" % i_, [128, 8, 16, 16], F32, es2) for i_ in range(2)]
            prod_l = [sb("prod_%d" % i_, [128, 8, 16, 16], F32, es2) for i_ in range(2)]
            k1s_l = [sb("k1s_%d" % i_, [128, 8, 16], F32, es2) for i_ in range(2)]
            k2f_l = [sb("k2f_%d" % i_, [128, 8, 16], F32, es2) for i_ in range(2)]
            res_l = [sb("res_%d" % i_, [128, 3, 128], F32, es2) for i_ in range(2)]
            rst = [sb("rst%d" % i, [128, 3, 128], F32, es2) for i in range(2)]
            for kc in range(KC):
                tk.dma("pool", Wq[:, kc, :], wq[kc * 128:(kc + 1) * 128, :], writes=["Wq"], key="wload")
            tk.dma("pool", kT[:], keysT, writes=["kT"], key="wload")
            PRIV = ['sc', 'tmp1', 'st', 'iu', 'itf', 'dd', 'cand', 'tmp2', 'cf', 'pu', 'posf', 'cs', 'zs', 'ge', 'prod', 'k1s', 'k2f', 'res']

            def b2_tile(ti, tk):
                sc = sc_l[ti % 2]; tmp1 = tmp1_l[ti % 2]; st = st_l[ti % 2]; iu = iu_l[ti % 2]; itf = itf_l[ti % 2]; dd = dd_l[ti % 2]; cand = cand_l[ti % 2]; tmp2 = tmp2_l[ti % 2]; cf = cf_l[ti % 2]; pu = pu_l[ti % 2]; posf = posf_l[ti % 2]; cs = cs_l[ti % 2]; zs = zs_l[ti % 2]; ge = ge_l[ti % 2]; prod = prod_l[ti % 2]; k1s = k1s_l[ti % 2]; k2f = k2f_l[ti % 2]; res = res_l[ti % 2]
                st4 = st[:].rearrange("p (h q) k -> p h q k", q=2)
                itf4 = itf[:].rearrange("p (h q) k -> p h q k", q=2)
                dd4 = dd[:].rearrange("p (h q) k -> p h q k", q=2)
                s = ti % 2
                cols = slice(ti * 128, (ti + 1) * 128)
                tk.dma("sp", h2t[s][:], h2T_d[:, :, cols], writes=["h2tb%d" % s])
                for blk in range(16):
                    b = s
                    p = blk % 2
                    for kc in range(KC):
                        tk.op("pe", lambda e, kc=kc, b=b, blk=blk: e.matmul(
                            psum[b][:, 0:128], lhsT=Wq[:, kc, blk * 128:(blk + 1) * 128], rhs=h2t[s][:, kc, :],
                            start=(kc == 0), stop=(kc == KC - 1)), reads=["Wq", "h2tb%d" % s], writes=["psb%d" % b], inc=(kc == KC - 1))
                    tk.op("act", lambda e, b=b: e.copy(out=qpT[b][:], in_=psum[b][:, 0:128]),
                          reads=["psb%d" % b], writes=["qpT%d" % b])
                    sbk = 2 + 2 * s + (blk // 4) % 2
                    tk.op("pe", lambda e, b=b, p=p, sbk=sbk, blk=blk: e.matmul(
                        psum[sbk][:, (blk % 4) * 128:(blk % 4 + 1) * 128], lhsT=qpT[b][:], rhs=kT[:, p, :],
                        start=True, stop=True), reads=["qpT%d" % b, "kT"], writes=["psb%d" % sbk])
                    if blk % 4 == 3:
                        tk.op("dve", lambda e, sbk=sbk, blk=blk: e.tensor_copy(
                            out=sc[:, blk - 3:blk + 1, :], in_=psum[sbk][:, :].rearrange("p (a n) -> p a n", a=4)),
                            reads=["psb%d" % sbk], writes=["sc"])
                for blk in range(16):
                    tk.op("dve", lambda e, blk=blk: e.max(out=st[:, blk, 0:8], in_=sc[:, blk, :]),
                          reads=["sc"], writes=["st"])
                    tk.op("dve", lambda e, blk=blk: e.max_index(out=iu[:, blk, 0:8], in_max=st[:, blk, 0:8],
                                                                in_values=sc[:, blk, :]),
                          reads=["sc", "st"], writes=["iu"])
                    tk.op("dve", lambda e, blk=blk: e.match_replace(out=tmp1[:], in_to_replace=st[:, blk, 0:8],
                                                                    in_values=sc[:, blk, :], imm_value=-1e30),
                          reads=["sc", "st"], writes=["tmp1"])
                    tk.op("dve", lambda e, blk=blk: e.max(out=st[:, blk, 8:16], in_=tmp1[:]),
                          reads=["tmp1"], writes=["st"])
                    tk.op("dve", lambda e, blk=blk: e.max_index(out=iu[:, blk, 8:16], in_max=st[:, blk, 8:16],
                                                                in_values=tmp1[:]),
                          reads=["tmp1", "st"], writes=["iu"])
                tk.op("dve", lambda e: e.tensor_copy(out=itf[:], in_=iu[:]), reads=["iu"], writes=["itf"])
                tk.op("dve", lambda e: e.tensor_copy(out=dd[:, :, 0:1], in_=itf[:, :, 0:1]), reads=["itf"], writes=["dd"])
                tk.op("dve", lambda e: e.tensor_tensor(out=dd[:, :, 1:16], in0=itf[:, :, 1:16], in1=itf[:, :, 0:15],
                                                       op=ALU.subtract), reads=["itf"], writes=["dd"])
                a0 = st4[:, :, 0, :].unsqueeze(3).broadcast_to([128, 8, 16, 16])
                a1 = st4[:, :, 1, :].unsqueeze(2).broadcast_to([128, 8, 16, 16])
                cand4 = cand[:].rearrange("p h (a b) -> p h a b", a=16)
                tk.op("dve", lambda e: e.tensor_tensor(out=cand4, in0=a0, in1=a1, op=ALU.add), reads=["st"], writes=["cand"])
                for h in range(8):
                    tk.op("dve", lambda e, h=h: e.max(out=cf[:, h, 0:8], in_=cand[:, h, :]), reads=["cand"], writes=["cf"])
                    tk.op("dve", lambda e, h=h: e.max_index(out=pu[:, h, 0:8], in_max=cf[:, h, 0:8], in_values=cand[:, h, :]),
                          reads=["cand", "cf"], writes=["pu"])
                    tk.op("dve", lambda e, h=h: e.match_replace(out=tmp2[:], in_to_replace=cf[:, h, 0:8],
                                                                in_values=cand[:, h, :], imm_value=-1e30),
                          reads=["cand", "cf"], writes=["tmp2"])
                    tk.op("dve", lambda e, h=h: e.max(out=cf[:, h, 8:16], in_=tmp2[:]), reads=["tmp2"], writes=["cf"])
                    tk.op("dve", lambda e, h=h: e.max_index(out=pu[:, h, 8:16], in_max=cf[:, h, 8:16], in_values=tmp2[:]),
                          reads=["tmp2", "cf"], writes=["pu"])
                tk.op("dve", lambda e: e.tensor_copy(out=posf[:], in_=pu[:]), reads=["pu"], writes=["posf"])
                tk.op("dve", lambda e: e.tensor_tensor(out=cs[:], in0=cf[:], in1=cf[:, :, 0:1].broadcast_to([128, 8, 16]),
                                                       op=ALU.subtract), reads=["cf"], writes=["cs"])
                tk.op("act", lambda e: e.activation(out=cs[:], in_=cs[:], func=AF.Exp), reads=["cs"], writes=["cs"])
                tk.op("dve", lambda e: e.tensor_reduce(out=zs[:], in_=cs[:], axis=AX.X, op=ALU.add), reads=["cs"], writes=["zs"])
                tk.op("dve", lambda e: e.reciprocal(out=zs[:], in_=zs[:]), reads=["zs"], writes=["zs"])
                res_g = res[:, 2, :].rearrange("p (h k) -> p h k", h=8)
                tk.op("dve", lambda e: e.tensor_tensor(out=res_g, in0=cs[:], in1=zs[:].unsqueeze(2).broadcast_to([128, 8, 16]),
                                                       op=ALU.mult), reads=["cs", "zs"], writes=["res"])
                pos_b = posf[:].unsqueeze(3).broadcast_to([128, 8, 16, 16])
                thr_b = thr16[:].unsqueeze(1).unsqueeze(1).broadcast_to([128, 8, 16, 16])
                tk.op("dve", lambda e: e.tensor_tensor(out=ge[:], in0=pos_b, in1=thr_b, op=ALU.is_ge),
                      reads=["posf", "c3"], writes=["ge"])
                d1_b = dd4[:, :, 0, :].unsqueeze(2).broadcast_to([128, 8, 16, 16])
                tk.op("dve", lambda e: e.tensor_tensor(out=prod[:], in0=ge[:], in1=d1_b, op=ALU.mult),
                      reads=["ge", "dd"], writes=["prod"])
                res_i = res[:, 0, :].rearrange("p (h k) -> p h k", h=8)
                tk.op("dve", lambda e: e.tensor_reduce(out=res_i, in_=prod[:], axis=AX.X, op=ALU.add),
                      reads=["prod"], writes=["res"])
                tk.op("dve", lambda e: e.tensor_reduce(out=k1s[:], in_=ge[:], axis=AX.X, op=ALU.add), reads=["ge"], writes=["k1s"])
                tk.op("dve", lambda e: e.tensor_scalar(out=k1s[:], in0=k1s[:], scalar1=-16.0, scalar2=16.0, op0=ALU.mult,
                                                       op1=ALU.add), reads=["k1s"], writes=["k1s"])
                tk.op("dve", lambda e: e.tensor_tensor(out=k2f[:], in0=posf[:], in1=k1s[:], op=ALU.add),
                      reads=["posf", "k1s"], writes=["k2f"])
                k2_b = k2f[:].unsqueeze(3).broadcast_to([128, 8, 16, 16])
                io_b = iota16[:].unsqueeze(1).unsqueeze(1).broadcast_to([128, 8, 16, 16])
                tk.op("dve", lambda e: e.tensor_tensor(out=ge[:], in0=k2_b, in1=io_b, op=ALU.is_ge),
                      reads=["k2f", "c4"], writes=["ge"])
                d2_b = dd4[:, :, 1, :].unsqueeze(2).broadcast_to([128, 8, 16, 16])
                tk.op("dve", lambda e: e.tensor_tensor(out=prod[:], in0=ge[:], in1=d2_b, op=ALU.mult),
                      reads=["ge", "dd"], writes=["prod"])
                res_j = res[:, 1, :].rearrange("p (h k) -> p h k", h=8)
                tk.op("dve", lambda e: e.tensor_reduce(out=res_j, in_=prod[:], axis=AX.X, op=ALU.add),
                      reads=["prod"], writes=["res"])
                for q in range(3):
                    tk.op("pe", lambda e, q=q: e.transpose(out=psum[6 + s][:, q * 128:(q + 1) * 128], in_=res[:, q, :],
                                                           identity=ident_f[:]), reads=["res", "c1"], writes=["psb%d" % (6 + s)])
                tk.op("act", lambda e, s=s: e.copy(out=rst[s][:], in_=psum[6 + s][:, 0:384].rearrange("p (q t) -> p q t", q=3)),
                      reads=["psb%d" % (6 + s)], writes=["rst%d" % s])
                tk.dma("pool", slot_d[:, :, cols], rst[s][:], reads=["rst%d" % s], key="rst%d" % s)

            for t0_ in range(0, NTT, 2):
                qs_ = []
                for ti in range(t0_, min(NTT, t0_ + 2)):
                    q_ = OpQueue()
                    q_.suffix = "_%d" % (ti % 2)
                    q_.priv = set(PRIV)
                    b2_tile(ti, q_)
                    qs_.append(q_)
                emit_interleaved(tk, qs_)
            tk.barrier()

        if os.environ.get('BSTOP') == '2':
            tk.final_wait('sp')
            return nc
        with ExitStack() as es3:
            G = sb("G", [128, 128, PB], BF16, es3)
            ut = [sb("ut%d" % i, [128, KC, 128], BF16, es3) for i in range(5)]
            vt = [sb("vt%d" % i, [128, D], BF16, es3) for i in range(5)]
            h2b = [sb("h2b%d" % i, [128, KC, PB], BF16, es3) for i in range(2)]
            slots = [sb("slots%d" % i, [128, 3, PB], F32, es3) for i in range(2)]
            gl = [sb("gl%d" % i, [128, PB], BF16, es3) for i in range(2)]
            ohi = [sb("ohi%d" % i, [128, 128], BF16, es3) for i in range(4)]
            ohj = [sb("ohj%d" % i, [128, 128], BF16, es3) for i in range(4)]
            x1t = sb("x1t", [128, D], F32, es3)
            xo = [sb("xo%d" % i, [128, D], F32, es3) for i in range(2)]
            junk = sb("junk3", [128, D], BF16, es3)
            fnb = sb("fnb", [128, D], F32, es3)
            ss = sb("ss3", [128, 4], F32, es3)
            tk.dma("sp", fnb[:], fnw, writes=["fnb"])
            ucnt = [0]
            vcnt = [0]
            ocnt = [0]
            for blk in range(NBLK):
                bs = blk % 2
                cols = slice(blk * PB, (blk + 1) * PB)
                tk.dma("sp", slots[bs][:], slot_d[:, :, cols], writes=["slots%d" % bs])
                tk.dma("sp", h2b[bs][:], h2T_d[:, :, cols], writes=["h2b%d" % bs])
                for t in range(PB):
                    o = t % 4
                    tk.op("dve", lambda e, o=o, t=t, bs=bs: e.tensor_scalar(
                        out=ohi[o][:], in0=iota_row[:], scalar1=slots[bs][:, 0, t:t + 1], scalar2=slots[bs][:, 2, t:t + 1],
                        op0=ALU.is_equal, op1=ALU.mult), reads=["slots%d" % bs, "c2"], writes=["ohi%d" % o])
                    tk.op("dve", lambda e, o=o, t=t, bs=bs: e.tensor_scalar(
                        out=ohj[o][:], in0=iota_row[:], scalar1=slots[bs][:, 1, t:t + 1], scalar2=None,
                        op0=ALU.is_equal), reads=["slots%d" % bs, "c2"], writes=["ohj%d" % o])
                    gb_ = (t // 4) % 2
                    tk.op("pe", lambda e, o=o, gb_=gb_: e.matmul(psum[gb_][:, o * 128:(o + 1) * 128], lhsT=ohi[o][:],
                                                                 rhs=ohj[o][:], start=True, stop=True),
                          reads=["ohi%d" % o, "ohj%d" % o], writes=["psb%d" % gb_])
                    if o == 3:
                        t0 = t - 3
                        tk.op("act", lambda e, gb_=gb_, t0=t0: e.copy(
                            out=G[:, :, t0:t0 + 4], in_=psum[gb_][:, :].rearrange("p (t j) -> p j t", t=4)),
                            reads=["psb%d" % gb_], writes=["G"])
                for j in range(NJ):
                    us = ucnt[0] % 5
                    ucnt[0] += 1
                    tk.dma("sp", ut[us][:], u16[j].rearrange("p (k i) -> p k i", k=KC), writes=["ut%d" % us])
                    b = 2 + j % 2
                    for kc in range(KC):
                        tk.op("pe", lambda e, kc=kc, b=b, us=us, bs=bs: e.matmul(
                            psum[b][:, 0:PB], lhsT=ut[us][:, kc, :], rhs=h2b[bs][:, kc, :], start=(kc == 0),
                            stop=(kc == KC - 1)), reads=["ut%d" % us, "h2b%d" % bs], writes=["psb%d" % b], inc=(kc == KC - 1))
                    g = j % 2
                    tk.op("act", lambda e, b=b, g=g: e.activation(out=gl[g][:], in_=psum[b][:, 0:PB], func=AF.Gelu),
                          reads=["psb%d" % b], writes=["gl%d" % g])
                    tk.op("dve", lambda e, g=g, j=j: e.tensor_tensor(out=G[:, j, :], in0=gl[g][:], in1=G[:, j, :], op=ALU.mult),
                          reads=["gl%d" % g, "G"], writes=["G"])
                for j in range(NJ):
                    vs = vcnt[0] % 5
                    vcnt[0] += 1
                    tk.dma("sp", vt[vs][:], v16[j], writes=["vt%d" % vs])
                    for tt in range(TPB):
                        for cg in range(4):
                            b = tt * 4 + cg
                            tk.op("pe", lambda e, j=j, tt=tt, cg=cg, b=b, vs=vs: e.matmul(
                                psum[b][:, :], lhsT=G[:, j, tt * 128:(tt + 1) * 128], rhs=vt[vs][:, cg * 512:(cg + 1) * 512],
                                start=(j == 0), stop=(j == NJ - 1)), reads=["G", "vt%d" % vs], writes=["psb%d" % b])
                for tt in range(TPB):
                    rows = slice(blk * PB + tt * 128, blk * PB + (tt + 1) * 128)
                    o = ocnt[0] % 2
                    ocnt[0] += 1
                    tk.dma("sp", x1t[:], x1_d[rows, :], writes=["x1t"])
                    for cg in range(4):
                        b = tt * 4 + cg
                        tk.op("dve", lambda e, b=b, cg=cg, o=o: e.tensor_tensor(
                            out=xo[o][:, cg * 512:(cg + 1) * 512], in0=psum[b][:, :], in1=x1t[:, cg * 512:(cg + 1) * 512],
                            op=ALU.add), reads=["psb%d" % b, "x1t"], writes=["xo%d" % o])
                    rms_rstd(xo[o][:], "xo%d" % o, ss, junk)
                    tk.op("dve", lambda e, o=o: e.scalar_tensor_tensor(out=xo[o][:], in0=xo[o][:], scalar=ss[:, 2:3],
                                                                       in1=fnb[:], op0=ALU.mult, op1=ALU.mult),
                          reads=["xo%d" % o, "ss2", "fnb"], writes=["xo%d" % o])
                    tk.dma("pool", out[rows, :], xo[o][:], reads=["xo%d" % o], key="ost%d" % o)
            tk.barrier()
        tk.final_wait("sp")
    print("phase B instructions (cumulative):", tk.ninst)
    return nc


def host_inputs_b(inp, xflat, mixed_full, TCs):
    c = host_consts_b()
    U = np.asarray(inp["peer_u"][0])
    V = np.asarray(inp["peer_v"][0])
    uh = np.ascontiguousarray(U.reshape(128, 128, KC, 128).transpose(1, 3, 2, 0)).reshape(128, 128, D)
    vh = np.ascontiguousarray(V.reshape(128, 128, D).transpose(1, 0, 2))
    keys = np.asarray(inp["peer_keys"][0])
    keysT = np.ascontiguousarray(keys.transpose(2, 0, 1))
    wout = np.ascontiguousarray(np.asarray(inp["w_out"][0]))
    wq = np.ascontiguousarray(np.asarray(inp["peer_w_q"][0]))
    n2w = np.ascontiguousarray(np.broadcast_to(np.asarray(inp["norm2_w"][0])[None, :], (128, D)))
    fnw = np.ascontiguousarray(np.broadcast_to(np.asarray(inp["final_norm_w"])[None, :], (128, D)))
    maps = []
    for core in range(8):
        r0 = core * TCs
        m = {"x": np.ascontiguousarray(xflat[r0:r0 + TCs]),
             "mixed": np.ascontiguousarray(mixed_full[r0:r0 + TCs]),
             "wout": wout, "wq": wq, "n2w": n2w, "fnw": fnw, "keysT": keysT, "uh": uh, "vh": vh}
        m.update(c)
        maps.append(m)
    return maps


def build_fused(nc, S):
    TC = 2 * S // 8
    NK = S // 512
    tk = Trk(nc)
    mixloc = [nc.dram_tensor("mixloc%d" % k, [512, 512], BF16) for k in range(NK)]
    gath = [nc.dram_tensor("gath%d" % k, [8 * 512, 512], BF16) for k in range(NK)]
    uh = nc.dram_tensor("b_uh", [128, 128, D], F32, kind="ExternalInput").ap()
    vh = nc.dram_tensor("b_vh", [128, 128, D], F32, kind="ExternalInput").ap()
    u16 = nc.dram_tensor("b_u16", [128, 128, D], BF16, kind="Internal").ap()
    v16 = nc.dram_tensor("b_v16", [128, 128, D], BF16, kind="Internal").ap()
    tk.lazy_keys.add("cvt")
    for j in range(128):
        tk.dma("pool", u16[j], uh[j], writes=["u16"], key="cvt")
        tk.dma("pool", v16[j], vh[j], writes=["v16"], key="cvt")
    build_phase_a(nc, S, tk=tk, mix_dst=lambda t0, c0, c1: mixloc[t0 // 512][t0 % 512:t0 % 512 + 128, c0:c1])
    for k in range(NK):
        tk.coll(lambda g, k=k: g.collective_compute("AllGather", ALU.bypass, replica_groups=[list(range(8))],
                                                    ins=[mixloc[k].ap().opt()], outs=[gath[k].ap().opt()]),
                writes=["gath%d" % k])
    tk.lazy_keys.discard("cvt")
    build_phase_b(nc, TC, tk=tk, gath=gath, pfx="b_", pre=(u16, v16))
    return nc


def wout_perm():
    perm = []
    for g in range(4):
        perm += list(range(g * 256, (g + 1) * 256)) + list(range(1024 + g * 256, 1024 + (g + 1) * 256))
    return np.array(perm)


def host_inputs_fused(inp, S):
    maps = host_inputs_a(inp, S)
    c = host_consts_b()
    TC = 2 * S // 8
    H = S // 8
    U = np.asarray(inp["peer_u"][0])
    V = np.asarray(inp["peer_v"][0])
    uh = np.ascontiguousarray(U.reshape(128, 128, KC, 128).transpose(1, 3, 2, 0)).reshape(128, 128, D)
    vh = np.ascontiguousarray(V.reshape(128, 128, D).transpose(1, 0, 2))
    keysT = np.ascontiguousarray(np.asarray(inp["peer_keys"][0]).transpose(2, 0, 1))
    wout = np.ascontiguousarray(np.asarray(inp["w_out"][0])[wout_perm(), :])
    wq = np.ascontiguousarray(np.asarray(inp["peer_w_q"][0]))
    n2w = np.ascontiguousarray(np.broadcast_to(np.asarray(inp["norm2_w"][0])[None, :], (128, D)))
    fnw = np.ascontiguousarray(np.broadcast_to(np.asarray(inp["final_norm_w"])[None, :], (128, D)))
    x = np.asarray(inp["x"])
    for d in range(8):
        m = maps[d]
        m["b_x"] = np.ascontiguousarray(np.concatenate([x[0, d * H:(d + 1) * H], x[1, d * H:(d + 1) * H]], axis=0))
        ws = np.zeros((128, 8), np.float32)
        ws[:, d] = 1.0
        m["b_wsel"] = ws
        m.update({"b_wout": wout, "b_wq": wq, "b_n2w": n2w, "b_fnw": fnw, "b_keysT": keysT, "b_uh": uh, "b_vh": vh})
        for k, v in c.items():
            m["b_" + k] = v
    return maps


def gather_out(res, S):
    H = S // 8
    out = np.zeros((2, S, D), np.float32)
    for d in range(8):
        o = res.results[d]["b_out"]
        out[0, d * H:(d + 1) * H] = o[0:H]
        out[1, d * H:(d + 1) * H] = o[H:2 * H]
    return out


S_FULL = 16384


def kernel(**inputs):
    inp = {k: np.asarray(v) for k, v in inputs.items()}
    S = S_FULL
    nc = bass.Bass("TRN2", target_bir_lowering=False)
    build_fused(nc, S)
    maps = host_inputs_fused(inp, S)
    res = run_bass_kernel_spmd(nc, maps, core_ids=list(range(8)))
    return gather_out(res, S).astype(np.float32)
```

```python
import numpy as np
import ml_dtypes
from contextlib import ExitStack
import concourse.bass as bass
import concourse.mybir as mybir
from concourse.bass_utils import run_bass_kernel_spmd

F32 = mybir.dt.float32
BF16 = mybir.dt.bfloat16
I32 = mybir.dt.int32
U32 = mybir.dt.uint32
AF = mybir.ActivationFunctionType
ALU = mybir.AluOpType
AX = mybir.AxisListType

D = 2048
KC = 16
EPS = 1e-6


class Trk:
    def __init__(self, nc):
        self.nc = nc
        self.engs = {"pe": nc.tensor, "act": nc.scalar, "dve": nc.vector,
                     "pool": nc.gpsimd, "sp": nc.sync}
        self.sem = {}
        self.cnt = {}
        for e in ("pe", "act", "dve", "pool"):
            self.sem[e] = nc.alloc_semaphore(name="s_" + e)
            self.cnt[e] = 0
        self.dsem = {}
        self.waited = {}
        self.lastw = {}
        self.readers = {}
        self.ninst = 0
        self.lazy_keys = set()

    def _wait(self, eng, ev):
        sem, val, src, key = ev
        k = (eng, key)
        if self.waited.get(k, 0) >= val:
            return
        self.engs[eng].wait_ge(sem, val)
        self.waited[k] = val

    def _deps(self, eng, reads, writes):
        for b in reads:
            ev = self.lastw.get(b)
            if ev is not None and not (ev[2] == eng and eng == "pe"):
                self._wait(eng, ev)
        for b in writes:
            ev = self.lastw.get(b)
            if ev is not None and ev[2] != eng:
                self._wait(eng, ev)
            for ev in self.readers.get(b, ()):
                if ev[2] != eng:
                    self._wait(eng, ev)

    def _record(self, ev, reads, writes):
        for b in reads:
            self.readers.setdefault(b, []).append(ev)
        for b in writes:
            self.lastw[b] = ev
            self.readers[b] = []

    def op(self, eng, fn, reads=(), writes=(), inc=True):
        self._deps(eng, reads, writes)
        inst = fn(self.engs[eng])
        if inc:
            self.cnt[eng] += 1
            inst.then_inc(self.sem[eng], 1)
            ev = (self.sem[eng], self.cnt[eng], eng, eng)
        else:
            assert eng == "pe"
            ev = (self.sem[eng], self.cnt[eng] + 1, eng, eng)
        self._record(ev, reads, writes)
        self.ninst += 1
        return inst

    def dma(self, q, out, in_, reads=(), writes=(), key=None, **kw):
        if key is None:
            key = writes[0] if writes else reads[0]
        if key not in self.dsem:
            self.dsem[key] = [self.nc.alloc_semaphore(name="d%d" % len(self.dsem)), 0]
        self._deps(q, reads, writes)
        ds = self.dsem[key]
        inst = self.engs[q].dma_start(out=out, in_=in_, **kw)
        ds[1] += 16
        inst.then_inc(ds[0], 16)
        ev = (ds[0], ds[1], "dma", ("d", key))
        self._record(ev, reads, writes)
        self.ninst += 1
        return inst

    def coll(self, fn, reads=(), writes=()):
        if "cc" not in self.sem:
            self.sem["cc"] = self.nc.alloc_semaphore(name="s_cc")
            self.cnt["cc"] = 0
        self._deps("pool", reads, writes)
        inst = fn(self.engs["pool"])
        self.cnt["cc"] += 1
        inst.then_inc(self.sem["cc"])
        ev = (self.sem["cc"], self.cnt["cc"], "cc", "cc")
        self._record(ev, reads, writes)
        self.ninst += 1
        return inst

    def barrier(self):
        evs = [(self.sem[e], self.cnt[e], e, e) for e in self.sem if self.cnt[e] > 0]
        evs += [(v[0], v[1], "dma", ("d", k)) for k, v in self.dsem.items()
                if v[1] > 0 and k not in self.lazy_keys]
        for e in ("pe", "act", "dve", "pool", "sp"):
            for ev in evs:
                if ev[2] != e:
                    self._wait(e, ev)
        keep = {b: ev for b, ev in self.lastw.items() if ev[2] == "dma" and ev[3][1] in self.lazy_keys}
        self.lastw = keep
        self.readers = {}

    def final_wait(self, q="sp"):
        for k, v in self.dsem.items():
            if v[1] > 0:
                self._wait(q, (v[0], v[1], "dma", ("d", k)))


class OpQueue:
    def __init__(self):
        self.q = []
        self.suffix = ""
        self.priv = set()

    def _fix(self, k):
        for nm in ("reads", "writes"):
            if nm in k:
                k[nm] = [x + self.suffix if x in self.priv else x for x in k[nm]]
        return k

    def op(self, *a, **k):
        self.q.append(("op", a, self._fix(k)))

    def dma(self, *a, **k):
        self.q.append(("dma", a, self._fix(k)))


def emit_interleaved(tk, queues):
    n = max(len(q.q) for q in queues)
    for i in range(n):
        for q in queues:
            if i < len(q.q):
                kind, a, k = q.q[i]
                getattr(tk, kind)(*a, **k)


import os
PH = os.environ.get('PH', 'PAM')
SK = os.environ.get('SK', '')

NCOL = 1540
C_FQ0, C_FQ1, C_FK0, C_FK1, C_FV, C_MQ, C_MK, C_MVO, C_G = 0, 128, 256, 384, 512, 768, 896, 1024, 1536


def host_consts():
    c = {}
    c["ident_bf"] = np.eye(128, dtype=np.float32).astype(ml_dtypes.bfloat16)
    c["ident_f"] = np.eye(128, dtype=np.float32)
    r = np.arange(128)
    c["triu"] = (r[:, None] <= r[None, :]).astype(np.float32)
    c["ones_f"] = np.ones((128, 128), np.float32)
    es = np.zeros((128, 128), np.float32); es[0, :] = 1.0
    c["esel"] = es
    t = np.arange(512)
    m = np.stack([(r[:, None] + 128 * rr <= t[None, :]) for rr in range(4)], axis=1)
    c["amask"] = m.astype(np.float32).astype(ml_dtypes.bfloat16)
    c["mmask"] = ((r[:, None] <= r[None, :]).astype(np.float32) * (128 ** -0.5)).astype(np.float32)
    return c


def build_phase_a(nc, S, tk=None, mix_dst=None, on_block_done=None):
    NT = S // 128
    NB = S // 512
    own_tk = tk is None
    if own_tk:
        tk = Trk(nc)
    dt = nc.dram_tensor
    x = dt("x", [int(os.environ.get("XROWS", S)), D], F32, kind="ExternalInput").ap()
    wc = dt("wc", [D, NCOL], F32, kind="ExternalInput").ap()
    n1w = dt("n1w", [128, KC], F32, kind="ExternalInput").ap()
    gbias = dt("gbias", [128, 4], F32, kind="ExternalInput").ap()
    convp = dt("convp", [128, 10], F32, kind="ExternalInput").ap()
    hnw = dt("hnw", [128, 512], F32, kind="ExternalInput").ap()
    ident_bf_d = dt("ident_bf", [128, 128], BF16, kind="ExternalInput").ap()
    ident_f_d = dt("ident_f", [128, 128], F32, kind="ExternalInput").ap()
    triu_d = dt("triu", [128, 128], F32, kind="ExternalInput").ap()
    ones_d = dt("ones_f", [128, 128], F32, kind="ExternalInput").ap()
    esel_d = dt("esel", [128, 128], F32, kind="ExternalInput").ap()
    amask_d = dt("amask", [128, 4, 512], BF16, kind="ExternalInput").ap()
    mmask_d = dt("mmask", [128, 128], F32, kind="ExternalInput").ap()
    if mix_dst is None:
        mixed = dt("mixed", [int(os.environ.get("MROWS", S)), 512], BF16, kind="ExternalOutput").ap()
        mix_dst = lambda t0, c0, c1: mixed[t0:t0 + 128, c0:c1]
    qt_d = [dt("qt%d" % h, [128, S], BF16, kind="Internal").ap() for h in range(2)]
    kt_d = [dt("kt%d" % h, [128, S], BF16, kind="Internal").ap() for h in range(2)]
    va_d = [dt("va%d" % h, [128, S // 128, 130], BF16, kind="Internal").ap() for h in range(2)]
    mq_d = dt("mqT", [128, S], F32, kind="Internal").ap()
    mk_d = dt("mkT", [128, S], F32, kind="Internal").ap()
    mvo_d = dt("mvo", [S, 512], F32, kind="Internal").ap()
    if os.environ.get("DUMMY"):
        dummy_d = dt("dummyx", [int(os.environ["DUMMY"]), 512], F32, kind="Internal").ap()

    with ExitStack() as es0:
        def sb(name, shape, dtype, es=es0):
            return es.enter_context(nc.sbuf_tensor(name, shape, dtype))
        ident_bf = sb("ident_bf_s", [128, 128], BF16)
        ident_f = sb("ident_f_s", [128, 128], F32)
        triu = sb("triu_s", [128, 128], F32)
        ones_f = sb("ones_s", [128, 128], F32)
        esel = sb("esel_s", [128, 128], F32)
        amask = sb("amask_s", [128, 4, 512], BF16)
        mmask = sb("mmask_s", [128, 128], F32)
        gb = sb("gb_s", [128, 4], F32)
        ngb = sb("ngb_s", [128, 4], F32)
        cvp = sb("cvp_s", [128, 10], F32)
        hnw_s = sb("hnw_s", [128, 512], F32)
        n1w_s = sb("n1w_s", [128, KC], F32)
        gates = sb("gates_s", [128, NT, 4], F32)
        epsc = sb("epsc", [128, 1], F32)
        onec = sb("onec", [128, 1], F32)
        psum = [es0.enter_context(nc.psum_tensor("ps%d" % i, [128, 512], F32)) for i in range(8)]
        for i, (s_, d_) in enumerate([(ident_bf, ident_bf_d), (ident_f, ident_f_d), (triu, triu_d),
                                      (ones_f, ones_d), (esel, esel_d), (amask, amask_d), (mmask, mmask_d),
                                      (gb, gbias), (cvp, convp), (hnw_s, hnw), (n1w_s, n1w)]):
            tk.dma("sp", s_[:], d_, writes=["c%d" % i], key="const")
        tk.barrier()
        tk.op("dve", lambda e: e.memset(epsc[:], EPS), writes=["epsc"])
        tk.op("dve", lambda e: e.memset(onec[:], 1.0), writes=["onec"])
        tk.op("dve", lambda e: e.tensor_scalar(out=ngb[:], in0=gb[:], scalar1=-1.0, scalar2=None, op0=ALU.mult),
              reads=["c7"], writes=["ngb"])

        with ExitStack() as es1:
            W = sb("W", [128, KC, NCOL], BF16, es1)
            wst = [sb("wst%d" % i, [128, NCOL], F32, es1) for i in range(2)]
            xt = [sb("xt%d" % i, [128, D], F32, es1) for i in range(2)]
            xn = [sb("xn%d" % i, [128, D], BF16, es1) for i in range(2)]
            junk = sb("junk", [128, D], BF16, es1)
            hT = [sb("hT%d" % i, [128, KC, 512], BF16, es1) for i in range(2)]
            ss = sb("ss", [128, 4], F32, es1)
            fst = [sb("fst%d" % i, [128, 512], BF16, es1) for i in range(4)]
            mst = [sb("mst%d" % i, [128, 512], F32, es1) for i in range(4)]
            vst = [sb("vst%d" % i, [128, 2, 130], BF16, es1) for i in range(2)]
            for i in range(2):
                tk.op("dve", lambda e, i=i: e.memset(vst[i][:, :, 128:129], 1.0), writes=["vst%d" % i])
                tk.op("dve", lambda e, i=i: e.memset(vst[i][:, :, 129:130], 0.0), writes=["vst%d" % i])
            for kc in range(KC):
                s = kc % 2
                tk.dma("sp", wst[s][:], wc[kc * 128:(kc + 1) * 128, :], writes=["wst%d" % s])
                tk.op("dve", lambda e, kc=kc, s=s: e.tensor_scalar(
                    out=W[:, kc, :], in0=wst[s][:], scalar1=n1w_s[:, kc:kc + 1], scalar2=None, op0=ALU.mult),
                    reads=["wst%d" % s, "c10"], writes=["W"])
            pb = [2]

            def nbank():
                b = pb[0]
                pb[0] = 2 + (pb[0] - 2 + 1) % 6
                return b
            fcnt = [0]
            mcnt = [0]
            def p_stage1(jb):
                hs = jb % 2
                for tt in range(4):
                    ti = jb * 4 + tt
                    s = ti % 2
                    tk.dma("sp", xt[s][:], x[ti * 128:(ti + 1) * 128, :], writes=["xt%d" % s])
                    tk.op("act", lambda e, s=s: e.activation(out=junk[:], in_=xt[s][:], func=AF.Square,
                                                             accum_out=ss[:, 0:1]),
                          reads=["xt%d" % s], writes=["junk", "ss0"])
                    tk.op("act", lambda e: e.activation(out=ss[:, 1:2], in_=ss[:, 0:1], func=AF.Ln,
                                                        scale=1.0 / D, bias=epsc[:]),
                          reads=["ss0", "epsc"], writes=["ss1"])
                    tk.op("act", lambda e: e.activation(out=ss[:, 2:3], in_=ss[:, 1:2], func=AF.Exp, scale=-0.5),
                          reads=["ss1"], writes=["ss2"])
                    tk.op("dve", lambda e, s=s: e.tensor_scalar(out=xn[s][:], in0=xt[s][:], scalar1=ss[:, 2:3],
                                                                scalar2=None, op0=ALU.mult),
                          reads=["xt%d" % s, "ss2"], writes=["xn%d" % s])
                    for half in range(2):
                        pst = psum[half][:, :].bitcast(BF16)
                        for k8 in range(8):
                            kc = half * 8 + k8
                            tk.op("pe", lambda e, kc=kc, k8=k8, pst=pst, s=s: e.transpose(
                                out=pst[:, k8 * 128:(k8 + 1) * 128], in_=xn[s][:, kc * 128:(kc + 1) * 128],
                                identity=ident_bf[:]),
                                reads=["xn%d" % s, "c0"], writes=["psb%d" % half], inc=(k8 == 7))
                        eng = "act" if half == 0 else "dve"
                        src = pst.rearrange("p (k t) -> p k t", k=8)
                        dst = hT[hs][:, half * 8:(half + 1) * 8, tt * 128:(tt + 1) * 128]
                        if eng == "act":
                            tk.op("act", lambda e, src=src, dst=dst: e.copy(out=dst, in_=src),
                                  reads=["psb%d" % half], writes=["hT%d" % hs])
                        else:
                            tk.op("dve", lambda e, src=src, dst=dst: e.tensor_copy(out=dst, in_=src),
                                  reads=["psb%d" % half], writes=["hT%d" % hs])

            def p_stage2(jb):
                hs = jb % 2
                for (c0, kind, dst_d) in [(C_FQ0, "f", qt_d[0]), (C_FQ1, "f", qt_d[1]), (C_FK0, "f", kt_d[0]),
                                          (C_FK1, "f", kt_d[1]), (C_MQ, "m", mq_d), (C_MK, "m", mk_d)]:
                    b = nbank()
                    for kc in range(KC):
                        tk.op("pe", lambda e, kc=kc, b=b, c0=c0: e.matmul(
                            psum[b][:, :], lhsT=W[:, kc, c0:c0 + 128], rhs=hT[hs][:, kc, :],
                            start=(kc == 0), stop=(kc == KC - 1)),
                            reads=["W", "hT%d" % hs], writes=["psb%d" % b], inc=(kc == KC - 1))
                    if kind == "f":
                        f = fcnt[0] % 4
                        fcnt[0] += 1
                        tk.op("act", lambda e, b=b, f=f: e.copy(out=fst[f][:], in_=psum[b][:, :]),
                              reads=["psb%d" % b], writes=["fst%d" % f])
                        if 'f' not in SK:
                            tk.dma("pool", dst_d[:, jb * 512:(jb + 1) * 512], fst[f][:], reads=["fst%d" % f],
                                   key="fst%d" % f)
                    else:
                        f = mcnt[0] % 4
                        mcnt[0] += 1
                        tk.op("dve", lambda e, b=b, f=f: e.tensor_copy(out=mst[f][:], in_=psum[b][:, :]),
                              reads=["psb%d" % b], writes=["mst%d" % f])
                        if 'm' not in SK:
                            tk.dma("pool", dst_d[:, jb * 512:(jb + 1) * 512], mst[f][:], reads=["mst%d" % f],
                                   key="mst%d" % f)
                for tt in range(4):
                    ti = jb * 4 + tt
                    tsl = slice(tt * 128, (tt + 1) * 128)
                    b = nbank()
                    for kc in range(KC):
                        tk.op("pe", lambda e, kc=kc, b=b, tsl=tsl: e.matmul(
                            psum[b][:, 0:256], lhsT=hT[hs][:, kc, tsl], rhs=W[:, kc, C_FV:C_FV + 256],
                            start=(kc == 0), stop=(kc == KC - 1)),
                            reads=["W", "hT%d" % hs], writes=["psb%d" % b], inc=(kc == KC - 1))
                    vs = ti % 2
                    tk.op("act", lambda e, b=b, vs=vs: e.copy(
                        out=vst[vs][:, :, 0:128], in_=psum[b][:, 0:256].rearrange("p (h d) -> p h d", h=2)),
                        reads=["psb%d" % b], writes=["vst%d" % vs])
                    for h in (range(2) if 'v' not in SK else []):
                        tk.dma("pool", va_d[h][:, ti, :], vst[vs][:, h, :],
                               reads=["vst%d" % vs], key="vst%d_%d" % (vs, h))
                    b = nbank()
                    for kc in range(KC):
                        tk.op("pe", lambda e, kc=kc, b=b, tsl=tsl: e.matmul(
                            psum[b][:, :], lhsT=hT[hs][:, kc, tsl], rhs=W[:, kc, C_MVO:C_MVO + 512],
                            start=(kc == 0), stop=(kc == KC - 1)),
                            reads=["W", "hT%d" % hs], writes=["psb%d" % b], inc=(kc == KC - 1))
                    f = mcnt[0] % 4
                    mcnt[0] += 1
                    tk.op("dve", lambda e, b=b, f=f: e.tensor_copy(out=mst[f][:], in_=psum[b][:, :]),
                          reads=["psb%d" % b], writes=["mst%d" % f])
                    if 'o' not in SK:
                        tk.dma("pool", mvo_d[ti * 128:(ti + 1) * 128, :], mst[f][:], reads=["mst%d" % f],
                               key="mst%d" % f)
                    b = nbank()
                    for kc in range(KC):
                        tk.op("pe", lambda e, kc=kc, b=b, tsl=tsl: e.matmul(
                            psum[b][:, 0:4], lhsT=hT[hs][:, kc, tsl], rhs=W[:, kc, C_G:C_G + 4],
                            start=(kc == 0), stop=(kc == KC - 1)),
                            reads=["W", "hT%d" % hs], writes=["psb%d" % b], inc=(kc == KC - 1))
                    tk.op("act", lambda e, b=b, ti=ti: e.copy(out=gates[:, ti, :], in_=psum[b][:, 0:4]),
                          reads=["psb%d" % b], writes=["gates"])

            NBP = min(NB, int(os.environ.get('NBLIM', '999')))
            p_stage1(0)
            for jb in range(NBP):
                if jb + 1 < NBP:
                    p_stage1(jb + 1)
                p_stage2(jb)
            tk.barrier()

        NQ = NB
        SC = 128 ** -0.5
        with ExitStack() as es2:
            KT = sb("KT", [128, S], BF16, es2)
            VA = sb("VA", [128, NT, 130], BF16, es2)
            qt = [sb("qtb%d" % i, [128, 512], BF16, es2) for i in range(2)]
            PT = [sb("PT%d" % i, [128, 512], BF16, es2) for i in range(4)]
            lfn = sb("lfn", [128, NT], F32, es2)
            tmpg = sb("tmpg", [128, NT], F32, es2)
            tot = sb("tot", [128, NT], F32, es2)
            offi = sb("offi", [128, NT], F32, es2)
            onesn = sb("onesn", [128, NT], F32, es2)
            G = sb("G", [128, NT], F32, es2)
            cb = sb("cb", [128, NQ], F32, es2)
            biasj = [sb("biasj%d" % i, [128, NT], F32, es2) for i in range(2)]
            sm = sb("sm", [128, 8], F32, es2)
            ot = [sb("ot%d" % i, [128, 128], F32, es2) for i in range(2)]
            oj = sb("oj", [128, 128], F32, es2)
            ob = [sb("ob%d" % i, [128, 128], BF16, es2) for i in range(2)]
            tk.op("dve", lambda e: e.memset(onesn[:], 1.0), writes=["onesn"])
            ptc = [0]
            oc = [0]
            for hh in (range(2) if 'A' in PH else []):
                for c0 in range(0, S, 2048):
                    tk.dma("sp", KT[:, c0:min(S, c0 + 2048)], kt_d[hh][:, c0:min(S, c0 + 2048)], writes=["KT"])
                for c0 in range(0, NT, 16):
                    tk.dma("sp", VA[:, c0:min(NT, c0 + 16), :], va_d[hh][:, c0:min(NT, c0 + 16), :], writes=["VA"])
                tk.op("act", lambda e, hh=hh: e.activation(out=tmpg[:], in_=gates[:, :, hh], func=AF.Exp,
                                                           scale=-1.0, bias=ngb[:, hh:hh + 1]),
                      reads=["gates", "ngb"], writes=["tmpg"])
                tk.op("act", lambda e: e.activation(out=lfn[:], in_=tmpg[:], func=AF.Ln, scale=1.0, bias=onec[:]),
                      reads=["tmpg", "onec"], writes=["lfn"])
                for c0 in range(0, NT, 32):
                    c1 = min(NT, c0 + 32)
                    tk.op("pe", lambda e, c0=c0, c1=c1: e.matmul(psum[0][:, c0:c1], lhsT=triu[:], rhs=lfn[:, c0:c1],
                                                                 start=True, stop=True),
                          reads=["lfn", "c2"], writes=["psb0"])
                    tk.op("pe", lambda e, c0=c0, c1=c1: e.matmul(psum[1][:, c0:c1], lhsT=ones_f[:], rhs=lfn[:, c0:c1],
                                                                 start=True, stop=True),
                          reads=["lfn", "c3"], writes=["psb1"])
                tk.op("act", lambda e: e.copy(out=tot[:], in_=psum[1][:, 0:NT]), reads=["psb1"], writes=["tot"])
                tk.op("act", lambda e: e.copy(out=G[:], in_=psum[0][:, 0:NT]), reads=["psb0"], writes=["G"])
                tk.op("dve", lambda e: e.tensor_tensor_scan(out=offi[:], data0=onesn[:], data1=tot[:], initial=0.0,
                                                            op0=ALU.mult, op1=ALU.add),
                      reads=["tot", "onesn"], writes=["offi"])
                tk.op("dve", lambda e: e.tensor_tensor(out=offi[:], in0=offi[:], in1=tot[:], op=ALU.subtract),
                      reads=["offi", "tot"], writes=["offi"])
                tk.op("dve", lambda e: e.tensor_tensor(out=G[:], in0=G[:], in1=offi[:], op=ALU.add),
                      reads=["G", "offi"], writes=["G"])
                Gsel = G[:].rearrange("p (j r) -> p j r", r=4)[:, :, 2]
                tk.op("dve", lambda e, Gsel=Gsel: e.tensor_copy(out=tmpg[:, 0:NQ], in_=Gsel), reads=["G"],
                      writes=["tmpg"])
                tk.op("pe", lambda e: e.matmul(psum[1][:, 0:NQ], lhsT=esel[:], rhs=tmpg[:, 0:NQ], start=True, stop=True),
                      reads=["tmpg", "c4"], writes=["psb1"])
                tk.op("act", lambda e: e.copy(out=cb[:], in_=psum[1][:, 0:NQ]), reads=["psb1"], writes=["cb"])
                blocks = [(j, i) for j in range(NQ) for i in range(4 * j + 4)]
                NBK = len(blocks)
                LOOK = 2

                def load_q(j):
                    tk.dma("sp", qt[j % 2][:], qt_d[hh][:, j * 512:(j + 1) * 512], writes=["qt%d" % (j % 2)])

                def make_bias(j):
                    nk = 4 * j + 4
                    tk.op("dve", lambda e, j=j, nk=nk: e.tensor_scalar(
                        out=biasj[j % 2][:, 0:nk], in0=G[:, 0:nk], scalar1=cb[:, j:j + 1], scalar2=None,
                        op0=ALU.subtract), reads=["G", "cb"], writes=["bj%d" % (j % 2)])

                def st_qk(n):
                    j, i = blocks[n]
                    if i == 0 and j + 1 < NQ:
                        load_q(j + 1)
                    sb_ = n % 4
                    qs = j % 2
                    tk.op("pe", lambda e, i=i, qs=qs, sb_=sb_: e.matmul(
                        psum[sb_][:, :], lhsT=KT[:, i * 128:(i + 1) * 128], rhs=qt[qs][:], start=True, stop=True),
                        reads=["KT", "qt%d" % qs], writes=["psb%d" % sb_])

                def st_ex(n):
                    j, i = blocks[n]
                    if i == 0 and j + 1 < NQ:
                        make_bias(j + 1)
                    r = i - 4 * j
                    sb_ = n % 4
                    p = n % 4
                    bj = j % 2
                    tk.op("act", lambda e, p=p, sb_=sb_, bj=bj, i=i: e.activation(
                        out=PT[p][:], in_=psum[sb_][:, :], func=AF.Exp, scale=SC, bias=biasj[bj][:, i:i + 1]),
                        reads=["psb%d" % sb_, "bj%d" % bj], writes=["PT%d" % p])
                    if r >= 0:
                        tk.op("dve", lambda e, p=p, r=r: e.tensor_tensor(
                            out=PT[p][:], in0=PT[p][:], in1=amask[:, r, :], op=ALU.mult),
                            reads=["PT%d" % p, "c5"], writes=["PT%d" % p])

                def st_pv(n):
                    j, i = blocks[n]
                    r = i - 4 * j
                    p = n % 4
                    for u in range(4):
                        if r > u:
                            continue
                        last = 4 * j + u
                        tk.op("pe", lambda e, p=p, u=u, i=i, last=last: e.matmul(
                            psum[4 + u][:, 0:130], lhsT=PT[p][:, u * 128:(u + 1) * 128], rhs=VA[:, i, :],
                            start=(i == 0), stop=(i == last)),
                            reads=["PT%d" % p, "VA"], writes=["psb%d" % (4 + u)], inc=(u == 3))
                    if i == 4 * j + 3:
                        epilogue(j)

                def epilogue(j):
                    for u in range(4):
                        o = oc[0] % 2
                        oc[0] += 1
                        pu = psum[4 + u]
                        tk.op("dve", lambda e, pu=pu: e.reciprocal(out=sm[:, 0:1], in_=pu[:, 128:129]),
                              reads=["psb%d" % (4 + u)], writes=["sm0"])
                        tk.op("dve", lambda e, pu=pu, o=o: e.tensor_scalar(
                            out=ot[o][:], in0=pu[:, 0:128], scalar1=sm[:, 0:1], scalar2=None, op0=ALU.mult),
                            reads=["psb%d" % (4 + u), "sm0"], writes=["ot%d" % o])
                        tk.op("act", lambda e, o=o: e.activation(out=oj[:], in_=ot[o][:], func=AF.Square,
                                                                 accum_out=sm[:, 1:2]),
                              reads=["ot%d" % o], writes=["oj", "sm1"])
                        tk.op("act", lambda e: e.activation(out=sm[:, 2:3], in_=sm[:, 1:2], func=AF.Ln,
                                                            scale=1.0 / 128, bias=epsc[:]),
                              reads=["sm1", "epsc"], writes=["sm2"])
                        tk.op("act", lambda e: e.activation(out=sm[:, 3:4], in_=sm[:, 2:3], func=AF.Exp, scale=-0.5),
                              reads=["sm2"], writes=["sm3"])
                        tk.op("dve", lambda e, o=o, hh=hh: e.scalar_tensor_tensor(
                            out=ob[o][:], in0=ot[o][:], scalar=sm[:, 3:4], in1=hnw_s[:, hh * 128:(hh + 1) * 128],
                            op0=ALU.mult, op1=ALU.mult),
                            reads=["ot%d" % o, "sm3", "c9"], writes=["ob%d" % o])
                        t0 = j * 512 + u * 128
                        tk.dma("pool", mix_dst(t0, hh * 128, (hh + 1) * 128), ob[o][:], reads=["ob%d" % o],
                               key="ob%d" % o)

                load_q(0)
                make_bias(0)
                for n in range(NBK + LOOK):
                    if n < NBK:
                        st_qk(n)
                    if n - LOOK >= 0:
                        st_ex(n - LOOK)
                        st_pv(n - LOOK)
                tk.barrier()

        NCH = NT
        with ExitStack() as es3:
            zq = [sb("zq%d" % i, [128, 515], F32, es3) for i in range(2)]
            zk = [sb("zk%d" % i, [128, 515], F32, es3) for i in range(2)]
            acc = sb("acc", [128, 512], F32, es3)
            ex = sb("ex", [128, 512], F32, es3)
            qT = [sb("qT%d" % i, [128, 512], BF16, es3) for i in range(2)]
            kT = [sb("kT%d" % i, [128, 512], BF16, es3) for i in range(2)]
            mvo = [sb("mvo%d" % i, [128, 512], F32, es3) for i in range(2)]
            vt = [sb("vt%d" % i, [128, 258], BF16, es3) for i in range(2)]
            ktok = [sb("ktok%d" % i, [128, 128], BF16, es3) for i in range(2)]
            smt = [sb("smt%d" % i, [128, 128], BF16, es3) for i in range(2)]
            Cf = sb("Cf", [128, 258], F32, es3)
            Cb = [sb("Cb%d" % i, [128, 258], BF16, es3) for i in range(2)]
            lf = sb("mlf", [128, NCH], F32, es3)
            tmpm = sb("tmpm", [128, NCH], F32, es3)
            bcs = sb("bcs", [128, NCH], F32, es3)
            eb = sb("eb", [128, NCH], F32, es3)
            ek = sb("ek", [128, NCH], F32, es3)
            ebl = sb("ebl", [128, NCH], F32, es3)
            hsm = sb("hsm", [128, 8], F32, es3)
            hv = [sb("hv%d" % i, [128, 256], F32, es3) for i in range(2)]
            sg = sb("sg", [128, 256], F32, es3)
            hj = sb("hj", [128, 256], F32, es3)
            hb = [sb("hb%d" % i, [128, 256], BF16, es3) for i in range(2)]
            if 'M' in PH or os.environ.get('MLPRE'):
                _lim = int(os.environ.get('MLPRE', '99'))
                _real_op = tk.op
                _cnt = [0]
                def _lop(*a, **k):
                    _cnt[0] += 1
                    if _cnt[0] <= _lim:
                        return _real_op(*a, **k)
                tk.op = _lop
                tk.op("act", lambda e: e.activation(out=tmpm[:], in_=gates[:, :, 3], func=AF.Exp, scale=-1.0,
                                                    bias=ngb[:, 3:4]), reads=["gates", "ngb"], writes=["tmpm"])
                tk.op("act", lambda e: e.activation(out=lf[:], in_=tmpm[:], func=AF.Ln, scale=1.0, bias=onec[:]),
                      reads=["tmpm", "onec"], writes=["mlf"])
                tk.op("dve", lambda e: e.tensor_scalar(out=lf[:], in0=lf[:], scalar1=-1.0, scalar2=None, op0=ALU.mult),
                      reads=["mlf"], writes=["mlf"])
                for c0 in range(0, NCH, 32):
                    c1 = min(NCH, c0 + 32)
                    tk.op("pe", lambda e, c0=c0, c1=c1: e.matmul(psum[0][:, c0:c1], lhsT=triu[:], rhs=lf[:, c0:c1],
                                                                 start=True, stop=True),
                          reads=["mlf", "c2"], writes=["psb0"])
                    tk.op("pe", lambda e, c0=c0, c1=c1: e.matmul(psum[1][:, c0:c1], lhsT=ones_f[:], rhs=lf[:, c0:c1],
                                                                 start=True, stop=True),
                          reads=["mlf", "c3"], writes=["psb1"])
                tk.op("act", lambda e: e.activation(out=eb[:], in_=psum[0][:, 0:NCH], func=AF.Exp),
                      reads=["psb0"], writes=["eb"])
                tk.op("act", lambda e: e.activation(out=ebl[:], in_=psum[1][:, 0:NCH], func=AF.Exp),
                      reads=["psb1"], writes=["ebl"])
                tk.op("act", lambda e: e.copy(out=tmpm[:], in_=psum[0][:, 0:NCH]), reads=["psb0"], writes=["tmpm"])
                tk.op("act", lambda e: e.copy(out=bcs[:], in_=gates[:, :, 2]), reads=["gates"], writes=["bcs"])
                tk.op("dve", lambda e: e.tensor_tensor(out=bcs[:], in0=bcs[:], in1=tmpm[:], op=ALU.subtract),
                      reads=["bcs", "tmpm"], writes=["bcs"])
                tk.op("act", lambda e: e.activation(out=ek[:], in_=bcs[:], func=AF.Exp, bias=gb[:, 2:3], scale=1.0),
                      reads=["bcs", "c7"], writes=["ek"])
            if 'M' in PH or os.environ.get('MLPRE'):
                tk.op = _real_op
            tk.op("dve", lambda e: e.memset(Cf[:], 0.0), writes=["Cf"])
            tk.op("dve", lambda e: e.memset(Cb[0][:], 0.0), writes=["Cb0"])
            for i in range(2):
                tk.op("dve", lambda e, i=i: e.memset(zq[i][:, 0:3], 0.0), writes=["zq%d" % i])
                tk.op("dve", lambda e, i=i: e.memset(zk[i][:, 0:3], 0.0), writes=["zk%d" % i])
                tk.op("dve", lambda e, i=i: e.memset(vt[i][:], 0.0), writes=["vt%d" % i])
            def ml_x(c, tk):
                jb, cc = c // 4, c % 4
                s = jb % 2
                cs = c % 2
                ob_ = 4 if cs == 0 else 6
                csl = slice(cc * 128, (cc + 1) * 128)
                tk.dma("sp", mvo[cs][:], mvo_d[c * 128:(c + 1) * 128, :], writes=["mvo%d" % cs])
                pkt = psum[2][:, :].bitcast(BF16)
                tk.op("pe", lambda e, s=s, csl=csl, pkt=pkt: e.transpose(out=pkt[:, 0:128], in_=kT[s][:, csl],
                                                                         identity=ident_bf[:]),
                      reads=["kT%d" % s, "c0"], writes=["psb2"])
                tk.op("act", lambda e, cs=cs, pkt=pkt: e.activation(out=ktok[cs][:], in_=pkt[:, 0:128],
                                                                    func=AF.Copy, scale=SC),
                      reads=["psb2"], writes=["ktok%d" % cs])
                tk.op("dve", lambda e, cs=cs, c=c: e.tensor_scalar(
                    out=vt[cs][:, 0:256], in0=mvo[cs][:, 0:256], scalar1=ek[:, c:c + 1], scalar2=None, op0=ALU.mult),
                    reads=["mvo%d" % cs, "ek"], writes=["vt%d" % cs])
                tk.op("dve", lambda e, cs=cs, c=c: e.tensor_copy(out=vt[cs][:, 256:257], in_=ek[:, c:c + 1]),
                      reads=["ek"], writes=["vt%d" % cs])
                tk.op("pe", lambda e, s=s, csl=csl: e.matmul(psum[3][:, 0:128], lhsT=kT[s][:, csl], rhs=qT[s][:, csl],
                                                             start=True, stop=True),
                      reads=["kT%d" % s, "qT%d" % s], writes=["psb3"])
                tk.op("dve", lambda e, cs=cs: e.tensor_tensor(out=smt[cs][:], in0=psum[3][:, 0:128], in1=mmask[:],
                                                              op=ALU.mult),
                      reads=["psb3", "c6"], writes=["smt%d" % cs])
                tk.op("pe", lambda e, cs=cs: e.matmul(psum[ob_][:, 0:258], lhsT=smt[cs][:], rhs=vt[cs][:],
                                                      start=True, stop=False),
                      reads=["smt%d" % cs, "vt%d" % cs], writes=["psb%d" % ob_])
                tk.op("pe", lambda e, s=s, csl=csl, cs=cs: e.matmul(psum[ob_][:, 0:258], lhsT=qT[s][:, csl],
                                                                    rhs=Cb[cs][:], start=False, stop=True),
                      reads=["qT%d" % s, "Cb%d" % cs], writes=["psb%d" % ob_])
                tk.op("pe", lambda e, cs=cs: e.matmul(psum[5][:, 0:258], lhsT=ktok[cs][:], rhs=vt[cs][:],
                                                      start=True, stop=True),
                      reads=["ktok%d" % cs, "vt%d" % cs], writes=["psb5"])
                tk.op("dve", lambda e: e.tensor_tensor(out=Cf[:], in0=psum[5][:, 0:258], in1=Cf[:], op=ALU.add),
                      reads=["psb5", "Cf"], writes=["Cf"])
                tk.op("dve", lambda e, c=c: e.tensor_scalar(out=Cf[:], in0=Cf[:], scalar1=ebl[:, c:c + 1],
                                                            scalar2=None, op0=ALU.mult),
                      reads=["Cf", "ebl"], writes=["Cf"])
                tk.op("act", lambda e, cs=cs: e.copy(out=Cb[1 - cs][:], in_=Cf[:]), reads=["Cf"],
                      writes=["Cb%d" % (1 - cs)])


            def ml_y(c, tk):
                jb, cc = c // 4, c % 4
                s = jb % 2
                cs = c % 2
                ob_ = 4 if cs == 0 else 6
                csl = slice(cc * 128, (cc + 1) * 128)
                tk.op("act", lambda e, c=c: e.activation(out=hsm[:, 6:7], in_=psum[ob_][:, 256:257], func=AF.Abs,
                                                         scale=eb[:, c:c + 1]),
                      reads=["psb%d" % ob_, "eb"], writes=["hsm6"])
                tk.op("dve", lambda e: e.tensor_scalar(out=hsm[:, 0:1], in0=hsm[:, 6:7], scalar1=1.0, scalar2=None,
                                                       op0=ALU.max),
                      reads=["hsm6"], writes=["hsm0"])
                tk.op("dve", lambda e: e.reciprocal(out=hsm[:, 1:2], in_=hsm[:, 0:1]), reads=["hsm0"], writes=["hsm1"])
                tk.op("dve", lambda e, c=c: e.tensor_tensor(out=hsm[:, 2:3], in0=hsm[:, 1:2], in1=eb[:, c:c + 1],
                                                            op=ALU.mult), reads=["hsm1", "eb"], writes=["hsm2"])
                tk.op("act", lambda e, cs=cs: e.activation(out=sg[:], in_=mvo[cs][:, 256:512], func=AF.Exp, scale=-1.0),
                      reads=["mvo%d" % cs], writes=["sg"])
                tk.op("dve", lambda e: e.tensor_scalar(out=sg[:], in0=sg[:], scalar1=1.0, scalar2=None, op0=ALU.add),
                      reads=["sg"], writes=["sg"])
                tk.op("dve", lambda e: e.reciprocal(out=sg[:], in_=sg[:]), reads=["sg"], writes=["sg"])
                tk.op("dve", lambda e, cs=cs: e.scalar_tensor_tensor(
                    out=hv[cs][:], in0=psum[ob_][:, 0:256], scalar=hsm[:, 2:3], in1=sg[:], op0=ALU.mult, op1=ALU.mult),
                    reads=["psb%d" % ob_, "hsm2", "sg"], writes=["hv%d" % cs])
                tk.op("act", lambda e, cs=cs: e.activation(out=hj[:], in_=hv[cs][:], func=AF.Square,
                                                           accum_out=hsm[:, 3:4]),
                      reads=["hv%d" % cs], writes=["hj", "hsm3"])
                tk.op("act", lambda e: e.activation(out=hsm[:, 4:5], in_=hsm[:, 3:4], func=AF.Ln, scale=1.0 / 256,
                                                    bias=epsc[:]), reads=["hsm3", "epsc"], writes=["hsm4"])
                tk.op("act", lambda e: e.activation(out=hsm[:, 5:6], in_=hsm[:, 4:5], func=AF.Exp, scale=-0.5),
                      reads=["hsm4"], writes=["hsm5"])
                tk.op("dve", lambda e, cs=cs: e.scalar_tensor_tensor(
                    out=hb[cs][:], in0=hv[cs][:], scalar=hsm[:, 5:6], in1=hnw_s[:, 256:512], op0=ALU.mult,
                    op1=ALU.mult), reads=["hv%d" % cs, "hsm5", "c9"], writes=["hb%d" % cs])
                tk.dma("pool", mix_dst(c * 128, 256, 512), hb[cs][:], reads=["hb%d" % cs], writes=["ml%d" % c],
                       key="hb%d" % cs)


            for jb in (range(NB) if 'M' in PH else []):
                s = jb % 2
                for (z, zn, zd, wo, bo, dstT, dn) in [(zq, "zq", mq_d, 0, 8, qT, "qT"), (zk, "zk", mk_d, 4, 9, kT, "kT")]:
                    tk.dma("sp", z[s][:, 3:515], zd[:, jb * 512:(jb + 1) * 512], writes=["%s%d" % (zn, s)])
                    if jb > 0:
                        tk.op("act", lambda e, z=z, s=s: e.copy(out=z[s][:, 0:3], in_=z[1 - s][:, 512:515]),
                              reads=["%s%d" % (zn, 1 - s)], writes=["%s%d" % (zn, s)])
                    tk.op("dve", lambda e, z=z, s=s, wo=wo, bo=bo: e.tensor_scalar(
                        out=acc[:], in0=z[s][:, 0:512], scalar1=cvp[:, wo:wo + 1], scalar2=cvp[:, bo:bo + 1],
                        op0=ALU.mult, op1=ALU.add), reads=["%s%d" % (zn, s), "c8"], writes=["acc"])
                    for jj in range(1, 4):
                        tk.op("dve", lambda e, z=z, s=s, wo=wo, jj=jj: e.scalar_tensor_tensor(
                            out=acc[:], in0=z[s][:, jj:jj + 512], scalar=cvp[:, wo + jj:wo + jj + 1], in1=acc[:],
                            op0=ALU.mult, op1=ALU.add), reads=["%s%d" % (zn, s), "c8", "acc"], writes=["acc"])
                    tk.op("act", lambda e: e.activation(out=ex[:], in_=acc[:], func=AF.Exp, scale=-1.0),
                          reads=["acc"], writes=["ex"])
                    tk.op("dve", lambda e: e.tensor_scalar(out=ex[:], in0=ex[:], scalar1=1.0, scalar2=None, op0=ALU.add),
                          reads=["ex"], writes=["ex"])
                    tk.op("dve", lambda e: e.reciprocal(out=ex[:], in_=ex[:]), reads=["ex"], writes=["ex"])
                    tk.op("dve", lambda e, dstT=dstT, s=s: e.tensor_tensor(out=dstT[s][:], in0=acc[:], in1=ex[:],
                                                                           op=ALU.mult),
                          reads=["acc", "ex"], writes=["%s%d" % (dn, s)])
                for cc in range(4):
                    c = jb * 4 + cc
                    qx = OpQueue()
                    ml_x(c, qx)
                    qs_ = [qx]
                    if c > 0:
                        qy = OpQueue()
                        ml_y(c - 1, qy)
                        qs_.append(qy)
                    emit_interleaved(tk, qs_)
                    if c > 0 and (c - 1) % 4 == 3 and on_block_done is not None:
                        on_block_done((c - 1) // 4)
            if 'M' in PH:
                ml_y(NCH - 1, tk)
                if on_block_done is not None:
                    on_block_done((NCH - 1) // 4)
            tk.barrier()
        if own_tk:
            tk.final_wait("sp")
    print("phase A instructions:", tk.ninst)
    return nc


def host_inputs_a(inp, S):
    c = host_consts()
    w_in = np.asarray(inp["w_in"][0])
    maps = []
    for core in range(8):
        b, g = core // 4, core % 4
        h0, h1 = 2 * g, 2 * g + 1
        cols = []
        for base in (0, 1024, 2048):
            cols += list(range(base + h0 * 128, base + h0 * 128 + 128)) + list(range(base + h1 * 128, base + h1 * 128 + 128))
        cols += list(range(3080 + g * 128, 3080 + g * 128 + 128))
        cols += list(range(3592 + g * 128, 3592 + g * 128 + 128))
        cols += list(range(4104 + g * 256, 4104 + g * 256 + 256))
        cols += list(range(5136 + g * 256, 5136 + g * 256 + 256))
        cols += [3072 + h0, 3072 + h1, 5128 + g, 5132 + g]
        wcs = np.ascontiguousarray(w_in[:, cols])
        gbv = np.array([inp["fox_f_bias"][0][h0], inp["fox_f_bias"][0][h1], inp["mlstm_i_bias"][0][g],
                        inp["mlstm_f_bias"][0][g]], np.float32)
        cw = np.asarray(inp["mlstm_conv_w"][0])
        cbv = np.asarray(inp["mlstm_conv_b"][0])
        convp = np.concatenate([cw[:, g * 128:(g + 1) * 128].T, cw[:, 512 + g * 128:512 + (g + 1) * 128].T,
                                cbv[g * 128:(g + 1) * 128][:, None], cbv[512 + g * 128:512 + (g + 1) * 128][:, None]],
                               axis=1).astype(np.float32)
        hn = np.concatenate([np.asarray(inp["fox_out_norm_w"][0])[h0 * 128:(h1 + 1) * 128],
                             np.asarray(inp["mlstm_out_norm_w"][0])[g * 256:(g + 1) * 256]])
        m = {"x": np.ascontiguousarray(np.asarray(inp["x"])[b, :int(os.environ.get("XROWS", S))]),
             "wc": wcs,
             "n1w": np.ascontiguousarray(np.asarray(inp["norm1_w"][0]).reshape(KC, 128).T),
             "gbias": np.ascontiguousarray(np.broadcast_to(gbv[None, :], (128, 4))),
             "convp": np.ascontiguousarray(convp),
             "hnw": np.ascontiguousarray(np.broadcast_to(hn[None, :], (128, 512))).astype(np.float32)}
        m.update(c)
        maps.append(m)
    return maps


import os


def host_consts_b():
    c = {}
    c["ident_bf"] = np.eye(128, dtype=np.float32).astype(ml_dtypes.bfloat16)
    c["ident_f"] = np.eye(128, dtype=np.float32)
    c["iota_row"] = np.ascontiguousarray(np.broadcast_to(np.arange(128, dtype=np.float32)[None, :], (128, 128)))
    c["thr16"] = np.ascontiguousarray(np.broadcast_to((16.0 * np.arange(16, dtype=np.float32))[None, :], (128, 16)))
    c["iota16"] = np.ascontiguousarray(np.broadcast_to(np.arange(16, dtype=np.float32)[None, :], (128, 16)))
    return c


def build_phase_b(nc, TC, NJ=128, tk=None, gath=None, pfx="", pre=None):
    NTT = TC // 128
    PB = min(256, TC)
    NBLK = TC // PB
    TPB = PB // 128
    own_tk = tk is None
    if own_tk:
        tk = Trk(nc)
    _dt = nc.dram_tensor

    def dt(name, *a, **k):
        return _dt(pfx + name, *a, **k)
    x = dt("x", [TC, D], F32, kind="ExternalInput").ap()
    if gath is None:
        mixed = dt("mixed", [TC, D], BF16, kind="ExternalInput").ap()
    else:
        wsel_d = dt("wsel", [128, 8], F32, kind="ExternalInput").ap()
    wout = dt("wout", [D, D], F32, kind="ExternalInput").ap()
    wq = dt("wq", [D, D], F32, kind="ExternalInput").ap()
    n2w = dt("n2w", [128, D], F32, kind="ExternalInput").ap()
    fnw = dt("fnw", [128, D], F32, kind="ExternalInput").ap()
    keysT = dt("keysT", [128, 2, 128], F32, kind="ExternalInput").ap()
    if pre is None:
        uh = dt("uh", [128, 128, D], F32, kind="ExternalInput").ap()
        vh = dt("vh", [128, 128, D], F32, kind="ExternalInput").ap()
    ident_bf_d = dt("ident_bf", [128, 128], BF16, kind="ExternalInput").ap()
    ident_f_d = dt("ident_f", [128, 128], F32, kind="ExternalInput").ap()
    iota_row_d = dt("iota_row", [128, 128], F32, kind="ExternalInput").ap()
    thr16_d = dt("thr16", [128, 16], F32, kind="ExternalInput").ap()
    iota16_d = dt("iota16", [128, 16], F32, kind="ExternalInput").ap()
    out = dt("out", [TC, D], F32, kind="ExternalOutput").ap()
    if pre is None:
        u16 = dt("u16", [128, 128, D], BF16, kind="Internal").ap()
        v16 = dt("v16", [128, 128, D], BF16, kind="Internal").ap()
    else:
        u16, v16 = pre
    x1_d = dt("x1_d", [TC, D], F32, kind="Internal").ap()
    h2T_d = dt("h2T_d", [128, KC, TC], BF16, kind="Internal").ap()
    slot_d = dt("slot_d", [128, 3, TC], F32, kind="Internal").ap()

    with ExitStack() as es0:
        def sb(name, shape, dtype, es=es0):
            return es.enter_context(nc.sbuf_tensor(pfx + name, shape, dtype))
        ident_bf = sb("ident_bf_s", [128, 128], BF16)
        ident_f = sb("ident_f_s", [128, 128], F32)
        iota_row = sb("iota_row_s", [128, 128], F32)
        thr16 = sb("thr16_s", [128, 16], F32)
        iota16 = sb("iota16_s", [128, 16], F32)
        epsc = sb("epsc", [128, 1], F32)
        psum = [es0.enter_context(nc.psum_tensor(pfx + "ps%d" % i, [128, 512], F32)) for i in range(8)]
        for i, (s_, d_) in enumerate([(ident_bf, ident_bf_d), (ident_f, ident_f_d), (iota_row, iota_row_d),
                                      (thr16, thr16_d), (iota16, iota16_d)]):
            tk.dma("sp", s_[:], d_, writes=["c%d" % i], key="const")
        for j in (range(NJ) if pre is None else []):
            tk.dma("pool", u16[j], uh[j], writes=["u16"], key="cvt")
            tk.dma("pool", v16[j], vh[j], writes=["v16"], key="cvt")
        tk.op("dve", lambda e: e.memset(epsc[:], EPS), writes=["epsc"])

        def rms_rstd(src_ap, srckey, ss, junk):
            tk.op("act", lambda e: e.activation(out=junk[:], in_=src_ap, func=AF.Square, accum_out=ss[:, 0:1]),
                  reads=[srckey], writes=["junk", "ss0"])
            tk.op("act", lambda e: e.activation(out=ss[:, 1:2], in_=ss[:, 0:1], func=AF.Ln, scale=1.0 / D, bias=epsc[:]),
                  reads=["ss0", "epsc"], writes=["ss1"])
            tk.op("act", lambda e: e.activation(out=ss[:, 2:3], in_=ss[:, 1:2], func=AF.Exp, scale=-0.5),
                  reads=["ss1"], writes=["ss2"])

        def transpose16(src, srckey, dst, dstkey, banks):
            for half in range(2):
                bk = banks[half]
                pst = psum[bk][:, :].bitcast(BF16)
                for k8 in range(8):
                    kc = half * 8 + k8
                    tk.op("pe", lambda e, kc=kc, k8=k8, pst=pst: e.transpose(
                        out=pst[:, k8 * 128:(k8 + 1) * 128], in_=src[:, kc * 128:(kc + 1) * 128], identity=ident_bf[:]),
                        reads=[srckey, "c0"], writes=["psb%d" % bk], inc=(k8 == 7))
                srcv = pst.rearrange("p (k t) -> p k t", k=8)
                dstv = dst[:, half * 8:(half + 1) * 8, :]
                if half == 0:
                    tk.op("act", lambda e, srcv=srcv, dstv=dstv: e.copy(out=dstv, in_=srcv),
                          reads=["psb%d" % bk], writes=[dstkey])
                else:
                    tk.op("dve", lambda e, srcv=srcv, dstv=dstv: e.tensor_copy(out=dstv, in_=srcv),
                          reads=["psb%d" % bk], writes=[dstkey])

        with ExitStack() as es1:
            Wo = sb("Wo", [128, KC, D], BF16, es1)
            n2b = sb("n2b", [128, D], F32, es1)
            mx = [sb("mx%d" % i, [128, D], BF16, es1) for i in range(2)]
            xt = [sb("xt%d" % i, [128, D], F32, es1) for i in range(2)]
            x1 = [sb("x1_%d" % i, [128, D], F32, es1) for i in range(2)]
            mT = sb("mT", [128, KC, 128], BF16, es1)
            h2 = sb("h2", [128, D], BF16, es1)
            h2t = [sb("h2t%d" % i, [128, KC, 128], BF16, es1) for i in range(2)]
            junk = sb("junk", [128, D], BF16, es1)
            ss = sb("ss", [128, 4], F32, es1)
            for kc in range(KC):
                tk.dma("pool", Wo[:, kc, :], wout[kc * 128:(kc + 1) * 128, :], writes=["Wo"], key="wload")
            tk.dma("sp", n2b[:], n2w, writes=["n2b"])
            ccnt = [0]
            if gath is not None:
                cand = [sb("cand%d" % i, [128, D], BF16, es1) for i in range(3)]
                wsel = sb("wsel_s", [128, 8], F32, es1)
                tk.dma("sp", wsel[:], wsel_d, writes=["wsel"])
            for ti in range(NTT):
                s = ti % 2
                rows = slice(ti * 128, (ti + 1) * 128)
                if gath is None:
                    tk.dma("sp", mx[s][:], mixed[rows, :], writes=["mx%d" % s])
                else:
                    bb_, lt = ti // (NTT // 2), ti % (NTT // 2)
                    for dp in range(8):
                        cs_ = ccnt[0] % 3
                        ccnt[0] += 1
                        gt_ = dp * (NTT // 2) + lt
                        kq = gt_ // 4
                        src = gath[kq].ap().rearrange("(r t) c -> r t c", r=8)[bb_ * 4:(bb_ + 1) * 4,
                                                                              (gt_ % 4) * 128:(gt_ % 4 + 1) * 128, :]
                        tk.dma("sp", cand[cs_][:].rearrange("p (g c) -> p g c", g=4), src.rearrange("g t c -> t g c"),
                               reads=["gath%d" % kq], writes=["cand%d" % cs_])
                        if dp == 0:
                            tk.op("dve", lambda e, cs_=cs_, s=s: e.tensor_scalar(
                                out=mx[s][:], in0=cand[cs_][:], scalar1=wsel[:, 0:1], scalar2=None, op0=ALU.mult),
                                reads=["cand%d" % cs_, "wsel"], writes=["mx%d" % s])
                        else:
                            tk.op("dve", lambda e, cs_=cs_, s=s, dp=dp: e.scalar_tensor_tensor(
                                out=mx[s][:], in0=cand[cs_][:], scalar=wsel[:, dp:dp + 1], in1=mx[s][:], op0=ALU.mult,
                                op1=ALU.add), reads=["cand%d" % cs_, "wsel", "mx%d" % s], writes=["mx%d" % s])
                tk.dma("sp", xt[s][:], x[rows, :], writes=["xt%d" % s])
                transpose16(mx[s], "mx%d" % s, mT, "mT", (0, 1))
                for cg in range(4):
                    b = 2 + cg
                    for kc in range(KC):
                        tk.op("pe", lambda e, kc=kc, b=b, cg=cg: e.matmul(
                            psum[b][:, :], lhsT=mT[:, kc, :], rhs=Wo[:, kc, cg * 512:(cg + 1) * 512],
                            start=(kc == 0), stop=(kc == KC - 1)), reads=["mT", "Wo"], writes=["psb%d" % b], inc=(kc == KC - 1))
                    tk.op("dve", lambda e, b=b, cg=cg, s=s: e.tensor_tensor(
                        out=x1[s][:, cg * 512:(cg + 1) * 512], in0=psum[b][:, :], in1=xt[s][:, cg * 512:(cg + 1) * 512],
                        op=ALU.add), reads=["psb%d" % b, "xt%d" % s], writes=["x1_%d" % s])
                tk.dma("pool", x1_d[rows, :], x1[s][:], reads=["x1_%d" % s], key="x1st%d" % s)
                rms_rstd(x1[s][:], "x1_%d" % s, ss, junk)
                tk.op("dve", lambda e, s=s: e.scalar_tensor_tensor(out=h2[:], in0=x1[s][:], scalar=ss[:, 2:3], in1=n2b[:],
                                                                   op0=ALU.mult, op1=ALU.mult),
                      reads=["x1_%d" % s, "ss2", "n2b"], writes=["h2"])
                transpose16(h2, "h2", h2t[s], "h2t%d" % s, (6, 7))
                tk.dma("pool", h2T_d[:, :, rows], h2t[s][:], reads=["h2t%d" % s], key="h2st%d" % s)
            tk.barrier()

        if os.environ.get('BSTOP') == '1':
            tk.final_wait('sp')
            return nc
        with ExitStack() as es2:
            Wq = sb("Wq", [128, KC, D], BF16, es2)
            kT = sb("kTs", [128, 2, 128], BF16, es2)
            h2t = [sb("h2tb%d" % i, [128, KC, 128], BF16, es2) for i in range(2)]
            qpT = [sb("qpT%d" % i, [128, 128], BF16, es2) for i in range(2)]
            sc_l = [sb("sc_%d" % i_, [128, 16, 128], F32, es2) for i_ in range(2)]
            tmp1_l = [sb("tmp1_%d" % i_, [128, 128], F32, es2) for i_ in range(2)]
            st_l = [sb("st_%d" % i_, [128, 16, 16], F32, es2) for i_ in range(2)]
            iu_l = [sb("iu_%d" % i_, [128, 16, 16], U32, es2) for i_ in range(2)]
            itf_l = [sb("itf_%d" % i_, [128, 16, 16], F32, es2) for i_ in range(2)]
            dd_l = [sb("dd_%d" % i_, [128, 16, 16], F32, es2) for i_ in range(2)]
            cand_l = [sb("cand_%d" % i_, [128, 8, 256], F32, es2) for i_ in range(2)]
            tmp2_l = [sb("tmp2_%d" % i_, [128, 256], F32, es2) for i_ in range(2)]
            cf_l = [sb("cf_%d" % i_, [128, 8, 16], F32, es2) for i_ in range(2)]
            pu_l = [sb("pu_%d" % i_, [128, 8, 16], U32, es2) for i_ in range(2)]
            posf_l = [sb("posf_%d" % i_, [128, 8, 16], F32, es2) for i_ in range(2)]
            cs_l = [sb("cs_%d" % i_, [128, 8, 16], F32, es2) for i_ in range(2)]
            zs_l = [sb("zs_%d" % i_, [128, 8], F32, es2) for i_ in range(2)]
            ge_l = [sb("ge_%d" % i_, [128, 8, 16, 16], F32, es2) for i_ in range(2)]
            prod_l = [sb("prod_%d" % i_, [128, 8, 16, 16], F32, es2) for i_ in range(2)]
            k1s_l = [sb("k1s_%d" % i_, [128, 8, 16], F32, es2) for i_ in range(2)]
            k2f_l = [sb("k2f_%d" % i_, [128, 8, 16], F32, es2) for i_ in range(2)]
            res_l = [sb("res_%d" % i_, [128, 3, 128], F32, es2) for i_ in range(2)]
            rst = [sb("rst%d" % i, [128, 3, 128], F32, es2) for i in range(2)]
            for kc in range(KC):
                tk.dma("pool", Wq[:, kc, :], wq[kc * 128:(kc + 1) * 128, :], writes=["Wq"], key="wload")
            tk.dma("pool", kT[:], keysT, writes=["kT"], key="wload")
            PRIV = ['sc', 'tmp1', 'st', 'iu', 'itf', 'dd', 'cand', 'tmp2', 'cf', 'pu', 'posf', 'cs', 'zs', 'ge', 'prod', 'k1s', 'k2f', 'res']

            def b2_tile(ti, tk):
                sc = sc_l[ti % 2]; tmp1 = tmp1_l[ti % 2]; st = st_l[ti % 2]; iu = iu_l[ti % 2]; itf = itf_l[ti % 2]; dd = dd_l[ti % 2]; cand = cand_l[ti % 2]; tmp2 = tmp2_l[ti % 2]; cf = cf_l[ti % 2]; pu = pu_l[ti % 2]; posf = posf_l[ti % 2]; cs = cs_l[ti % 2]; zs = zs_l[ti % 2]; ge = ge_l[ti % 2]; prod = prod_l[ti % 2]; k1s = k1s_l[ti % 2]; k2f = k2f_l[ti % 2]; res = res_l[ti % 2]
                st4 = st[:].rearrange("p (h q) k -> p h q k", q=2)
                itf4 = itf[:].rearrange("p (h q) k -> p h q k", q=2)
                dd4 = dd[:].rearrange("p (h q) k -> p h q k", q=2)
                s = ti % 2
                cols = slice(ti * 128, (ti + 1) * 128)
                tk.dma("sp", h2t[s][:], h2T_d[:, :, cols], writes=["h2tb%d" % s])
                for blk in range(16):
                    b = s
                    p = blk % 2
                    for kc in range(KC):
                        tk.op("pe", lambda e, kc=kc, b=b, blk=blk: e.matmul(
                            psum[b][:, 0:128], lhsT=Wq[:, kc, blk * 128:(blk + 1) * 128], rhs=h2t[s][:, kc, :],
                            start=(kc == 0), stop=(kc == KC - 1)), reads=["Wq", "h2tb%d" % s], writes=["psb%d" % b], inc=(kc == KC - 1))
                    tk.op("act", lambda e, b=b: e.copy(out=qpT[b][:], in_=psum[b][:, 0:128]),
                          reads=["psb%d" % b], writes=["qpT%d" % b])
                    sbk = 2 + 2 * s + (blk // 4) % 2
                    tk.op("pe", lambda e, b=b, p=p, sbk=sbk, blk=blk: e.matmul(
                        psum[sbk][:, (blk % 4) * 128:(blk % 4 + 1) * 128], lhsT=qpT[b][:], rhs=kT[:, p, :],
                        start=True, stop=True), reads=["qpT%d" % b, "kT"], writes=["psb%d" % sbk])
                    if blk % 4 == 3:
                        tk.op("dve", lambda e, sbk=sbk, blk=blk: e.tensor_copy(
                            out=sc[:, blk - 3:blk + 1, :], in_=psum[sbk][:, :].rearrange("p (a n) -> p a n", a=4)),
                            reads=["psb%d" % sbk], writes=["sc"])
                for blk in range(16):
                    tk.op("dve", lambda e, blk=blk: e.max(out=st[:, blk, 0:8], in_=sc[:, blk, :]),
                          reads=["sc"], writes=["st"])
                    tk.op("dve", lambda e, blk=blk: e.max_index(out=iu[:, blk, 0:8], in_max=st[:, blk, 0:8],
                                                                in_values=sc[:, blk, :]),
                          reads=["sc", "st"], writes=["iu"])
                    tk.op("dve", lambda e, blk=blk: e.match_replace(out=tmp1[:], in_to_replace=st[:, blk, 0:8],
                                                                    in_values=sc[:, blk, :], imm_value=-1e30),
                          reads=["sc", "st"], writes=["tmp1"])
                    tk.op("dve", lambda e, blk=blk: e.max(out=st[:, blk, 8:16], in_=tmp1[:]),
                          reads=["tmp1"], writes=["st"])
                    tk.op("dve", lambda e, blk=blk: e.max_index(out=iu[:, blk, 8:16], in_max=st[:, blk, 8:16],
                                                                in_values=tmp1[:]),
                          reads=["tmp1", "st"], writes=["iu"])
                tk.op("dve", lambda e: e.tensor_copy(out=itf[:], in_=iu[:]), reads=["iu"], writes=["itf"])
                tk.op("dve", lambda e: e.tensor_copy(out=dd[:, :, 0:1], in_=itf[:, :, 0:1]), reads=["itf"], writes=["dd"])
                tk.op("dve", lambda e: e.tensor_tensor(out=dd[:, :, 1:16], in0=itf[:, :, 1:16], in1=itf[:, :, 0:15],
                                                       op=ALU.subtract), reads=["itf"], writes=["dd"])
                a0 = st4[:, :, 0, :].unsqueeze(3).broadcast_to([128, 8, 16, 16])
                a1 = st4[:, :, 1, :].unsqueeze(2).broadcast_to([128, 8, 16, 16])
                cand4 = cand[:].rearrange("p h (a b) -> p h a b", a=16)
                tk.op("dve", lambda e: e.tensor_tensor(out=cand4, in0=a0, in1=a1, op=ALU.add), reads=["st"], writes=["cand"])
                for h in range(8):
                    tk.op("dve", lambda e, h=h: e.max(out=cf[:, h, 0:8], in_=cand[:, h, :]), reads=["cand"], writes=["cf"])
                    tk.op("dve", lambda e, h=h: e.max_index(out=pu[:, h, 0:8], in_max=cf[:, h, 0:8], in_values=cand[:, h, :]),
                          reads=["cand", "cf"], writes=["pu"])
                    tk.op("dve", lambda e, h=h: e.match_replace(out=tmp2[:], in_to_replace=cf[:, h, 0:8],
                                                                in_values=cand[:, h, :], imm_value=-1e30),
                          reads=["cand", "cf"], writes=["tmp2"])
                    tk.op("dve", lambda e, h=h: e.max(out=cf[:, h, 8:16], in_=tmp2[:]), reads=["tmp2"], writes=["cf"])
                    tk.op("dve", lambda e, h=h: e.max_index(out=pu[:, h, 8:16], in_max=cf[:, h, 8:16], in_values=tmp2[:]),
                          reads=["tmp2", "cf"], writes=["pu"])
                tk.op("dve", lambda e: e.tensor_copy(out=posf[:], in_=pu[:]), reads=["pu"], writes=["posf"])
                tk.op("dve", lambda e: e.tensor_tensor(out=cs[:], in0=cf[:], in1=cf[:, :, 0:1].broadcast_to([128, 8, 16]),
                                                       op=ALU.subtract), reads=["cf"], writes=["cs"])
                tk.op("act", lambda e: e.activation(out=cs[:], in_=cs[:], func=AF.Exp), reads=["cs"], writes=["cs"])
                tk.op("dve", lambda e: e.tensor_reduce(out=zs[:], in_=cs[:], axis=AX.X, op=ALU.add), reads=["cs"], writes=["zs"])
                tk.op("dve", lambda e: e.reciprocal(out=zs[:], in_=zs[:]), reads=["zs"], writes=["zs"])
                res_g = res[:, 2, :].rearrange("p (h k) -> p h k", h=8)
                tk.op("dve", lambda e: e.tensor_tensor(out=res_g, in0=cs[:], in1=zs[:].unsqueeze(2).broadcast_to([128, 8, 16]),
                                                       op=ALU.mult), reads=["cs", "zs"], writes=["res"])
                pos_b = posf[:].unsqueeze(3).broadcast_to([128, 8, 16, 16])
                thr_b = thr16[:].unsqueeze(1).unsqueeze(1).broadcast_to([128, 8, 16, 16])
                tk.op("dve", lambda e: e.tensor_tensor(out=ge[:], in0=pos_b, in1=thr_b, op=ALU.is_ge),
                      reads=["posf", "c3"], writes=["ge"])
                d1_b = dd4[:, :, 0, :].unsqueeze(2).broadcast_to([128, 8, 16, 16])
                tk.op("dve", lambda e: e.tensor_tensor(out=prod[:], in0=ge[:], in1=d1_b, op=ALU.mult),
                      reads=["ge", "dd"], writes=["prod"])
                res_i = res[:, 0, :].rearrange("p (h k) -> p h k", h=8)
                tk.op("dve", lambda e: e.tensor_reduce(out=res_i, in_=prod[:], axis=AX.X, op=ALU.add),
                      reads=["prod"], writes=["res"])
                tk.op("dve", lambda e: e.tensor_reduce(out=k1s[:], in_=ge[:], axis=AX.X, op=ALU.add), reads=["ge"], writes=["k1s"])
                tk.op("dve", lambda e: e.tensor_scalar(out=k1s[:], in0=k1s[:], scalar1=-16.0, scalar2=16.0, op0=ALU.mult,
                                                       op1=ALU.add), reads=["k1s"], writes=["k1s"])
                tk.op("dve", lambda e: e.tensor_tensor(out=k2f[:], in0=posf[:], in1=k1s[:], op=ALU.add),
                      reads=["posf", "k1s"], writes=["k2f"])
                k2_b = k2f[:].unsqueeze(3).broadcast_to([128, 8, 16, 16])
                io_b = iota16[:].unsqueeze(1).unsqueeze(1).broadcast_to([128, 8, 16, 16])
                tk.op("dve", lambda e: e.tensor_tensor(out=ge[:], in0=k2_b, in1=io_b, op=ALU.is_ge),
                      reads=["k2f", "c4"], writes=["ge"])
                d2_b = dd4[:, :, 1, :].unsqueeze(2).broadcast_to([128, 8, 16, 16])
                tk.op("dve", lambda e: e.tensor_tensor(out=prod[:], in0=ge[:], in1=d2_b, op=ALU.mult),
                      reads=["ge", "dd"], writes=["prod"])
                res_j = res[:, 1, :].rearrange("p (h k) -> p h k", h=8)
                tk.op("dve", lambda e: e.tensor_reduce(out=res_j, in_=prod[:], axis=AX.X, op=ALU.add),
                      reads=["prod"], writes=["res"])
                for q in range(3):
                    tk.op("pe", lambda e, q=q: e.transpose(out=psum[6 + s][:, q * 128:(q + 1) * 128], in_=res[:, q, :],
                                                           identity=ident_f[:]), reads=["res", "c1"], writes=["psb%d" % (6 + s)])
                tk.op("act", lambda e, s=s: e.copy(out=rst[s][:], in_=psum[6 + s][:, 0:384].rearrange("p (q t) -> p q t", q=3)),
                      reads=["psb%d" % (6 + s)], writes=["rst%d" % s])
                tk.dma("pool", slot_d[:, :, cols], rst[s][:], reads=["rst%d" % s], key="rst%d" % s)

            for t0_ in range(0, NTT, 2):
                qs_ = []
                for ti in range(t0_, min(NTT, t0_ + 2)):
                    q_ = OpQueue()
                    q_.suffix = "_%d" % (ti % 2)
                    q_.priv = set(PRIV)
                    b2_tile(ti, q_)
                    qs_.append(q_)
                emit_interleaved(tk, qs_)
            tk.barrier()

        if os.environ.get('BSTOP') == '2':
            tk.final_wait('sp')
            return nc
        with ExitStack() as es3:
            G = sb("G", [128, 128, PB], BF16, es3)
            ut = [sb("ut%d" % i, [128, KC, 128], BF16, es3) for i in range(7)]
            vt = [sb("vt%d" % i, [128, D], BF16, es3) for i in range(7)]
            h2b = [sb("h2b%d" % i, [128, KC, PB], BF16, es3) for i in range(2)]
            slots = [sb("slots%d" % i, [128, 3, PB], F32, es3) for i in range(2)]
            gl = [sb("gl%d" % i, [128, PB], BF16, es3) for i in range(2)]
            ohi = [sb("ohi%d" % i, [128, 128], BF16, es3) for i in range(4)]
            ohj = [sb("ohj%d" % i, [128, 128], BF16, es3) for i in range(4)]
            x1t = sb("x1t", [128, D], F32, es3)
            xo = [sb("xo%d" % i, [128, D], F32, es3) for i in range(2)]
            junk = sb("junk3", [128, D], BF16, es3)
            fnb = sb("fnb", [128, D], F32, es3)
            ss = sb("ss3", [128, 4], F32, es3)
            tk.dma("sp", fnb[:], fnw, writes=["fnb"])
            ucnt = [0]
            vcnt = [0]
            ocnt = [0]
            for blk in range(NBLK):
                bs = blk % 2
                cols = slice(blk * PB, (blk + 1) * PB)
                tk.dma("sp", slots[bs][:], slot_d[:, :, cols], writes=["slots%d" % bs])
                tk.dma("sp", h2b[bs][:], h2T_d[:, :, cols], writes=["h2b%d" % bs])
                for t in range(PB):
                    o = t % 4
                    tk.op("dve", lambda e, o=o, t=t, bs=bs: e.tensor_scalar(
                        out=ohi[o][:], in0=iota_row[:], scalar1=slots[bs][:, 0, t:t + 1], scalar2=slots[bs][:, 2, t:t + 1],
                        op0=ALU.is_equal, op1=ALU.mult), reads=["slots%d" % bs, "c2"], writes=["ohi%d" % o])
                    tk.op("dve", lambda e, o=o, t=t, bs=bs: e.tensor_scalar(
                        out=ohj[o][:], in0=iota_row[:], scalar1=slots[bs][:, 1, t:t + 1], scalar2=None,
                        op0=ALU.is_equal), reads=["slots%d" % bs, "c2"], writes=["ohj%d" % o])
                    gb_ = (t // 4) % 2
                    tk.op("pe", lambda e, o=o, gb_=gb_: e.matmul(psum[gb_][:, o * 128:(o + 1) * 128], lhsT=ohi[o][:],
                                                                 rhs=ohj[o][:], start=True, stop=True),
                          reads=["ohi%d" % o, "ohj%d" % o], writes=["psb%d" % gb_])
                    if o == 3:
                        t0 = t - 3
                        tk.op("act", lambda e, gb_=gb_, t0=t0: e.copy(
                            out=G[:, :, t0:t0 + 4], in_=psum[gb_][:, :].rearrange("p (t j) -> p j t", t=4)),
                            reads=["psb%d" % gb_], writes=["G"])
                for j in range(NJ):
                    us = ucnt[0] % 7
                    ucnt[0] += 1
                    tk.dma("sp", ut[us][:], u16[j].rearrange("p (k i) -> p k i", k=KC), writes=["ut%d" % us])
                    b = 2 + j % 2
                    for kc in range(KC):
                        tk.op("pe", lambda e, kc=kc, b=b, us=us, bs=bs: e.matmul(
                            psum[b][:, 0:PB], lhsT=ut[us][:, kc, :], rhs=h2b[bs][:, kc, :], start=(kc == 0),
                            stop=(kc == KC - 1)), reads=["ut%d" % us, "h2b%d" % bs], writes=["psb%d" % b], inc=(kc == KC - 1))
                    g = j % 2
                    tk.op("act", lambda e, b=b, g=g: e.activation(out=gl[g][:], in_=psum[b][:, 0:PB], func=AF.Gelu),
                          reads=["psb%d" % b], writes=["gl%d" % g])
                    tk.op("dve", lambda e, g=g, j=j: e.tensor_tensor(out=G[:, j, :], in0=gl[g][:], in1=G[:, j, :], op=ALU.mult),
                          reads=["gl%d" % g, "G"], writes=["G"])
                for j in range(NJ):
                    vs = vcnt[0] % 7
                    vcnt[0] += 1
                    tk.dma("sp", vt[vs][:], v16[j], writes=["vt%d" % vs])
                    for tt in range(TPB):
                        for cg in range(4):
                            b = tt * 4 + cg
                            tk.op("pe", lambda e, j=j, tt=tt, cg=cg, b=b, vs=vs: e.matmul(
                                psum[b][:, :], lhsT=G[:, j, tt * 128:(tt + 1) * 128], rhs=vt[vs][:, cg * 512:(cg + 1) * 512],
                                start=(j == 0), stop=(j == NJ - 1)), reads=["G", "vt%d" % vs], writes=["psb%d" % b])
                for tt in range(TPB):
                    rows = slice(blk * PB + tt * 128, blk * PB + (tt + 1) * 128)
                    o = ocnt[0] % 2
                    ocnt[0] += 1
                    tk.dma("sp", x1t[:], x1_d[rows, :], writes=["x1t"])
                    for cg in range(4):
                        b = tt * 4 + cg
                        tk.op("dve", lambda e, b=b, cg=cg, o=o: e.tensor_tensor(
                            out=xo[o][:, cg * 512:(cg + 1) * 512], in0=psum[b][:, :], in1=x1t[:, cg * 512:(cg + 1) * 512],
                            op=ALU.add), reads=["psb%d" % b, "x1t"], writes=["xo%d" % o])
                    rms_rstd(xo[o][:], "xo%d" % o, ss, junk)
                    tk.op("dve", lambda e, o=o: e.scalar_tensor_tensor(out=xo[o][:], in0=xo[o][:], scalar=ss[:, 2:3],
                                                                       in1=fnb[:], op0=ALU.mult, op1=ALU.mult),
                          reads=["xo%d" % o, "ss2", "fnb"], writes=["xo%d" % o])
                    tk.dma("pool", out[rows, :], xo[o][:], reads=["xo%d" % o], key="ost%d" % o)
            tk.barrier()
        tk.final_wait("sp")
    print("phase B instructions (cumulative):", tk.ninst)
    return nc


def host_inputs_b(inp, xflat, mixed_full, TCs):
    c = host_consts_b()
    U = np.asarray(inp["peer_u"][0])
    V = np.asarray(inp["peer_v"][0])
    uh = np.ascontiguousarray(U.reshape(128, 128, KC, 128).transpose(1, 3, 2, 0)).reshape(128, 128, D)
    vh = np.ascontiguousarray(V.reshape(128, 128, D).transpose(1, 0, 2))
    keys = np.asarray(inp["peer_keys"][0])
    keysT = np.ascontiguousarray(keys.transpose(2, 0, 1))
    wout = np.ascontiguousarray(np.asarray(inp["w_out"][0]))
    wq = np.ascontiguousarray(np.asarray(inp["peer_w_q"][0]))
    n2w = np.ascontiguousarray(np.broadcast_to(np.asarray(inp["norm2_w"][0])[None, :], (128, D)))
    fnw = np.ascontiguousarray(np.broadcast_to(np.asarray(inp["final_norm_w"])[None, :], (128, D)))
    maps = []
    for core in range(8):
        r0 = core * TCs
        m = {"x": np.ascontiguousarray(xflat[r0:r0 + TCs]),
             "mixed": np.ascontiguousarray(mixed_full[r0:r0 + TCs]),
             "wout": wout, "wq": wq, "n2w": n2w, "fnw": fnw, "keysT": keysT, "uh": uh, "vh": vh}
        m.update(c)
        maps.append(m)
    return maps


def build_fused(nc, S):
    TC = 2 * S // 8
    NK = S // 512
    tk = Trk(nc)
    mixloc = [nc.dram_tensor("mixloc%d" % k, [512, 512], BF16) for k in range(NK)]
    gath = [nc.dram_tensor("gath%d" % k, [8 * 512, 512], BF16) for k in range(NK)]
    uh = nc.dram_tensor("b_uh", [128, 128, D], F32, kind="ExternalInput").ap()
    vh = nc.dram_tensor("b_vh", [128, 128, D], F32, kind="ExternalInput").ap()
    u16 = nc.dram_tensor("b_u16", [128, 128, D], BF16, kind="Internal").ap()
    v16 = nc.dram_tensor("b_v16", [128, 128, D], BF16, kind="Internal").ap()
    tk.lazy_keys.add("cvt")
    for j in range(128):
        tk.dma("pool", u16[j], uh[j], writes=["u16"], key="cvt")
        tk.dma("pool", v16[j], vh[j], writes=["v16"], key="cvt")
    def gather_block(k):
        tk.coll(lambda g, k=k: g.collective_compute("AllGather", ALU.bypass, replica_groups=[list(range(8))],
                                                    ins=[mixloc[k].ap().opt()], outs=[gath[k].ap().opt()]),
                reads=["ml%d" % c for c in range(4 * k, 4 * k + 4)], writes=["gath%d" % k])

    build_phase_a(nc, S, tk=tk, mix_dst=lambda t0, c0, c1: mixloc[t0 // 512][t0 % 512:t0 % 512 + 128, c0:c1],
                  on_block_done=gather_block)
    tk.lazy_keys.discard("cvt")
    build_phase_b(nc, TC, tk=tk, gath=gath, pfx="b_", pre=(u16, v16))
    return nc


def wout_perm():
    perm = []
    for g in range(4):
        perm += list(range(g * 256, (g + 1) * 256)) + list(range(1024 + g * 256, 1024 + (g + 1) * 256))
    return np.array(perm)


def host_inputs_fused(inp, S):
    maps = host_inputs_a(inp, S)
    c = host_consts_b()
    TC = 2 * S // 8
    H = S // 8
    U = np.asarray(inp["peer_u"][0])
    V = np.asarray(inp["peer_v"][0])
    uh = np.ascontiguousarray(U.reshape(128, 128, KC, 128).transpose(1, 3, 2, 0)).reshape(128, 128, D)
    vh = np.ascontiguousarray(V.reshape(128, 128, D).transpose(1, 0, 2))
    keysT = np.ascontiguousarray(np.asarray(inp["peer_keys"][0]).transpose(2, 0, 1))
    wout = np.ascontiguousarray(np.asarray(inp["w_out"][0])[wout_perm(), :])
    wq = np.ascontiguousarray(np.asarray(inp["peer_w_q"][0]))
    n2w = np.ascontiguousarray(np.broadcast_to(np.asarray(inp["norm2_w"][0])[None, :], (128, D)))
    fnw = np.ascontiguousarray(np.broadcast_to(np.asarray(inp["final_norm_w"])[None, :], (128, D)))
    x = np.asarray(inp["x"])
    for d in range(8):
        m = maps[d]
        m["b_x"] = np.ascontiguousarray(np.concatenate([x[0, d * H:(d + 1) * H], x[1, d * H:(d + 1) * H]], axis=0))
        ws = np.zeros((128, 8), np.float32)
        ws[:, d] = 1.0
        m["b_wsel"] = ws
        m.update({"b_wout": wout, "b_wq": wq, "b_n2w": n2w, "b_fnw": fnw, "b_keysT": keysT, "b_uh": uh, "b_vh": vh})
        for k, v in c.items():
            m["b_" + k] = v
    return maps


def gather_out(res, S):
    H = S // 8
    out = np.zeros((2, S, D), np.float32)
    for d in range(8):
        o = res.results[d]["b_out"]
        out[0, d * H:(d + 1) * H] = o[0:H]
        out[1, d * H:(d + 1) * H] = o[H:2 * H]
    return out


S_FULL = 16384


def kernel(**inputs):
    inp = {k: np.asarray(v) for k, v in inputs.items()}
    S = S_FULL
    nc = bass.Bass("TRN2", target_bir_lowering=False)
    build_fused(nc, S)
    maps = host_inputs_fused(inp, S)
    res = run_bass_kernel_spmd(nc, maps, core_ids=list(range(8)))
    return gather_out(res, S).astype(np.float32)
```

```python
import numpy as np
import ml_dtypes
from contextlib import ExitStack
import concourse.bass as bass
import concourse.mybir as mybir
from concourse.bass_utils import run_bass_kernel_spmd

F32 = mybir.dt.float32
BF16 = mybir.dt.bfloat16
I32 = mybir.dt.int32
U32 = mybir.dt.uint32
AF = mybir.ActivationFunctionType
ALU = mybir.AluOpType
AX = mybir.AxisListType

D = 2048
KC = 16
EPS = 1e-6


class Trk:
    def __init__(self, nc):
        self.nc = nc
        self.engs = {"pe": nc.tensor, "act": nc.scalar, "dve": nc.vector,
                     "pool": nc.gpsimd, "sp": nc.sync}
        self.sem = {}
        self.cnt = {}
        for e in ("pe", "act", "dve", "pool"):
            self.sem[e] = nc.alloc_semaphore(name="s_" + e)
            self.cnt[e] = 0
        self.dsem = {}
        self.waited = {}
        self.lastw = {}
        self.readers = {}
        self.ninst = 0
        self.lazy_keys = set()

    def _wait(self, eng, ev):
        sem, val, src, key = ev
        k = (eng, key)
        if self.waited.get(k, 0) >= val:
            return
        self.engs[eng].wait_ge(sem, val)
        self.waited[k] = val

    def _deps(self, eng, reads, writes):
        for b in reads:
            ev = self.lastw.get(b)
            if ev is not None and not (ev[2] == eng and eng == "pe"):
                self._wait(eng, ev)
        for b in writes:
            ev = self.lastw.get(b)
            if ev is not None and ev[2] != eng:
                self._wait(eng, ev)
            for ev in self.readers.get(b, ()):
                if ev[2] != eng:
                    self._wait(eng, ev)

    def _record(self, ev, reads, writes):
        for b in reads:
            self.readers.setdefault(b, []).append(ev)
        for b in writes:
            self.lastw[b] = ev
            self.readers[b] = []

    def op(self, eng, fn, reads=(), writes=(), inc=True):
        self._deps(eng, reads, writes)
        inst = fn(self.engs[eng])
        if inc:
            self.cnt[eng] += 1
            inst.then_inc(self.sem[eng], 1)
            ev = (self.sem[eng], self.cnt[eng], eng, eng)
        else:
            assert eng == "pe"
            ev = (self.sem[eng], self.cnt[eng] + 1, eng, eng)
        self._record(ev, reads, writes)
        self.ninst += 1
        return inst

    def dma(self, q, out, in_, reads=(), writes=(), key=None, **kw):
        if key is None:
            key = writes[0] if writes else reads[0]
        if key not in self.dsem:
            self.dsem[key] = [self.nc.alloc_semaphore(name="d%d" % len(self.dsem)), 0]
        self._deps(q, reads, writes)
        ds = self.dsem[key]
        inst = self.engs[q].dma_start(out=out, in_=in_, **kw)
        ds[1] += 16
        inst.then_inc(ds[0], 16)
        ev = (ds[0], ds[1], "dma", ("d", key))
        self._record(ev, reads, writes)
        self.ninst += 1
        return inst

    def coll(self, fn, reads=(), writes=()):
        if "cc" not in self.sem:
            self.sem["cc"] = self.nc.alloc_semaphore(name="s_cc")
            self.cnt["cc"] = 0
        self._deps("pool", reads, writes)
        inst = fn(self.engs["pool"])
        self.cnt["cc"] += 1
        inst.then_inc(self.sem["cc"])
        ev = (self.sem["cc"], self.cnt["cc"], "cc", "cc")
        self._record(ev, reads, writes)
        self.ninst += 1
        return inst

    def barrier(self):
        evs = [(self.sem[e], self.cnt[e], e, e) for e in self.sem if self.cnt[e] > 0]
        evs += [(v[0], v[1], "dma", ("d", k)) for k, v in self.dsem.items()
                if v[1] > 0 and k not in self.lazy_keys]
        for e in ("pe", "act", "dve", "pool", "sp"):
            for ev in evs:
                if ev[2] != e:
                    self._wait(e, ev)
        keep = {b: ev for b, ev in self.lastw.items() if ev[2] == "dma" and ev[3][1] in self.lazy_keys}
        self.lastw = keep
        self.readers = {}

    def final_wait(self, q="sp"):
        for k, v in self.dsem.items():
            if v[1] > 0:
                self._wait(q, (v[0], v[1], "dma", ("d", k)))


class OpQueue:
    def __init__(self):
        self.q = []
        self.suffix = ""
        self.priv = set()

    def _fix(self, k):
        for nm in ("reads", "writes"):
            if nm in k:
                k[nm] = [x + self.suffix if x in self.priv else x for x in k[nm]]
        return k

    def op(self, *a, **k):
        self.q.append(("op", a, self._fix(k)))

    def dma(self, *a, **k):
        self.q.append(("dma", a, self._fix(k)))


def emit_interleaved(tk, queues):
    n = max(len(q.q) for q in queues)
    for i in range(n):
        for q in queues:
            if i < len(q.q):
                kind, a, k = q.q[i]
                getattr(tk, kind)(*a, **k)


import os
PH = os.environ.get('PH', 'PAM')
SK = os.environ.get('SK', '')

NCOL = 1540
C_FQ0, C_FQ1, C_FK0, C_FK1, C_FV, C_G, C_MQ, C_MK, C_MVO = 0, 128, 256, 384, 512, 768, 772, 900, 1028


def host_consts():
    c = {}
    c["ident_bf"] = np.eye(128, dtype=np.float32).astype(ml_dtypes.bfloat16)
    c["ident_f"] = np.eye(128, dtype=np.float32)
    r = np.arange(128)
    c["triu"] = (r[:, None] <= r[None, :]).astype(np.float32)
    c["ones_f"] = np.ones((128, 128), np.float32)
    es = np.zeros((128, 128), np.float32); es[0, :] = 1.0
    c["esel"] = es
    t = np.arange(512)
    m = np.stack([(r[:, None] + 128 * rr <= t[None, :]) for rr in range(4)], axis=1)
    c["amask"] = m.astype(np.float32).astype(ml_dtypes.bfloat16)
    c["mmask"] = ((r[:, None] <= r[None, :]).astype(np.float32) * (128 ** -0.5)).astype(np.float32)
    return c


def build_phase_a(nc, S, tk=None, mix_dst=None, on_block_done=None):
    NT = S // 128
    NB = S // 512
    own_tk = tk is None
    if own_tk:
        tk = Trk(nc)
    dt = nc.dram_tensor
    x = dt("x", [int(os.environ.get("XROWS", S)), D], F32, kind="ExternalInput").ap()
    wc = dt("wc", [D, NCOL], F32, kind="ExternalInput").ap()
    n1w = dt("n1w", [128, KC], F32, kind="ExternalInput").ap()
    gbias = dt("gbias", [128, 4], F32, kind="ExternalInput").ap()
    convp = dt("convp", [128, 10], F32, kind="ExternalInput").ap()
    hnw = dt("hnw", [128, 512], F32, kind="ExternalInput").ap()
    ident_bf_d = dt("ident_bf", [128, 128], BF16, kind="ExternalInput").ap()
    ident_f_d = dt("ident_f", [128, 128], F32, kind="ExternalInput").ap()
    triu_d = dt("triu", [128, 128], F32, kind="ExternalInput").ap()
    ones_d = dt("ones_f", [128, 128], F32, kind="ExternalInput").ap()
    esel_d = dt("esel", [128, 128], F32, kind="ExternalInput").ap()
    amask_d = dt("amask", [128, 4, 512], BF16, kind="ExternalInput").ap()
    mmask_d = dt("mmask", [128, 128], F32, kind="ExternalInput").ap()
    if mix_dst is None:
        mixed = dt("mixed", [int(os.environ.get("MROWS", S)), 512], BF16, kind="ExternalOutput").ap()
        mix_dst = lambda t0, c0, c1: mixed[t0:t0 + 128, c0:c1]
    qt_d = [dt("qt%d" % h, [128, S], BF16, kind="Internal").ap() for h in range(2)]
    kt_d = [dt("kt%d" % h, [128, S], BF16, kind="Internal").ap() for h in range(2)]
    va_d = [dt("va%d" % h, [128, S // 128, 130], BF16, kind="Internal").ap() for h in range(2)]
    mq_d = dt("mqT", [128, S], F32, kind="Internal").ap()
    mk_d = dt("mkT", [128, S], F32, kind="Internal").ap()
    mvo_d = dt("mvo", [S, 512], F32, kind="Internal").ap()
    if os.environ.get("DUMMY"):
        dummy_d = dt("dummyx", [int(os.environ["DUMMY"]), 512], F32, kind="Internal").ap()

    with ExitStack() as es0:
        def sb(name, shape, dtype, es=es0):
            return es.enter_context(nc.sbuf_tensor(name, shape, dtype))
        ident_bf = sb("ident_bf_s", [128, 128], BF16)
        ident_f = sb("ident_f_s", [128, 128], F32)
        triu = sb("triu_s", [128, 128], F32)
        ones_f = sb("ones_s", [128, 128], F32)
        esel = sb("esel_s", [128, 128], F32)
        amask = sb("amask_s", [128, 4, 512], BF16)
        mmask = sb("mmask_s", [128, 128], F32)
        gb = sb("gb_s", [128, 4], F32)
        ngb = sb("ngb_s", [128, 4], F32)
        cvp = sb("cvp_s", [128, 10], F32)
        hnw_s = sb("hnw_s", [128, 512], F32)
        n1w_s = sb("n1w_s", [128, KC], F32)
        gates = sb("gates_s", [128, NT, 4], F32)
        epsc = sb("epsc", [128, 1], F32)
        onec = sb("onec", [128, 1], F32)
        psum = [es0.enter_context(nc.psum_tensor("ps%d" % i, [128, 512], F32)) for i in range(8)]
        for i, (s_, d_) in enumerate([(ident_bf, ident_bf_d), (ident_f, ident_f_d), (triu, triu_d),
                                      (ones_f, ones_d), (esel, esel_d), (amask, amask_d), (mmask, mmask_d),
                                      (gb, gbias), (cvp, convp), (hnw_s, hnw), (n1w_s, n1w)]):
            tk.dma("sp", s_[:], d_, writes=["c%d" % i], key="const")
        tk.barrier()
        tk.op("dve", lambda e: e.memset(epsc[:], EPS), writes=["epsc"])
        tk.op("dve", lambda e: e.memset(onec[:], 1.0), writes=["onec"])
        tk.op("dve", lambda e: e.tensor_scalar(out=ngb[:], in0=gb[:], scalar1=-1.0, scalar2=None, op0=ALU.mult),
              reads=["c7"], writes=["ngb"])

        with ExitStack() as es1:
            W = sb("W", [128, KC, NCOL], BF16, es1)
            wst = [sb("wst%d" % i, [128, NCOL], F32, es1) for i in range(2)]
            xt = [sb("xt%d" % i, [128, D], F32, es1) for i in range(2)]
            xn = [sb("xn%d" % i, [128, D], BF16, es1) for i in range(2)]
            junk = sb("junk", [128, D], BF16, es1)
            hT = [sb("hT%d" % i, [128, KC, 512], BF16, es1) for i in range(2)]
            ss = sb("ss", [128, 4], F32, es1)
            fst = [sb("fst%d" % i, [128, 512], BF16, es1) for i in range(4)]
            mst = [sb("mst%d" % i, [128, 512], F32, es1) for i in range(4)]
            vst = [sb("vst%d" % i, [128, 2, 130], BF16, es1) for i in range(2)]
            for i in range(2):
                tk.op("dve", lambda e, i=i: e.memset(vst[i][:, :, 128:129], 1.0), writes=["vst%d" % i])
                tk.op("dve", lambda e, i=i: e.memset(vst[i][:, :, 129:130], 0.0), writes=["vst%d" % i])
            for kc in range(KC):
                s = kc % 2
                tk.dma("sp", wst[s][:], wc[kc * 128:(kc + 1) * 128, :], writes=["wst%d" % s])
                tk.op("dve", lambda e, kc=kc, s=s: e.tensor_scalar(
                    out=W[:, kc, :], in0=wst[s][:], scalar1=n1w_s[:, kc:kc + 1], scalar2=None, op0=ALU.mult),
                    reads=["wst%d" % s, "c10"], writes=["W"])
            pb = [2]

            def nbank():
                b = pb[0]
                pb[0] = 2 + (pb[0] - 2 + 1) % 6
                return b
            fcnt = [0]
            mcnt = [0]
            def p_stage1(jb):
                hs = jb % 2
                for tt in range(4):
                    ti = jb * 4 + tt
                    s = ti % 2
                    tk.dma("sp", xt[s][:], x[ti * 128:(ti + 1) * 128, :], writes=["xt%d" % s])
                    tk.op("act", lambda e, s=s: e.activation(out=junk[:], in_=xt[s][:], func=AF.Square,
                                                             accum_out=ss[:, 0:1]),
                          reads=["xt%d" % s], writes=["junk", "ss0"])
                    tk.op("act", lambda e: e.activation(out=ss[:, 1:2], in_=ss[:, 0:1], func=AF.Ln,
                                                        scale=1.0 / D, bias=epsc[:]),
                          reads=["ss0", "epsc"], writes=["ss1"])
                    tk.op("act", lambda e: e.activation(out=ss[:, 2:3], in_=ss[:, 1:2], func=AF.Exp, scale=-0.5),
                          reads=["ss1"], writes=["ss2"])
                    tk.op("dve", lambda e, s=s: e.tensor_scalar(out=xn[s][:], in0=xt[s][:], scalar1=ss[:, 2:3],
                                                                scalar2=None, op0=ALU.mult),
                          reads=["xt%d" % s, "ss2"], writes=["xn%d" % s])
                    for half in range(2):
                        pst = psum[half][:, :].bitcast(BF16)
                        for k8 in range(8):
                            kc = half * 8 + k8
                            tk.op("pe", lambda e, kc=kc, k8=k8, pst=pst, s=s: e.transpose(
                                out=pst[:, k8 * 128:(k8 + 1) * 128], in_=xn[s][:, kc * 128:(kc + 1) * 128],
                                identity=ident_bf[:]),
                                reads=["xn%d" % s, "c0"], writes=["psb%d" % half], inc=(k8 == 7))
                        eng = "act" if half == 0 else "dve"
                        src = pst.rearrange("p (k t) -> p k t", k=8)
                        dst = hT[hs][:, half * 8:(half + 1) * 8, tt * 128:(tt + 1) * 128]
                        if eng == "act":
                            tk.op("act", lambda e, src=src, dst=dst: e.copy(out=dst, in_=src),
                                  reads=["psb%d" % half], writes=["hT%d" % hs])
                        else:
                            tk.op("dve", lambda e, src=src, dst=dst: e.tensor_copy(out=dst, in_=src),
                                  reads=["psb%d" % half], writes=["hT%d" % hs])

            def p_stage2(jb):
                hs = jb % 2
                for (c0, kind, dst_d) in [(C_FQ0, "f", qt_d[0]), (C_FQ1, "f", qt_d[1]), (C_FK0, "f", kt_d[0]),
                                          (C_FK1, "f", kt_d[1]), (C_MQ, "m", mq_d), (C_MK, "m", mk_d)]:
                    b = nbank()
                    for kc in range(KC):
                        tk.op("pe", lambda e, kc=kc, b=b, c0=c0: e.matmul(
                            psum[b][:, :], lhsT=W[:, kc, c0:c0 + 128], rhs=hT[hs][:, kc, :],
                            start=(kc == 0), stop=(kc == KC - 1)),
                            reads=["W", "hT%d" % hs], writes=["psb%d" % b], inc=(kc == KC - 1))
                    if kind == "f":
                        f = fcnt[0] % 4
                        fcnt[0] += 1
                        tk.op("act", lambda e, b=b, f=f: e.copy(out=fst[f][:], in_=psum[b][:, :]),
                              reads=["psb%d" % b], writes=["fst%d" % f])
                        if 'f' not in SK:
                            tk.dma("pool", dst_d[:, jb * 512:(jb + 1) * 512], fst[f][:], reads=["fst%d" % f],
                                   key="fst%d" % f)
                    else:
                        f = mcnt[0] % 4
                        mcnt[0] += 1
                        tk.op("dve", lambda e, b=b, f=f: e.tensor_copy(out=mst[f][:], in_=psum[b][:, :]),
                              reads=["psb%d" % b], writes=["mst%d" % f])
                        if 'm' not in SK:
                            tk.dma("pool", dst_d[:, jb * 512:(jb + 1) * 512], mst[f][:], reads=["mst%d" % f],
                                   key="mst%d" % f)
                for tt in range(4):
                    ti = jb * 4 + tt
                    tsl = slice(tt * 128, (tt + 1) * 128)
                    b = nbank()
                    for kc in range(KC):
                        tk.op("pe", lambda e, kc=kc, b=b, tsl=tsl: e.matmul(
                            psum[b][:, 0:260], lhsT=hT[hs][:, kc, tsl], rhs=W[:, kc, C_FV:C_FV + 260],
                            start=(kc == 0), stop=(kc == KC - 1)),
                            reads=["W", "hT%d" % hs], writes=["psb%d" % b], inc=(kc == KC - 1))
                    vs = ti % 2
                    tk.op("act", lambda e, b=b, vs=vs: e.copy(
                        out=vst[vs][:, :, 0:128], in_=psum[b][:, 0:256].rearrange("p (h d) -> p h d", h=2)),
                        reads=["psb%d" % b], writes=["vst%d" % vs])
                    tk.op("act", lambda e, b=b, ti=ti: e.copy(out=gates[:, ti, :], in_=psum[b][:, 256:260]),
                          reads=["psb%d" % b], writes=["gates"])
                    for h in (range(2) if 'v' not in SK else []):
                        tk.dma("pool", va_d[h][:, ti, :], vst[vs][:, h, :],
                               reads=["vst%d" % vs], key="vst%d_%d" % (vs, h))
                    b = nbank()
                    for kc in range(KC):
                        tk.op("pe", lambda e, kc=kc, b=b, tsl=tsl: e.matmul(
                            psum[b][:, :], lhsT=hT[hs][:, kc, tsl], rhs=W[:, kc, C_MVO:C_MVO + 512],
                            start=(kc == 0), stop=(kc == KC - 1)),
                            reads=["W", "hT%d" % hs], writes=["psb%d" % b], inc=(kc == KC - 1))
                    f = mcnt[0] % 4
                    mcnt[0] += 1
                    tk.op("dve", lambda e, b=b, f=f: e.tensor_copy(out=mst[f][:], in_=psum[b][:, :]),
                          reads=["psb%d" % b], writes=["mst%d" % f])
                    if 'o' not in SK:
                        tk.dma("pool", mvo_d[ti * 128:(ti + 1) * 128, :], mst[f][:], reads=["mst%d" % f],
                               key="mst%d" % f)

            NBP = min(NB, int(os.environ.get('NBLIM', '999')))
            p_stage1(0)
            for jb in range(NBP):
                if jb + 1 < NBP:
                    p_stage1(jb + 1)
                p_stage2(jb)
            tk.barrier()

        NQ = NB
        SC = 128 ** -0.5
        with ExitStack() as es2:
            KT = sb("KT", [128, S], BF16, es2)
            VA = sb("VA", [128, NT, 130], BF16, es2)
            qt = [sb("qtb%d" % i, [128, 512], BF16, es2) for i in range(2)]
            PT = [sb("PT%d" % i, [128, 512], BF16, es2) for i in range(4)]
            lfn = sb("lfn", [128, NT], F32, es2)
            tmpg = sb("tmpg", [128, NT], F32, es2)
            tot = sb("tot", [128, NT], F32, es2)
            offi = sb("offi", [128, NT], F32, es2)
            onesn = sb("onesn", [128, NT], F32, es2)
            G = sb("G", [128, NT], F32, es2)
            cb = sb("cb", [128, NQ], F32, es2)
            biasj = [sb("biasj%d" % i, [128, NT], F32, es2) for i in range(2)]
            sm = sb("sm", [128, 8], F32, es2)
            ot = [sb("ot%d" % i, [128, 128], F32, es2) for i in range(2)]
            oj = sb("oj", [128, 128], F32, es2)
            ob = [sb("ob%d" % i, [128, 128], BF16, es2) for i in range(2)]
            tk.op("dve", lambda e: e.memset(onesn[:], 1.0), writes=["onesn"])
            ptc = [0]
            oc = [0]
            for hh in (range(2) if 'A' in PH else []):
                for c0 in range(0, S, 2048):
                    tk.dma("sp", KT[:, c0:min(S, c0 + 2048)], kt_d[hh][:, c0:min(S, c0 + 2048)], writes=["KT"])
                for c0 in range(0, NT, 16):
                    tk.dma("sp", VA[:, c0:min(NT, c0 + 16), :], va_d[hh][:, c0:min(NT, c0 + 16), :], writes=["VA"])
                tk.op("act", lambda e, hh=hh: e.activation(out=tmpg[:], in_=gates[:, :, hh], func=AF.Exp,
                                                           scale=-1.0, bias=ngb[:, hh:hh + 1]),
                      reads=["gates", "ngb"], writes=["tmpg"])
                tk.op("act", lambda e: e.activation(out=lfn[:], in_=tmpg[:], func=AF.Ln, scale=1.0, bias=onec[:]),
                      reads=["tmpg", "onec"], writes=["lfn"])
                for c0 in range(0, NT, 32):
                    c1 = min(NT, c0 + 32)
                    tk.op("pe", lambda e, c0=c0, c1=c1: e.matmul(psum[0][:, c0:c1], lhsT=triu[:], rhs=lfn[:, c0:c1],
                                                                 start=True, stop=True),
                          reads=["lfn", "c2"], writes=["psb0"])
                    tk.op("pe", lambda e, c0=c0, c1=c1: e.matmul(psum[1][:, c0:c1], lhsT=ones_f[:], rhs=lfn[:, c0:c1],
                                                                 start=True, stop=True),
                          reads=["lfn", "c3"], writes=["psb1"])
                tk.op("act", lambda e: e.copy(out=tot[:], in_=psum[1][:, 0:NT]), reads=["psb1"], writes=["tot"])
                tk.op("act", lambda e: e.copy(out=G[:], in_=psum[0][:, 0:NT]), reads=["psb0"], writes=["G"])
                tk.op("dve", lambda e: e.tensor_tensor_scan(out=offi[:], data0=onesn[:], data1=tot[:], initial=0.0,
                                                            op0=ALU.mult, op1=ALU.add),
                      reads=["tot", "onesn"], writes=["offi"])
                tk.op("dve", lambda e: e.tensor_tensor(out=offi[:], in0=offi[:], in1=tot[:], op=ALU.subtract),
                      reads=["offi", "tot"], writes=["offi"])
                tk.op("dve", lambda e: e.tensor_tensor(out=G[:], in0=G[:], in1=offi[:], op=ALU.add),
                      reads=["G", "offi"], writes=["G"])
                Gsel = G[:].rearrange("p (j r) -> p j r", r=4)[:, :, 2]
                tk.op("dve", lambda e, Gsel=Gsel: e.tensor_copy(out=tmpg[:, 0:NQ], in_=Gsel), reads=["G"],
                      writes=["tmpg"])
                tk.op("pe", lambda e: e.matmul(psum[1][:, 0:NQ], lhsT=esel[:], rhs=tmpg[:, 0:NQ], start=True, stop=True),
                      reads=["tmpg", "c4"], writes=["psb1"])
                tk.op("act", lambda e: e.copy(out=cb[:], in_=psum[1][:, 0:NQ]), reads=["psb1"], writes=["cb"])
                blocks = [(j, i) for j in range(NQ) for i in range(4 * j + 4)]
                NBK = len(blocks)
                LOOK = 2

                def load_q(j):
                    tk.dma("sp", qt[j % 2][:], qt_d[hh][:, j * 512:(j + 1) * 512], writes=["qt%d" % (j % 2)])

                def make_bias(j):
                    nk = 4 * j + 4
                    tk.op("dve", lambda e, j=j, nk=nk: e.tensor_scalar(
                        out=biasj[j % 2][:, 0:nk], in0=G[:, 0:nk], scalar1=cb[:, j:j + 1], scalar2=None,
                        op0=ALU.subtract), reads=["G", "cb"], writes=["bj%d" % (j % 2)])

                def st_qk(n):
                    j, i = blocks[n]
                    if i == 0 and j + 1 < NQ:
                        load_q(j + 1)
                    sb_ = n % 4
                    qs = j % 2
                    tk.op("pe", lambda e, i=i, qs=qs, sb_=sb_: e.matmul(
                        psum[sb_][:, :], lhsT=KT[:, i * 128:(i + 1) * 128], rhs=qt[qs][:], start=True, stop=True),
                        reads=["KT", "qt%d" % qs], writes=["psb%d" % sb_])

                def st_ex(n):
                    j, i = blocks[n]
                    if i == 0 and j + 1 < NQ:
                        make_bias(j + 1)
                    r = i - 4 * j
                    sb_ = n % 4
                    p = n % 4
                    bj = j % 2
                    tk.op("act", lambda e, p=p, sb_=sb_, bj=bj, i=i: e.activation(
                        out=PT[p][:], in_=psum[sb_][:, :], func=AF.Exp, scale=SC, bias=biasj[bj][:, i:i + 1]),
                        reads=["psb%d" % sb_, "bj%d" % bj], writes=["PT%d" % p])
                    if r >= 0:
                        tk.op("dve", lambda e, p=p, r=r: e.tensor_tensor(
                            out=PT[p][:], in0=PT[p][:], in1=amask[:, r, :], op=ALU.mult),
                            reads=["PT%d" % p, "c5"], writes=["PT%d" % p])

                def st_pv(n):
                    j, i = blocks[n]
                    r = i - 4 * j
                    p = n % 4
                    for u in range(4):
                        if r > u:
                            continue
                        last = 4 * j + u
                        tk.op("pe", lambda e, p=p, u=u, i=i, last=last: e.matmul(
                            psum[4 + u][:, 0:130], lhsT=PT[p][:, u * 128:(u + 1) * 128], rhs=VA[:, i, :],
                            start=(i == 0), stop=(i == last)),
                            reads=["PT%d" % p, "VA"], writes=["psb%d" % (4 + u)], inc=(u == 3))
                    if i == 4 * j + 3:
                        epilogue(j)

                def epilogue(j):
                    for u in range(4):
                        o = oc[0] % 2
                        oc[0] += 1
                        pu = psum[4 + u]
                        tk.op("dve", lambda e, pu=pu: e.reciprocal(out=sm[:, 0:1], in_=pu[:, 128:129]),
                              reads=["psb%d" % (4 + u)], writes=["sm0"])
                        tk.op("dve", lambda e, pu=pu, o=o: e.tensor_scalar(
                            out=ot[o][:], in0=pu[:, 0:128], scalar1=sm[:, 0:1], scalar2=None, op0=ALU.mult),
                            reads=["psb%d" % (4 + u), "sm0"], writes=["ot%d" % o])
                        tk.op("act", lambda e, o=o: e.activation(out=oj[:], in_=ot[o][:], func=AF.Square,
                                                                 accum_out=sm[:, 1:2]),
                              reads=["ot%d" % o], writes=["oj", "sm1"])
                        tk.op("act", lambda e: e.activation(out=sm[:, 2:3], in_=sm[:, 1:2], func=AF.Ln,
                                                            scale=1.0 / 128, bias=epsc[:]),
                              reads=["sm1", "epsc"], writes=["sm2"])
                        tk.op("act", lambda e: e.activation(out=sm[:, 3:4], in_=sm[:, 2:3], func=AF.Exp, scale=-0.5),
                              reads=["sm2"], writes=["sm3"])
                        tk.op("dve", lambda e, o=o, hh=hh: e.scalar_tensor_tensor(
                            out=ob[o][:], in0=ot[o][:], scalar=sm[:, 3:4], in1=hnw_s[:, hh * 128:(hh + 1) * 128],
                            op0=ALU.mult, op1=ALU.mult),
                            reads=["ot%d" % o, "sm3", "c9"], writes=["ob%d" % o])
                        t0 = j * 512 + u * 128
                        tk.dma("pool", mix_dst(t0, hh * 128, (hh + 1) * 128), ob[o][:], reads=["ob%d" % o],
                               key="ob%d" % o)

                load_q(0)
                make_bias(0)
                for n in range(NBK + LOOK):
                    if n < NBK:
                        st_qk(n)
                    if n - LOOK >= 0:
                        st_ex(n - LOOK)
                        st_pv(n - LOOK)
                tk.barrier()

        NCH = NT
        with ExitStack() as es3:
            zq = [sb("zq%d" % i, [128, 515], F32, es3) for i in range(2)]
            zk = [sb("zk%d" % i, [128, 515], F32, es3) for i in range(2)]
            acc = sb("acc", [128, 512], F32, es3)
            ex = sb("ex", [128, 512], F32, es3)
            qT = [sb("qT%d" % i, [128, 512], BF16, es3) for i in range(2)]
            kT = [sb("kT%d" % i, [128, 512], BF16, es3) for i in range(2)]
            mvo = [sb("mvo%d" % i, [128, 512], F32, es3) for i in range(2)]
            vt = [sb("vt%d" % i, [128, 258], BF16, es3) for i in range(2)]
            ktok = [sb("ktok%d" % i, [128, 128], BF16, es3) for i in range(2)]
            smt = [sb("smt%d" % i, [128, 128], BF16, es3) for i in range(2)]
            Cf = sb("Cf", [128, 258], F32, es3)
            Cb = [sb("Cb%d" % i, [128, 258], BF16, es3) for i in range(2)]
            lf = sb("mlf", [128, NCH], F32, es3)
            tmpm = sb("tmpm", [128, NCH], F32, es3)
            bcs = sb("bcs", [128, NCH], F32, es3)
            eb = sb("eb", [128, NCH], F32, es3)
            ek = sb("ek", [128, NCH], F32, es3)
            ebl = sb("ebl", [128, NCH], F32, es3)
            hsm = sb("hsm", [128, 8], F32, es3)
            hv = [sb("hv%d" % i, [128, 256], F32, es3) for i in range(2)]
            sg = sb("sg", [128, 256], F32, es3)
            hj = sb("hj", [128, 256], F32, es3)
            hb = [sb("hb%d" % i, [128, 256], BF16, es3) for i in range(2)]
            if 'M' in PH or os.environ.get('MLPRE'):
                _lim = int(os.environ.get('MLPRE', '99'))
                _real_op = tk.op
                _cnt = [0]
                def _lop(*a, **k):
                    _cnt[0] += 1
                    if _cnt[0] <= _lim:
                        return _real_op(*a, **k)
                tk.op = _lop
                tk.op("act", lambda e: e.activation(out=tmpm[:], in_=gates[:, :, 3], func=AF.Exp, scale=-1.0,
                                                    bias=ngb[:, 3:4]), reads=["gates", "ngb"], writes=["tmpm"])
                tk.op("act", lambda e: e.activation(out=lf[:], in_=tmpm[:], func=AF.Ln, scale=1.0, bias=onec[:]),
                      reads=["tmpm", "onec"], writes=["mlf"])
                tk.op("dve", lambda e: e.tensor_scalar(out=lf[:], in0=lf[:], scalar1=-1.0, scalar2=None, op0=ALU.mult),
                      reads=["mlf"], writes=["mlf"])
                for c0 in range(0, NCH, 32):
                    c1 = min(NCH, c0 + 32)
                    tk.op("pe", lambda e, c0=c0, c1=c1: e.matmul(psum[0][:, c0:c1], lhsT=triu[:], rhs=lf[:, c0:c1],
                                                                 start=True, stop=True),
                          reads=["mlf", "c2"], writes=["psb0"])
                    tk.op("pe", lambda e, c0=c0, c1=c1: e.matmul(psum[1][:, c0:c1], lhsT=ones_f[:], rhs=lf[:, c0:c1],
                                                                 start=True, stop=True),
                          reads=["mlf", "c3"], writes=["psb1"])
                tk.op("act", lambda e: e.activation(out=eb[:], in_=psum[0][:, 0:NCH], func=AF.Exp),
                      reads=["psb0"], writes=["eb"])
                tk.op("act", lambda e: e.activation(out=ebl[:], in_=psum[1][:, 0:NCH], func=AF.Exp),
                      reads=["psb1"], writes=["ebl"])
                tk.op("act", lambda e: e.copy(out=tmpm[:], in_=psum[0][:, 0:NCH]), reads=["psb0"], writes=["tmpm"])
                tk.op("act", lambda e: e.copy(out=bcs[:], in_=gates[:, :, 2]), reads=["gates"], writes=["bcs"])
                tk.op("dve", lambda e: e.tensor_tensor(out=bcs[:], in0=bcs[:], in1=tmpm[:], op=ALU.subtract),
                      reads=["bcs", "tmpm"], writes=["bcs"])
                tk.op("act", lambda e: e.activation(out=ek[:], in_=bcs[:], func=AF.Exp, bias=gb[:, 2:3], scale=1.0),
                      reads=["bcs", "c7"], writes=["ek"])
            if 'M' in PH or os.environ.get('MLPRE'):
                tk.op = _real_op
            tk.op("dve", lambda e: e.memset(Cf[:], 0.0), writes=["Cf"])
            tk.op("dve", lambda e: e.memset(Cb[0][:], 0.0), writes=["Cb0"])
            for i in range(2):
                tk.op("dve", lambda e, i=i: e.memset(zq[i][:, 0:3], 0.0), writes=["zq%d" % i])
                tk.op("dve", lambda e, i=i: e.memset(zk[i][:, 0:3], 0.0), writes=["zk%d" % i])
                tk.op("dve", lambda e, i=i: e.memset(vt[i][:], 0.0), writes=["vt%d" % i])
            def ml_x(c, tk):
                jb, cc = c // 4, c % 4
                s = jb % 2
                cs = c % 2
                ob_ = 4 if cs == 0 else 6
                csl = slice(cc * 128, (cc + 1) * 128)
                tk.dma("sp", mvo[cs][:], mvo_d[c * 128:(c + 1) * 128, :], writes=["mvo%d" % cs])
                pkt = psum[2][:, :].bitcast(BF16)
                tk.op("pe", lambda e, s=s, csl=csl, pkt=pkt: e.transpose(out=pkt[:, 0:128], in_=kT[s][:, csl],
                                                                         identity=ident_bf[:]),
                      reads=["kT%d" % s, "c0"], writes=["psb2"])
                tk.op("act", lambda e, cs=cs, pkt=pkt: e.activation(out=ktok[cs][:], in_=pkt[:, 0:128],
                                                                    func=AF.Copy, scale=SC),
                      reads=["psb2"], writes=["ktok%d" % cs])
                tk.op("dve", lambda e, cs=cs, c=c: e.tensor_scalar(
                    out=vt[cs][:, 0:256], in0=mvo[cs][:, 0:256], scalar1=ek[:, c:c + 1], scalar2=None, op0=ALU.mult),
                    reads=["mvo%d" % cs, "ek"], writes=["vt%d" % cs])
                tk.op("dve", lambda e, cs=cs, c=c: e.tensor_copy(out=vt[cs][:, 256:257], in_=ek[:, c:c + 1]),
                      reads=["ek"], writes=["vt%d" % cs])
                tk.op("pe", lambda e, s=s, csl=csl: e.matmul(psum[3][:, 0:128], lhsT=kT[s][:, csl], rhs=qT[s][:, csl],
                                                             start=True, stop=True),
                      reads=["kT%d" % s, "qT%d" % s], writes=["psb3"])
                tk.op("dve", lambda e, cs=cs: e.tensor_tensor(out=smt[cs][:], in0=psum[3][:, 0:128], in1=mmask[:],
                                                              op=ALU.mult),
                      reads=["psb3", "c6"], writes=["smt%d" % cs])
                tk.op("pe", lambda e, cs=cs: e.matmul(psum[ob_][:, 0:258], lhsT=smt[cs][:], rhs=vt[cs][:],
                                                      start=True, stop=False),
                      reads=["smt%d" % cs, "vt%d" % cs], writes=["psb%d" % ob_])
                tk.op("pe", lambda e, s=s, csl=csl, cs=cs: e.matmul(psum[ob_][:, 0:258], lhsT=qT[s][:, csl],
                                                                    rhs=Cb[cs][:], start=False, stop=True),
                      reads=["qT%d" % s, "Cb%d" % cs], writes=["psb%d" % ob_])
                tk.op("pe", lambda e, cs=cs: e.matmul(psum[5][:, 0:258], lhsT=ktok[cs][:], rhs=vt[cs][:],
                                                      start=True, stop=True),
                      reads=["ktok%d" % cs, "vt%d" % cs], writes=["psb5"])
                tk.op("dve", lambda e: e.tensor_tensor(out=Cf[:], in0=psum[5][:, 0:258], in1=Cf[:], op=ALU.add),
                      reads=["psb5", "Cf"], writes=["Cf"])
                tk.op("dve", lambda e, c=c: e.tensor_scalar(out=Cf[:], in0=Cf[:], scalar1=ebl[:, c:c + 1],
                                                            scalar2=None, op0=ALU.mult),
                      reads=["Cf", "ebl"], writes=["Cf"])
                tk.op("act", lambda e, cs=cs: e.copy(out=Cb[1 - cs][:], in_=Cf[:]), reads=["Cf"],
                      writes=["Cb%d" % (1 - cs)])


            def ml_y(c, tk):
                jb, cc = c // 4, c % 4
                s = jb % 2
                cs = c % 2
                ob_ = 4 if cs == 0 else 6
                csl = slice(cc * 128, (cc + 1) * 128)
                tk.op("act", lambda e, c=c: e.activation(out=hsm[:, 6:7], in_=psum[ob_][:, 256:257], func=AF.Abs,
                                                         scale=eb[:, c:c + 1]),
                      reads=["psb%d" % ob_, "eb"], writes=["hsm6"])
                tk.op("dve", lambda e: e.tensor_scalar(out=hsm[:, 0:1], in0=hsm[:, 6:7], scalar1=1.0, scalar2=None,
                                                       op0=ALU.max),
                      reads=["hsm6"], writes=["hsm0"])
                tk.op("dve", lambda e: e.reciprocal(out=hsm[:, 1:2], in_=hsm[:, 0:1]), reads=["hsm0"], writes=["hsm1"])
                tk.op("dve", lambda e, c=c: e.tensor_tensor(out=hsm[:, 2:3], in0=hsm[:, 1:2], in1=eb[:, c:c + 1],
                                                            op=ALU.mult), reads=["hsm1", "eb"], writes=["hsm2"])
                tk.op("act", lambda e, cs=cs: e.activation(out=sg[:], in_=mvo[cs][:, 256:512], func=AF.Exp, scale=-1.0),
                      reads=["mvo%d" % cs], writes=["sg"])
                tk.op("dve", lambda e: e.tensor_scalar(out=sg[:], in0=sg[:], scalar1=1.0, scalar2=None, op0=ALU.add),
                      reads=["sg"], writes=["sg"])
                tk.op("dve", lambda e: e.reciprocal(out=sg[:], in_=sg[:]), reads=["sg"], writes=["sg"])
                tk.op("dve", lambda e, cs=cs: e.scalar_tensor_tensor(
                    out=hv[cs][:], in0=psum[ob_][:, 0:256], scalar=hsm[:, 2:3], in1=sg[:], op0=ALU.mult, op1=ALU.mult),
                    reads=["psb%d" % ob_, "hsm2", "sg"], writes=["hv%d" % cs])
                tk.op("act", lambda e, cs=cs: e.activation(out=hj[:], in_=hv[cs][:], func=AF.Square,
                                                           accum_out=hsm[:, 3:4]),
                      reads=["hv%d" % cs], writes=["hj", "hsm3"])
                tk.op("act", lambda e: e.activation(out=hsm[:, 4:5], in_=hsm[:, 3:4], func=AF.Ln, scale=1.0 / 256,
                                                    bias=epsc[:]), reads=["hsm3", "epsc"], writes=["hsm4"])
                tk.op("act", lambda e: e.activation(out=hsm[:, 5:6], in_=hsm[:, 4:5], func=AF.Exp, scale=-0.5),
                      reads=["hsm4"], writes=["hsm5"])
                tk.op("dve", lambda e, cs=cs: e.scalar_tensor_tensor(
                    out=hb[cs][:], in0=hv[cs][:], scalar=hsm[:, 5:6], in1=hnw_s[:, 256:512], op0=ALU.mult,
                    op1=ALU.mult), reads=["hv%d" % cs, "hsm5", "c9"], writes=["hb%d" % cs])
                tk.dma("pool", mix_dst(c * 128, 256, 512), hb[cs][:], reads=["hb%d" % cs], writes=["ml%d" % c],
                       key="hb%d" % cs)


            for jb in (range(NB) if 'M' in PH else []):
                s = jb % 2
                for (z, zn, zd, wo, bo, dstT, dn) in [(zq, "zq", mq_d, 0, 8, qT, "qT"), (zk, "zk", mk_d, 4, 9, kT, "kT")]:
                    tk.dma("sp", z[s][:, 3:515], zd[:, jb * 512:(jb + 1) * 512], writes=["%s%d" % (zn, s)])
                    if jb > 0:
                        tk.op("act", lambda e, z=z, s=s: e.copy(out=z[s][:, 0:3], in_=z[1 - s][:, 512:515]),
                              reads=["%s%d" % (zn, 1 - s)], writes=["%s%d" % (zn, s)])
                    tk.op("dve", lambda e, z=z, s=s, wo=wo, bo=bo: e.tensor_scalar(
                        out=acc[:], in0=z[s][:, 0:512], scalar1=cvp[:, wo:wo + 1], scalar2=cvp[:, bo:bo + 1],
                        op0=ALU.mult, op1=ALU.add), reads=["%s%d" % (zn, s), "c8"], writes=["acc"])
                    for jj in range(1, 4):
                        tk.op("dve", lambda e, z=z, s=s, wo=wo, jj=jj: e.scalar_tensor_tensor(
                            out=acc[:], in0=z[s][:, jj:jj + 512], scalar=cvp[:, wo + jj:wo + jj + 1], in1=acc[:],
                            op0=ALU.mult, op1=ALU.add), reads=["%s%d" % (zn, s), "c8", "acc"], writes=["acc"])
                    tk.op("act", lambda e: e.activation(out=ex[:], in_=acc[:], func=AF.Exp, scale=-1.0),
                          reads=["acc"], writes=["ex"])
                    tk.op("dve", lambda e: e.tensor_scalar(out=ex[:], in0=ex[:], scalar1=1.0, scalar2=None, op0=ALU.add),
                          reads=["ex"], writes=["ex"])
                    tk.op("dve", lambda e: e.reciprocal(out=ex[:], in_=ex[:]), reads=["ex"], writes=["ex"])
                    tk.op("dve", lambda e, dstT=dstT, s=s: e.tensor_tensor(out=dstT[s][:], in0=acc[:], in1=ex[:],
                                                                           op=ALU.mult),
                          reads=["acc", "ex"], writes=["%s%d" % (dn, s)])
                for cc in range(4):
                    c = jb * 4 + cc
                    qx = OpQueue()
                    ml_x(c, qx)
                    qs_ = [qx]
                    if c > 0:
                        qy = OpQueue()
                        ml_y(c - 1, qy)
                        qs_.append(qy)
                    emit_interleaved(tk, qs_)
                    if c > 0 and (c - 1) % 4 == 3 and on_block_done is not None:
                        on_block_done((c - 1) // 4)
            if 'M' in PH:
                ml_y(NCH - 1, tk)
                if on_block_done is not None:
                    on_block_done((NCH - 1) // 4)
            tk.barrier()
        if own_tk:
            tk.final_wait("sp")
    print("phase A instructions:", tk.ninst)
    return nc


def host_inputs_a(inp, S):
    c = host_consts()
    w_in = np.asarray(inp["w_in"][0])
    maps = []
    for core in range(8):
        b, g = core // 4, core % 4
        h0, h1 = 2 * g, 2 * g + 1
        cols = []
        for base in (0, 1024, 2048):
            cols += list(range(base + h0 * 128, base + h0 * 128 + 128)) + list(range(base + h1 * 128, base + h1 * 128 + 128))
        cols += [3072 + h0, 3072 + h1, 5128 + g, 5132 + g]
        cols += list(range(3080 + g * 128, 3080 + g * 128 + 128))
        cols += list(range(3592 + g * 128, 3592 + g * 128 + 128))
        cols += list(range(4104 + g * 256, 4104 + g * 256 + 256))
        cols += list(range(5136 + g * 256, 5136 + g * 256 + 256))
        wcs = np.ascontiguousarray(w_in[:, cols])
        gbv = np.array([inp["fox_f_bias"][0][h0], inp["fox_f_bias"][0][h1], inp["mlstm_i_bias"][0][g],
                        inp["mlstm_f_bias"][0][g]], np.float32)
        cw = np.asarray(inp["mlstm_conv_w"][0])
        cbv = np.asarray(inp["mlstm_conv_b"][0])
        convp = np.concatenate([cw[:, g * 128:(g + 1) * 128].T, cw[:, 512 + g * 128:512 + (g + 1) * 128].T,
                                cbv[g * 128:(g + 1) * 128][:, None], cbv[512 + g * 128:512 + (g + 1) * 128][:, None]],
                               axis=1).astype(np.float32)
        hn = np.concatenate([np.asarray(inp["fox_out_norm_w"][0])[h0 * 128:(h1 + 1) * 128],
                             np.asarray(inp["mlstm_out_norm_w"][0])[g * 256:(g + 1) * 256]])
        m = {"x": np.ascontiguousarray(np.asarray(inp["x"])[b, :int(os.environ.get("XROWS", S))]),
             "wc": wcs,
             "n1w": np.ascontiguousarray(np.asarray(inp["norm1_w"][0]).reshape(KC, 128).T),
             "gbias": np.ascontiguousarray(np.broadcast_to(gbv[None, :], (128, 4))),
             "convp": np.ascontiguousarray(convp),
             "hnw": np.ascontiguousarray(np.broadcast_to(hn[None, :], (128, 512))).astype(np.float32)}
        m.update(c)
        maps.append(m)
    return maps


import os


def host_consts_b():
    c = {}
    c["ident_bf"] = np.eye(128, dtype=np.float32).astype(ml_dtypes.bfloat16)
    c["ident_f"] = np.eye(128, dtype=np.float32)
    c["iota_row"] = np.ascontiguousarray(np.broadcast_to(np.arange(128, dtype=np.float32)[None, :], (128, 128)))
    c["thr16"] = np.ascontiguousarray(np.broadcast_to((16.0 * np.arange(16, dtype=np.float32))[None, :], (128, 16)))
    c["iota16"] = np.ascontiguousarray(np.broadcast_to(np.arange(16, dtype=np.float32)[None, :], (128, 16)))
    return c


def build_phase_b(nc, TC, NJ=128, tk=None, gath=None, pfx="", pre=None):
    NTT = TC // 128
    PB = min(256, TC)
    NBLK = TC // PB
    TPB = PB // 128
    own_tk = tk is None
    if own_tk:
        tk = Trk(nc)
    _dt = nc.dram_tensor

    def dt(name, *a, **k):
        return _dt(pfx + name, *a, **k)
    x = dt("x", [TC, D], F32, kind="ExternalInput").ap()
    if gath is None:
        mixed = dt("mixed", [TC, D], BF16, kind="ExternalInput").ap()
    else:
        wsel_d = dt("wsel", [128, 8], F32, kind="ExternalInput").ap()
    wout = dt("wout", [D, D], F32, kind="ExternalInput").ap()
    wq = dt("wq", [D, D], F32, kind="ExternalInput").ap()
    n2w = dt("n2w", [128, D], F32, kind="ExternalInput").ap()
    fnw = dt("fnw", [128, D], F32, kind="ExternalInput").ap()
    keysT = dt("keysT", [128, 2, 128], F32, kind="ExternalInput").ap()
    if pre is None:
        uh = dt("uh", [128, 128, D], F32, kind="ExternalInput").ap()
        vh = dt("vh", [128, 128, D], F32, kind="ExternalInput").ap()
    ident_bf_d = dt("ident_bf", [128, 128], BF16, kind="ExternalInput").ap()
    ident_f_d = dt("ident_f", [128, 128], F32, kind="ExternalInput").ap()
    iota_row_d = dt("iota_row", [128, 128], F32, kind="ExternalInput").ap()
    thr16_d = dt("thr16", [128, 16], F32, kind="ExternalInput").ap()
    iota16_d = dt("iota16", [128, 16], F32, kind="ExternalInput").ap()
    out = dt("out", [TC, D], F32, kind="ExternalOutput").ap()
    if pre is None:
        u16 = dt("u16", [128, 128, D], BF16, kind="Internal").ap()
        v16 = dt("v16", [128, 128, D], BF16, kind="Internal").ap()
    else:
        u16, v16 = pre
    x1_d = dt("x1_d", [TC, D], F32, kind="Internal").ap()
    h2T_d = dt("h2T_d", [128, KC, TC], BF16, kind="Internal").ap()
    slot_d = dt("slot_d", [128, 3, TC], F32, kind="Internal").ap()

    with ExitStack() as es0:
        def sb(name, shape, dtype, es=es0):
            return es.enter_context(nc.sbuf_tensor(pfx + name, shape, dtype))
        ident_bf = sb("ident_bf_s", [128, 128], BF16)
        ident_f = sb("ident_f_s", [128, 128], F32)
        iota_row = sb("iota_row_s", [128, 128], F32)
        thr16 = sb("thr16_s", [128, 16], F32)
        iota16 = sb("iota16_s", [128, 16], F32)
        epsc = sb("epsc", [128, 1], F32)
        psum = [es0.enter_context(nc.psum_tensor(pfx + "ps%d" % i, [128, 512], F32)) for i in range(8)]
        for i, (s_, d_) in enumerate([(ident_bf, ident_bf_d), (ident_f, ident_f_d), (iota_row, iota_row_d),
                                      (thr16, thr16_d), (iota16, iota16_d)]):
            tk.dma("sp", s_[:], d_, writes=["c%d" % i], key="const")
        for j in (range(NJ) if pre is None else []):
            tk.dma("pool", u16[j], uh[j], writes=["u16"], key="cvt")
            tk.dma("pool", v16[j], vh[j], writes=["v16"], key="cvt")
        tk.op("dve", lambda e: e.memset(epsc[:], EPS), writes=["epsc"])

        def rms_rstd(src_ap, srckey, ss, junk):
            tk.op("act", lambda e: e.activation(out=junk[:], in_=src_ap, func=AF.Square, accum_out=ss[:, 0:1]),
                  reads=[srckey], writes=["junk", "ss0"])
            tk.op("act", lambda e: e.activation(out=ss[:, 1:2], in_=ss[:, 0:1], func=AF.Ln, scale=1.0 / D, bias=epsc[:]),
                  reads=["ss0", "epsc"], writes=["ss1"])
            tk.op("act", lambda e: e.activation(out=ss[:, 2:3], in_=ss[:, 1:2], func=AF.Exp, scale=-0.5),
                  reads=["ss1"], writes=["ss2"])

        def transpose16(src, srckey, dst, dstkey, banks):
            for half in range(2):
                bk = banks[half]
                pst = psum[bk][:, :].bitcast(BF16)
                for k8 in range(8):
                    kc = half * 8 + k8
                    tk.op("pe", lambda e, kc=kc, k8=k8, pst=pst: e.transpose(
                        out=pst[:, k8 * 128:(k8 + 1) * 128], in_=src[:, kc * 128:(kc + 1) * 128], identity=ident_bf[:]),
                        reads=[srckey, "c0"], writes=["psb%d" % bk], inc=(k8 == 7))
                srcv = pst.rearrange("p (k t) -> p k t", k=8)
                dstv = dst[:, half * 8:(half + 1) * 8, :]
                if half == 0:
                    tk.op("act", lambda e, srcv=srcv, dstv=dstv: e.copy(out=dstv, in_=srcv),
                          reads=["psb%d" % bk], writes=[dstkey])
                else:
                    tk.op("dve", lambda e, srcv=srcv, dstv=dstv: e.tensor_copy(out=dstv, in_=srcv),
                          reads=["psb%d" % bk], writes=[dstkey])

        with ExitStack() as es1:
            Wo = sb("Wo", [128, KC, D], BF16, es1)
            n2b = sb("n2b", [128, D], F32, es1)
            mx = [sb("mx%d" % i, [128, D], BF16, es1) for i in range(2)]
            xt = [sb("xt%d" % i, [128, D], F32, es1) for i in range(2)]
            x1 = [sb("x1_%d" % i, [128, D], F32, es1) for i in range(2)]
            mT = sb("mT", [128, KC, 128], BF16, es1)
            h2 = sb("h2", [128, D], BF16, es1)
            h2t = [sb("h2t%d" % i, [128, KC, 128], BF16, es1) for i in range(2)]
            junk = sb("junk", [128, D], BF16, es1)
            ss = sb("ss", [128, 4], F32, es1)
            for kc in range(KC):
                tk.dma("pool", Wo[:, kc, :], wout[kc * 128:(kc + 1) * 128, :], writes=["Wo"], key="wload")
            tk.dma("sp", n2b[:], n2w, writes=["n2b"])
            ccnt = [0]
            if gath is not None:
                cand = [sb("cand%d" % i, [128, D], BF16, es1) for i in range(3)]
                wsel = sb("wsel_s", [128, 8], F32, es1)
                tk.dma("sp", wsel[:], wsel_d, writes=["wsel"])
            for ti in range(NTT):
                s = ti % 2
                rows = slice(ti * 128, (ti + 1) * 128)
                if gath is None:
                    tk.dma("sp", mx[s][:], mixed[rows, :], writes=["mx%d" % s])
                else:
                    bb_, lt = ti // (NTT // 2), ti % (NTT // 2)
                    for dp in range(8):
                        cs_ = ccnt[0] % 3
                        ccnt[0] += 1
                        gt_ = dp * (NTT // 2) + lt
                        kq = gt_ // 4
                        src = gath[kq].ap().rearrange("(r t) c -> r t c", r=8)[bb_ * 4:(bb_ + 1) * 4,
                                                                              (gt_ % 4) * 128:(gt_ % 4 + 1) * 128, :]
                        tk.dma("sp", cand[cs_][:].rearrange("p (g c) -> p g c", g=4), src.rearrange("g t c -> t g c"),
                               reads=["gath%d" % kq], writes=["cand%d" % cs_])
                        if dp == 0:
                            tk.op("dve", lambda e, cs_=cs_, s=s: e.tensor_scalar(
                                out=mx[s][:], in0=cand[cs_][:], scalar1=wsel[:, 0:1], scalar2=None, op0=ALU.mult),
                                reads=["cand%d" % cs_, "wsel"], writes=["mx%d" % s])
                        else:
                            tk.op("dve", lambda e, cs_=cs_, s=s, dp=dp: e.scalar_tensor_tensor(
                                out=mx[s][:], in0=cand[cs_][:], scalar=wsel[:, dp:dp + 1], in1=mx[s][:], op0=ALU.mult,
                                op1=ALU.add), reads=["cand%d" % cs_, "wsel", "mx%d" % s], writes=["mx%d" % s])
                tk.dma("sp", xt[s][:], x[rows, :], writes=["xt%d" % s])
                transpose16(mx[s], "mx%d" % s, mT, "mT", (0, 1))
                for cg in range(4):
                    b = 2 + cg
                    for kc in range(KC):
                        tk.op("pe", lambda e, kc=kc, b=b, cg=cg: e.matmul(
                            psum[b][:, :], lhsT=mT[:, kc, :], rhs=Wo[:, kc, cg * 512:(cg + 1) * 512],
                            start=(kc == 0), stop=(kc == KC - 1)), reads=["mT", "Wo"], writes=["psb%d" % b], inc=(kc == KC - 1))
                    tk.op("dve", lambda e, b=b, cg=cg, s=s: e.tensor_tensor(
                        out=x1[s][:, cg * 512:(cg + 1) * 512], in0=psum[b][:, :], in1=xt[s][:, cg * 512:(cg + 1) * 512],
                        op=ALU.add), reads=["psb%d" % b, "xt%d" % s], writes=["x1_%d" % s])
                tk.dma("pool", x1_d[rows, :], x1[s][:], reads=["x1_%d" % s], key="x1st%d" % s)
                rms_rstd(x1[s][:], "x1_%d" % s, ss, junk)
                tk.op("dve", lambda e, s=s: e.scalar_tensor_tensor(out=h2[:], in0=x1[s][:], scalar=ss[:, 2:3], in1=n2b[:],
                                                                   op0=ALU.mult, op1=ALU.mult),
                      reads=["x1_%d" % s, "ss2", "n2b"], writes=["h2"])
                transpose16(h2, "h2", h2t[s], "h2t%d" % s, (6, 7))
                tk.dma("pool", h2T_d[:, :, rows], h2t[s][:], reads=["h2t%d" % s], key="h2st%d" % s)
            tk.barrier()

        if os.environ.get('BSTOP') == '1':
            tk.final_wait('sp')
            return nc
        with ExitStack() as es2:
            Wq = sb("Wq", [128, KC, D], BF16, es2)
            kT = sb("kTs", [128, 2, 128], BF16, es2)
            h2t = [sb("h2tb%d" % i, [128, KC, 128], BF16, es2) for i in range(2)]
            qpT = [sb("qpT%d" % i, [128, 128], BF16, es2) for i in range(2)]
            sc_l = [sb("sc_%d" % i_, [128, 16, 128], F32, es2) for i_ in range(2)]
            tmp1_l = [sb("tmp1_%d" % i_, [128, 128], F32, es2) for i_ in range(2)]
            st_l = [sb("st_%d" % i_, [128, 16, 16], F32, es2) for i_ in range(2)]
            iu_l = [sb("iu_%d" % i_, [128, 16, 16], U32, es2) for i_ in range(2)]
            itf_l = [sb("itf_%d" % i_, [128, 16, 16], F32, es2) for i_ in range(2)]
            dd_l = [sb("dd_%d" % i_, [128, 16, 16], F32, es2) for i_ in range(2)]
            cand_l = [sb("cand_%d" % i_, [128, 8, 256], F32, es2) for i_ in range(2)]
            tmp2_l = [sb("tmp2_%d" % i_, [128, 256], F32, es2) for i_ in range(2)]
            cf_l = [sb("cf_%d" % i_, [128, 8, 16], F32, es2) for i_ in range(2)]
            pu_l = [sb("pu_%d" % i_, [128, 8, 16], U32, es2) for i_ in range(2)]
            posf_l = [sb("posf_%d" % i_, [128, 8, 16], F32, es2) for i_ in range(2)]
            cs_l = [sb("cs_%d" % i_, [128, 8, 16], F32, es2) for i_ in range(2)]
            zs_l = [sb("zs_%d" % i_, [128, 8], F32, es2) for i_ in range(2)]
            ge_l = [sb("ge_%d" % i_, [128, 8, 16, 16], F32, es2) for i_ in range(2)]
            prod_l = [sb("prod_%d" % i_, [128, 8, 16, 16], F32, es2) for i_ in range(2)]
            k1s_l = [sb("k1s_%d" % i_, [128, 8, 16], F32, es2) for i_ in range(2)]
            k2f_l = [sb("k2f_%d" % i_, [128, 8, 16], F32, es2) for i_ in range(2)]
            res_l = [sb("res_%d" % i_, [128, 3, 128], F32, es2) for i_ in range(2)]
            rst = [sb("rst%d" % i, [128, 3, 128], F32, es2) for i in range(2)]
            for kc in range(KC):
                tk.dma("pool", Wq[:, kc, :], wq[kc * 128:(kc + 1) * 128, :], writes=["Wq"], key="wload")
            tk.dma("pool", kT[:], keysT, writes=["kT"], key="wload")
            PRIV = ['sc', 'tmp1', 'st', 'iu', 'itf', 'dd', 'cand', 'tmp2', 'cf', 'pu', 'posf', 'cs', 'zs', 'ge', 'prod', 'k1s', 'k2f', 'res']

            def b2_tile(ti, tk):
                sc = sc_l[ti % 2]; tmp1 = tmp1_l[ti % 2]; st = st_l[ti % 2]; iu = iu_l[ti % 2]; itf = itf_l[ti % 2]; dd = dd_l[ti % 2]; cand = cand_l[ti % 2]; tmp2 = tmp2_l[ti % 2]; cf = cf_l[ti % 2]; pu = pu_l[ti % 2]; posf = posf_l[ti % 2]; cs = cs_l[ti % 2]; zs = zs_l[ti % 2]; ge = ge_l[ti % 2]; prod = prod_l[ti % 2]; k1s = k1s_l[ti % 2]; k2f = k2f_l[ti % 2]; res = res_l[ti % 2]
                st4 = st[:].rearrange("p (h q) k -> p h q k", q=2)
                itf4 = itf[:].rearrange("p (h q) k -> p h q k", q=2)
                dd4 = dd[:].rearrange("p (h q) k -> p h q k", q=2)
                s = ti % 2
                cols = slice(ti * 128, (ti + 1) * 128)
                tk.dma("sp", h2t[s][:], h2T_d[:, :, cols], writes=["h2tb%d" % s])
                for blk in range(16):
                    b = s
                    p = blk % 2
                    for kc in range(KC):
                        tk.op("pe", lambda e, kc=kc, b=b, blk=blk: e.matmul(
                            psum[b][:, 0:128], lhsT=Wq[:, kc, blk * 128:(blk + 1) * 128], rhs=h2t[s][:, kc, :],
                            start=(kc == 0), stop=(kc == KC - 1)), reads=["Wq", "h2tb%d" % s], writes=["psb%d" % b], inc=(kc == KC - 1))
                    tk.op("act", lambda e, b=b: e.copy(out=qpT[b][:], in_=psum[b][:, 0:128]),
                          reads=["psb%d" % b], writes=["qpT%d" % b])
                    sbk = 2 + 2 * s + (blk // 4) % 2
                    tk.op("pe", lambda e, b=b, p=p, sbk=sbk, blk=blk: e.matmul(
                        psum[sbk][:, (blk % 4) * 128:(blk % 4 + 1) * 128], lhsT=qpT[b][:], rhs=kT[:, p, :],
                        start=True, stop=True), reads=["qpT%d" % b, "kT"], writes=["psb%d" % sbk])
                    if blk % 4 == 3:
                        tk.op("act", lambda e, sbk=sbk, blk=blk: e.copy(
                            out=sc[:, blk - 3:blk + 1, :], in_=psum[sbk][:, :].rearrange("p (a n) -> p a n", a=4)),
                            reads=["psb%d" % sbk], writes=["sc"])
                for blk in range(16):
                    tk.op("dve", lambda e, blk=blk: e.max(out=st[:, blk, 0:8], in_=sc[:, blk, :]),
                          reads=["sc"], writes=["st"])
                    tk.op("dve", lambda e, blk=blk: e.max_index(out=iu[:, blk, 0:8], in_max=st[:, blk, 0:8],
                                                                in_values=sc[:, blk, :]),
                          reads=["sc", "st"], writes=["iu"])
                    tk.op("dve", lambda e, blk=blk: e.match_replace(out=tmp1[:], in_to_replace=st[:, blk, 0:8],
                                                                    in_values=sc[:, blk, :], imm_value=-1e30),
                          reads=["sc", "st"], writes=["tmp1"])
                    tk.op("dve", lambda e, blk=blk: e.max(out=st[:, blk, 8:16], in_=tmp1[:]),
                          reads=["tmp1"], writes=["st"])
                    tk.op("dve", lambda e, blk=blk: e.max_index(out=iu[:, blk, 8:16], in_max=st[:, blk, 8:16],
                                                                in_values=tmp1[:]),
                          reads=["tmp1", "st"], writes=["iu"])
                tk.op("dve", lambda e: e.tensor_copy(out=itf[:], in_=iu[:]), reads=["iu"], writes=["itf"])
                tk.op("dve", lambda e: e.tensor_copy(out=dd[:, :, 0:1], in_=itf[:, :, 0:1]), reads=["itf"], writes=["dd"])
                tk.op("dve", lambda e: e.tensor_tensor(out=dd[:, :, 1:16], in0=itf[:, :, 1:16], in1=itf[:, :, 0:15],
                                                       op=ALU.subtract), reads=["itf"], writes=["dd"])
                a0 = st4[:, :, 0, :].unsqueeze(3).broadcast_to([128, 8, 16, 16])
                a1 = st4[:, :, 1, :].unsqueeze(2).broadcast_to([128, 8, 16, 16])
                cand4 = cand[:].rearrange("p h (a b) -> p h a b", a=16)
                tk.op("pool", lambda e: e.tensor_tensor(out=cand4, in0=a0, in1=a1, op=ALU.add), reads=["st"], writes=["cand"])
                for h in range(8):
                    tk.op("dve", lambda e, h=h: e.max(out=cf[:, h, 0:8], in_=cand[:, h, :]), reads=["cand"], writes=["cf"])
                    tk.op("dve", lambda e, h=h: e.max_index(out=pu[:, h, 0:8], in_max=cf[:, h, 0:8], in_values=cand[:, h, :]),
                          reads=["cand", "cf"], writes=["pu"])
                    tk.op("dve", lambda e, h=h: e.match_replace(out=tmp2[:], in_to_replace=cf[:, h, 0:8],
                                                                in_values=cand[:, h, :], imm_value=-1e30),
                          reads=["cand", "cf"], writes=["tmp2"])
                    tk.op("dve", lambda e, h=h: e.max(out=cf[:, h, 8:16], in_=tmp2[:]), reads=["tmp2"], writes=["cf"])
                    tk.op("dve", lambda e, h=h: e.max_index(out=pu[:, h, 8:16], in_max=cf[:, h, 8:16], in_values=tmp2[:]),
                          reads=["tmp2", "cf"], writes=["pu"])
                tk.op("dve", lambda e: e.tensor_copy(out=posf[:], in_=pu[:]), reads=["pu"], writes=["posf"])
                tk.op("dve", lambda e: e.tensor_tensor(out=cs[:], in0=cf[:], in1=cf[:, :, 0:1].broadcast_to([128, 8, 16]),
                                                       op=ALU.subtract), reads=["cf"], writes=["cs"])
                tk.op("act", lambda e: e.activation(out=cs[:], in_=cs[:], func=AF.Exp), reads=["cs"], writes=["cs"])
                tk.op("dve", lambda e: e.tensor_reduce(out=zs[:], in_=cs[:], axis=AX.X, op=ALU.add), reads=["cs"], writes=["zs"])
                tk.op("dve", lambda e: e.reciprocal(out=zs[:], in_=zs[:]), reads=["zs"], writes=["zs"])
                res_g = res[:, 2, :].rearrange("p (h k) -> p h k", h=8)
                tk.op("dve", lambda e: e.tensor_tensor(out=res_g, in0=cs[:], in1=zs[:].unsqueeze(2).broadcast_to([128, 8, 16]),
                                                       op=ALU.mult), reads=["cs", "zs"], writes=["res"])
                pos_b = posf[:].unsqueeze(3).broadcast_to([128, 8, 16, 16])
                thr_b = thr16[:].unsqueeze(1).unsqueeze(1).broadcast_to([128, 8, 16, 16])
                tk.op("dve", lambda e: e.tensor_tensor(out=ge[:], in0=pos_b, in1=thr_b, op=ALU.is_ge),
                      reads=["posf", "c3"], writes=["ge"])
                d1_b = dd4[:, :, 0, :].unsqueeze(2).broadcast_to([128, 8, 16, 16])
                tk.op("pool", lambda e: e.tensor_tensor(out=prod[:], in0=ge[:], in1=d1_b, op=ALU.mult),
                      reads=["ge", "dd"], writes=["prod"])
                res_i = res[:, 0, :].rearrange("p (h k) -> p h k", h=8)
                tk.op("dve", lambda e: e.tensor_reduce(out=res_i, in_=prod[:], axis=AX.X, op=ALU.add),
                      reads=["prod"], writes=["res"])
                tk.op("dve", lambda e: e.tensor_reduce(out=k1s[:], in_=ge[:], axis=AX.X, op=ALU.add), reads=["ge"], writes=["k1s"])
                tk.op("dve", lambda e: e.tensor_scalar(out=k1s[:], in0=k1s[:], scalar1=-16.0, scalar2=16.0, op0=ALU.mult,
                                                       op1=ALU.add), reads=["k1s"], writes=["k1s"])
                tk.op("dve", lambda e: e.tensor_tensor(out=k2f[:], in0=posf[:], in1=k1s[:], op=ALU.add),
                      reads=["posf", "k1s"], writes=["k2f"])
                k2_b = k2f[:].unsqueeze(3).broadcast_to([128, 8, 16, 16])
                io_b = iota16[:].unsqueeze(1).unsqueeze(1).broadcast_to([128, 8, 16, 16])
                tk.op("dve", lambda e: e.tensor_tensor(out=ge[:], in0=k2_b, in1=io_b, op=ALU.is_ge),
                      reads=["k2f", "c4"], writes=["ge"])
                d2_b = dd4[:, :, 1, :].unsqueeze(2).broadcast_to([128, 8, 16, 16])
                tk.op("pool", lambda e: e.tensor_tensor(out=prod[:], in0=ge[:], in1=d2_b, op=ALU.mult),
                      reads=["ge", "dd"], writes=["prod"])
                res_j = res[:, 1, :].rearrange("p (h k) -> p h k", h=8)
                tk.op("dve", lambda e: e.tensor_reduce(out=res_j, in_=prod[:], axis=AX.X, op=ALU.add),
                      reads=["prod"], writes=["res"])
                for q in range(3):
                    tk.op("pe", lambda e, q=q: e.transpose(out=psum[6 + s][:, q * 128:(q + 1) * 128], in_=res[:, q, :],
                                                           identity=ident_f[:]), reads=["res", "c1"], writes=["psb%d" % (6 + s)])
                tk.op("act", lambda e, s=s: e.copy(out=rst[s][:], in_=psum[6 + s][:, 0:384].rearrange("p (q t) -> p q t", q=3)),
                      reads=["psb%d" % (6 + s)], writes=["rst%d" % s])
                tk.dma("pool", slot_d[:, :, cols], rst[s][:], reads=["rst%d" % s], key="rst%d" % s)

            for t0_ in range(0, NTT, 2):
                qs_ = []
                for ti in range(t0_, min(NTT, t0_ + 2)):
                    q_ = OpQueue()
                    q_.suffix = "_%d" % (ti % 2)
                    q_.priv = set(PRIV)
                    b2_tile(ti, q_)
                    qs_.append(q_)
                emit_interleaved(tk, qs_)
            tk.barrier()

        if os.environ.get('BSTOP') == '2':
            tk.final_wait('sp')
            return nc
        with ExitStack() as es3:
            G = sb("G", [128, 128, PB], BF16, es3)
            ut = [sb("ut%d" % i, [128, KC, 128], BF16, es3) for i in range(7)]
            vt = [sb("vt%d" % i, [128, D], BF16, es3) for i in range(7)]
            h2b = [sb("h2b%d" % i, [128, KC, PB], BF16, es3) for i in range(2)]
            slots = [sb("slots%d" % i, [128, 3, PB], F32, es3) for i in range(2)]
            gl = [sb("gl%d" % i, [128, PB], BF16, es3) for i in range(2)]
            ohi = [sb("ohi%d" % i, [128, 128], BF16, es3) for i in range(4)]
            ohj = [sb("ohj%d" % i, [128, 128], BF16, es3) for i in range(4)]
            x1t = sb("x1t", [128, D], F32, es3)
            xo = [sb("xo%d" % i, [128, D], F32, es3) for i in range(2)]
            junk = sb("junk3", [128, D], BF16, es3)
            fnb = sb("fnb", [128, D], F32, es3)
            ss = sb("ss3", [128, 4], F32, es3)
            tk.dma("sp", fnb[:], fnw, writes=["fnb"])
            ucnt = [0]
            vcnt = [0]
            ocnt = [0]
            for blk in range(NBLK):
                bs = blk % 2
                cols = slice(blk * PB, (blk + 1) * PB)
                tk.dma("sp", slots[bs][:], slot_d[:, :, cols], writes=["slots%d" % bs])
                tk.dma("sp", h2b[bs][:], h2T_d[:, :, cols], writes=["h2b%d" % bs])
                for t in range(PB):
                    o = t % 4
                    tk.op("dve", lambda e, o=o, t=t, bs=bs: e.tensor_scalar(
                        out=ohi[o][:], in0=iota_row[:], scalar1=slots[bs][:, 0, t:t + 1], scalar2=slots[bs][:, 2, t:t + 1],
                        op0=ALU.is_equal, op1=ALU.mult), reads=["slots%d" % bs, "c2"], writes=["ohi%d" % o])
                    tk.op("dve", lambda e, o=o, t=t, bs=bs: e.tensor_scalar(
                        out=ohj[o][:], in0=iota_row[:], scalar1=slots[bs][:, 1, t:t + 1], scalar2=None,
                        op0=ALU.is_equal), reads=["slots%d" % bs, "c2"], writes=["ohj%d" % o])
                    gb_ = (t // 4) % 2
                    tk.op("pe", lambda e, o=o, gb_=gb_: e.matmul(psum[gb_][:, o * 128:(o + 1) * 128], lhsT=ohi[o][:],
                                                                 rhs=ohj[o][:], start=True, stop=True),
                          reads=["ohi%d" % o, "ohj%d" % o], writes=["psb%d" % gb_])
                    if o == 3:
                        t0 = t - 3
                        tk.op("act", lambda e, gb_=gb_, t0=t0: e.copy(
                            out=G[:, :, t0:t0 + 4], in_=psum[gb_][:, :].rearrange("p (t j) -> p j t", t=4)),
                            reads=["psb%d" % gb_], writes=["G"])
                for j in range(NJ):
                    us = ucnt[0] % 7
                    ucnt[0] += 1
                    tk.dma("sp", ut[us][:], u16[j].rearrange("p (k i) -> p k i", k=KC), writes=["ut%d" % us])
                    b = 2 + j % 2
                    for kc in range(KC):
                        tk.op("pe", lambda e, kc=kc, b=b, us=us, bs=bs: e.matmul(
                            psum[b][:, 0:PB], lhsT=ut[us][:, kc, :], rhs=h2b[bs][:, kc, :], start=(kc == 0),
                            stop=(kc == KC - 1)), reads=["ut%d" % us, "h2b%d" % bs], writes=["psb%d" % b], inc=(kc == KC - 1))
                    g = j % 2
                    tk.op("act", lambda e, b=b, g=g: e.activation(out=gl[g][:], in_=psum[b][:, 0:PB], func=AF.Gelu),
                          reads=["psb%d" % b], writes=["gl%d" % g])
                    tk.op("dve", lambda e, g=g, j=j: e.tensor_tensor(out=G[:, j, :], in0=gl[g][:], in1=G[:, j, :], op=ALU.mult),
                          reads=["gl%d" % g, "G"], writes=["G"])
                for j in range(NJ):
                    vs = vcnt[0] % 7
                    vcnt[0] += 1
                    tk.dma("sp", vt[vs][:], v16[j], writes=["vt%d" % vs])
                    for tt in range(TPB):
                        for cg in range(4):
                            b = tt * 4 + cg
                            tk.op("pe", lambda e, j=j, tt=tt, cg=cg, b=b, vs=vs: e.matmul(
                                psum[b][:, :], lhsT=G[:, j, tt * 128:(tt + 1) * 128], rhs=vt[vs][:, cg * 512:(cg + 1) * 512],
                                start=(j == 0), stop=(j == NJ - 1)), reads=["G", "vt%d" % vs], writes=["psb%d" % b])
                for tt in range(TPB):
                    rows = slice(blk * PB + tt * 128, blk * PB + (tt + 1) * 128)
                    o = ocnt[0] % 2
                    ocnt[0] += 1
                    tk.dma("sp", x1t[:], x1_d[rows, :], writes=["x1t"])
                    for cg in range(4):
                        b = tt * 4 + cg
                        tk.op("dve", lambda e, b=b, cg=cg, o=o: e.tensor_tensor(
                            out=xo[o][:, cg * 512:(cg + 1) * 512], in0=psum[b][:, :], in1=x1t[:, cg * 512:(cg + 1) * 512],
                            op=ALU.add), reads=["psb%d" % b, "x1t"], writes=["xo%d" % o])
                    rms_rstd(xo[o][:], "xo%d" % o, ss, junk)
                    tk.op("dve", lambda e, o=o: e.scalar_tensor_tensor(out=xo[o][:], in0=xo[o][:], scalar=ss[:, 2:3],
                                                                       in1=fnb[:], op0=ALU.mult, op1=ALU.mult),
                          reads=["xo%d" % o, "ss2", "fnb"], writes=["xo%d" % o])
                    tk.dma("pool", out[rows, :], xo[o][:], reads=["xo%d" % o], key="ost%d" % o)
            tk.barrier()
        tk.final_wait("sp")
    print("phase B instructions (cumulative):", tk.ninst)
    return nc


def host_inputs_b(inp, xflat, mixed_full, TCs):
    c = host_consts_b()
    U = np.asarray(inp["peer_u"][0])
    V = np.asarray(inp["peer_v"][0])
    uh = np.ascontiguousarray(U.reshape(128, 128, KC, 128).transpose(1, 3, 2, 0)).reshape(128, 128, D)
    vh = np.ascontiguousarray(V.reshape(128, 128, D).transpose(1, 0, 2))
    keys = np.asarray(inp["peer_keys"][0])
    keysT = np.ascontiguousarray(keys.transpose(2, 0, 1))
    wout = np.ascontiguousarray(np.asarray(inp["w_out"][0]))
    wq = np.ascontiguousarray(np.asarray(inp["peer_w_q"][0]))
    n2w = np.ascontiguousarray(np.broadcast_to(np.asarray(inp["norm2_w"][0])[None, :], (128, D)))
    fnw = np.ascontiguousarray(np.broadcast_to(np.asarray(inp["final_norm_w"])[None, :], (128, D)))
    maps = []
    for core in range(8):
        r0 = core * TCs
        m = {"x": np.ascontiguousarray(xflat[r0:r0 + TCs]),
             "mixed": np.ascontiguousarray(mixed_full[r0:r0 + TCs]),
             "wout": wout, "wq": wq, "n2w": n2w, "fnw": fnw, "keysT": keysT, "uh": uh, "vh": vh}
        m.update(c)
        maps.append(m)
    return maps


def build_fused(nc, S):
    TC = 2 * S // 8
    NK = S // 512
    tk = Trk(nc)
    mixloc = [nc.dram_tensor("mixloc%d" % k, [512, 512], BF16) for k in range(NK)]
    gath = [nc.dram_tensor("gath%d" % k, [8 * 512, 512], BF16) for k in range(NK)]
    uh = nc.dram_tensor("b_uh", [128, 128, D], F32, kind="ExternalInput").ap()
    vh = nc.dram_tensor("b_vh", [128, 128, D], F32, kind="ExternalInput").ap()
    u16 = nc.dram_tensor("b_u16", [128, 128, D], BF16, kind="Internal").ap()
    v16 = nc.dram_tensor("b_v16", [128, 128, D], BF16, kind="Internal").ap()
    tk.lazy_keys.add("cvt")
    for j in range(128):
        tk.dma("pool", u16[j], uh[j], writes=["u16"], key="cvt")
        tk.dma("pool", v16[j], vh[j], writes=["v16"], key="cvt")
    def gather_block(k):
        tk.coll(lambda g, k=k: g.collective_compute("AllGather", ALU.bypass, replica_groups=[list(range(8))],
                                                    ins=[mixloc[k].ap().opt()], outs=[gath[k].ap().opt()]),
                reads=["ml%d" % c for c in range(4 * k, 4 * k + 4)], writes=["gath%d" % k])

    build_phase_a(nc, S, tk=tk, mix_dst=lambda t0, c0, c1: mixloc[t0 // 512][t0 % 512:t0 % 512 + 128, c0:c1],
                  on_block_done=gather_block)
    tk.lazy_keys.discard("cvt")
    build_phase_b(nc, TC, tk=tk, gath=gath, pfx="b_", pre=(u16, v16))
    return nc


def wout_perm():
    perm = []
    for g in range(4):
        perm += list(range(g * 256, (g + 1) * 256)) + list(range(1024 + g * 256, 1024 + (g + 1) * 256))
    return np.array(perm)


def host_inputs_fused(inp, S):
    maps = host_inputs_a(inp, S)
    c = host_consts_b()
    TC = 2 * S // 8
    H = S // 8
    U = np.asarray(inp["peer_u"][0])
    V = np.asarray(inp["peer_v"][0])
    uh = np.ascontiguousarray(U.reshape(128, 128, KC, 128).transpose(1, 3, 2, 0)).reshape(128, 128, D)
    vh = np.ascontiguousarray(V.reshape(128, 128, D).transpose(1, 0, 2))
    keysT = np.ascontiguousarray(np.asarray(inp["peer_keys"][0]).transpose(2, 0, 1))
    wout = np.ascontiguousarray(np.asarray(inp["w_out"][0])[wout_perm(), :])
    wq = np.ascontiguousarray(np.asarray(inp["peer_w_q"][0]))
    n2w = np.ascontiguousarray(np.broadcast_to(np.asarray(inp["norm2_w"][0])[None, :], (128, D)))
    fnw = np.ascontiguousarray(np.broadcast_to(np.asarray(inp["final_norm_w"])[None, :], (128, D)))
    x = np.asarray(inp["x"])
    for d in range(8):
        m = maps[d]
        m["b_x"] = np.ascontiguousarray(np.concatenate([x[0, d * H:(d + 1) * H], x[1, d * H:(d + 1) * H]], axis=0))
        ws = np.zeros((128, 8), np.float32)
        ws[:, d] = 1.0
        m["b_wsel"] = ws
        m.update({"b_wout": wout, "b_wq": wq, "b_n2w": n2w, "b_fnw": fnw, "b_keysT": keysT, "b_uh": uh, "b_vh": vh})
        for k, v in c.items():
            m["b_" + k] = v
    return maps


def gather_out(res, S):
    H = S // 8
    out = np.zeros((2, S, D), np.float32)
    for d in range(8):
        o = res.results[d]["b_out"]
        out[0, d * H:(d + 1) * H] = o[0:H]
        out[1, d * H:(d + 1) * H] = o[H:2 * H]
    return out


S_FULL = 16384


def kernel(**inputs):
    inp = {k: np.asarray(v) for k, v in inputs.items()}
    S = S_FULL
    nc = bass.Bass("TRN2", target_bir_lowering=False)
    build_fused(nc, S)
    maps = host_inputs_fused(inp, S)
    res = run_bass_kernel_spmd(nc, maps, core_ids=list(range(8)))
    return gather_out(res, S).astype(np.float32)
```

```python
import numpy as np
import ml_dtypes
from contextlib import ExitStack
import concourse.bass as bass
import concourse.mybir as mybir
from concourse.bass_utils import run_bass_kernel_spmd

F32 = mybir.dt.float32
BF16 = mybir.dt.bfloat16
I32 = mybir.dt.int32
U32 = mybir.dt.uint32
AF = mybir.ActivationFunctionType
ALU = mybir.AluOpType
AX = mybir.AxisListType

D = 2048
KC = 16
EPS = 1e-6


class Trk:
    def __init__(self, nc):
        self.nc = nc
        self.engs = {"pe": nc.tensor, "act": nc.scalar, "dve": nc.vector,
                     "pool": nc.gpsimd, "sp": nc.sync}
        self.sem = {}
        self.cnt = {}
        for e in ("pe", "act", "dve", "pool"):
            self.sem[e] = nc.alloc_semaphore(name="s_" + e)
            self.cnt[e] = 0
        self.dsem = {}
        self.waited = {}
        self.lastw = {}
        self.readers = {}
        self.ninst = 0
        self.lazy_keys = set()

    def _wait(self, eng, ev):
        sem, val, src, key = ev
        k = (eng, key)
        if self.waited.get(k, 0) >= val:
            return
        self.engs[eng].wait_ge(sem, val)
        self.waited[k] = val

    def _deps(self, eng, reads, writes):
        for b in reads:
            ev = self.lastw.get(b)
            if ev is not None and not (ev[2] == eng and eng == "pe"):
                self._wait(eng, ev)
        for b in writes:
            ev = self.lastw.get(b)
            if ev is not None and ev[2] != eng:
                self._wait(eng, ev)
            for ev in self.readers.get(b, ()):
                if ev[2] != eng:
                    self._wait(eng, ev)

    def _record(self, ev, reads, writes):
        for b in reads:
            self.readers.setdefault(b, []).append(ev)
        for b in writes:
            self.lastw[b] = ev
            self.readers[b] = []

    def op(self, eng, fn, reads=(), writes=(), inc=True):
        self._deps(eng, reads, writes)
        inst = fn(self.engs[eng])
        if inc:
            self.cnt[eng] += 1
            inst.then_inc(self.sem[eng], 1)
            ev = (self.sem[eng], self.cnt[eng], eng, eng)
        else:
            assert eng == "pe"
            ev = (self.sem[eng], self.cnt[eng] + 1, eng, eng)
        self._record(ev, reads, writes)
        self.ninst += 1
        return inst

    def dma(self, q, out, in_, reads=(), writes=(), key=None, **kw):
        if key is None:
            key = writes[0] if writes else reads[0]
        if key not in self.dsem:
            self.dsem[key] = [self.nc.alloc_semaphore(name="d%d" % len(self.dsem)), 0]
        self._deps(q, reads, writes)
        ds = self.dsem[key]
        inst = self.engs[q].dma_start(out=out, in_=in_, **kw)
        ds[1] += 16
        inst.then_inc(ds[0], 16)
        ev = (ds[0], ds[1], "dma", ("d", key))
        self._record(ev, reads, writes)
        self.ninst += 1
        return inst

    def coll(self, fn, reads=(), writes=()):
        if "cc" not in self.sem:
            self.sem["cc"] = self.nc.alloc_semaphore(name="s_cc")
            self.cnt["cc"] = 0
        self._deps("pool", reads, writes)
        inst = fn(self.engs["pool"])
        self.cnt["cc"] += 1
        inst.then_inc(self.sem["cc"])
        ev = (self.sem["cc"], self.cnt["cc"], "cc", "cc")
        self._record(ev, reads, writes)
        self.ninst += 1
        return inst

    def barrier(self):
        evs = [(self.sem[e], self.cnt[e], e, e) for e in self.sem if self.cnt[e] > 0]
        evs += [(v[0], v[1], "dma", ("d", k)) for k, v in self.dsem.items()
                if v[1] > 0 and k not in self.lazy_keys]
        for e in ("pe", "act", "dve", "pool", "sp"):
            for ev in evs:
                if ev[2] != e:
                    self._wait(e, ev)
        keep = {b: ev for b, ev in self.lastw.items() if ev[2] == "dma" and ev[3][1] in self.lazy_keys}
        self.lastw = keep
        self.readers = {}

    def final_wait(self, q="sp"):
        for k, v in self.dsem.items():
            if v[1] > 0:
                self._wait(q, (v[0], v[1], "dma", ("d", k)))


class OpQueue:
    def __init__(self):
        self.q = []
        self.suffix = ""
        self.priv = set()

    def _fix(self, k):
        for nm in ("reads", "writes"):
            if nm in k:
                k[nm] = [x + self.suffix if x in self.priv else x for x in k[nm]]
        return k

    def op(self, *a, **k):
        self.q.append(("op", a, self._fix(k)))

    def dma(self, *a, **k):
        self.q.append(("dma", a, self._fix(k)))


def emit_interleaved(tk, queues):
    n = max(len(q.q) for q in queues)
    for i in range(n):
        for q in queues:
            if i < len(q.q):
                kind, a, k = q.q[i]
                getattr(tk, kind)(*a, **k)


import os
PH = os.environ.get('PH', 'PAM')
SK = os.environ.get('SK', '')

NCOL = 1540
C_FQ0, C_FQ1, C_FK0, C_FK1, C_FV, C_G, C_MQ, C_MK, C_MVO = 0, 128, 256, 384, 512, 768, 772, 900, 1028


def host_consts():
    c = {}
    c["ident_bf"] = np.eye(128, dtype=np.float32).astype(ml_dtypes.bfloat16)
    c["ident_f"] = np.eye(128, dtype=np.float32)
    r = np.arange(128)
    c["triu"] = (r[:, None] <= r[None, :]).astype(np.float32)
    c["ones_f"] = np.ones((128, 128), np.float32)
    es = np.zeros((128, 128), np.float32); es[0, :] = 1.0
    c["esel"] = es
    t = np.arange(512)
    m = np.stack([(r[:, None] + 128 * rr <= t[None, :]) for rr in range(4)], axis=1)
    c["amask"] = m.astype(np.float32).astype(ml_dtypes.bfloat16)
    c["mmask"] = ((r[:, None] <= r[None, :]).astype(np.float32) * (128 ** -0.5)).astype(np.float32)
    return c


def build_phase_a(nc, S, tk=None, mix_dst=None, on_block_done=None, p_hook=None):
    NT = S // 128
    NB = S // 512
    own_tk = tk is None
    if own_tk:
        tk = Trk(nc)
    dt = nc.dram_tensor
    x = dt("x", [int(os.environ.get("XROWS", S)), D], F32, kind="ExternalInput").ap()
    wc = dt("wc", [D, NCOL], F32, kind="ExternalInput").ap()
    n1w = dt("n1w", [128, KC], F32, kind="ExternalInput").ap()
    gbias = dt("gbias", [128, 4], F32, kind="ExternalInput").ap()
    convp = dt("convp", [128, 10], F32, kind="ExternalInput").ap()
    hnw = dt("hnw", [128, 512], F32, kind="ExternalInput").ap()
    ident_bf_d = dt("ident_bf", [128, 128], BF16, kind="ExternalInput").ap()
    ident_f_d = dt("ident_f", [128, 128], F32, kind="ExternalInput").ap()
    triu_d = dt("triu", [128, 128], F32, kind="ExternalInput").ap()
    ones_d = dt("ones_f", [128, 128], F32, kind="ExternalInput").ap()
    esel_d = dt("esel", [128, 128], F32, kind="ExternalInput").ap()
    amask_d = dt("amask", [128, 4, 512], BF16, kind="ExternalInput").ap()
    mmask_d = dt("mmask", [128, 128], F32, kind="ExternalInput").ap()
    if mix_dst is None:
        mixed = dt("mixed", [int(os.environ.get("MROWS", S)), 512], BF16, kind="ExternalOutput").ap()
        mix_dst = lambda t0, c0, c1: mixed[t0:t0 + 128, c0:c1]
    qt_d = [dt("qt%d" % h, [128, S], BF16, kind="Internal").ap() for h in range(2)]
    kt_d = [dt("kt%d" % h, [128, S], BF16, kind="Internal").ap() for h in range(2)]
    va_d = [dt("va%d" % h, [128, S // 128, 130], BF16, kind="Internal").ap() for h in range(2)]
    mq_d = dt("mqT", [128, S], F32, kind="Internal").ap()
    mk_d = dt("mkT", [128, S], F32, kind="Internal").ap()
    mvo_d = dt("mvo", [S, 512], F32, kind="Internal").ap()
    if os.environ.get("DUMMY"):
        dummy_d = dt("dummyx", [int(os.environ["DUMMY"]), 512], F32, kind="Internal").ap()

    with ExitStack() as es0:
        def sb(name, shape, dtype, es=es0):
            return es.enter_context(nc.sbuf_tensor(name, shape, dtype))
        ident_bf = sb("ident_bf_s", [128, 128], BF16)
        ident_f = sb("ident_f_s", [128, 128], F32)
        triu = sb("triu_s", [128, 128], F32)
        ones_f = sb("ones_s", [128, 128], F32)
        esel = sb("esel_s", [128, 128], F32)
        amask = sb("amask_s", [128, 4, 512], BF16)
        mmask = sb("mmask_s", [128, 128], F32)
        gb = sb("gb_s", [128, 4], F32)
        ngb = sb("ngb_s", [128, 4], F32)
        cvp = sb("cvp_s", [128, 10], F32)
        hnw_s = sb("hnw_s", [128, 512], F32)
        n1w_s = sb("n1w_s", [128, KC], F32)
        gates = sb("gates_s", [128, NT, 4], F32)
        epsc = sb("epsc", [128, 1], F32)
        onec = sb("onec", [128, 1], F32)
        psum = [es0.enter_context(nc.psum_tensor("ps%d" % i, [128, 512], F32)) for i in range(8)]
        for i, (s_, d_) in enumerate([(ident_bf, ident_bf_d), (ident_f, ident_f_d), (triu, triu_d),
                                      (ones_f, ones_d), (esel, esel_d), (amask, amask_d), (mmask, mmask_d),
                                      (gb, gbias), (cvp, convp), (hnw_s, hnw), (n1w_s, n1w)]):
            tk.dma("sp", s_[:], d_, writes=["c%d" % i], key="const")
        tk.barrier()
        tk.op("dve", lambda e: e.memset(epsc[:], EPS), writes=["epsc"])
        tk.op("dve", lambda e: e.memset(onec[:], 1.0), writes=["onec"])
        tk.op("dve", lambda e: e.tensor_scalar(out=ngb[:], in0=gb[:], scalar1=-1.0, scalar2=None, op0=ALU.mult),
              reads=["c7"], writes=["ngb"])

        with ExitStack() as es1:
            W = sb("W", [128, KC, NCOL], BF16, es1)
            wst = [sb("wst%d" % i, [128, NCOL], F32, es1) for i in range(2)]
            xt = [sb("xt%d" % i, [128, D], F32, es1) for i in range(2)]
            xn = [sb("xn%d" % i, [128, D], BF16, es1) for i in range(2)]
            junk = sb("junk", [128, D], BF16, es1)
            hT = [sb("hT%d" % i, [128, KC, 512], BF16, es1) for i in range(2)]
            ss = sb("ss", [128, 4], F32, es1)
            fst = [sb("fst%d" % i, [128, 512], BF16, es1) for i in range(4)]
            mst = [sb("mst%d" % i, [128, 512], F32, es1) for i in range(4)]
            vst = [sb("vst%d" % i, [128, 2, 130], BF16, es1) for i in range(2)]
            for i in range(2):
                tk.op("dve", lambda e, i=i: e.memset(vst[i][:, :, 128:129], 1.0), writes=["vst%d" % i])
                tk.op("dve", lambda e, i=i: e.memset(vst[i][:, :, 129:130], 0.0), writes=["vst%d" % i])
            for kc in range(KC):
                s = kc % 2
                tk.dma("sp", wst[s][:], wc[kc * 128:(kc + 1) * 128, :], writes=["wst%d" % s])
                tk.op("dve", lambda e, kc=kc, s=s: e.tensor_scalar(
                    out=W[:, kc, :], in0=wst[s][:], scalar1=n1w_s[:, kc:kc + 1], scalar2=None, op0=ALU.mult),
                    reads=["wst%d" % s, "c10"], writes=["W"])
            pb = [2]

            def nbank():
                b = pb[0]
                pb[0] = 2 + (pb[0] - 2 + 1) % 6
                return b
            fcnt = [0]
            mcnt = [0]
            def p_stage1(jb):
                hs = jb % 2
                for tt in range(4):
                    ti = jb * 4 + tt
                    s = ti % 2
                    tk.dma("sp", xt[s][:], x[ti * 128:(ti + 1) * 128, :], writes=["xt%d" % s])
                    tk.op("act", lambda e, s=s: e.activation(out=junk[:], in_=xt[s][:], func=AF.Square,
                                                             accum_out=ss[:, 0:1]),
                          reads=["xt%d" % s], writes=["junk", "ss0"])
                    tk.op("act", lambda e: e.activation(out=ss[:, 1:2], in_=ss[:, 0:1], func=AF.Ln,
                                                        scale=1.0 / D, bias=epsc[:]),
                          reads=["ss0", "epsc"], writes=["ss1"])
                    tk.op("act", lambda e: e.activation(out=ss[:, 2:3], in_=ss[:, 1:2], func=AF.Exp, scale=-0.5),
                          reads=["ss1"], writes=["ss2"])
                    tk.op("dve", lambda e, s=s: e.tensor_scalar(out=xn[s][:], in0=xt[s][:], scalar1=ss[:, 2:3],
                                                                scalar2=None, op0=ALU.mult),
                          reads=["xt%d" % s, "ss2"], writes=["xn%d" % s])
                    for half in range(2):
                        pst = psum[half][:, :].bitcast(BF16)
                        for k8 in range(8):
                            kc = half * 8 + k8
                            tk.op("pe", lambda e, kc=kc, k8=k8, pst=pst, s=s: e.transpose(
                                out=pst[:, k8 * 128:(k8 + 1) * 128], in_=xn[s][:, kc * 128:(kc + 1) * 128],
                                identity=ident_bf[:]),
                                reads=["xn%d" % s, "c0"], writes=["psb%d" % half], inc=(k8 == 7))
                        eng = "act" if half == 0 else "dve"
                        src = pst.rearrange("p (k t) -> p k t", k=8)
                        dst = hT[hs][:, half * 8:(half + 1) * 8, tt * 128:(tt + 1) * 128]
                        if eng == "act":
                            tk.op("act", lambda e, src=src, dst=dst: e.copy(out=dst, in_=src),
                                  reads=["psb%d" % half], writes=["hT%d" % hs])
                        else:
                            tk.op("dve", lambda e, src=src, dst=dst: e.tensor_copy(out=dst, in_=src),
                                  reads=["psb%d" % half], writes=["hT%d" % hs])

            def p_stage2(jb):
                hs = jb % 2
                if p_hook is not None:
                    p_hook(jb, NB)
                for (c0, kind, dst_d) in [(C_FQ0, "f", qt_d[0]), (C_FQ1, "f", qt_d[1]), (C_FK0, "f", kt_d[0]),
                                          (C_FK1, "f", kt_d[1]), (C_MQ, "m", mq_d), (C_MK, "m", mk_d)]:
                    b = nbank()
                    for kc in range(KC):
                        tk.op("pe", lambda e, kc=kc, b=b, c0=c0: e.matmul(
                            psum[b][:, :], lhsT=W[:, kc, c0:c0 + 128], rhs=hT[hs][:, kc, :],
                            start=(kc == 0), stop=(kc == KC - 1)),
                            reads=["W", "hT%d" % hs], writes=["psb%d" % b], inc=(kc == KC - 1))
                    if kind == "f":
                        f = fcnt[0] % 4
                        fcnt[0] += 1
                        tk.op("act", lambda e, b=b, f=f: e.copy(out=fst[f][:], in_=psum[b][:, :]),
                              reads=["psb%d" % b], writes=["fst%d" % f])
                        if 'f' not in SK:
                            tk.dma("pool", dst_d[:, jb * 512:(jb + 1) * 512], fst[f][:], reads=["fst%d" % f],
                                   key="fst%d" % f)
                    else:
                        f = mcnt[0] % 4
                        mcnt[0] += 1
                        tk.op("dve", lambda e, b=b, f=f: e.tensor_copy(out=mst[f][:], in_=psum[b][:, :]),
                              reads=["psb%d" % b], writes=["mst%d" % f])
                        if 'm' not in SK:
                            tk.dma("pool", dst_d[:, jb * 512:(jb + 1) * 512], mst[f][:], reads=["mst%d" % f],
                                   key="mst%d" % f)
                for tt in range(4):
                    ti = jb * 4 + tt
                    tsl = slice(tt * 128, (tt + 1) * 128)
                    b = nbank()
                    for kc in range(KC):
                        tk.op("pe", lambda e, kc=kc, b=b, tsl=tsl: e.matmul(
                            psum[b][:, 0:260], lhsT=hT[hs][:, kc, tsl], rhs=W[:, kc, C_FV:C_FV + 260],
                            start=(kc == 0), stop=(kc == KC - 1)),
                            reads=["W", "hT%d" % hs], writes=["psb%d" % b], inc=(kc == KC - 1))
                    vs = ti % 2
                    tk.op("act", lambda e, b=b, vs=vs: e.copy(
                        out=vst[vs][:, :, 0:128], in_=psum[b][:, 0:256].rearrange("p (h d) -> p h d", h=2)),
                        reads=["psb%d" % b], writes=["vst%d" % vs])
                    tk.op("act", lambda e, b=b, ti=ti: e.copy(out=gates[:, ti, :], in_=psum[b][:, 256:260]),
                          reads=["psb%d" % b], writes=["gates"])
                    for h in (range(2) if 'v' not in SK else []):
                        tk.dma("pool", va_d[h][:, ti, :], vst[vs][:, h, :],
                               reads=["vst%d" % vs], key="vst%d_%d" % (vs, h))
                    b = nbank()
                    for kc in range(KC):
                        tk.op("pe", lambda e, kc=kc, b=b, tsl=tsl: e.matmul(
                            psum[b][:, :], lhsT=hT[hs][:, kc, tsl], rhs=W[:, kc, C_MVO:C_MVO + 512],
                            start=(kc == 0), stop=(kc == KC - 1)),
                            reads=["W", "hT%d" % hs], writes=["psb%d" % b], inc=(kc == KC - 1))
                    f = mcnt[0] % 4
                    mcnt[0] += 1
                    tk.op("dve", lambda e, b=b, f=f: e.tensor_copy(out=mst[f][:], in_=psum[b][:, :]),
                          reads=["psb%d" % b], writes=["mst%d" % f])
                    if 'o' not in SK:
                        tk.dma("pool", mvo_d[ti * 128:(ti + 1) * 128, :], mst[f][:], reads=["mst%d" % f],
                               key="mst%d" % f)

            NBP = min(NB, int(os.environ.get('NBLIM', '999')))
            p_stage1(0)
            for jb in range(NBP):
                if jb + 1 < NBP:
                    p_stage1(jb + 1)
                p_stage2(jb)
            tk.barrier()

        NQ = NB
        SC = 128 ** -0.5
        with ExitStack() as es2:
            KT = sb("KT", [128, S], BF16, es2)
            VA = sb("VA", [128, NT, 130], BF16, es2)
            qt = [sb("qtb%d" % i, [128, 512], BF16, es2) for i in range(2)]
            PT = [sb("PT%d" % i, [128, 512], BF16, es2) for i in range(4)]
            lfn = sb("lfn", [128, NT], F32, es2)
            tmpg = sb("tmpg", [128, NT], F32, es2)
            tot = sb("tot", [128, NT], F32, es2)
            offi = sb("offi", [128, NT], F32, es2)
            onesn = sb("onesn", [128, NT], F32, es2)
            G = sb("G", [128, NT], F32, es2)
            cb = sb("cb", [128, NQ], F32, es2)
            biasj = [sb("biasj%d" % i, [128, NT], F32, es2) for i in range(2)]
            sm = sb("sm", [128, 8], F32, es2)
            ot = [sb("ot%d" % i, [128, 128], F32, es2) for i in range(2)]
            oj = sb("oj", [128, 128], F32, es2)
            ob = [sb("ob%d" % i, [128, 128], BF16, es2) for i in range(2)]
            tk.op("dve", lambda e: e.memset(onesn[:], 1.0), writes=["onesn"])
            ptc = [0]
            oc = [0]
            for hh in (range(2) if 'A' in PH else []):
                for c0 in range(0, S, 2048):
                    tk.dma("sp", KT[:, c0:min(S, c0 + 2048)], kt_d[hh][:, c0:min(S, c0 + 2048)], writes=["KT"])
                for c0 in range(0, NT, 16):
                    tk.dma("sp", VA[:, c0:min(NT, c0 + 16), :], va_d[hh][:, c0:min(NT, c0 + 16), :], writes=["VA"])
                tk.op("act", lambda e, hh=hh: e.activation(out=tmpg[:], in_=gates[:, :, hh], func=AF.Exp,
                                                           scale=-1.0, bias=ngb[:, hh:hh + 1]),
                      reads=["gates", "ngb"], writes=["tmpg"])
                tk.op("act", lambda e: e.activation(out=lfn[:], in_=tmpg[:], func=AF.Ln, scale=1.0, bias=onec[:]),
                      reads=["tmpg", "onec"], writes=["lfn"])
                for c0 in range(0, NT, 32):
                    c1 = min(NT, c0 + 32)
                    tk.op("pe", lambda e, c0=c0, c1=c1: e.matmul(psum[0][:, c0:c1], lhsT=triu[:], rhs=lfn[:, c0:c1],
                                                                 start=True, stop=True),
                          reads=["lfn", "c2"], writes=["psb0"])
                    tk.op("pe", lambda e, c0=c0, c1=c1: e.matmul(psum[1][:, c0:c1], lhsT=ones_f[:], rhs=lfn[:, c0:c1],
                                                                 start=True, stop=True),
                          reads=["lfn", "c3"], writes=["psb1"])
                tk.op("act", lambda e: e.copy(out=tot[:], in_=psum[1][:, 0:NT]), reads=["psb1"], writes=["tot"])
                tk.op("act", lambda e: e.copy(out=G[:], in_=psum[0][:, 0:NT]), reads=["psb0"], writes=["G"])
                tk.op("dve", lambda e: e.tensor_tensor_scan(out=offi[:], data0=onesn[:], data1=tot[:], initial=0.0,
                                                            op0=ALU.mult, op1=ALU.add),
                      reads=["tot", "onesn"], writes=["offi"])
                tk.op("dve", lambda e: e.tensor_tensor(out=offi[:], in0=offi[:], in1=tot[:], op=ALU.subtract),
                      reads=["offi", "tot"], writes=["offi"])
                tk.op("dve", lambda e: e.tensor_tensor(out=G[:], in0=G[:], in1=offi[:], op=ALU.add),
                      reads=["G", "offi"], writes=["G"])
                Gsel = G[:].rearrange("p (j r) -> p j r", r=4)[:, :, 2]
                tk.op("dve", lambda e, Gsel=Gsel: e.tensor_copy(out=tmpg[:, 0:NQ], in_=Gsel), reads=["G"],
                      writes=["tmpg"])
                tk.op("pe", lambda e: e.matmul(psum[1][:, 0:NQ], lhsT=esel[:], rhs=tmpg[:, 0:NQ], start=True, stop=True),
                      reads=["tmpg", "c4"], writes=["psb1"])
                tk.op("act", lambda e: e.copy(out=cb[:], in_=psum[1][:, 0:NQ]), reads=["psb1"], writes=["cb"])
                blocks = [(j, i) for j in range(NQ) for i in range(4 * j + 4)]
                NBK = len(blocks)
                LOOK = 2

                def load_q(j):
                    tk.dma("sp", qt[j % 2][:], qt_d[hh][:, j * 512:(j + 1) * 512], writes=["qt%d" % (j % 2)])

                def make_bias(j):
                    nk = 4 * j + 4
                    tk.op("dve", lambda e, j=j, nk=nk: e.tensor_scalar(
                        out=biasj[j % 2][:, 0:nk], in0=G[:, 0:nk], scalar1=cb[:, j:j + 1], scalar2=None,
                        op0=ALU.subtract), reads=["G", "cb"], writes=["bj%d" % (j % 2)])

                def st_qk(n):
                    j, i = blocks[n]
                    if i == 0 and j + 1 < NQ:
                        load_q(j + 1)
                    sb_ = n % 4
                    qs = j % 2
                    tk.op("pe", lambda e, i=i, qs=qs, sb_=sb_: e.matmul(
                        psum[sb_][:, :], lhsT=KT[:, i * 128:(i + 1) * 128], rhs=qt[qs][:], start=True, stop=True),
                        reads=["KT", "qt%d" % qs], writes=["psb%d" % sb_])

                def st_ex(n):
                    j, i = blocks[n]
                    if i == 0 and j + 1 < NQ:
                        make_bias(j + 1)
                    r = i - 4 * j
                    sb_ = n % 4
                    p = n % 4
                    bj = j % 2
                    tk.op("act", lambda e, p=p, sb_=sb_, bj=bj, i=i: e.activation(
                        out=PT[p][:], in_=psum[sb_][:, :], func=AF.Exp, scale=SC, bias=biasj[bj][:, i:i + 1]),
                        reads=["psb%d" % sb_, "bj%d" % bj], writes=["PT%d" % p])
                    if r >= 0:
                        tk.op("dve", lambda e, p=p, r=r: e.tensor_tensor(
                            out=PT[p][:], in0=PT[p][:], in1=amask[:, r, :], op=ALU.mult),
                            reads=["PT%d" % p, "c5"], writes=["PT%d" % p])

                def st_pv(n):
                    j, i = blocks[n]
                    r = i - 4 * j
                    p = n % 4
                    for u in range(4):
                        if r > u:
                            continue
                        last = 4 * j + u
                        tk.op("pe", lambda e, p=p, u=u, i=i, last=last: e.matmul(
                            psum[4 + u][:, 0:130], lhsT=PT[p][:, u * 128:(u + 1) * 128], rhs=VA[:, i, :],
                            start=(i == 0), stop=(i == last)),
                            reads=["PT%d" % p, "VA"], writes=["psb%d" % (4 + u)], inc=(u == 3))
                    if i == 4 * j + 3:
                        epilogue(j)

                def epilogue(j):
                    for u in range(4):
                        o = oc[0] % 2
                        oc[0] += 1
                        pu = psum[4 + u]
                        tk.op("dve", lambda e, pu=pu: e.reciprocal(out=sm[:, 0:1], in_=pu[:, 128:129]),
                              reads=["psb%d" % (4 + u)], writes=["sm0"])
                        tk.op("dve", lambda e, pu=pu, o=o: e.tensor_scalar(
                            out=ot[o][:], in0=pu[:, 0:128], scalar1=sm[:, 0:1], scalar2=None, op0=ALU.mult),
                            reads=["psb%d" % (4 + u), "sm0"], writes=["ot%d" % o])
                        tk.op("act", lambda e, o=o: e.activation(out=oj[:], in_=ot[o][:], func=AF.Square,
                                                                 accum_out=sm[:, 1:2]),
                              reads=["ot%d" % o], writes=["oj", "sm1"])
                        tk.op("act", lambda e: e.activation(out=sm[:, 2:3], in_=sm[:, 1:2], func=AF.Ln,
                                                            scale=1.0 / 128, bias=epsc[:]),
                              reads=["sm1", "epsc"], writes=["sm2"])
                        tk.op("act", lambda e: e.activation(out=sm[:, 3:4], in_=sm[:, 2:3], func=AF.Exp, scale=-0.5),
                              reads=["sm2"], writes=["sm3"])
                        tk.op("dve", lambda e, o=o, hh=hh: e.scalar_tensor_tensor(
                            out=ob[o][:], in0=ot[o][:], scalar=sm[:, 3:4], in1=hnw_s[:, hh * 128:(hh + 1) * 128],
                            op0=ALU.mult, op1=ALU.mult),
                            reads=["ot%d" % o, "sm3", "c9"], writes=["ob%d" % o])
                        t0 = j * 512 + u * 128
                        tk.dma("pool", mix_dst(t0, hh * 128, (hh + 1) * 128), ob[o][:], reads=["ob%d" % o],
                               key="ob%d" % o)

                load_q(0)
                make_bias(0)
                for n in range(NBK + LOOK):
                    if n < NBK:
                        st_qk(n)
                    if n - LOOK >= 0:
                        st_ex(n - LOOK)
                        st_pv(n - LOOK)
                tk.barrier()

        NCH = NT
        with ExitStack() as es3:
            zq = [sb("zq%d" % i, [128, 515], F32, es3) for i in range(2)]
            zk = [sb("zk%d" % i, [128, 515], F32, es3) for i in range(2)]
            acc = sb("acc", [128, 512], F32, es3)
            ex = sb("ex", [128, 512], F32, es3)
            qT = [sb("qT%d" % i, [128, 512], BF16, es3) for i in range(2)]
            kT = [sb("kT%d" % i, [128, 512], BF16, es3) for i in range(2)]
            mvo = [sb("mvo%d" % i, [128, 512], F32, es3) for i in range(2)]
            vt = [sb("vt%d" % i, [128, 258], BF16, es3) for i in range(2)]
            ktok = [sb("ktok%d" % i, [128, 128], BF16, es3) for i in range(2)]
            smt = [sb("smt%d" % i, [128, 128], BF16, es3) for i in range(2)]
            Cf = sb("Cf", [128, 258], F32, es3)
            Cb = [sb("Cb%d" % i, [128, 258], BF16, es3) for i in range(2)]
            lf = sb("mlf", [128, NCH], F32, es3)
            tmpm = sb("tmpm", [128, NCH], F32, es3)
            bcs = sb("bcs", [128, NCH], F32, es3)
            eb = sb("eb", [128, NCH], F32, es3)
            ek = sb("ek", [128, NCH], F32, es3)
            ebl = sb("ebl", [128, NCH], F32, es3)
            hsm = sb("hsm", [128, 8], F32, es3)
            hv = [sb("hv%d" % i, [128, 256], F32, es3) for i in range(2)]
            sg = sb("sg", [128, 256], F32, es3)
            hj = sb("hj", [128, 256], F32, es3)
            hb = [sb("hb%d" % i, [128, 256], BF16, es3) for i in range(2)]
            if 'M' in PH or os.environ.get('MLPRE'):
                _lim = int(os.environ.get('MLPRE', '99'))
                _real_op = tk.op
                _cnt = [0]
                def _lop(*a, **k):
                    _cnt[0] += 1
                    if _cnt[0] <= _lim:
                        return _real_op(*a, **k)
                tk.op = _lop
                tk.op("act", lambda e: e.activation(out=tmpm[:], in_=gates[:, :, 3], func=AF.Exp, scale=-1.0,
                                                    bias=ngb[:, 3:4]), reads=["gates", "ngb"], writes=["tmpm"])
                tk.op("act", lambda e: e.activation(out=lf[:], in_=tmpm[:], func=AF.Ln, scale=1.0, bias=onec[:]),
                      reads=["tmpm", "onec"], writes=["mlf"])
                tk.op("dve", lambda e: e.tensor_scalar(out=lf[:], in0=lf[:], scalar1=-1.0, scalar2=None, op0=ALU.mult),
                      reads=["mlf"], writes=["mlf"])
                for c0 in range(0, NCH, 32):
                    c1 = min(NCH, c0 + 32)
                    tk.op("pe", lambda e, c0=c0, c1=c1: e.matmul(psum[0][:, c0:c1], lhsT=triu[:], rhs=lf[:, c0:c1],
                                                                 start=True, stop=True),
                          reads=["mlf", "c2"], writes=["psb0"])
                    tk.op("pe", lambda e, c0=c0, c1=c1: e.matmul(psum[1][:, c0:c1], lhsT=ones_f[:], rhs=lf[:, c0:c1],
                                                                 start=True, stop=True),
                          reads=["mlf", "c3"], writes=["psb1"])
                tk.op("act", lambda e: e.activation(out=eb[:], in_=psum[0][:, 0:NCH], func=AF.Exp),
                      reads=["psb0"], writes=["eb"])
                tk.op("act", lambda e: e.activation(out=ebl[:], in_=psum[1][:, 0:NCH], func=AF.Exp),
                      reads=["psb1"], writes=["ebl"])
                tk.op("act", lambda e: e.copy(out=tmpm[:], in_=psum[0][:, 0:NCH]), reads=["psb0"], writes=["tmpm"])
                tk.op("act", lambda e: e.copy(out=bcs[:], in_=gates[:, :, 2]), reads=["gates"], writes=["bcs"])
                tk.op("dve", lambda e: e.tensor_tensor(out=bcs[:], in0=bcs[:], in1=tmpm[:], op=ALU.subtract),
                      reads=["bcs", "tmpm"], writes=["bcs"])
                tk.op("act", lambda e: e.activation(out=ek[:], in_=bcs[:], func=AF.Exp, bias=gb[:, 2:3], scale=1.0),
                      reads=["bcs", "c7"], writes=["ek"])
            if 'M' in PH or os.environ.get('MLPRE'):
                tk.op = _real_op
            tk.op("dve", lambda e: e.memset(Cf[:], 0.0), writes=["Cf"])
            tk.op("dve", lambda e: e.memset(Cb[0][:], 0.0), writes=["Cb0"])
            for i in range(2):
                tk.op("dve", lambda e, i=i: e.memset(zq[i][:, 0:3], 0.0), writes=["zq%d" % i])
                tk.op("dve", lambda e, i=i: e.memset(zk[i][:, 0:3], 0.0), writes=["zk%d" % i])
                tk.op("dve", lambda e, i=i: e.memset(vt[i][:], 0.0), writes=["vt%d" % i])
            def ml_x(c, tk):
                jb, cc = c // 4, c % 4
                s = jb % 2
                cs = c % 2
                ob_ = 4 if cs == 0 else 6
                csl = slice(cc * 128, (cc + 1) * 128)
                tk.dma("sp", mvo[cs][:], mvo_d[c * 128:(c + 1) * 128, :], writes=["mvo%d" % cs])
                pkt = psum[2][:, :].bitcast(BF16)
                tk.op("pe", lambda e, s=s, csl=csl, pkt=pkt: e.transpose(out=pkt[:, 0:128], in_=kT[s][:, csl],
                                                                         identity=ident_bf[:]),
                      reads=["kT%d" % s, "c0"], writes=["psb2"])
                tk.op("act", lambda e, cs=cs, pkt=pkt: e.activation(out=ktok[cs][:], in_=pkt[:, 0:128],
                                                                    func=AF.Copy, scale=SC),
                      reads=["psb2"], writes=["ktok%d" % cs])
                tk.op("dve", lambda e, cs=cs, c=c: e.tensor_scalar(
                    out=vt[cs][:, 0:256], in0=mvo[cs][:, 0:256], scalar1=ek[:, c:c + 1], scalar2=None, op0=ALU.mult),
                    reads=["mvo%d" % cs, "ek"], writes=["vt%d" % cs])
                tk.op("dve", lambda e, cs=cs, c=c: e.tensor_copy(out=vt[cs][:, 256:257], in_=ek[:, c:c + 1]),
                      reads=["ek"], writes=["vt%d" % cs])
                tk.op("pe", lambda e, s=s, csl=csl: e.matmul(psum[3][:, 0:128], lhsT=kT[s][:, csl], rhs=qT[s][:, csl],
                                                             start=True, stop=True),
                      reads=["kT%d" % s, "qT%d" % s], writes=["psb3"])
                tk.op("dve", lambda e, cs=cs: e.tensor_tensor(out=smt[cs][:], in0=psum[3][:, 0:128], in1=mmask[:],
                                                              op=ALU.mult),
                      reads=["psb3", "c6"], writes=["smt%d" % cs])
                tk.op("pe", lambda e, cs=cs: e.matmul(psum[ob_][:, 0:258], lhsT=smt[cs][:], rhs=vt[cs][:],
                                                      start=True, stop=False),
                      reads=["smt%d" % cs, "vt%d" % cs], writes=["psb%d" % ob_])
                tk.op("pe", lambda e, s=s, csl=csl, cs=cs: e.matmul(psum[ob_][:, 0:258], lhsT=qT[s][:, csl],
                                                                    rhs=Cb[cs][:], start=False, stop=True),
                      reads=["qT%d" % s, "Cb%d" % cs], writes=["psb%d" % ob_])
                tk.op("pe", lambda e, cs=cs: e.matmul(psum[5][:, 0:258], lhsT=ktok[cs][:], rhs=vt[cs][:],
                                                      start=True, stop=True),
                      reads=["ktok%d" % cs, "vt%d" % cs], writes=["psb5"])
                tk.op("dve", lambda e: e.tensor_tensor(out=Cf[:], in0=psum[5][:, 0:258], in1=Cf[:], op=ALU.add),
                      reads=["psb5", "Cf"], writes=["Cf"])
                tk.op("dve", lambda e, c=c: e.tensor_scalar(out=Cf[:], in0=Cf[:], scalar1=ebl[:, c:c + 1],
                                                            scalar2=None, op0=ALU.mult),
                      reads=["Cf", "ebl"], writes=["Cf"])
                tk.op("act", lambda e, cs=cs: e.copy(out=Cb[1 - cs][:], in_=Cf[:]), reads=["Cf"],
                      writes=["Cb%d" % (1 - cs)])


            def ml_y(c, tk):
                jb, cc = c // 4, c % 4
                s = jb % 2
                cs = c % 2
                ob_ = 4 if cs == 0 else 6
                csl = slice(cc * 128, (cc + 1) * 128)
                tk.op("act", lambda e, c=c: e.activation(out=hsm[:, 6:7], in_=psum[ob_][:, 256:257], func=AF.Abs,
                                                         scale=eb[:, c:c + 1]),
                      reads=["psb%d" % ob_, "eb"], writes=["hsm6"])
                tk.op("dve", lambda e: e.tensor_scalar(out=hsm[:, 0:1], in0=hsm[:, 6:7], scalar1=1.0, scalar2=None,
                                                       op0=ALU.max),
                      reads=["hsm6"], writes=["hsm0"])
                tk.op("dve", lambda e: e.reciprocal(out=hsm[:, 1:2], in_=hsm[:, 0:1]), reads=["hsm0"], writes=["hsm1"])
                tk.op("dve", lambda e, c=c: e.tensor_tensor(out=hsm[:, 2:3], in0=hsm[:, 1:2], in1=eb[:, c:c + 1],
                                                            op=ALU.mult), reads=["hsm1", "eb"], writes=["hsm2"])
                tk.op("act", lambda e, cs=cs: e.activation(out=sg[:], in_=mvo[cs][:, 256:512], func=AF.Exp, scale=-1.0),
                      reads=["mvo%d" % cs], writes=["sg"])
                tk.op("dve", lambda e: e.tensor_scalar(out=sg[:], in0=sg[:], scalar1=1.0, scalar2=None, op0=ALU.add),
                      reads=["sg"], writes=["sg"])
                tk.op("dve", lambda e: e.reciprocal(out=sg[:], in_=sg[:]), reads=["sg"], writes=["sg"])
                tk.op("dve", lambda e, cs=cs: e.scalar_tensor_tensor(
                    out=hv[cs][:], in0=psum[ob_][:, 0:256], scalar=hsm[:, 2:3], in1=sg[:], op0=ALU.mult, op1=ALU.mult),
                    reads=["psb%d" % ob_, "hsm2", "sg"], writes=["hv%d" % cs])
                tk.op("act", lambda e, cs=cs: e.activation(out=hj[:], in_=hv[cs][:], func=AF.Square,
                                                           accum_out=hsm[:, 3:4]),
                      reads=["hv%d" % cs], writes=["hj", "hsm3"])
                tk.op("act", lambda e: e.activation(out=hsm[:, 4:5], in_=hsm[:, 3:4], func=AF.Ln, scale=1.0 / 256,
                                                    bias=epsc[:]), reads=["hsm3", "epsc"], writes=["hsm4"])
                tk.op("act", lambda e: e.activation(out=hsm[:, 5:6], in_=hsm[:, 4:5], func=AF.Exp, scale=-0.5),
                      reads=["hsm4"], writes=["hsm5"])
                tk.op("dve", lambda e, cs=cs: e.scalar_tensor_tensor(
                    out=hb[cs][:], in0=hv[cs][:], scalar=hsm[:, 5:6], in1=hnw_s[:, 256:512], op0=ALU.mult,
                    op1=ALU.mult), reads=["hv%d" % cs, "hsm5", "c9"], writes=["hb%d" % cs])
                tk.dma("pool", mix_dst(c * 128, 256, 512), hb[cs][:], reads=["hb%d" % cs], writes=["ml%d" % c],
                       key="hb%d" % cs)


            for jb in (range(NB) if 'M' in PH else []):
                s = jb % 2
                for (z, zn, zd, wo, bo, dstT, dn) in [(zq, "zq", mq_d, 0, 8, qT, "qT"), (zk, "zk", mk_d, 4, 9, kT, "kT")]:
                    tk.dma("sp", z[s][:, 3:515], zd[:, jb * 512:(jb + 1) * 512], writes=["%s%d" % (zn, s)])
                    if jb > 0:
                        tk.op("act", lambda e, z=z, s=s: e.copy(out=z[s][:, 0:3], in_=z[1 - s][:, 512:515]),
                              reads=["%s%d" % (zn, 1 - s)], writes=["%s%d" % (zn, s)])
                    tk.op("dve", lambda e, z=z, s=s, wo=wo, bo=bo: e.tensor_scalar(
                        out=acc[:], in0=z[s][:, 0:512], scalar1=cvp[:, wo:wo + 1], scalar2=cvp[:, bo:bo + 1],
                        op0=ALU.mult, op1=ALU.add), reads=["%s%d" % (zn, s), "c8"], writes=["acc"])
                    for jj in range(1, 4):
                        tk.op("dve", lambda e, z=z, s=s, wo=wo, jj=jj: e.scalar_tensor_tensor(
                            out=acc[:], in0=z[s][:, jj:jj + 512], scalar=cvp[:, wo + jj:wo + jj + 1], in1=acc[:],
                            op0=ALU.mult, op1=ALU.add), reads=["%s%d" % (zn, s), "c8", "acc"], writes=["acc"])
                    tk.op("act", lambda e: e.activation(out=ex[:], in_=acc[:], func=AF.Exp, scale=-1.0),
                          reads=["acc"], writes=["ex"])
                    tk.op("dve", lambda e: e.tensor_scalar(out=ex[:], in0=ex[:], scalar1=1.0, scalar2=None, op0=ALU.add),
                          reads=["ex"], writes=["ex"])
                    tk.op("dve", lambda e: e.reciprocal(out=ex[:], in_=ex[:]), reads=["ex"], writes=["ex"])
                    tk.op("dve", lambda e, dstT=dstT, s=s: e.tensor_tensor(out=dstT[s][:], in0=acc[:], in1=ex[:],
                                                                           op=ALU.mult),
                          reads=["acc", "ex"], writes=["%s%d" % (dn, s)])
                for cc in range(4):
                    c = jb * 4 + cc
                    qx = OpQueue()
                    ml_x(c, qx)
                    qs_ = [qx]
                    if c > 0:
                        qy = OpQueue()
                        ml_y(c - 1, qy)
                        qs_.append(qy)
                    emit_interleaved(tk, qs_)
                    if c > 0 and (c - 1) % 4 == 3 and on_block_done is not None:
                        on_block_done((c - 1) // 4)
            if 'M' in PH:
                ml_y(NCH - 1, tk)
                if on_block_done is not None:
                    on_block_done((NCH - 1) // 4)
            tk.barrier()
        if own_tk:
            tk.final_wait("sp")
    print("phase A instructions:", tk.ninst)
    return nc


def host_inputs_a(inp, S):
    c = host_consts()
    w_in = np.asarray(inp["w_in"][0])
    maps = []
    for core in range(8):
        b, g = core // 4, core % 4
        h0, h1 = 2 * g, 2 * g + 1
        cols = []
        for base in (0, 1024, 2048):
            cols += list(range(base + h0 * 128, base + h0 * 128 + 128)) + list(range(base + h1 * 128, base + h1 * 128 + 128))
        cols += [3072 + h0, 3072 + h1, 5128 + g, 5132 + g]
        cols += list(range(3080 + g * 128, 3080 + g * 128 + 128))
        cols += list(range(3592 + g * 128, 3592 + g * 128 + 128))
        cols += list(range(4104 + g * 256, 4104 + g * 256 + 256))
        cols += list(range(5136 + g * 256, 5136 + g * 256 + 256))
        wcs = np.ascontiguousarray(w_in[:, cols])
        gbv = np.array([inp["fox_f_bias"][0][h0], inp["fox_f_bias"][0][h1], inp["mlstm_i_bias"][0][g],
                        inp["mlstm_f_bias"][0][g]], np.float32)
        cw = np.asarray(inp["mlstm_conv_w"][0])
        cbv = np.asarray(inp["mlstm_conv_b"][0])
        convp = np.concatenate([cw[:, g * 128:(g + 1) * 128].T, cw[:, 512 + g * 128:512 + (g + 1) * 128].T,
                                cbv[g * 128:(g + 1) * 128][:, None], cbv[512 + g * 128:512 + (g + 1) * 128][:, None]],
                               axis=1).astype(np.float32)
        hn = np.concatenate([np.asarray(inp["fox_out_norm_w"][0])[h0 * 128:(h1 + 1) * 128],
                             np.asarray(inp["mlstm_out_norm_w"][0])[g * 256:(g + 1) * 256]])
        m = {"x": np.ascontiguousarray(np.asarray(inp["x"])[b, :int(os.environ.get("XROWS", S))]),
             "wc": wcs,
             "n1w": np.ascontiguousarray(np.asarray(inp["norm1_w"][0]).reshape(KC, 128).T),
             "gbias": np.ascontiguousarray(np.broadcast_to(gbv[None, :], (128, 4))),
             "convp": np.ascontiguousarray(convp),
             "hnw": np.ascontiguousarray(np.broadcast_to(hn[None, :], (128, 512))).astype(np.float32)}
        m.update(c)
        maps.append(m)
    return maps


import os


def host_consts_b():
    c = {}
    c["ident_bf"] = np.eye(128, dtype=np.float32).astype(ml_dtypes.bfloat16)
    c["ident_f"] = np.eye(128, dtype=np.float32)
    c["iota_row"] = np.ascontiguousarray(np.broadcast_to(np.arange(128, dtype=np.float32)[None, :], (128, 128)))
    c["thr16"] = np.ascontiguousarray(np.broadcast_to((16.0 * np.arange(16, dtype=np.float32))[None, :], (128, 16)))
    c["iota16"] = np.ascontiguousarray(np.broadcast_to(np.arange(16, dtype=np.float32)[None, :], (128, 16)))
    return c


def build_phase_b(nc, TC, NJ=128, tk=None, gath=None, pfx="", pre=None):
    NTT = TC // 128
    PB = min(256, TC)
    NBLK = TC // PB
    TPB = PB // 128
    own_tk = tk is None
    if own_tk:
        tk = Trk(nc)
    _dt = nc.dram_tensor

    def dt(name, *a, **k):
        return _dt(pfx + name, *a, **k)
    x = dt("x", [TC, D], F32, kind="ExternalInput").ap()
    if gath is None:
        mixed = dt("mixed", [TC, D], BF16, kind="ExternalInput").ap()
    else:
        wsel_d = dt("wsel", [128, 8], F32, kind="ExternalInput").ap()
    wout = dt("wout", [D, D], F32, kind="ExternalInput").ap()
    wq = dt("wq", [D, D], F32, kind="ExternalInput").ap()
    n2w = dt("n2w", [128, D], F32, kind="ExternalInput").ap()
    fnw = dt("fnw", [128, D], F32, kind="ExternalInput").ap()
    keysT = dt("keysT", [128, 2, 128], F32, kind="ExternalInput").ap()
    if pre is None:
        uh = dt("uh", [128, 128, D], F32, kind="ExternalInput").ap()
        vh = dt("vh", [128, 128, D], F32, kind="ExternalInput").ap()
    ident_bf_d = dt("ident_bf", [128, 128], BF16, kind="ExternalInput").ap()
    ident_f_d = dt("ident_f", [128, 128], F32, kind="ExternalInput").ap()
    iota_row_d = dt("iota_row", [128, 128], F32, kind="ExternalInput").ap()
    thr16_d = dt("thr16", [128, 16], F32, kind="ExternalInput").ap()
    iota16_d = dt("iota16", [128, 16], F32, kind="ExternalInput").ap()
    out = dt("out", [TC, D], F32, kind="ExternalOutput").ap()
    if pre is None:
        u16 = dt("u16", [128, 128, D], BF16, kind="Internal").ap()
        v16 = dt("v16", [128, 128, D], BF16, kind="Internal").ap()
    else:
        u16, v16 = pre
    x1_d = dt("x1_d", [TC, D], F32, kind="Internal").ap()
    h2T_d = dt("h2T_d", [128, KC, TC], BF16, kind="Internal").ap()
    slot_d = dt("slot_d", [128, 3, TC], F32, kind="Internal").ap()

    with ExitStack() as es0:
        def sb(name, shape, dtype, es=es0):
            return es.enter_context(nc.sbuf_tensor(pfx + name, shape, dtype))
        ident_bf = sb("ident_bf_s", [128, 128], BF16)
        ident_f = sb("ident_f_s", [128, 128], F32)
        iota_row = sb("iota_row_s", [128, 128], F32)
        thr16 = sb("thr16_s", [128, 16], F32)
        iota16 = sb("iota16_s", [128, 16], F32)
        epsc = sb("epsc", [128, 1], F32)
        psum = [es0.enter_context(nc.psum_tensor(pfx + "ps%d" % i, [128, 512], F32)) for i in range(8)]
        for i, (s_, d_) in enumerate([(ident_bf, ident_bf_d), (ident_f, ident_f_d), (iota_row, iota_row_d),
                                      (thr16, thr16_d), (iota16, iota16_d)]):
            tk.dma("sp", s_[:], d_, writes=["c%d" % i], key="const")
        for j in (range(NJ) if pre is None else []):
            tk.dma("pool", u16[j], uh[j], writes=["u16"], key="cvt")
            tk.dma("pool", v16[j], vh[j], writes=["v16"], key="cvt")
        tk.op("dve", lambda e: e.memset(epsc[:], EPS), writes=["epsc"])

        def rms_rstd(src_ap, srckey, ss, junk):
            tk.op("act", lambda e: e.activation(out=junk[:], in_=src_ap, func=AF.Square, accum_out=ss[:, 0:1]),
                  reads=[srckey], writes=["junk", "ss0"])
            tk.op("act", lambda e: e.activation(out=ss[:, 1:2], in_=ss[:, 0:1], func=AF.Ln, scale=1.0 / D, bias=epsc[:]),
                  reads=["ss0", "epsc"], writes=["ss1"])
            tk.op("act", lambda e: e.activation(out=ss[:, 2:3], in_=ss[:, 1:2], func=AF.Exp, scale=-0.5),
                  reads=["ss1"], writes=["ss2"])

        def transpose16(src, srckey, dst, dstkey, banks):
            for half in range(2):
                bk = banks[half]
                pst = psum[bk][:, :].bitcast(BF16)
                for k8 in range(8):
                    kc = half * 8 + k8
                    tk.op("pe", lambda e, kc=kc, k8=k8, pst=pst: e.transpose(
                        out=pst[:, k8 * 128:(k8 + 1) * 128], in_=src[:, kc * 128:(kc + 1) * 128], identity=ident_bf[:]),
                        reads=[srckey, "c0"], writes=["psb%d" % bk], inc=(k8 == 7))
                srcv = pst.rearrange("p (k t) -> p k t", k=8)
                dstv = dst[:, half * 8:(half + 1) * 8, :]
                if half == 0:
                    tk.op("act", lambda e, srcv=srcv, dstv=dstv: e.copy(out=dstv, in_=srcv),
                          reads=["psb%d" % bk], writes=[dstkey])
                else:
                    tk.op("dve", lambda e, srcv=srcv, dstv=dstv: e.tensor_copy(out=dstv, in_=srcv),
                          reads=["psb%d" % bk], writes=[dstkey])

        with ExitStack() as es1:
            Wo = sb("Wo", [128, KC, D], BF16, es1)
            n2b = sb("n2b", [128, D], F32, es1)
            mx = [sb("mx%d" % i, [128, D], BF16, es1) for i in range(2)]
            xt = [sb("xt%d" % i, [128, D], F32, es1) for i in range(2)]
            x1 = [sb("x1_%d" % i, [128, D], F32, es1) for i in range(2)]
            mT = sb("mT", [128, KC, 128], BF16, es1)
            h2 = sb("h2", [128, D], BF16, es1)
            h2t = [sb("h2t%d" % i, [128, KC, 128], BF16, es1) for i in range(2)]
            junk = sb("junk", [128, D], BF16, es1)
            ss = sb("ss", [128, 4], F32, es1)
            for kc in range(KC):
                tk.dma("pool", Wo[:, kc, :], wout[kc * 128:(kc + 1) * 128, :], writes=["Wo"], key="wload")
            tk.dma("sp", n2b[:], n2w, writes=["n2b"])
            ccnt = [0]
            if gath is not None:
                cand = [sb("cand%d" % i, [128, D], BF16, es1) for i in range(3)]
                wsel = sb("wsel_s", [128, 8], F32, es1)
                tk.dma("sp", wsel[:], wsel_d, writes=["wsel"])
            for ti in range(NTT):
                s = ti % 2
                rows = slice(ti * 128, (ti + 1) * 128)
                if gath is None:
                    tk.dma("sp", mx[s][:], mixed[rows, :], writes=["mx%d" % s])
                else:
                    bb_, lt = ti // (NTT // 2), ti % (NTT // 2)
                    for dp in range(8):
                        cs_ = ccnt[0] % 3
                        ccnt[0] += 1
                        gt_ = dp * (NTT // 2) + lt
                        kq = gt_ // 4
                        src = gath[kq].ap().rearrange("(r t) c -> r t c", r=8)[bb_ * 4:(bb_ + 1) * 4,
                                                                              (gt_ % 4) * 128:(gt_ % 4 + 1) * 128, :]
                        tk.dma("sp", cand[cs_][:].rearrange("p (g c) -> p g c", g=4), src.rearrange("g t c -> t g c"),
                               reads=["gath%d" % kq], writes=["cand%d" % cs_])
                        if dp == 0:
                            tk.op("dve", lambda e, cs_=cs_, s=s: e.tensor_scalar(
                                out=mx[s][:], in0=cand[cs_][:], scalar1=wsel[:, 0:1], scalar2=None, op0=ALU.mult),
                                reads=["cand%d" % cs_, "wsel"], writes=["mx%d" % s])
                        else:
                            tk.op("dve", lambda e, cs_=cs_, s=s, dp=dp: e.scalar_tensor_tensor(
                                out=mx[s][:], in0=cand[cs_][:], scalar=wsel[:, dp:dp + 1], in1=mx[s][:], op0=ALU.mult,
                                op1=ALU.add), reads=["cand%d" % cs_, "wsel", "mx%d" % s], writes=["mx%d" % s])
                tk.dma("sp", xt[s][:], x[rows, :], writes=["xt%d" % s])
                transpose16(mx[s], "mx%d" % s, mT, "mT", (0, 1))
                for cg in range(4):
                    b = 2 + cg
                    for kc in range(KC):
                        tk.op("pe", lambda e, kc=kc, b=b, cg=cg: e.matmul(
                            psum[b][:, :], lhsT=mT[:, kc, :], rhs=Wo[:, kc, cg * 512:(cg + 1) * 512],
                            start=(kc == 0), stop=(kc == KC - 1)), reads=["mT", "Wo"], writes=["psb%d" % b], inc=(kc == KC - 1))
                    tk.op("dve", lambda e, b=b, cg=cg, s=s: e.tensor_tensor(
                        out=x1[s][:, cg * 512:(cg + 1) * 512], in0=psum[b][:, :], in1=xt[s][:, cg * 512:(cg + 1) * 512],
                        op=ALU.add), reads=["psb%d" % b, "xt%d" % s], writes=["x1_%d" % s])
                tk.dma("pool", x1_d[rows, :], x1[s][:], reads=["x1_%d" % s], key="x1st%d" % s)
                rms_rstd(x1[s][:], "x1_%d" % s, ss, junk)
                tk.op("dve", lambda e, s=s: e.scalar_tensor_tensor(out=h2[:], in0=x1[s][:], scalar=ss[:, 2:3], in1=n2b[:],
                                                                   op0=ALU.mult, op1=ALU.mult),
                      reads=["x1_%d" % s, "ss2", "n2b"], writes=["h2"])
                transpose16(h2, "h2", h2t[s], "h2t%d" % s, (6, 7))
                tk.dma("pool", h2T_d[:, :, rows], h2t[s][:], reads=["h2t%d" % s], key="h2st%d" % s)
            tk.barrier()

        if os.environ.get('BSTOP') == '1':
            tk.final_wait('sp')
            return nc
        with ExitStack() as es2:
            Wq = sb("Wq", [128, KC, D], BF16, es2)
            kT = sb("kTs", [128, 2, 128], BF16, es2)
            h2t = [sb("h2tb%d" % i, [128, KC, 128], BF16, es2) for i in range(2)]
            qpT = [sb("qpT%d" % i, [128, 128], BF16, es2) for i in range(2)]
            sc_l = [sb("sc_%d" % i_, [128, 16, 128], F32, es2) for i_ in range(2)]
            tmp1_l = [sb("tmp1_%d" % i_, [128, 128], F32, es2) for i_ in range(2)]
            st_l = [sb("st_%d" % i_, [128, 16, 16], F32, es2) for i_ in range(2)]
            iu_l = [sb("iu_%d" % i_, [128, 16, 16], U32, es2) for i_ in range(2)]
            itf_l = [sb("itf_%d" % i_, [128, 16, 16], F32, es2) for i_ in range(2)]
            dd_l = [sb("dd_%d" % i_, [128, 16, 16], F32, es2) for i_ in range(2)]
            cand_l = [sb("cand_%d" % i_, [128, 8, 256], F32, es2) for i_ in range(2)]
            tmp2_l = [sb("tmp2_%d" % i_, [128, 256], F32, es2) for i_ in range(2)]
            cf_l = [sb("cf_%d" % i_, [128, 8, 16], F32, es2) for i_ in range(2)]
            pu_l = [sb("pu_%d" % i_, [128, 8, 16], U32, es2) for i_ in range(2)]
            posf_l = [sb("posf_%d" % i_, [128, 8, 16], F32, es2) for i_ in range(2)]
            cs_l = [sb("cs_%d" % i_, [128, 8, 16], F32, es2) for i_ in range(2)]
            zs_l = [sb("zs_%d" % i_, [128, 8], F32, es2) for i_ in range(2)]
            ge_l = [sb("ge_%d" % i_, [128, 8, 16, 16], F32, es2) for i_ in range(2)]
            prod_l = [sb("prod_%d" % i_, [128, 8, 16, 16], F32, es2) for i_ in range(2)]
            k1s_l = [sb("k1s_%d" % i_, [128, 8, 16], F32, es2) for i_ in range(2)]
            k2f_l = [sb("k2f_%d" % i_, [128, 8, 16], F32, es2) for i_ in range(2)]
            res_l = [sb("res_%d" % i_, [128, 3, 128], F32, es2) for i_ in range(2)]
            rst = [sb("rst%d" % i, [128, 3, 128], F32, es2) for i in range(2)]
            for kc in range(KC):
                tk.dma("pool", Wq[:, kc, :], wq[kc * 128:(kc + 1) * 128, :], writes=["Wq"], key="wload")
            tk.dma("pool", kT[:], keysT, writes=["kT"], key="wload")
            PRIV = ['sc', 'tmp1', 'st', 'iu', 'itf', 'dd', 'cand', 'tmp2', 'cf', 'pu', 'posf', 'cs', 'zs', 'ge', 'prod', 'k1s', 'k2f', 'res']

            def b2_tile(ti, tk):
                sc = sc_l[ti % 2]; tmp1 = tmp1_l[ti % 2]; st = st_l[ti % 2]; iu = iu_l[ti % 2]; itf = itf_l[ti % 2]; dd = dd_l[ti % 2]; cand = cand_l[ti % 2]; tmp2 = tmp2_l[ti % 2]; cf = cf_l[ti % 2]; pu = pu_l[ti % 2]; posf = posf_l[ti % 2]; cs = cs_l[ti % 2]; zs = zs_l[ti % 2]; ge = ge_l[ti % 2]; prod = prod_l[ti % 2]; k1s = k1s_l[ti % 2]; k2f = k2f_l[ti % 2]; res = res_l[ti % 2]
                st4 = st[:].rearrange("p (h q) k -> p h q k", q=2)
                itf4 = itf[:].rearrange("p (h q) k -> p h q k", q=2)
                dd4 = dd[:].rearrange("p (h q) k -> p h q k", q=2)
                s = ti % 2
                cols = slice(ti * 128, (ti + 1) * 128)
                tk.dma("sp", h2t[s][:], h2T_d[:, :, cols], writes=["h2tb%d" % s])
                for blk in range(16):
                    b = s
                    p = blk % 2
                    for kc in range(KC):
                        tk.op("pe", lambda e, kc=kc, b=b, blk=blk: e.matmul(
                            psum[b][:, 0:128], lhsT=Wq[:, kc, blk * 128:(blk + 1) * 128], rhs=h2t[s][:, kc, :],
                            start=(kc == 0), stop=(kc == KC - 1)), reads=["Wq", "h2tb%d" % s], writes=["psb%d" % b], inc=(kc == KC - 1))
                    tk.op("act", lambda e, b=b: e.copy(out=qpT[b][:], in_=psum[b][:, 0:128]),
                          reads=["psb%d" % b], writes=["qpT%d" % b])
                    sbk = 2 + 2 * s + (blk // 4) % 2
                    tk.op("pe", lambda e, b=b, p=p, sbk=sbk, blk=blk: e.matmul(
                        psum[sbk][:, (blk % 4) * 128:(blk % 4 + 1) * 128], lhsT=qpT[b][:], rhs=kT[:, p, :],
                        start=True, stop=True), reads=["qpT%d" % b, "kT"], writes=["psb%d" % sbk])
                    if blk % 4 == 3:
                        tk.op("act", lambda e, sbk=sbk, blk=blk: e.copy(
                            out=sc[:, blk - 3:blk + 1, :], in_=psum[sbk][:, :].rearrange("p (a n) -> p a n", a=4)),
                            reads=["psb%d" % sbk], writes=["sc"])
                for blk in range(16):
                    tk.op("dve", lambda e, blk=blk: e.max(out=st[:, blk, 0:8], in_=sc[:, blk, :]),
                          reads=["sc"], writes=["st"])
                    tk.op("dve", lambda e, blk=blk: e.max_index(out=iu[:, blk, 0:8], in_max=st[:, blk, 0:8],
                                                                in_values=sc[:, blk, :]),
                          reads=["sc", "st"], writes=["iu"])
                    tk.op("dve", lambda e, blk=blk: e.match_replace(out=tmp1[:], in_to_replace=st[:, blk, 0:8],
                                                                    in_values=sc[:, blk, :], imm_value=-1e30),
                          reads=["sc", "st"], writes=["tmp1"])
                    tk.op("dve", lambda e, blk=blk: e.max(out=st[:, blk, 8:16], in_=tmp1[:]),
                          reads=["tmp1"], writes=["st"])
                    tk.op("dve", lambda e, blk=blk: e.max_index(out=iu[:, blk, 8:16], in_max=st[:, blk, 8:16],
                                                                in_values=tmp1[:]),
                          reads=["tmp1", "st"], writes=["iu"])
                tk.op("dve", lambda e: e.tensor_copy(out=itf[:], in_=iu[:]), reads=["iu"], writes=["itf"])
                tk.op("dve", lambda e: e.tensor_copy(out=dd[:, :, 0:1], in_=itf[:, :, 0:1]), reads=["itf"], writes=["dd"])
                tk.op("dve", lambda e: e.tensor_tensor(out=dd[:, :, 1:16], in0=itf[:, :, 1:16], in1=itf[:, :, 0:15],
                                                       op=ALU.subtract), reads=["itf"], writes=["dd"])
                a0 = st4[:, :, 0, :].unsqueeze(3).broadcast_to([128, 8, 16, 16])
                a1 = st4[:, :, 1, :].unsqueeze(2).broadcast_to([128, 8, 16, 16])
                cand4 = cand[:].rearrange("p h (a b) -> p h a b", a=16)
                tk.op("pool", lambda e: e.tensor_tensor(out=cand4, in0=a0, in1=a1, op=ALU.add), reads=["st"], writes=["cand"])
                for h in range(8):
                    tk.op("dve", lambda e, h=h: e.max(out=cf[:, h, 0:8], in_=cand[:, h, :]), reads=["cand"], writes=["cf"])
                    tk.op("dve", lambda e, h=h: e.max_index(out=pu[:, h, 0:8], in_max=cf[:, h, 0:8], in_values=cand[:, h, :]),
                          reads=["cand", "cf"], writes=["pu"])
                    tk.op("dve", lambda e, h=h: e.match_replace(out=tmp2[:], in_to_replace=cf[:, h, 0:8],
                                                                in_values=cand[:, h, :], imm_value=-1e30),
                          reads=["cand", "cf"], writes=["tmp2"])
                    tk.op("dve", lambda e, h=h: e.max(out=cf[:, h, 8:16], in_=tmp2[:]), reads=["tmp2"], writes=["cf"])
                    tk.op("dve", lambda e, h=h: e.max_index(out=pu[:, h, 8:16], in_max=cf[:, h, 8:16], in_values=tmp2[:]),
                          reads=["tmp2", "cf"], writes=["pu"])
                tk.op("dve", lambda e: e.tensor_copy(out=posf[:], in_=pu[:]), reads=["pu"], writes=["posf"])
                tk.op("dve", lambda e: e.tensor_tensor(out=cs[:], in0=cf[:], in1=cf[:, :, 0:1].broadcast_to([128, 8, 16]),
                                                       op=ALU.subtract), reads=["cf"], writes=["cs"])
                tk.op("act", lambda e: e.activation(out=cs[:], in_=cs[:], func=AF.Exp), reads=["cs"], writes=["cs"])
                tk.op("dve", lambda e: e.tensor_reduce(out=zs[:], in_=cs[:], axis=AX.X, op=ALU.add), reads=["cs"], writes=["zs"])
                tk.op("dve", lambda e: e.reciprocal(out=zs[:], in_=zs[:]), reads=["zs"], writes=["zs"])
                res_g = res[:, 2, :].rearrange("p (h k) -> p h k", h=8)
                tk.op("dve", lambda e: e.tensor_tensor(out=res_g, in0=cs[:], in1=zs[:].unsqueeze(2).broadcast_to([128, 8, 16]),
                                                       op=ALU.mult), reads=["cs", "zs"], writes=["res"])
                pos_b = posf[:].unsqueeze(3).broadcast_to([128, 8, 16, 16])
                thr_b = thr16[:].unsqueeze(1).unsqueeze(1).broadcast_to([128, 8, 16, 16])
                tk.op("dve", lambda e: e.tensor_tensor(out=ge[:], in0=pos_b, in1=thr_b, op=ALU.is_ge),
                      reads=["posf", "c3"], writes=["ge"])
                d1_b = dd4[:, :, 0, :].unsqueeze(2).broadcast_to([128, 8, 16, 16])
                tk.op("pool", lambda e: e.tensor_tensor(out=prod[:], in0=ge[:], in1=d1_b, op=ALU.mult),
                      reads=["ge", "dd"], writes=["prod"])
                res_i = res[:, 0, :].rearrange("p (h k) -> p h k", h=8)
                tk.op("dve", lambda e: e.tensor_reduce(out=res_i, in_=prod[:], axis=AX.X, op=ALU.add),
                      reads=["prod"], writes=["res"])
                tk.op("dve", lambda e: e.tensor_reduce(out=k1s[:], in_=ge[:], axis=AX.X, op=ALU.add), reads=["ge"], writes=["k1s"])
                tk.op("dve", lambda e: e.tensor_scalar(out=k1s[:], in0=k1s[:], scalar1=-16.0, scalar2=16.0, op0=ALU.mult,
                                                       op1=ALU.add), reads=["k1s"], writes=["k1s"])
                tk.op("dve", lambda e: e.tensor_tensor(out=k2f[:], in0=posf[:], in1=k1s[:], op=ALU.add),
                      reads=["posf", "k1s"], writes=["k2f"])
                k2_b = k2f[:].unsqueeze(3).broadcast_to([128, 8, 16, 16])
                io_b = iota16[:].unsqueeze(1).unsqueeze(1).broadcast_to([128, 8, 16, 16])
                tk.op("dve", lambda e: e.tensor_tensor(out=ge[:], in0=k2_b, in1=io_b, op=ALU.is_ge),
                      reads=["k2f", "c4"], writes=["ge"])
                d2_b = dd4[:, :, 1, :].unsqueeze(2).broadcast_to([128, 8, 16, 16])
                tk.op("pool", lambda e: e.tensor_tensor(out=prod[:], in0=ge[:], in1=d2_b, op=ALU.mult),
                      reads=["ge", "dd"], writes=["prod"])
                res_j = res[:, 1, :].rearrange("p (h k) -> p h k", h=8)
                tk.op("dve", lambda e: e.tensor_reduce(out=res_j, in_=prod[:], axis=AX.X, op=ALU.add),
                      reads=["prod"], writes=["res"])
                for q in range(3):
                    tk.op("pe", lambda e, q=q: e.transpose(out=psum[6 + s][:, q * 128:(q + 1) * 128], in_=res[:, q, :],
                                                           identity=ident_f[:]), reads=["res", "c1"], writes=["psb%d" % (6 + s)])
                tk.op("act", lambda e, s=s: e.copy(out=rst[s][:], in_=psum[6 + s][:, 0:384].rearrange("p (q t) -> p q t", q=3)),
                      reads=["psb%d" % (6 + s)], writes=["rst%d" % s])
                tk.dma("pool", slot_d[:, :, cols], rst[s][:], reads=["rst%d" % s], key="rst%d" % s)

            for t0_ in range(0, NTT, 2):
                qs_ = []
                for ti in range(t0_, min(NTT, t0_ + 2)):
                    q_ = OpQueue()
                    q_.suffix = "_%d" % (ti % 2)
                    q_.priv = set(PRIV)
                    b2_tile(ti, q_)
                    qs_.append(q_)
                emit_interleaved(tk, qs_)
            tk.barrier()

        if os.environ.get('BSTOP') == '2':
            tk.final_wait('sp')
            return nc
        with ExitStack() as es3:
            G = sb("G", [128, 128, PB], BF16, es3)
            ut = [sb("ut%d" % i, [128, KC, 128], BF16, es3) for i in range(7)]
            vt = [sb("vt%d" % i, [128, D], BF16, es3) for i in range(7)]
            h2b = [sb("h2b%d" % i, [128, KC, PB], BF16, es3) for i in range(2)]
            slots = [sb("slots%d" % i, [128, 3, PB], F32, es3) for i in range(2)]
            gl = [sb("gl%d" % i, [128, PB], BF16, es3) for i in range(2)]
            ohi = [sb("ohi%d" % i, [128, 128], BF16, es3) for i in range(4)]
            ohj = [sb("ohj%d" % i, [128, 128], BF16, es3) for i in range(4)]
            x1t = sb("x1t", [128, D], F32, es3)
            xo = [sb("xo%d" % i, [128, D], F32, es3) for i in range(2)]
            junk = sb("junk3", [128, D], BF16, es3)
            fnb = sb("fnb", [128, D], F32, es3)
            ss = sb("ss3", [128, 4], F32, es3)
            tk.dma("sp", fnb[:], fnw, writes=["fnb"])
            ucnt = [0]
            vcnt = [0]
            ocnt = [0]
            for blk in range(NBLK):
                bs = blk % 2
                cols = slice(blk * PB, (blk + 1) * PB)
                tk.dma("sp", slots[bs][:], slot_d[:, :, cols], writes=["slots%d" % bs])
                tk.dma("sp", h2b[bs][:], h2T_d[:, :, cols], writes=["h2b%d" % bs])
                for t in range(PB):
                    o = t % 4
                    tk.op("dve", lambda e, o=o, t=t, bs=bs: e.tensor_scalar(
                        out=ohi[o][:], in0=iota_row[:], scalar1=slots[bs][:, 0, t:t + 1], scalar2=slots[bs][:, 2, t:t + 1],
                        op0=ALU.is_equal, op1=ALU.mult), reads=["slots%d" % bs, "c2"], writes=["ohi%d" % o])
                    tk.op("dve", lambda e, o=o, t=t, bs=bs: e.tensor_scalar(
                        out=ohj[o][:], in0=iota_row[:], scalar1=slots[bs][:, 1, t:t + 1], scalar2=None,
                        op0=ALU.is_equal), reads=["slots%d" % bs, "c2"], writes=["ohj%d" % o])
                    gb_ = (t // 4) % 2
                    tk.op("pe", lambda e, o=o, gb_=gb_: e.matmul(psum[gb_][:, o * 128:(o + 1) * 128], lhsT=ohi[o][:],
                                                                 rhs=ohj[o][:], start=True, stop=True),
                          reads=["ohi%d" % o, "ohj%d" % o], writes=["psb%d" % gb_])
                    if o == 3:
                        t0 = t - 3
                        tk.op("act", lambda e, gb_=gb_, t0=t0: e.copy(
                            out=G[:, :, t0:t0 + 4], in_=psum[gb_][:, :].rearrange("p (t j) -> p j t", t=4)),
                            reads=["psb%d" % gb_], writes=["G"])
                for j in range(NJ):
                    us = ucnt[0] % 7
                    ucnt[0] += 1
                    tk.dma("sp", ut[us][:], u16[j].rearrange("p (k i) -> p k i", k=KC),
                           reads=(["u16_%d" % j] if pre is not None else ["u16"]), writes=["ut%d" % us], key="ut%d" % us)
                    b = 2 + j % 2
                    for kc in range(KC):
                        tk.op("pe", lambda e, kc=kc, b=b, us=us, bs=bs: e.matmul(
                            psum[b][:, 0:PB], lhsT=ut[us][:, kc, :], rhs=h2b[bs][:, kc, :], start=(kc == 0),
                            stop=(kc == KC - 1)), reads=["ut%d" % us, "h2b%d" % bs], writes=["psb%d" % b], inc=(kc == KC - 1))
                    g = j % 2
                    tk.op("act", lambda e, b=b, g=g: e.activation(out=gl[g][:], in_=psum[b][:, 0:PB], func=AF.Gelu),
                          reads=["psb%d" % b], writes=["gl%d" % g])
                    tk.op("dve", lambda e, g=g, j=j: e.tensor_tensor(out=G[:, j, :], in0=gl[g][:], in1=G[:, j, :], op=ALU.mult),
                          reads=["gl%d" % g, "G"], writes=["G"])
                for j in range(NJ):
                    vs = vcnt[0] % 7
                    vcnt[0] += 1
                    tk.dma("sp", vt[vs][:], v16[j], reads=(["v16_%d" % j] if pre is not None else ["v16"]),
                           writes=["vt%d" % vs], key="vt%d" % vs)
                    for tt in range(TPB):
                        for cg in range(4):
                            b = tt * 4 + cg
                            tk.op("pe", lambda e, j=j, tt=tt, cg=cg, b=b, vs=vs: e.matmul(
                                psum[b][:, :], lhsT=G[:, j, tt * 128:(tt + 1) * 128], rhs=vt[vs][:, cg * 512:(cg + 1) * 512],
                                start=(j == 0), stop=(j == NJ - 1)), reads=["G", "vt%d" % vs], writes=["psb%d" % b])
                for tt in range(TPB):
                    rows = slice(blk * PB + tt * 128, blk * PB + (tt + 1) * 128)
                    o = ocnt[0] % 2
                    ocnt[0] += 1
                    tk.dma("sp", x1t[:], x1_d[rows, :], writes=["x1t"])
                    for cg in range(4):
                        b = tt * 4 + cg
                        tk.op("dve", lambda e, b=b, cg=cg, o=o: e.tensor_tensor(
                            out=xo[o][:, cg * 512:(cg + 1) * 512], in0=psum[b][:, :], in1=x1t[:, cg * 512:(cg + 1) * 512],
                            op=ALU.add), reads=["psb%d" % b, "x1t"], writes=["xo%d" % o])
                    rms_rstd(xo[o][:], "xo%d" % o, ss, junk)
                    tk.op("dve", lambda e, o=o: e.scalar_tensor_tensor(out=xo[o][:], in0=xo[o][:], scalar=ss[:, 2:3],
                                                                       in1=fnb[:], op0=ALU.mult, op1=ALU.mult),
                          reads=["xo%d" % o, "ss2", "fnb"], writes=["xo%d" % o])
                    tk.dma("pool", out[rows, :], xo[o][:], reads=["xo%d" % o], key="ost%d" % o)
            tk.barrier()
        tk.final_wait("sp")
    print("phase B instructions (cumulative):", tk.ninst)
    return nc


def host_inputs_b(inp, xflat, mixed_full, TCs):
    c = host_consts_b()
    U = np.asarray(inp["peer_u"][0])
    V = np.asarray(inp["peer_v"][0])
    uh = np.ascontiguousarray(U.reshape(128, 128, KC, 128).transpose(1, 3, 2, 0)).reshape(128, 128, D)
    vh = np.ascontiguousarray(V.reshape(128, 128, D).transpose(1, 0, 2))
    keys = np.asarray(inp["peer_keys"][0])
    keysT = np.ascontiguousarray(keys.transpose(2, 0, 1))
    wout = np.ascontiguousarray(np.asarray(inp["w_out"][0]))
    wq = np.ascontiguousarray(np.asarray(inp["peer_w_q"][0]))
    n2w = np.ascontiguousarray(np.broadcast_to(np.asarray(inp["norm2_w"][0])[None, :], (128, D)))
    fnw = np.ascontiguousarray(np.broadcast_to(np.asarray(inp["final_norm_w"])[None, :], (128, D)))
    maps = []
    for core in range(8):
        r0 = core * TCs
        m = {"x": np.ascontiguousarray(xflat[r0:r0 + TCs]),
             "mixed": np.ascontiguousarray(mixed_full[r0:r0 + TCs]),
             "wout": wout, "wq": wq, "n2w": n2w, "fnw": fnw, "keysT": keysT, "uh": uh, "vh": vh}
        m.update(c)
        maps.append(m)
    return maps


def build_fused(nc, S):
    TC = 2 * S // 8
    NK = S // 512
    tk = Trk(nc)
    mixloc = [nc.dram_tensor("mixloc%d" % k, [512, 512], BF16) for k in range(NK)]
    gath = [nc.dram_tensor("gath%d" % k, [8 * 512, 512], BF16) for k in range(NK)]
    uh = nc.dram_tensor("b_uh", [128, 128, D], F32, kind="ExternalInput").ap()
    vh = nc.dram_tensor("b_vh", [128, 128, D], F32, kind="ExternalInput").ap()
    u16 = nc.dram_tensor("b_u16", [128, 128, D], BF16, kind="Internal").ap()
    v16 = nc.dram_tensor("b_v16", [128, 128, D], BF16, kind="Internal").ap()
    tk.lazy_keys.add("cvt")

    def cvt_some(jb, nb):
        per = (128 + nb - 1) // nb
        for j in range(jb * per, min(128, (jb + 1) * per)):
            tk.dma("pool", u16[j], uh[j], writes=["u16_%d" % j], key="cvt")
            tk.dma("pool", v16[j], vh[j], writes=["v16_%d" % j], key="cvt")
    def gather_block(k):
        tk.coll(lambda g, k=k: g.collective_compute("AllGather", ALU.bypass, replica_groups=[list(range(8))],
                                                    ins=[mixloc[k].ap().opt()], outs=[gath[k].ap().opt()]),
                reads=["ml%d" % c for c in range(4 * k, 4 * k + 4)], writes=["gath%d" % k])

    build_phase_a(nc, S, tk=tk, mix_dst=lambda t0, c0, c1: mixloc[t0 // 512][t0 % 512:t0 % 512 + 128, c0:c1],
                  on_block_done=gather_block, p_hook=cvt_some)
    tk.lazy_keys.discard("cvt")
    build_phase_b(nc, TC, tk=tk, gath=gath, pfx="b_", pre=(u16, v16))
    return nc


def wout_perm():
    perm = []
    for g in range(4):
        perm += list(range(g * 256, (g + 1) * 256)) + list(range(1024 + g * 256, 1024 + (g + 1) * 256))
    return np.array(perm)


def host_inputs_fused(inp, S):
    maps = host_inputs_a(inp, S)
    c = host_consts_b()
    TC = 2 * S // 8
    H = S // 8
    U = np.asarray(inp["peer_u"][0])
    V = np.asarray(inp["peer_v"][0])
    uh = np.ascontiguousarray(U.reshape(128, 128, KC, 128).transpose(1, 3, 2, 0)).reshape(128, 128, D)
    vh = np.ascontiguousarray(V.reshape(128, 128, D).transpose(1, 0, 2))
    keysT = np.ascontiguousarray(np.asarray(inp["peer_keys"][0]).transpose(2, 0, 1))
    wout = np.ascontiguousarray(np.asarray(inp["w_out"][0])[wout_perm(), :])
    wq = np.ascontiguousarray(np.asarray(inp["peer_w_q"][0]))
    n2w = np.ascontiguousarray(np.broadcast_to(np.asarray(inp["norm2_w"][0])[None, :], (128, D)))
    fnw = np.ascontiguousarray(np.broadcast_to(np.asarray(inp["final_norm_w"])[None, :], (128, D)))
    x = np.asarray(inp["x"])
    for d in range(8):
        m = maps[d]
        m["b_x"] = np.ascontiguousarray(np.concatenate([x[0, d * H:(d + 1) * H], x[1, d * H:(d + 1) * H]], axis=0))
        ws = np.zeros((128, 8), np.float32)
        ws[:, d] = 1.0
        m["b_wsel"] = ws
        m.update({"b_wout": wout, "b_wq": wq, "b_n2w": n2w, "b_fnw": fnw, "b_keysT": keysT, "b_uh": uh, "b_vh": vh})
        for k, v in c.items():
            m["b_" + k] = v
    return maps


def gather_out(res, S):
    H = S // 8
    out = np.zeros((2, S, D), np.float32)
    for d in range(8):
        o = res.results[d]["b_out"]
        out[0, d * H:(d + 1) * H] = o[0:H]
        out[1, d * H:(d + 1) * H] = o[H:2 * H]
    return out


S_FULL = 16384


def kernel(**inputs):
    inp = {k: np.asarray(v) for k, v in inputs.items()}
    S = S_FULL
    nc = bass.Bass("TRN2", target_bir_lowering=False)
    build_fused(nc, S)
    maps = host_inputs_fused(inp, S)
    res = run_bass_kernel_spmd(nc, maps, core_ids=list(range(8)))
    return gather_out(res, S).astype(np.float32)
```
